# Optimizing a Trainium2 kernel written in Bass

```python
import math
import jax, jax.numpy as jnp
from jax import lax
import numpy as np

D_MODEL = 1024
BATCH = 2
SEQ = 8192
DEPTH = 2

HEAD_DIM = 64
MEM_LEN = 256
MEM_HEADS = 4
MEM_WIDTH = MEM_HEADS * HEAD_DIM
MIX_WIDTH = D_MODEL
CONV_CH = MIX_WIDTH - MEM_WIDTH
CONV_WIDTH = 31
SWA_Q_HEADS = 12
SWA_KV_HEADS = 4
SWA_GROUP = SWA_Q_HEADS // SWA_KV_HEADS
SWA_Q_WIDTH = SWA_Q_HEADS * HEAD_DIM
SWA_KV_WIDTH = SWA_KV_HEADS * HEAD_DIM
WINDOW = 128
BLOCK = 128
D_FF_DENSE = 2816
N_EXPERTS = 8
TOP_K = 2
D_FF_EXPERT = 3584
N_EVEN = (DEPTH + 1) // 2
N_ODD = DEPTH // 2
RMS_EPS = 1e-6
LN_EPS = 1e-5
ALIBI_MAX_BIAS = 8.0
NEG_INF = -1e30

kernel_name = "hybrid_conformer_swa_alibi_mem_moe"


def rms_norm(x, g):
    xf = x.astype(jnp.float32)
    y = xf * lax.rsqrt(jnp.mean(xf * xf, axis=-1, keepdims=True) + RMS_EPS)
    return (y * g.astype(jnp.float32)).astype(x.dtype)


def layer_norm(x, g, b):
    xf = x.astype(jnp.float32)
    mu = jnp.mean(xf, axis=-1, keepdims=True)
    var = jnp.mean(jnp.square(xf - mu), axis=-1, keepdims=True)
    y = (xf - mu) * lax.rsqrt(var + LN_EPS)
    return (y * g.astype(jnp.float32) + b.astype(jnp.float32)).astype(x.dtype)


def swiglu(h, w_gate, w_up, w_down):
    return (jax.nn.silu(h @ w_gate) * (h @ w_up)) @ w_down


def alibi_slopes(n_heads):
    return jnp.exp2(-ALIBI_MAX_BIAS * (jnp.arange(n_heads, dtype=jnp.float32) + 1.0) / n_heads)


def memory_attention(q, q_g, mem_k, mem_v):
    b, s, _ = q.shape
    q = rms_norm(q.reshape(b, s, MEM_HEADS, HEAD_DIM), q_g)
    sc = jnp.einsum('bshd,bmhd->bhsm', q, mem_k).astype(jnp.float32) * (HEAD_DIM ** -0.5)
    p = jax.nn.softmax(sc, axis=-1).astype(mem_v.dtype)
    o = jnp.einsum('bhsm,bmhd->bshd', p, mem_v)
    return o.reshape(b, s, MEM_WIDTH)


def conv_mixer(h, w_in, b_glu, dw_w, dw_b, ln_g, ln_b, memq_g, w_out, mem_k, mem_v):
    proj = h @ w_in
    a = proj[..., :CONV_CH] + b_glu[:CONV_CH]
    gate = proj[..., CONV_CH:2 * CONV_CH] + b_glu[CONV_CH:]
    q_mem = proj[..., 2 * CONV_CH:]
    u = a * jax.nn.sigmoid(gate)
    u_pad = jnp.pad(u, ((0, 0), (CONV_WIDTH - 1, 0), (0, 0)))
    c = lax.conv_general_dilated(
        u_pad, dw_w[:, None, :], window_strides=(1,), padding='VALID',
        dimension_numbers=('NWC', 'WIO', 'NWC'), feature_group_count=CONV_CH) + dw_b
    c = jax.nn.silu(layer_norm(c, ln_g, ln_b))
    m = memory_attention(q_mem, memq_g, mem_k, mem_v)
    return jnp.concatenate([c, m], axis=-1) @ w_out


def swa_mixer(h, w_in, q_g, k_g, sinks, memq_g, w_out, mem_k, mem_v):
    b, s, _ = h.shape
    nb = s // BLOCK
    proj = h @ w_in
    o1 = SWA_Q_WIDTH
    o2 = o1 + SWA_KV_WIDTH
    o3 = o2 + SWA_KV_WIDTH
    q = rms_norm(proj[..., :o1].reshape(b, s, SWA_KV_HEADS, SWA_GROUP, HEAD_DIM), q_g)
    k = rms_norm(proj[..., o1:o2].reshape(b, s, SWA_KV_HEADS, HEAD_DIM), k_g)
    v = proj[..., o2:o3].reshape(b, s, SWA_KV_HEADS, HEAD_DIM)
    q_mem = proj[..., o3:]

    qb = q.reshape(b, nb, BLOCK, SWA_KV_HEADS, SWA_GROUP, HEAD_DIM)
    def band(t):
        prev = jnp.pad(t, ((0, 0), (BLOCK, 0), (0, 0), (0, 0)))[:, :s]
        return jnp.concatenate([prev.reshape(b, nb, BLOCK, SWA_KV_HEADS, HEAD_DIM),
                                t.reshape(b, nb, BLOCK, SWA_KV_HEADS, HEAD_DIM)], axis=2)
    kk = band(k)
    vv = band(v)
    sc = jnp.einsum('bnqhgd,bnchd->bnhgqc', qb, kk).astype(jnp.float32) * (HEAD_DIM ** -0.5)

    qpos = jnp.arange(BLOCK) + BLOCK
    kpos = jnp.arange(2 * BLOCK)
    dist = (qpos[:, None] - kpos[None, :]).astype(jnp.float32)
    win = (dist >= 0) & (dist < WINDOW)
    first = (jnp.arange(nb)[:, None] > 0) | (kpos[None, :] >= BLOCK)
    mask = win[None, :, :] & first[:, None, :]
    slopes = alibi_slopes(SWA_Q_HEADS).reshape(SWA_KV_HEADS, SWA_GROUP)
    bias = -slopes[:, :, None, None] * dist[None, None]
    sc = jnp.where(mask[None, :, None, None], sc + bias[None, None], NEG_INF)

    sink = jnp.broadcast_to(sinks.astype(jnp.float32).reshape(1, 1, SWA_KV_HEADS, SWA_GROUP, 1, 1),
                            sc.shape[:-1] + (1,))
    p = jax.nn.softmax(jnp.concatenate([sc, sink], axis=-1), axis=-1)[..., :-1]
    o = jnp.einsum('bnhgqc,bnchd->bnqhgd', p.astype(vv.dtype), vv).reshape(b, s, SWA_Q_WIDTH)
    m = memory_attention(q_mem, memq_g, mem_k, mem_v)
    return jnp.concatenate([o, m], axis=-1) @ w_out


def moe_swiglu(h, router, we_gate, we_up, we_down):
    logits = (h @ router).astype(jnp.float32)
    top_v, top_i = lax.top_k(logits, TOP_K)
    top_w = jax.nn.softmax(top_v, axis=-1)
    gates = jnp.sum(jax.nn.one_hot(top_i, N_EXPERTS, dtype=jnp.float32) * top_w[..., None], axis=-2)
    gates = gates.astype(h.dtype)
    out = jnp.zeros_like(h)
    for e in range(N_EXPERTS):
        out = out + gates[..., e:e + 1] * swiglu(h, we_gate[e], we_up[e], we_down[e])
    return out


def setup_inputs(seed: int = 0) -> dict:
    key = jax.random.key(seed)
    ks = iter(jax.random.split(key, 40))
    f32 = jnp.float32
    def nrm(shape, scale):
        return jax.random.normal(next(ks), shape, f32) * scale
    def gain(shape):
        return 1.0 + 0.05 * jax.random.normal(next(ks), shape, f32)
    D = D_MODEL
    return {
        "x": nrm((BATCH, SEQ, D), 1.0),
        "mem": nrm((BATCH, MEM_LEN, D), 1.0),
        "mem_norm_g": gain((D,)),
        "w_mem_kv": nrm((D, 2 * MEM_WIDTH), D ** -0.5),
        "mem_k_norm_g": gain((HEAD_DIM,)),
        "cv_attn_norm_g": gain((N_EVEN, D)),
        "cv_w_in": nrm((N_EVEN, D, 2 * CONV_CH + MEM_WIDTH), D ** -0.5),
        "cv_b_glu": nrm((N_EVEN, 2 * CONV_CH), 0.02),
        "cv_dw_w": nrm((N_EVEN, CONV_WIDTH, CONV_CH), CONV_WIDTH ** -0.5),
        "cv_dw_b": nrm((N_EVEN, CONV_CH), 0.02),
        "cv_ln_g": gain((N_EVEN, CONV_CH)),
        "cv_ln_b": nrm((N_EVEN, CONV_CH), 0.02),
        "cv_memq_norm_g": gain((N_EVEN, HEAD_DIM)),
        "cv_w_out": nrm((N_EVEN, MIX_WIDTH, D), MIX_WIDTH ** -0.5),
        "cv_ffn_norm_g": gain((N_EVEN, D)),
        "cv_w_gate": nrm((N_EVEN, D, D_FF_DENSE), D ** -0.5),
        "cv_w_up": nrm((N_EVEN, D, D_FF_DENSE), D ** -0.5),
        "cv_w_down": nrm((N_EVEN, D_FF_DENSE, D), D_FF_DENSE ** -0.5),
        "sw_attn_norm_g": gain((N_ODD, D)),
        "sw_w_in": nrm((N_ODD, D, SWA_Q_WIDTH + 2 * SWA_KV_WIDTH + MEM_WIDTH), D ** -0.5),
        "sw_q_norm_g": gain((N_ODD, HEAD_DIM)),
        "sw_k_norm_g": gain((N_ODD, HEAD_DIM)),
        "sw_sinks": nrm((N_ODD, SWA_Q_HEADS), 0.5),
        "sw_memq_norm_g": gain((N_ODD, HEAD_DIM)),
        "sw_w_out": nrm((N_ODD, MIX_WIDTH, D), MIX_WIDTH ** -0.5),
        "sw_ffn_norm_g": gain((N_ODD, D)),
        "sw_router": nrm((N_ODD, D, N_EXPERTS), D ** -0.5),
        "sw_we_gate": nrm((N_ODD, N_EXPERTS, D, D_FF_EXPERT), D ** -0.5),
        "sw_we_up": nrm((N_ODD, N_EXPERTS, D, D_FF_EXPERT), D ** -0.5),
        "sw_we_down": nrm((N_ODD, N_EXPERTS, D_FF_EXPERT, D), D_FF_EXPERT ** -0.5),
    }


def reference(x, mem, mem_norm_g, w_mem_kv, mem_k_norm_g,
              cv_attn_norm_g, cv_w_in, cv_b_glu, cv_dw_w, cv_dw_b, cv_ln_g, cv_ln_b,
              cv_memq_norm_g, cv_w_out, cv_ffn_norm_g, cv_w_gate, cv_w_up, cv_w_down,
              sw_attn_norm_g, sw_w_in, sw_q_norm_g, sw_k_norm_g, sw_sinks, sw_memq_norm_g,
              sw_w_out, sw_ffn_norm_g, sw_router, sw_we_gate, sw_we_up, sw_we_down):
    b, m_len, _ = mem.shape
    mkv = rms_norm(mem, mem_norm_g) @ w_mem_kv
    mem_k = rms_norm(mkv[..., :MEM_WIDTH].reshape(b, m_len, MEM_HEADS, HEAD_DIM), mem_k_norm_g)
    mem_v = mkv[..., MEM_WIDTH:].reshape(b, m_len, MEM_HEADS, HEAD_DIM)

    h = x
    for i in range(DEPTH):
        j = i // 2
        if i % 2 == 0:
            hn = rms_norm(h, cv_attn_norm_g[j])
            h = h + conv_mixer(hn, cv_w_in[j], cv_b_glu[j], cv_dw_w[j], cv_dw_b[j], cv_ln_g[j],
                               cv_ln_b[j], cv_memq_norm_g[j], cv_w_out[j], mem_k, mem_v)
            hn = rms_norm(h, cv_ffn_norm_g[j])
            h = h + swiglu(hn, cv_w_gate[j], cv_w_up[j], cv_w_down[j])
        else:
            hn = rms_norm(h, sw_attn_norm_g[j])
            h = h + swa_mixer(hn, sw_w_in[j], sw_q_norm_g[j], sw_k_norm_g[j], sw_sinks[j],
                              sw_memq_norm_g[j], sw_w_out[j], mem_k, mem_v)
            hn = rms_norm(h, sw_ffn_norm_g[j])
            h = h + moe_swiglu(hn, sw_router[j], sw_we_gate[j], sw_we_up[j], sw_we_down[j])
    return h
```

```python
import os
from contextlib import ExitStack

import numpy as np
import concourse.bass as bass
import concourse.mybir as mybir
from concourse.bass_utils import run_bass_kernel_spmd

F32 = mybir.dt.float32
BF16 = mybir.dt.bfloat16
AF = mybir.ActivationFunctionType
ALU = mybir.AluOpType
AX = mybir.AxisListType

NCORES = 8
D = 1024
SEQ = 8192
TOK = 2048
NT = 17
TC = 2208
HD = 64
DFF0 = 2816
DFFE = 3584
NE = 8
CONV_W = 31
CONV_CH = 768
RMS_EPS = 1e-6
LN_EPS = 1e-5

C_GA0, C_GF0, C_GA1, C_GF1, C_GM = 0, 8, 16, 24, 32
C_BA, C_BG = 40, 46
C_DW = 52
C_DWB, C_LNG, C_LNB = 238, 244, 250
C_MQ0, C_MQ1, C_MK, C_QG, C_KG, C_FLAG = 256, 257, 258, 259, 260, 261
NCP = 264

ENGS = ("pe", "act", "dve", "pool", "sp")


class Res:
    __slots__ = ("name", "w", "r", "dsem", "dcnt")

    def __init__(self, name):
        self.name = name
        self.w = None
        self.r = []
        self.dsem = None
        self.dcnt = 0


class Sched:
    def __init__(self, esem, dsems):
        self.esem = dict(esem)
        self.free_dsems = list(dsems)
        self.cnt = {e: 0 for e in ENGS}
        self.seen = {e: {} for e in ENGS}
        self.q = {e: [] for e in ENGS}
        self.dlast = {}
        self.nbank = 0

    def _deps(self, eng, reads, writes, skip_key=None):
        deps = {}

        def add(ev, same_ok):
            if ev is None:
                return
            k, v = ev
            if (k == eng or k == skip_key) and not same_ok:
                return
            if deps.get(k, 0) < v:
                deps[k] = v
        for r in reads:
            add(r.w, True)
        for w in writes:
            add(w.w, False)
            for ev in w.r:
                add(ev, False)
        waits = []
        for k, v in deps.items():
            if self.seen[eng].get(k, 0) < v:
                self.seen[eng][k] = v
                waits.append((self.esem[k], v))
        return waits

    def _commit(self, ev, reads, writes):
        for r in reads:
            r.r.append(ev)
        for w in writes:
            w.w = ev
            w.r = []

    def op(self, eng, fn, reads=(), writes=()):
        waits = self._deps(eng, reads, writes)
        self.cnt[eng] += 1
        ev = (eng, self.cnt[eng])
        sem = self.esem[eng]

        def run(e, waits=waits, fn=fn, sem=sem):
            for s, v in waits:
                e.wait_ge(s, v)
            fn(e).then_inc(sem, 1)
        self.q[eng].append(run)
        self._commit(ev, reads, writes)
        return ev

    def dma(self, q, out, in_, reads=(), writes=(), owner=None, **kw):
        owner = owner or (writes[0] if writes else reads[0])
        if owner.dsem is None:
            owner.dsem = self.free_dsems.pop()
            self.esem[("d", id(owner))] = owner.dsem
        key = ("d", id(owner))
        waits = self._deps(q, reads, writes, skip_key=key)
        owner.dcnt += 16
        ev = (key, owner.dcnt)
        self.dlast[key] = owner.dcnt
        sem = owner.dsem

        def run(e, waits=waits, sem=sem, out=out, in_=in_, kw=kw):
            for s, v in waits:
                e.wait_ge(s, v)
            e.dma_start(out=out, in_=in_, **kw).then_inc(sem, 16)
        self.q[q].append(run)
        self._commit(ev, reads, writes)
        return ev

    def barrier(self):
        evs = [(e, self.cnt[e]) for e in ENGS if self.cnt[e] > 0]
        evs += list(self.dlast.items())
        for eng in ENGS:
            waits = []
            for k, v in evs:
                if k == eng:
                    continue
                if self.seen[eng].get(k, 0) < v:
                    self.seen[eng][k] = v
                    waits.append((self.esem[k], v))
            if waits:
                def run(e, waits=waits):
                    for s, v in waits:
                        e.wait_ge(s, v)
                self.q[eng].append(run)

    def final_wait(self, eng):
        waits = []
        for k, v in self.dlast.items():
            if self.seen[eng].get(k, 0) < v:
                self.seen[eng][k] = v
                waits.append((self.esem[k], v))

        def run(e, waits=waits):
            for s, v in waits:
                e.wait_ge(s, v)
        self.q[eng].append(run)


def build_program(debug=()):
    nc = bass.Bass("TRN2", target_bir_lowering=False)

    def din(name, shape):
        return nc.dram_tensor(name, list(shape), F32, kind="ExternalInput").ap()

    xin = din("xin", [TC, D])
    mem = din("mem", [256, D])
    cpack = din("cpack", [128, NCP])
    ident = din("ident", [128, 128])
    bdm = din("bdm", [128, 128])
    maskexp = din("maskexp", [128, 3072])
    sinks_rep = din("sinks_rep", [1, 1536])
    w_mem_kv = din("w_mem_kv", [D, 512])
    cv_w_in = din("cv_w_in", [D, 1792])
    cv_w_out = din("cv_w_out", [D, D])
    cv_w_gate = din("cv_w_gate", [D, DFF0])
    cv_w_up = din("cv_w_up", [D, DFF0])
    cv_w_down = din("cv_w_down", [DFF0, D])
    sw_w_in = din("sw_w_in", [D, 1536])
    sw_w_out = din("sw_w_out", [D, D])
    sw_router = din("sw_router", [D, NE])
    sw_we_gate = din("sw_we_gate", [NE, D, DFFE])
    sw_we_up = din("sw_we_up", [NE, D, DFFE])
    sw_we_down = din("sw_we_down", [NE, DFFE, D])
    out = nc.dram_tensor("out", [TOK, D], F32, kind="ExternalOutput").ap()
    dbg_out = {}
    for name, shape in debug:
        dbg_out[name] = nc.dram_tensor("dbg_" + name, list(shape), F32, kind="ExternalOutput").ap()

    es = ExitStack()
    with es:
        def sb(name, shape, dt):
            return es.enter_context(nc.sbuf_tensor(name, list(shape), dt))

        HT = sb("HT", [128, NT, D], F32)
        RA = sb("RA", [128, 8, TC], BF16)
        AR = sb("AR", [128, 40960], BF16)
        CP = sb("CP", [128, NCP], F32)
        IDF = sb("IDF", [128, 128], F32)
        IDB = sb("IDB", [128, 128], BF16)
        BDB = sb("BDB", [128, 128], BF16)
        ONB = sb("ONB", [128, 128], BF16)
        ME = sb("ME", [128, 2, 4, 384], BF16)
        ME0 = sb("ME0", [128, 4, 384], BF16)
        SRB = sb("SRB", [1, 4, 384], BF16)
        MKT = sb("MKT", [128, 2, 256], BF16)
        MV = sb("MV", [128, 2, 256], BF16)
        SS = sb("SS", [128, 3, 32], F32)
        GT = sb("GT", [128, 16, 8], F32)
        WR = sb("WR", [128, 8, NE], F32)
        PS = es.enter_context(nc.psum_tensor("PS", [128, 8, 512], F32))

        esem = {e: es.enter_context(nc.semaphore("sem_" + e)) for e in ENGS}
        dsems = [es.enter_context(nc.semaphore("dsem%d" % i)) for i in range(48)]
        S = Sched(esem, dsems)

        banks = [Res("bank%d" % i) for i in range(8)]
        reserved = set()

        def bank(pair=False):
            while True:
                b = S.nbank % 8
                if pair and (b % 2 == 1 or (b + 1) in reserved):
                    S.nbank += 1
                    continue
                if b in reserved:
                    S.nbank += 1
                    continue
                break
            if pair:
                S.nbank += 2
                return PS[:, b:b + 2, :], [banks[b], banks[b + 1]]
            S.nbank += 1
            return PS[:, b, :], [banks[b]]

        def mmg(out_ap, pairs, reads, writes):
            def fn(e, out_ap=out_ap, pairs=pairs):
                n = len(pairs)
                ins = None
                for i, (l, r) in enumerate(pairs):
                    ins = e.matmul(out_ap, l, r, start=(i == 0), stop=(i == n - 1))
                return ins
            S.op("pe", fn, reads, writes)

        def act(out_ap, in_ap, func, reads, writes, **kw):
            S.op("act", lambda e: e.activation(out=out_ap, in_=in_ap, func=func, **kw), reads, writes)

        def tt(out_ap, in0, in1, op, reads, writes, eng="dve"):
            S.op(eng, lambda e: e.tensor_tensor(out=out_ap, in0=in0, in1=in1, op=op), reads, writes)

        def ts(out_ap, in0, s1, op0, reads, writes, s2=None, op1=None, eng="dve"):
            if op1 is None:
                S.op(eng, lambda e: e.tensor_scalar(out=out_ap, in0=in0, scalar1=s1, scalar2=None, op0=op0),
                     reads, writes)
            else:
                S.op(eng, lambda e: e.tensor_scalar(out=out_ap, in0=in0, scalar1=s1, scalar2=s2, op0=op0, op1=op1),
                     reads, writes)

        def stt(out_ap, in0, scalar, in1, op0, op1, reads, writes):
            S.op("dve", lambda e: e.scalar_tensor_tensor(out=out_ap, in0=in0, scalar=scalar, in1=in1,
                                                         op0=op0, op1=op1), reads, writes)

        def recip(out_ap, in_ap, reads, writes):
            S.op("dve", lambda e: e.reciprocal(out=out_ap, in_=in_ap), reads, writes)

        def copy(eng, out_ap, in_ap, reads, writes):
            if eng == "act":
                act(out_ap, in_ap, AF.Copy, reads, writes)
            else:
                S.op(eng, lambda e: e.tensor_copy(out=out_ap, in_=in_ap), reads, writes)

        def dump(name, ap, reads, view=None):
            if name in dbg_out:
                dst = dbg_out[name]
                if view:
                    dst = dst.rearrange(view[0], **view[1])
                S.dma("pool", dst, ap, reads=reads, writes=(), owner=R_dbg)

        R_dbg = Res("dbg")

        def cpc(c, n=1):
            return CP[:, c:c + n]

        def arv(off, n, dt=BF16):
            if dt == F32:
                return AR[:, off:off + 2 * n].bitcast(F32)
            return AR[:, off:off + n]

        R_HT = [Res("HT%d" % j) for j in range(NT)]
        R_RA = [Res("RA%d" % j) for j in range(NT + 1)]
        R_CP, R_ID, R_ME, R_SR = Res("CP"), Res("ID"), Res("ME"), Res("SR")
        R_MKV = Res("MKV")
        R_SS = Res("SS")

        def tcols(j):
            if j == 17:
                return 0, 32
            return 32 + 128 * j, 128

        S.dma("sp", CP[:], cpack[:, :], writes=[R_CP])
        S.dma("sp", IDF[:], ident[:, :], writes=[R_ID])
        R_BD = Res("BD")
        PB0 = 22016
        BDF = arv(PB0, 128, F32)
        S.dma("sp", BDF, bdm[:, :], writes=[R_BD])
        MEf = ME[:].rearrange("p a b c -> p (a b c)")
        S.dma("pool", MEf[:, 0:1536], maskexp[:, 0:1536], writes=[R_ME])
        S.dma("pool", MEf[:, 1536:3072], maskexp[:, 1536:3072], writes=[R_ME])
        SRF = arv(14336, 1536, F32)[0:1, :]
        S.dma("sp", SRF, sinks_rep[:, :], writes=[R_SR])
        WIN0 = arv(0, 8 * 1792).rearrange("p (k n) -> p k n", k=8)
        R_WIN0 = Res("WIN0")
        S.dma("pool", WIN0, cv_w_in.rearrange("(k p) n -> p k n", p=128), writes=[R_WIN0])
        R_XP = Res("XP")
        XP = arv(PB0 + 256, D, F32)
        S.dma("sp", XP[0:32, :], xin[0:32, :], writes=[R_XP])
        R_MEM = Res("MEM")
        MEMT = arv(PB0 + 2304, 2 * D, F32).rearrange("p (t d) -> p t d", t=2)
        S.dma("sp", MEMT, mem.rearrange("(t p) d -> p t d", p=128), writes=[R_MEM])
        R_WKV = Res("WKV")
        WKV = arv(PB0 + 6400, 8 * 512).rearrange("p (k n) -> p k n", k=8)
        S.dma("pool", WKV, w_mem_kv.rearrange("(k p) n -> p k n", p=128), writes=[R_WKV])
        for j0, j1 in ((0, 2), (2, 5), (5, 9), (9, 13), (13, 17)):
            S.dma("sp", HT[:, j0:j1, :],
                  xin[32 + 128 * j0:32 + 128 * j1, :].rearrange("(j p) d -> p j d", p=128),
                  writes=R_HT[j0:j1])
        R_WR = Res("WR")
        S.dma("sp", WR[:], sw_router.rearrange("(k p) n -> p k n", p=128), writes=[R_WR])

        copy("act", IDB[:], IDF[:], [R_ID], [R_ID])
        copy("act", BDB[:], BDF, [R_BD], [R_BD])
        S.op("dve", lambda e: e.memset(ONB[:], 1.0), [], [R_ID])
        act(SRB[:].rearrange("p a b -> p (a b)"), SRF, AF.Exp, [R_SR], [R_SR])
        ts(ME0[:], ME[:, 0], cpc(C_FLAG), ALU.mult, [R_ME, R_CP], [R_ME])

        A_TAIL = 37888
        HNB = [arv(A_TAIL + i * 1024, 1024) for i in range(2)]
        R_HNB = [Res("HNB%d" % i) for i in range(2)]
        JUNK = arv(A_TAIL + 2048, 1024)
        R_JUNK = Res("JUNK")
        ss_state = {"n": 0}

        def norm_transpose(j, gcol, src_ap=None, src_res=None, npart=128, fp32_path=None):
            k = ss_state["n"] % 32
            ss_state["n"] += 1
            if src_ap is None:
                src_ap, src_res = HT[:, j, :], R_HT[j]
            c0, n = tcols(j)
            P = slice(0, npart)
            act(JUNK[P, :], src_ap, AF.Square, [src_res], [R_JUNK, R_SS], accum_out=SS[P, 0, k:k + 1])
            act(SS[P, 1, k:k + 1], SS[P, 0, k:k + 1], AF.Sqrt, [R_SS], [R_SS], scale=1.0 / D, bias=RMS_EPS)
            recip(SS[P, 2, k:k + 1], SS[P, 1, k:k + 1], [R_SS], [R_SS])
            if fp32_path is None:
                s = j % 2
                ts(HNB[s][P, :], src_ap, SS[P, 2, k:k + 1], ALU.mult, [src_res, R_SS], [R_HNB[s]])
                pb, pr = bank()
                pv = pb.bitcast(BF16).rearrange("p (k c) -> p k c", k=8)

                def fn(e, pv=pv, s=s, P=P, n=n):
                    ins = None
                    for kc in range(8):
                        ins = e.transpose(out=pv[:, kc, 0:n], in_=HNB[s][P, kc * 128:(kc + 1) * 128],
                                          identity=IDB[P, 0:n])
                    return ins
                S.op("pe", fn, [R_HNB[s], R_ID], pr)
                g_b = CP[:, gcol:gcol + 8].unsqueeze(2).to_broadcast([128, 8, n])
                tt(RA[:, :, c0:c0 + n], pv[:, :, 0:n], g_b, ALU.mult, pr + [R_CP], [R_RA[j]])
            else:
                HN32, R_HN32, H32T, R_H32T = fp32_path
                s = j % 2
                ts(HN32[s], src_ap, SS[:, 2, k:k + 1], ALU.mult, [src_res, R_SS], [R_HN32[s]])
                pb, pr = bank(pair=True)
                pv = pb.rearrange("p b (k c) -> p (b k) c", k=4)

                def fn(e, pv=pv, s=s):
                    ins = None
                    for kc in range(8):
                        ins = e.transpose(out=pv[:, kc, :], in_=HN32[s][:, kc * 128:(kc + 1) * 128],
                                          identity=IDF[:])
                    return ins
                S.op("pe", fn, [R_HN32[s], R_ID], pr)
                g_b = CP[:, gcol:gcol + 8].unsqueeze(2).to_broadcast([128, 8, 128])
                tt(RA[:, :, c0:c0 + n], pv, g_b, ALU.mult, pr + [R_CP], [R_RA[j]])
                tt(H32T[s], pv, g_b, ALU.mult, pr + [R_CP], [R_H32T[s]])

        hn_state = {"n": 0}

        def headnorm(q_ap, q_res, gcol, out_ap, out_res, n, bufs, split=None):
            SQ, R_SQ, RT, R_RT = bufs
            s = hn_state["n"] % 2
            hn_state["n"] += 1
            act(SQ[s][:, :n], q_ap, AF.Square, q_res, [R_SQ[s]])
            sb_, sr_ = bank()
            mmg(sb_[:, :n], [(BDB[:], SQ[s][:, :n])], [R_SQ[s], R_BD], sr_)
            act(RT[s][:, :n], sb_[:, :n], AF.Sqrt, sr_, [R_RT[s]], bias=RMS_EPS)
            recip(RT[s][:, :n], RT[s][:, :n], [R_RT[s]], [R_RT[s]])
            qv, rv = q_ap, RT[s][:, :n]
            if split:
                qv = qv.rearrange("p (a b) -> p a b", b=split)
                rv = rv.rearrange("p (a b) -> p a b", b=split)
            stt(out_ap, qv, cpc(gcol), rv, ALU.mult, ALU.mult, q_res + [R_RT[s], R_CP], out_res)

        def mem_attention(QM, R_QM, blocks, qcol_off, bufs):
            PT, R_PT, RD, R_RD = bufs
            st = {"n": 0}
            for (c0, n, tiles) in blocks:
                q0 = c0 - qcol_off
                for hd in range(4):
                    c2, hb = hd // 2, hd % 2
                    rows = slice(hb * 64, hb * 64 + 64)
                    pts = []
                    for mc in range(2):
                        s = st["n"] % 4
                        st["n"] += 1
                        sb_, sr_ = bank()
                        mmg(sb_[:, :n], [(MKT[rows, c2, mc * 128:(mc + 1) * 128], QM[rows, c2, q0:q0 + n])],
                            [R_MKV, R_QM], sr_)
                        act(PT[s][:, :n], sb_[:, :n], AF.Exp, sr_, [R_PT[s]], scale=0.125)
                        pts.append(s)
                    ob, orr = bank()
                    mmg(ob[rows, :n], [(MV[:, mc, hd * 64:(hd + 1) * 64], PT[pts[mc]][:, :n]) for mc in range(2)],
                        [R_MKV] + [R_PT[s] for s in pts], orr)
                    db, dr = bank()
                    mmg(db[rows, :n], [(ONB[:, 0:64], PT[pts[mc]][:, :n]) for mc in range(2)],
                        [R_ID] + [R_PT[s] for s in pts], dr)
                    s2 = (st["n"] // 2) % 2
                    recip(RD[s2][rows, :n], db[rows, :n], dr, [R_RD[s2]])
                    tt(RA[rows, 6 + c2, c0:c0 + n], ob[rows, :n], RD[s2][rows, :n], ALU.mult,
                       orr + [R_RD[s2]], [R_RA[t] for t in tiles])

        def out_proj(WO, R_WO, tiles):
            for j in tiles:
                c0, n = tcols(j)
                yb, yr = bank(pair=True)
                for half in range(2):
                    mmg(yb[:, half, :], [(RA[:, kc, c0:c0 + 128], WO[:, kc, half * 512:(half + 1) * 512])
                                         for kc in range(8)], [R_RA[j], R_WO], [yr[half]])
                tt(HT[:, j, :], yb.rearrange("p b c -> p (b c)"), HT[:, j, :], ALU.add, yr + [R_HT[j]], [R_HT[j]])

        def ffn(groups, blocks, WB, R_WB, ACTB, R_ACTB, SIL, R_SIL):
            steps = [(gi, bi) for gi in range(len(groups)) for bi in range(len(blocks))]

            def load(gi):
                g = groups[gi]
                s = gi % 2
                nfc = g["nfc"]
                wg, wu, wd = WB[s]
                S.dma("pool", wg[:, :, 0:nfc * 128], g["g"].rearrange("(k p) n -> p k n", p=128),
                      writes=[R_WB[s][0]])
                S.dma("pool", wu[:, :, 0:nfc * 128], g["u"].rearrange("(k p) n -> p k n", p=128),
                      writes=[R_WB[s][1]])
                S.dma("pool", wd[:, 0:nfc, :], g["d"].rearrange("(f p) n -> p f n", p=128),
                      writes=[R_WB[s][2]])

            def gu(si):
                gi, bi = steps[si]
                g = groups[gi]
                s = gi % 2
                a = si % 2
                wg, wu, wd = WB[s]
                c0, n, tiles = blocks[bi]
                rr = [R_RA[t] for t in tiles]
                for fc in range(g["nfc"]):
                    gb, gr = bank()
                    mmg(gb[:, :n], [(wg[:, kc, fc * 128:(fc + 1) * 128], RA[:, kc, c0:c0 + n]) for kc in range(8)],
                        rr + [R_WB[s][0]], gr)
                    ub, ur = bank()
                    mmg(ub[:, :n], [(wu[:, kc, fc * 128:(fc + 1) * 128], RA[:, kc, c0:c0 + n]) for kc in range(8)],
                        rr + [R_WB[s][1]], ur)
                    sl = (si * 4 + fc) % 2
                    act(SIL[sl][:, :n], gb[:, :n], AF.Silu, gr, [R_SIL[sl]])
                    tt(ACTB[a][:, fc, :n], ub[:, :n], SIL[sl][:, :n], ALU.mult, ur + [R_SIL[sl]], [R_ACTB[a][fc]])

            def down(si):
                gi, bi = steps[si]
                g = groups[gi]
                s = gi % 2
                a = si % 2
                wg, wu, wd = WB[s]
                c0, n, tiles = blocks[bi]
                nfc = g["nfc"]
                for ti, j in enumerate(tiles):
                    yb, yr = bank(pair=True)
                    for half in range(2):
                        mmg(yb[:, half, :], [(ACTB[a][:, fc, ti * 128:(ti + 1) * 128],
                                              wd[:, fc, half * 512:(half + 1) * 512]) for fc in range(nfc)],
                            [R_ACTB[a][fc] for fc in range(nfc)] + [R_WB[s][2]], [yr[half]])
                    yv = yb.rearrange("p b c -> p (b c)")
                    if g["gate"] is None:
                        tt(HT[:, j, :], yv, HT[:, j, :], ALU.add, yr + [R_HT[j]], [R_HT[j]])
                    else:
                        stt(HT[:, j, :], yv, g["gate"](j), HT[:, j, :], ALU.mult, ALU.add,
                            yr + [R_HT[j], R_GT], [R_HT[j]])

            load(0)
            if len(groups) > 1:
                load(1)
            nb = len(blocks)
            for si in range(len(steps)):
                gu(si)
                if si > 0:
                    down(si - 1)
                    gi_prev, bi_prev = steps[si - 1]
                    if bi_prev == nb - 1 and gi_prev + 2 < len(groups):
                        load(gi_prev + 2)
            down(len(steps) - 1)

        R_GT = Res("GT")

        MEMX = arv(PB0 + 10496, 8 * 256).rearrange("p (k c) -> p k c", k=8)
        R_MEMX = [Res("MEMX0"), Res("MEMX1")]
        SQb = [arv(PB0 + 12544 + i * 512, 512) for i in range(2)]
        R_SQb = [Res("SQ%d" % i) for i in range(2)]
        RTb = [arv(PB0 + 13568 + i * 1024, 512, F32) for i in range(2)]
        R_RTb = [Res("RT%d" % i) for i in range(2)]
        hbufs = (SQb, R_SQb, RTb, R_RTb)
        for t in range(2):
            k = ss_state["n"] % 32
            ss_state["n"] += 1
            act(JUNK[:], MEMT[:, t, :], AF.Square, [R_MEM], [R_JUNK, R_SS], accum_out=SS[:, 0, k:k + 1])
            act(SS[:, 1, k:k + 1], SS[:, 0, k:k + 1], AF.Sqrt, [R_SS], [R_SS], scale=1.0 / D, bias=RMS_EPS)
            recip(SS[:, 2, k:k + 1], SS[:, 1, k:k + 1], [R_SS], [R_SS])
            ts(HNB[t][:], MEMT[:, t, :], SS[:, 2, k:k + 1], ALU.mult, [R_MEM, R_SS], [R_HNB[t]])
            pb, pr = bank()
            pv = pb.bitcast(BF16).rearrange("p (k c) -> p k c", k=8)

            def fn(e, pv=pv, t=t):
                ins = None
                for kc in range(8):
                    ins = e.transpose(out=pv[:, kc, :], in_=HNB[t][:, kc * 128:(kc + 1) * 128], identity=IDB[:])
                return ins
            S.op("pe", fn, [R_HNB[t], R_ID], pr)
            g_b = CP[:, C_GM:C_GM + 8].unsqueeze(2).to_broadcast([128, 8, 128])
            tt(MEMX[:, :, t * 128:(t + 1) * 128], pv, g_b, ALU.mult, pr + [R_CP], [R_MEMX[t]])
        for c in range(2):
            kb_, kr_ = bank()
            mmg(kb_[:, :256], [(WKV[:, kc, c * 128:(c + 1) * 128], MEMX[:, kc, :]) for kc in range(8)],
                R_MEMX + [R_WKV], kr_)
            headnorm(kb_[:, :256], kr_, C_MK, MKT[:, c, :], [R_MKV], 256, hbufs)
        for t in range(2):
            vb_, vr_ = bank()
            mmg(vb_[:, :256], [(MEMX[:, kc, t * 128:(t + 1) * 128], WKV[:, kc, 256:512]) for kc in range(8)],
                [R_MEMX[t], R_WKV], vr_)
            copy("act", MV[:, t, :], vb_[:, :256], vr_, [R_MKV])
        dump("mkt", MKT[:].rearrange("p a b -> p (a b)"), [R_MKV])
        dump("mv", MV[:].rearrange("p a b -> p (a b)"), [R_MKV])

        norm_transpose(17, C_GA0, src_ap=XP[0:32, :], src_res=R_XP, npart=32)
        for j in range(NT):
            norm_transpose(j, C_GA0)
        S.barrier()
        U = arv(14336, 6 * TC).rearrange("p (c t) -> p c t", c=6)
        R_U = [Res("U%d" % c) for c in range(6)]
        QM0 = arv(27584, 2 * 2176).rearrange("p (c t) -> p c t", c=2)
        R_QM0 = Res("QM0")
        SG = [arv(31936 + i * 1024, 512, F32) for i in range(2)]
        R_SG = [Res("SG%d" % i) for i in range(2)]
        SQb = [arv(33984 + i * 512, 512) for i in range(2)]
        RTb = [arv(35008 + i * 1024, 512, F32) for i in range(2)]
        hbufs = (SQb, R_SQb, RTb, R_RTb)
        TB0 = [(0, 160, [17, 0])] + [(160 + 512 * i, 512, [1 + 4 * i + t for t in range(4)]) for i in range(4)]
        sgi = 0
        for c in range(6):
            for (c0, n, tiles) in TB0:
                rr = [R_RA[t] for t in tiles]
                ab, ar_ = bank()
                mmg(ab[:, :n], [(WIN0[:, kc, c * 128:(c + 1) * 128], RA[:, kc, c0:c0 + n]) for kc in range(8)],
                    rr + [R_WIN0], ar_)
                gb, gr = bank()
                mmg(gb[:, :n], [(WIN0[:, kc, (6 + c) * 128:(7 + c) * 128], RA[:, kc, c0:c0 + n]) for kc in range(8)],
                    rr + [R_WIN0], gr)
                s = sgi % 2
                sgi += 1
                act(SG[s][:, :n], gb[:, :n], AF.Sigmoid, gr + [R_CP], [R_SG[s]], bias=cpc(C_BG + c))
                stt(U[:, c, c0:c0 + n], ab[:, :n], cpc(C_BA + c), SG[s][:, :n], ALU.add, ALU.mult,
                    ar_ + [R_SG[s], R_CP], [R_U[c]])
                if c0 == 0:
                    ts(U[:, c, 0:160], U[:, c, 0:160], cpc(C_FLAG), ALU.mult, [R_U[c], R_CP], [R_U[c]])
        TBC = [(32, 128, [0])] + TB0[1:]
        for c2 in range(2):
            for (c0, n, tiles) in TBC:
                rr = [R_RA[t] for t in tiles]
                qb, qr = bank()
                mmg(qb[:, :n], [(WIN0[:, kc, (12 + c2) * 128:(13 + c2) * 128], RA[:, kc, c0:c0 + n])
                                for kc in range(8)], rr + [R_WIN0], qr)
                headnorm(qb[:, :n], qr, C_MQ0, QM0[:, c2, c0 - 32:c0 - 32 + n], [R_QM0], n, hbufs)
        dump("u", U[:, 0, :], [R_U[0]])
        dump("qm0", QM0[:, 0, :], [R_QM0])
        S.barrier()
        DG = [arv(i * 3968, 3968).rearrange("p (k c) -> p k c", k=CONV_W) for i in range(2)]
        R_DG = [Res("DG%d" % i) for i in range(2)]
        for c in range(6):
            s = c % 2
            S.op("dve", lambda e, s=s, c=c: e.tensor_tensor(
                out=DG[s], in0=IDB[:].unsqueeze(1).to_broadcast([128, CONV_W, 128]),
                in1=CP[:, C_DW + c * CONV_W:C_DW + (c + 1) * CONV_W].unsqueeze(2).to_broadcast([128, CONV_W, 128]),
                op=ALU.mult), [R_ID, R_CP], [R_DG[s]])
            for (c0, n, tiles) in TBC:
                cb, cr = bank()
                mmg(cb[:, :n], [(DG[s][:, k, :], U[:, c, c0 - 30 + k:c0 - 30 + k + n]) for k in range(CONV_W)],
                    [R_DG[s], R_U[c]], cr)
                act(RA[:, c, c0:c0 + n], cb[:, :n], AF.Identity, cr + [R_CP], [R_RA[t] for t in tiles],
                    bias=cpc(C_DWB + c))
        dump("conv", RA[:, 0, 32:TC], [R_RA[t] for t in range(NT)])
        SQL = [arv(7936 + i * 512, 512) for i in range(2)]
        R_SQL = [Res("SQL%d" % i) for i in range(2)]
        MSQ = arv(8960, 512, F32)
        VAR = arv(9984, 512, F32)
        R_ST = Res("LNST")
        T1 = [arv(11008 + i * 1024, 512, F32) for i in range(2)]
        R_T1 = [Res("T1%d" % i) for i in range(2)]
        sqi = 0
        for (c0, n, tiles) in TBC:
            rr = [R_RA[t] for t in tiles]
            s1b, s1r = bank()
            mmg(s1b[:, :n], [(ONB[:], RA[:, c, c0:c0 + n]) for c in range(6)], rr + [R_ID], s1r)
            s2b, s2r = bank()
            sqs = []
            for c in range(6):
                s = sqi % 2
                sqi += 1
                act(SQL[s][:, :n], RA[:, c, c0:c0 + n], AF.Square, rr, [R_SQL[s]])
                S.op("pe", lambda e, s2b=s2b, s=s, n=n, c=c: e.matmul(s2b[:, :n], ONB[:], SQL[s][:, :n],
                                                                    start=(c == 0), stop=(c == 5)),
                     [R_SQL[s], R_ID], s2r)
            act(MSQ[:, :n], s1b[:, :n], AF.Square, s1r, [R_ST], scale=1.0 / CONV_CH)
            stt(VAR[:, :n], s2b[:, :n], 1.0 / CONV_CH, MSQ[:, :n], ALU.mult, ALU.subtract, s2r + [R_ST], [R_ST])
            act(VAR[:, :n], VAR[:, :n], AF.Sqrt, [R_ST], [R_ST], bias=LN_EPS)
            recip(VAR[:, :n], VAR[:, :n], [R_ST], [R_ST])
            for c in range(6):
                s = c % 2
                stt(T1[s][:, :n], s1b[:, :n], -1.0 / CONV_CH, RA[:, c, c0:c0 + n], ALU.mult, ALU.add,
                    s1r + rr, [R_T1[s]])
                tt(T1[s][:, :n], T1[s][:, :n], VAR[:, :n], ALU.mult, [R_T1[s], R_ST], [R_T1[s]])
                act(RA[:, c, c0:c0 + n], T1[s][:, :n], AF.Silu, [R_T1[s], R_CP], rr,
                    scale=cpc(C_LNG + c), bias=cpc(C_LNB + c))
        dump("cln", RA[:, 0, 32:TC], [R_RA[t] for t in range(NT)])
        S.barrier()
        PT = [arv(i * 512, 512) for i in range(4)]
        R_PT = [Res("PT%d" % i) for i in range(4)]
        RD = [arv(2048 + i * 1024, 512, F32) for i in range(2)]
        R_RD = [Res("RD%d" % i) for i in range(2)]
        WO0 = arv(14336, 8 * D).rearrange("p (k n) -> p k n", k=8)
        R_WO0 = Res("WO0")
        S.dma("pool", WO0, cv_w_out.rearrange("(k p) n -> p k n", p=128), writes=[R_WO0])
        mem_attention(QM0, R_QM0, TBC, 32, (PT, R_PT, RD, R_RD))
        dump("cat0", RA[:, 6, 32:TC], [R_RA[t] for t in range(NT)])
        out_proj(WO0, R_WO0, list(range(NT)))
        dump("h0a", HT[:, 1, :], [R_HT[1]])
        S.barrier()
        WB = []
        R_WB = []
        for s in range(2):
            o = s * 12288
            WB.append((arv(o, 4096).rearrange("p (k n) -> p k n", k=8),
                       arv(o + 4096, 4096).rearrange("p (k n) -> p k n", k=8),
                       arv(o + 8192, 4096).rearrange("p (f n) -> p f n", f=4)))
            R_WB.append([Res("WB%d_%d" % (s, i)) for i in range(3)])
        ACTB = [arv(24576 + a * 2048, 2048).rearrange("p (f n) -> p f n", f=4) for a in range(2)]
        R_ACTB = [[Res("ACTB%d_%d" % (a, f)) for f in range(4)] for a in range(2)]
        SIL = [arv(28672 + i * 512, 512) for i in range(2)]
        R_SIL = [Res("SIL%d" % i) for i in range(2)]
        for j in range(NT):
            norm_transpose(j, C_GF0)
        groups0 = []
        for f0 in range(0, DFF0, 512):
            nf = min(512, DFF0 - f0)
            groups0.append(dict(g=cv_w_gate[:, f0:f0 + nf], u=cv_w_up[:, f0:f0 + nf], d=cv_w_down[f0:f0 + nf, :],
                                nfc=nf // 128, gate=None))
        ffn(groups0, TBC, WB, R_WB, ACTB, R_ACTB, SIL, R_SIL)
        dump("h1", HT[:, 1, :], [R_HT[1]])
        S.barrier()

        for j in range(NT):
            norm_transpose(j, C_GA1)
        WIN1 = arv(0, 8 * 1536).rearrange("p (k n) -> p k n", k=8)
        R_WIN1 = Res("WIN1")
        QPAIR = [(0, 3), (1, 4), (2, 5), (6, 9), (7, 10), (8, 11)]
        for i, (ha, hb_) in enumerate(QPAIR):
            for half, hh in enumerate((ha, hb_)):
                S.dma("pool", WIN1[:, :, i * 128 + half * 64:i * 128 + half * 64 + 64],
                      sw_w_in[:, hh * 64:(hh + 1) * 64].rearrange("(k p) n -> p k n", p=128), writes=[R_WIN1])
        S.dma("pool", WIN1[:, :, 768:1536], sw_w_in[:, 768:1536].rearrange("(k p) n -> p k n", p=128),
              writes=[R_WIN1])
        QT = arv(12288, 6 * TOK).rearrange("p (a n g q) -> p a n g q", a=2, n=16, g=3)
        R_QT = Res("QT")
        KT = arv(24576, 2 * 2176).rearrange("p (c t) -> p c t", c=2)
        R_KT = Res("KT")
        VT = arv(28928, NT * 256).rearrange("p (j d) -> p j d", j=NT)
        R_VT = Res("VT")
        QM1 = arv(33280, 2 * TOK).rearrange("p (c t) -> p c t", c=2)
        R_QM1 = Res("QM1")
        SQb = [arv(37376 + i * 512, 512) for i in range(2)]
        RTb = [arv(38400 + i * 1024, 512, F32) for i in range(2)]
        hbufs = (SQb, R_SQb, RTb, R_RTb)
        S.barrier()
        TB1 = TB0[1:]
        for i in range(6):
            for bi, (c0, n, tiles) in enumerate(TB1):
                rr = [R_RA[t] for t in tiles]
                qb, qr = bank()
                mmg(qb[:, :n], [(WIN1[:, kc, i * 128:(i + 1) * 128], RA[:, kc, c0:c0 + n]) for kc in range(8)],
                    rr + [R_WIN1], qr)
                headnorm(qb[:, :n], qr, C_QG, QT[:, i // 3, 4 * bi:4 * bi + 4, i % 3, :], [R_QT], n, hbufs,
                         split=128)
        for c2 in range(2):
            for (c0, n, tiles) in TBC:
                rr = [R_RA[t] for t in tiles]
                kb_, kr_ = bank()
                mmg(kb_[:, :n], [(WIN1[:, kc, 768 + c2 * 128:768 + (c2 + 1) * 128], RA[:, kc, c0:c0 + n])
                                 for kc in range(8)], rr + [R_WIN1], kr_)
                headnorm(kb_[:, :n], kr_, C_KG, KT[:, c2, c0 - 32:c0 - 32 + n], [R_KT], n, hbufs)
        for j in range(NT):
            c0, n = tcols(j)
            vb_, vr_ = bank()
            mmg(vb_[:, :256], [(RA[:, kc, c0:c0 + 128], WIN1[:, kc, 1024:1280]) for kc in range(8)],
                [R_RA[j], R_WIN1], vr_)
            copy("act", VT[:, j, :], vb_[:, :256], vr_, [R_VT])
        for c2 in range(2):
            for (c0, n, tiles) in TB1:
                rr = [R_RA[t] for t in tiles]
                qb, qr = bank()
                mmg(qb[:, :n], [(WIN1[:, kc, 1280 + c2 * 128:1280 + (c2 + 1) * 128], RA[:, kc, c0:c0 + n])
                                for kc in range(8)], rr + [R_WIN1], qr)
                headnorm(qb[:, :n], qr, C_MQ1, QM1[:, c2, c0 - 160:c0 - 160 + n], [R_QM1], n, hbufs)
        dump("qt", QT[:, 0, :, 0, :], [R_QT], view=("p (n q) -> p n q", dict(n=16)))
        dump("kt", KT[:, 0, :], [R_KT])
        S.barrier()
        ET = [arv(i * 384, 384) for i in range(2)]
        R_ET = [Res("ET%d" % i) for i in range(2)]
        PT1 = [arv(768 + i * 384, 384) for i in range(4)]
        R_PT1 = [Res("PT1%d" % i) for i in range(4)]
        RD1 = [arv(2304 + i * 768, 384, F32) for i in range(2)]
        R_RD1 = [Res("RD1%d" % i) for i in range(2)]
        ei = 0
        pi = 0
        for nblk in range(16):
            j = nblk + 1
            rc0, _ = tcols(j)
            for kvh in range(4):
                rows = slice((kvh % 2) * 64, (kvh % 2) * 64 + 64)
                qc0 = (kvh // 2) * 3
                pts = []
                for kb in range(2):
                    jk = j - 1 + kb
                    sb_, sr_ = bank()
                    mmg(sb_[:, :384], [(KT[rows, kvh // 2, jk * 128:(jk + 1) * 128],
                                        QT[rows, kvh // 2, nblk, :, :].rearrange("p g q -> p (g q)"))],
                        [R_KT, R_QT], sr_)
                    s = ei % 2
                    ei += 1
                    act(ET[s][:], sb_[:, :384], AF.Exp, sr_, [R_ET[s]], scale=0.125)
                    p = pi % 4
                    pi += 1
                    msk = ME0[:, kvh, :] if (nblk == 0 and kb == 0) else ME[:, kb, kvh, :]
                    tt(PT1[p][:], ET[s][:], msk, ALU.mult, [R_ET[s], R_ME], [R_PT1[p]])
                    pts.append((p, jk))
                ob, orr = bank()
                mmg(ob[rows, :384], [(VT[:, jk, kvh * 64:(kvh + 1) * 64], PT1[p][:]) for (p, jk) in pts],
                    [R_VT] + [R_PT1[p] for (p, _) in pts], orr)
                db, dr = bank()
                mmg(db[rows, :384], [(ONB[:, 0:64], PT1[p][:]) for (p, _) in pts] + [(ONB[0:1, 0:64], SRB[0:1, kvh, :])],
                    [R_ID, R_SR] + [R_PT1[p] for (p, _) in pts], dr)
                s2 = (nblk * 4 + kvh) % 2
                recip(RD1[s2][rows, :], db[rows, :384], dr, [R_RD1[s2]])
                tt(RA[rows, qc0:qc0 + 3, rc0:rc0 + 128], ob[rows, :384].rearrange("p (g q) -> p g q", g=3),
                   RD1[s2][rows, :].rearrange("p (g q) -> p g q", g=3), ALU.mult,
                   orr + [R_RD1[s2]], [R_RA[j]])
        dump("swa", RA[:, 0, 160:TC], [R_RA[t] for t in range(1, NT)])
        S.barrier()
        WO1 = arv(14336, 8 * D).rearrange("p (k n) -> p k n", k=8)
        R_WO1 = Res("WO1")
        for i, (ha, hb_) in enumerate(QPAIR):
            S.dma("pool", WO1[0:64, i, :], sw_w_out[ha * 64:(ha + 1) * 64, :], writes=[R_WO1])
            S.dma("pool", WO1[64:128, i, :], sw_w_out[hb_ * 64:(hb_ + 1) * 64, :], writes=[R_WO1])
        S.dma("pool", WO1[:, 6:8, :], sw_w_out[768:1024, :].rearrange("(k p) n -> p k n", p=128), writes=[R_WO1])
        PT = [arv(i * 512, 512) for i in range(4)]
        RD = [arv(2048 + i * 1024, 512, F32) for i in range(2)]
        mem_attention(QM1, R_QM1, TB1, 160, (PT, R_PT, RD, R_RD))
        out_proj(WO1, R_WO1, list(range(1, NT)))
        dump("h1a", HT[:, 1, :], [R_HT[1]])
        S.barrier()
        HN32 = [arv(29696 + i * 2048, 1024, F32) for i in range(2)]
        R_HN32 = [Res("HN32%d" % i) for i in range(2)]
        H32T = [arv(33792 + i * 2048, 1024, F32).rearrange("p (k c) -> p k c", k=8) for i in range(2)]
        R_H32T = [Res("H32T%d" % i) for i in range(2)]
        reserved.add(7)
        LG = PS[:, 7, 0:128].rearrange("p (j e) -> p j e", j=16)
        R_LG = banks[7]
        for j in range(1, NT):
            norm_transpose(j, C_GF1, fp32_path=(HN32, R_HN32, H32T, R_H32T))
            s = j % 2
            mmg(LG[:, j - 1, :], [(H32T[s][:, kc, :], WR[:, kc, :]) for kc in range(8)],
                [R_H32T[s], R_WR], [R_LG])
        def gw(i):
            return arv(i * 256, 128, F32).rearrange("p (j e) -> p j e", j=16)

        def g16(i):
            return arv(i * 256, 128, F32)[:, 0:16]
        R_GW = Res("GW")
        copy("act", gw(0), LG, [R_LG], [R_GW])
        S.op("dve", lambda e: e.tensor_reduce(out=g16(8), in_=gw(0), op=ALU.max, axis=AX.X), [R_GW], [R_GW])
        tt(gw(1), gw(0), g16(8).unsqueeze(2).to_broadcast([128, 16, 8]), ALU.is_equal, [R_GW], [R_GW])
        stt(gw(2), gw(1), -1e30, gw(0), ALU.mult, ALU.add, [R_GW], [R_GW])
        S.op("dve", lambda e: e.tensor_reduce(out=g16(9), in_=gw(2), op=ALU.max, axis=AX.X), [R_GW], [R_GW])
        tt(gw(3), gw(2), g16(9).unsqueeze(2).to_broadcast([128, 16, 8]), ALU.is_equal, [R_GW], [R_GW])
        tt(g16(10), g16(9), g16(8), ALU.subtract, [R_GW], [R_GW])
        act(g16(10), g16(10), AF.Exp, [R_GW], [R_GW])
        ts(g16(11), g16(10), 1.0, ALU.add, [R_GW], [R_GW])
        recip(g16(11), g16(11), [R_GW], [R_GW])
        tt(g16(10), g16(10), g16(11), ALU.mult, [R_GW], [R_GW])
        tt(gw(1), gw(1), g16(11).unsqueeze(2).to_broadcast([128, 16, 8]), ALU.mult, [R_GW], [R_GW])
        tt(gw(3), gw(3), g16(10).unsqueeze(2).to_broadcast([128, 16, 8]), ALU.mult, [R_GW], [R_GW])
        tt(GT[:], gw(1), gw(3), ALU.add, [R_GW], [R_GT])
        dump("gates", GT[:].rearrange("p a b -> p (a b)"), [R_GT])
        reserved.discard(7)
        S.barrier()
        groups1 = []
        for ex in range(NE):
            for f0 in range(0, DFFE, 512):
                groups1.append(dict(g=sw_we_gate[ex, :, f0:f0 + 512], u=sw_we_up[ex, :, f0:f0 + 512],
                                    d=sw_we_down[ex, f0:f0 + 512, :], nfc=4,
                                    gate=(lambda j, ex=ex: GT[:, j - 1, ex:ex + 1])))
        if os.environ.get("MK_MOE_GROUPS"):
            groups1 = groups1[:int(os.environ["MK_MOE_GROUPS"])]
        ffn(groups1, TB1, WB, R_WB, ACTB, R_ACTB, SIL, R_SIL)
        R_OUT = Res("OUT")
        for j0 in range(1, NT, 4):
            S.dma("sp", out[(j0 - 1) * 128:(j0 + 3) * 128, :].rearrange("(j p) d -> p j d", p=128),
                  HT[:, j0:j0 + 4, :], reads=R_HT[j0:j0 + 4], owner=R_OUT)
        S.final_wait("sp")

        block = es.enter_context(nc.Block())

        @block.tensor
        def _(e):
            for f in S.q["pe"]:
                f(e)

        @block.scalar
        def _(e):
            for f in S.q["act"]:
                f(e)

        @block.vector
        def _(e):
            for f in S.q["dve"]:
                f(e)

        @block.gpsimd
        def _(e):
            for f in S.q["pool"]:
                f(e)

        @block.sync
        def _(e):
            for f in S.q["sp"]:
                f(e)
    return nc


def _consts():
    ident = np.eye(128, dtype=np.float32)
    bd = np.zeros((128, 128), np.float32)
    bd[:64, :64] = 1.0 / 64
    bd[64:, 64:] = 1.0 / 64
    slopes = np.exp2(-8.0 * (np.arange(12, dtype=np.float32) + 1.0) / 12).astype(np.float32)
    k = np.arange(128)[:, None].astype(np.float32)
    q = np.arange(128)[None, :].astype(np.float32)
    me = np.zeros((128, 2, 4, 3, 128), np.float32)
    for kb in range(2):
        dist = q + 128.0 - k if kb == 0 else q - k
        valid = (dist >= 0) & (dist < 128)
        for kvh in range(4):
            for g in range(3):
                s = slopes[kvh * 3 + g]
                me[:, kb, kvh, g, :] = np.where(valid, np.exp(-s * dist), 0.0)
    return ident, bd, me.reshape(128, 3072)


def _cpack(inp, flag):
    cp = np.zeros((128, NCP), np.float32)

    def pk(v):
        return np.asarray(v, np.float32).reshape(8, 128).T
    cp[:, C_GA0:C_GA0 + 8] = pk(inp["cv_attn_norm_g"][0])
    cp[:, C_GF0:C_GF0 + 8] = pk(inp["cv_ffn_norm_g"][0])
    cp[:, C_GA1:C_GA1 + 8] = pk(inp["sw_attn_norm_g"][0])
    cp[:, C_GF1:C_GF1 + 8] = pk(inp["sw_ffn_norm_g"][0])
    cp[:, C_GM:C_GM + 8] = pk(inp["mem_norm_g"])
    bg = np.asarray(inp["cv_b_glu"][0], np.float32)
    cp[:, C_BA:C_BA + 6] = bg[:768].reshape(6, 128).T
    cp[:, C_BG:C_BG + 6] = bg[768:].reshape(6, 128).T
    dw = np.asarray(inp["cv_dw_w"][0], np.float32)
    cp[:, C_DW:C_DW + 186] = dw.reshape(31, 6, 128).transpose(2, 1, 0).reshape(128, 186)
    cp[:, C_DWB:C_DWB + 6] = np.asarray(inp["cv_dw_b"][0], np.float32).reshape(6, 128).T
    cp[:, C_LNG:C_LNG + 6] = np.asarray(inp["cv_ln_g"][0], np.float32).reshape(6, 128).T
    cp[:, C_LNB:C_LNB + 6] = np.asarray(inp["cv_ln_b"][0], np.float32).reshape(6, 128).T

    def h2(v):
        v = np.asarray(v, np.float32).reshape(64)
        return np.concatenate([v, v])
    cp[:, C_MQ0] = h2(inp["cv_memq_norm_g"][0])
    cp[:, C_MQ1] = h2(inp["sw_memq_norm_g"][0])
    cp[:, C_MK] = h2(inp["mem_k_norm_g"])
    cp[:, C_QG] = h2(inp["sw_q_norm_g"][0])
    cp[:, C_KG] = h2(inp["sw_k_norm_g"][0])
    cp[:, C_FLAG] = flag
    return cp


def make_in_maps(inp):
    f = lambda a: np.ascontiguousarray(np.asarray(a, dtype=np.float32))
    ident, bd, me = _consts()
    x = f(inp["x"])
    memf = f(inp["mem"])
    sinks = f(inp["sw_sinks"][0])
    sinks_rep = np.ascontiguousarray(np.repeat(sinks, 128)[None, :])
    shared = dict(
        ident=ident, bdm=bd, maskexp=me, sinks_rep=sinks_rep,
        w_mem_kv=f(inp["w_mem_kv"]), cv_w_in=f(inp["cv_w_in"][0]), cv_w_out=f(inp["cv_w_out"][0]),
        cv_w_gate=f(inp["cv_w_gate"][0]), cv_w_up=f(inp["cv_w_up"][0]), cv_w_down=f(inp["cv_w_down"][0]),
        sw_w_in=f(inp["sw_w_in"][0]), sw_w_out=f(inp["sw_w_out"][0]), sw_router=f(inp["sw_router"][0]),
        sw_we_gate=f(inp["sw_we_gate"][0]), sw_we_up=f(inp["sw_we_up"][0]), sw_we_down=f(inp["sw_we_down"][0]),
    )
    maps = []
    for c in range(NCORES):
        b, qtr = divmod(c, 4)
        xin = np.zeros((TC, D), np.float32)
        t0 = qtr * TOK
        if qtr == 0:
            xin[160:] = x[b, 0:TOK]
        else:
            xin[:] = x[b, t0 - 160:t0 + TOK]
        m = dict(shared)
        m["xin"] = xin
        m["mem"] = memf[b]
        m["cpack"] = _cpack(inp, 0.0 if qtr == 0 else 1.0)
        maps.append(m)
    return maps


def kernel(**inputs):
    nc = build_program()
    maps = make_in_maps(inputs)
    res = run_bass_kernel_spmd(nc, maps, core_ids=list(range(NCORES)))
    outs = [np.asarray(r["out"], np.float32) for r in res.results]
    full = np.concatenate(outs, axis=0).reshape(2, SEQ, D)
    return full
```

```python
import os
from contextlib import ExitStack

import numpy as np
import concourse.bass as bass
import concourse.mybir as mybir
from concourse.bass_utils import run_bass_kernel_spmd

F32 = mybir.dt.float32
BF16 = mybir.dt.bfloat16
I32 = mybir.dt.int32
AF = mybir.ActivationFunctionType
ALU = mybir.AluOpType
AX = mybir.AxisListType

NCORES = 8
D = 1024
SEQ = 8192
TOK = 2048
NT = 17
TC = 2208
HD = 64
DFF0 = 2816
DFFE = 3584
NE = 8
CONV_W = 31
CONV_CH = 768
RMS_EPS = 1e-6
LN_EPS = 1e-5

C_GA0, C_GF0, C_GA1, C_GF1, C_GM = 0, 8, 16, 24, 32
C_BA, C_BG = 40, 46
C_DW = 52
C_DWB, C_LNG, C_LNB = 238, 244, 250
C_MQ0, C_MQ1, C_MK, C_QG, C_KG, C_FLAG = 256, 257, 258, 259, 260, 261
NCP = 264

ENGS = ("pe", "act", "dve", "pool", "sp")


class Res:
    __slots__ = ("name", "w", "r", "dsem", "dcnt")

    def __init__(self, name):
        self.name = name
        self.w = None
        self.r = []
        self.dsem = None
        self.dcnt = 0


class Sched:
    def __init__(self, esem, dsems):
        self.esem = dict(esem)
        self.free_dsems = list(dsems)
        self.cnt = {e: 0 for e in ENGS}
        self.seen = {e: {} for e in ENGS}
        self.q = {e: [] for e in ENGS}
        self.dlast = {}
        self.nbank = 0

    def _deps(self, eng, reads, writes, skip_key=None):
        deps = {}

        def add(ev, same_ok):
            if ev is None:
                return
            k, v = ev
            if (k == eng or k == skip_key) and not same_ok:
                return
            if deps.get(k, 0) < v:
                deps[k] = v
        for r in reads:
            add(r.w, True)
        for w in writes:
            add(w.w, False)
            for ev in w.r:
                add(ev, False)
        waits = []
        for k, v in deps.items():
            if self.seen[eng].get(k, 0) < v:
                self.seen[eng][k] = v
                waits.append((self.esem[k], v))
        return waits

    def _commit(self, ev, reads, writes):
        for r in reads:
            r.r.append(ev)
        for w in writes:
            w.w = ev
            w.r = []

    def op(self, eng, fn, reads=(), writes=()):
        waits = self._deps(eng, reads, writes)
        self.cnt[eng] += 1
        ev = (eng, self.cnt[eng])
        sem = self.esem[eng]

        def run(e, waits=waits, fn=fn, sem=sem):
            for s, v in waits:
                e.wait_ge(s, v)
            fn(e).then_inc(sem, 1)
        self.q[eng].append(run)
        self._commit(ev, reads, writes)
        return ev

    def dma(self, q, out, in_, reads=(), writes=(), owner=None, fn=None, **kw):
        owner = owner or (writes[0] if writes else reads[0])
        if owner.dsem is None:
            owner.dsem = self.free_dsems.pop()
            self.esem[("d", id(owner))] = owner.dsem
        key = ("d", id(owner))
        waits = self._deps(q, reads, writes, skip_key=key)
        owner.dcnt += 16
        ev = (key, owner.dcnt)
        self.dlast[key] = owner.dcnt
        sem = owner.dsem

        def run(e, waits=waits, sem=sem, out=out, in_=in_, kw=kw, fn=fn):
            for s, v in waits:
                e.wait_ge(s, v)
            if fn is not None:
                fn(e).then_inc(sem, 16)
            else:
                e.dma_start(out=out, in_=in_, **kw).then_inc(sem, 16)
        self.q[q].append(run)
        self._commit(ev, reads, writes)
        return ev

    def raw(self, eng, fn, reads=()):
        waits = self._deps(eng, reads, ())

        def run(e, waits=waits, fn=fn):
            for s, v in waits:
                e.wait_ge(s, v)
            fn(e)
        self.q[eng].append(run)

    def barrier(self):
        evs = [(e, self.cnt[e]) for e in ENGS if self.cnt[e] > 0]
        evs += list(self.dlast.items())
        for eng in ENGS:
            waits = []
            for k, v in evs:
                if k == eng:
                    continue
                if self.seen[eng].get(k, 0) < v:
                    self.seen[eng][k] = v
                    waits.append((self.esem[k], v))
            if waits:
                def run(e, waits=waits):
                    for s, v in waits:
                        e.wait_ge(s, v)
                self.q[eng].append(run)

    def final_wait(self, eng):
        waits = []
        for k, v in self.dlast.items():
            if self.seen[eng].get(k, 0) < v:
                self.seen[eng][k] = v
                waits.append((self.esem[k], v))

        def run(e, waits=waits):
            for s, v in waits:
                e.wait_ge(s, v)
        self.q[eng].append(run)


def build_program(debug=()):
    nc = bass.Bass("TRN2", target_bir_lowering=False)

    def din(name, shape):
        return nc.dram_tensor(name, list(shape), F32, kind="ExternalInput").ap()

    xin = din("xin", [TC, D])
    mem = din("mem", [256, D])
    cpack = din("cpack", [128, NCP])
    ident = din("ident", [128, 128])
    bdm = din("bdm", [128, 128])
    maskexp = din("maskexp", [128, 3072])
    sinks_rep = din("sinks_rep", [1, 1536])
    w_mem_kv = din("w_mem_kv", [D, 512])
    cv_w_in = din("cv_w_in", [D, 1792])
    cv_w_out = din("cv_w_out", [D, D])
    cv_w_gate = din("cv_w_gate", [D, DFF0])
    cv_w_up = din("cv_w_up", [D, DFF0])
    cv_w_down = din("cv_w_down", [DFF0, D])
    sw_w_in = din("sw_w_in", [D, 1536])
    sw_w_out = din("sw_w_out", [D, D])
    sw_router = din("sw_router", [D, NE])
    sw_we_gate = din("sw_we_gate", [NE, D, DFFE])
    sw_we_up = din("sw_we_up", [NE, D, DFFE])
    sw_we_down = din("sw_we_down", [NE, DFFE, D])
    cst2 = din("cst2", [128, 296])
    WSG = nc.dram_tensor("wsg_scr", [NE * 7 * 128, 4096], BF16, kind="Internal").ap()
    WSU = nc.dram_tensor("wsu_scr", [NE * 7 * 128, 4096], BF16, kind="Internal").ap()
    XS = nc.dram_tensor("xs_scr", [8192, D], BF16, kind="Internal").ap()
    YS = nc.dram_tensor("ys_scr", [8192, D], F32, kind="Internal").ap()
    out = nc.dram_tensor("out", [TOK, D], F32, kind="ExternalOutput").ap()
    dbg_out = {}
    for name, shape in debug:
        dbg_out[name] = nc.dram_tensor("dbg_" + name, list(shape), F32, kind="ExternalOutput").ap()

    es = ExitStack()
    with es:
        def sb(name, shape, dt):
            return es.enter_context(nc.sbuf_tensor(name, list(shape), dt))

        HT = sb("HT", [128, NT, D], F32)
        RA = sb("RA", [128, 8, TC], BF16)
        AR = sb("AR", [128, 40960], BF16)
        CP = sb("CP", [128, NCP], F32)
        IDF = sb("IDF", [128, 128], F32)
        IDB = sb("IDB", [128, 128], BF16)
        BDB = sb("BDB", [128, 128], BF16)
        ONB = sb("ONB", [128, 128], BF16)
        ME = sb("ME", [128, 2, 4, 384], BF16)
        ME0 = sb("ME0", [128, 4, 384], BF16)
        SRB = sb("SRB", [1, 4, 384], BF16)
        MKT = sb("MKT", [128, 2, 256], BF16)
        MV = sb("MV", [128, 2, 256], BF16)
        SS = sb("SS", [128, 3, 32], F32)
        GT = sb("GT", [128, 16, 8], F32)
        WR = sb("WR", [128, 8, NE], F32)
        PS = es.enter_context(nc.psum_tensor("PS", [128, 8, 512], F32))

        esem = {e: es.enter_context(nc.semaphore("sem_" + e)) for e in ENGS}
        dsems = [es.enter_context(nc.semaphore("dsem%d" % i)) for i in range(72)]
        S = Sched(esem, dsems)

        banks = [Res("bank%d" % i) for i in range(8)]
        reserved = set()

        def bank(pair=False):
            while True:
                b = S.nbank % 8
                if pair and (b % 2 == 1 or (b + 1) in reserved):
                    S.nbank += 1
                    continue
                if b in reserved:
                    S.nbank += 1
                    continue
                break
            if pair:
                S.nbank += 2
                return PS[:, b:b + 2, :], [banks[b], banks[b + 1]]
            S.nbank += 1
            return PS[:, b, :], [banks[b]]

        def mmg(out_ap, pairs, reads, writes):
            def fn(e, out_ap=out_ap, pairs=pairs):
                n = len(pairs)
                ins = None
                for i, (l, r) in enumerate(pairs):
                    ins = e.matmul(out_ap, l, r, start=(i == 0), stop=(i == n - 1))
                return ins
            S.op("pe", fn, reads, writes)

        def act(out_ap, in_ap, func, reads, writes, **kw):
            S.op("act", lambda e: e.activation(out=out_ap, in_=in_ap, func=func, **kw), reads, writes)

        def tt(out_ap, in0, in1, op, reads, writes, eng="dve"):
            S.op(eng, lambda e: e.tensor_tensor(out=out_ap, in0=in0, in1=in1, op=op), reads, writes)

        def ts(out_ap, in0, s1, op0, reads, writes, s2=None, op1=None, eng="dve"):
            if op1 is None:
                S.op(eng, lambda e: e.tensor_scalar(out=out_ap, in0=in0, scalar1=s1, scalar2=None, op0=op0),
                     reads, writes)
            else:
                S.op(eng, lambda e: e.tensor_scalar(out=out_ap, in0=in0, scalar1=s1, scalar2=s2, op0=op0, op1=op1),
                     reads, writes)

        def stt(out_ap, in0, scalar, in1, op0, op1, reads, writes):
            S.op("dve", lambda e: e.scalar_tensor_tensor(out=out_ap, in0=in0, scalar=scalar, in1=in1,
                                                         op0=op0, op1=op1), reads, writes)

        def recip(out_ap, in_ap, reads, writes):
            S.op("dve", lambda e: e.reciprocal(out=out_ap, in_=in_ap), reads, writes)

        def copy(eng, out_ap, in_ap, reads, writes):
            if eng == "act":
                act(out_ap, in_ap, AF.Copy, reads, writes)
            else:
                S.op(eng, lambda e: e.tensor_copy(out=out_ap, in_=in_ap), reads, writes)

        def dump(name, ap, reads, view=None):
            if name in dbg_out:
                dst = dbg_out[name]
                if view:
                    dst = dst.rearrange(view[0], **view[1])
                S.dma("pool", dst, ap, reads=reads, writes=(), owner=R_dbg)

        R_dbg = Res("dbg")

        def cpc(c, n=1):
            return CP[:, c:c + n]

        def arv(off, n, dt=BF16):
            if dt == F32:
                return AR[:, off:off + 2 * n].bitcast(F32)
            return AR[:, off:off + n]

        R_HT = [Res("HT%d" % j) for j in range(NT)]
        R_RA = [Res("RA%d" % j) for j in range(NT + 1)]
        R_CP, R_ID, R_ME, R_SR = Res("CP"), Res("ID"), Res("ME"), Res("SR")
        R_MKV = Res("MKV")
        R_SS = Res("SS")

        def tcols(j):
            if j == 17:
                return 0, 32
            return 32 + 128 * j, 128

        S.dma("sp", CP[:], cpack[:, :], writes=[R_CP])
        S.dma("sp", IDF[:], ident[:, :], writes=[R_ID])
        R_BD = Res("BD")
        PB0 = 22016
        BDF = arv(PB0, 128, F32)
        S.dma("sp", BDF, bdm[:, :], writes=[R_BD])
        MEf = ME[:].rearrange("p a b c -> p (a b c)")
        S.dma("pool", MEf[:, 0:1536], maskexp[:, 0:1536], writes=[R_ME])
        S.dma("pool", MEf[:, 1536:3072], maskexp[:, 1536:3072], writes=[R_ME])
        SRF = arv(14336, 1536, F32)[0:1, :]
        S.dma("sp", SRF, sinks_rep[:, :], writes=[R_SR])
        WIN0 = arv(0, 8 * 1792).rearrange("p (k n) -> p k n", k=8)
        R_WIN0 = Res("WIN0")
        S.dma("pool", WIN0, cv_w_in.rearrange("(k p) n -> p k n", p=128), writes=[R_WIN0])
        R_XP = Res("XP")
        R_WS = Res("WS")
        pp_list = [(ws_, w_, ex, g_) for ex in range(NE) for g_ in range(7) for (ws_, w_) in ((WSG, sw_we_gate), (WSU, sw_we_up))]
        pp_state = {"i": 0}

        def prepass(n):
            for _ in range(n):
                if pp_state["i"] >= len(pp_list):
                    return
                ws_, w_, ex, g_ = pp_list[pp_state["i"]]
                pp_state["i"] += 1
                r0 = (ex * 7 + g_) * 128
                S.dma("pool", ws_[r0:r0 + 128, :].rearrange("p (k n) -> p k n", k=8),
                      w_[ex, :, g_ * 512:(g_ + 1) * 512].rearrange("(k p) n -> p k n", p=128),
                      writes=[R_WS], owner=R_WS)
        ZT = sb("ZT", [128, 2048], BF16)
        R_ZT = Res("ZT")
        R_XS = Res("XS")
        S.op("dve", lambda e: e.memset(ZT[:], 0.0), [], [R_ZT])
        XP = arv(PB0 + 256, D, F32)
        S.dma("sp", XP[0:32, :], xin[0:32, :], writes=[R_XP])
        R_MEM = Res("MEM")
        MEMT = arv(PB0 + 2304, 2 * D, F32).rearrange("p (t d) -> p t d", t=2)
        S.dma("sp", MEMT, mem.rearrange("(t p) d -> p t d", p=128), writes=[R_MEM])
        R_WKV = Res("WKV")
        WKV = arv(PB0 + 6400, 8 * 512).rearrange("p (k n) -> p k n", k=8)
        S.dma("pool", WKV, w_mem_kv.rearrange("(k p) n -> p k n", p=128), writes=[R_WKV])
        for j0, j1 in ((0, 2), (2, 5), (5, 9), (9, 13), (13, 17)):
            S.dma("sp", HT[:, j0:j1, :],
                  xin[32 + 128 * j0:32 + 128 * j1, :].rearrange("(j p) d -> p j d", p=128),
                  writes=R_HT[j0:j1])
        R_WR = Res("WR")
        S.dma("sp", WR[:], sw_router.rearrange("(k p) n -> p k n", p=128), writes=[R_WR])
        XSz = XS.rearrange("(a p r) d -> a p (r d)", p=128, r=2)
        for a_ in range(32):
            S.dma("sp", XSz[a_], ZT[:], reads=[R_ZT], writes=[R_XS], owner=R_XS)
        prepass(8)

        copy("act", IDB[:], IDF[:], [R_ID], [R_ID])
        copy("act", BDB[:], BDF, [R_BD], [R_BD])
        S.op("dve", lambda e: e.memset(ONB[:], 1.0), [], [R_ID])
        act(SRB[:].rearrange("p a b -> p (a b)"), SRF, AF.Exp, [R_SR], [R_SR])
        ts(ME0[:], ME[:, 0], cpc(C_FLAG), ALU.mult, [R_ME, R_CP], [R_ME])

        A_TAIL = 37888
        HNB = [arv(A_TAIL + i * 1024, 1024) for i in range(2)]
        R_HNB = [Res("HNB%d" % i) for i in range(2)]
        JUNK = arv(A_TAIL + 2048, 1024)
        R_JUNK = Res("JUNK")
        ss_state = {"n": 0}

        def norm_transpose(j, gcol, src_ap=None, src_res=None, npart=128, fp32_path=None):
            k = ss_state["n"] % 32
            ss_state["n"] += 1
            if src_ap is None:
                src_ap, src_res = HT[:, j, :], R_HT[j]
            c0, n = tcols(j)
            P = slice(0, npart)
            act(JUNK[P, :], src_ap, AF.Square, [src_res], [R_JUNK, R_SS], accum_out=SS[P, 0, k:k + 1])
            act(SS[P, 1, k:k + 1], SS[P, 0, k:k + 1], AF.Sqrt, [R_SS], [R_SS], scale=1.0 / D, bias=RMS_EPS)
            recip(SS[P, 2, k:k + 1], SS[P, 1, k:k + 1], [R_SS], [R_SS])
            if fp32_path is None:
                s = j % 2
                ts(HNB[s][P, :], src_ap, SS[P, 2, k:k + 1], ALU.mult, [src_res, R_SS], [R_HNB[s]])
                pb, pr = bank()
                pv = pb.bitcast(BF16).rearrange("p (k c) -> p k c", k=8)

                def fn(e, pv=pv, s=s, P=P, n=n):
                    ins = None
                    for kc in range(8):
                        ins = e.transpose(out=pv[:, kc, 0:n], in_=HNB[s][P, kc * 128:(kc + 1) * 128],
                                          identity=IDB[P, 0:n])
                    return ins
                S.op("pe", fn, [R_HNB[s], R_ID], pr)
                g_b = CP[:, gcol:gcol + 8].unsqueeze(2).to_broadcast([128, 8, n])
                tt(RA[:, :, c0:c0 + n], pv[:, :, 0:n], g_b, ALU.mult, pr + [R_CP], [R_RA[j]])
            else:
                HN32, R_HN32, H32T, R_H32T = fp32_path
                s = j % 2
                ts(HN32[s], src_ap, SS[:, 2, k:k + 1], ALU.mult, [src_res, R_SS], [R_HN32[s]])
                pb, pr = bank(pair=True)
                pv = pb.rearrange("p b (k c) -> p (b k) c", k=4)

                def fn(e, pv=pv, s=s):
                    ins = None
                    for kc in range(8):
                        ins = e.transpose(out=pv[:, kc, :], in_=HN32[s][:, kc * 128:(kc + 1) * 128],
                                          identity=IDF[:])
                    return ins
                S.op("pe", fn, [R_HN32[s], R_ID], pr)
                g_b = CP[:, gcol:gcol + 8].unsqueeze(2).to_broadcast([128, 8, 128])
                tt(RA[:, :, c0:c0 + n], pv, g_b, ALU.mult, pr + [R_CP], [R_RA[j]])
                tt(H32T[s], pv, g_b, ALU.mult, pr + [R_CP], [R_H32T[s]])

        hn_state = {"n": 0}

        def headnorm(q_ap, q_res, gcol, out_ap, out_res, n, bufs, split=None):
            SQ, R_SQ, RT, R_RT = bufs
            s = hn_state["n"] % 2
            hn_state["n"] += 1
            act(SQ[s][:, :n], q_ap, AF.Square, q_res, [R_SQ[s]])
            sb_, sr_ = bank()
            mmg(sb_[:, :n], [(BDB[:], SQ[s][:, :n])], [R_SQ[s], R_BD], sr_)
            act(RT[s][:, :n], sb_[:, :n], AF.Sqrt, sr_, [R_RT[s]], bias=RMS_EPS)
            recip(RT[s][:, :n], RT[s][:, :n], [R_RT[s]], [R_RT[s]])
            qv, rv = q_ap, RT[s][:, :n]
            if split:
                qv = qv.rearrange("p (a b) -> p a b", b=split)
                rv = rv.rearrange("p (a b) -> p a b", b=split)
            stt(out_ap, qv, cpc(gcol), rv, ALU.mult, ALU.mult, q_res + [R_RT[s], R_CP], out_res)

        def mem_attention(QM, R_QM, blocks, qcol_off, bufs):
            PT, R_PT, RD, R_RD = bufs
            st = {"n": 0}
            for (c0, n, tiles) in blocks:
                q0 = c0 - qcol_off
                for hd in range(4):
                    c2, hb = hd // 2, hd % 2
                    rows = slice(hb * 64, hb * 64 + 64)
                    pts = []
                    for mc in range(2):
                        s = st["n"] % 4
                        st["n"] += 1
                        sb_, sr_ = bank()
                        mmg(sb_[:, :n], [(MKT[rows, c2, mc * 128:(mc + 1) * 128], QM[rows, c2, q0:q0 + n])],
                            [R_MKV, R_QM], sr_)
                        act(PT[s][:, :n], sb_[:, :n], AF.Exp, sr_, [R_PT[s]], scale=0.125)
                        pts.append(s)
                    ob, orr = bank()
                    mmg(ob[rows, :n], [(MV[:, mc, hd * 64:(hd + 1) * 64], PT[pts[mc]][:, :n]) for mc in range(2)],
                        [R_MKV] + [R_PT[s] for s in pts], orr)
                    db, dr = bank()
                    mmg(db[rows, :n], [(ONB[:, 0:64], PT[pts[mc]][:, :n]) for mc in range(2)],
                        [R_ID] + [R_PT[s] for s in pts], dr)
                    s2 = (st["n"] // 2) % 2
                    recip(RD[s2][rows, :n], db[rows, :n], dr, [R_RD[s2]])
                    tt(RA[rows, 6 + c2, c0:c0 + n], ob[rows, :n], RD[s2][rows, :n], ALU.mult,
                       orr + [R_RD[s2]], [R_RA[t] for t in tiles])

        def out_proj(WO, R_WO, tiles):
            for j in tiles:
                c0, n = tcols(j)
                yb, yr = bank(pair=True)
                for half in range(2):
                    mmg(yb[:, half, :], [(RA[:, kc, c0:c0 + 128], WO[:, kc, half * 512:(half + 1) * 512])
                                         for kc in range(8)], [R_RA[j], R_WO], [yr[half]])
                tt(HT[:, j, :], yb.rearrange("p b c -> p (b c)"), HT[:, j, :], ALU.add, yr + [R_HT[j]], [R_HT[j]])

        def ffn(groups, blocks, WB, R_WB, ACTB, R_ACTB, SIL, R_SIL, after_load=None):
            steps = [(gi, bi) for gi in range(len(groups)) for bi in range(len(blocks))]

            def load(gi):
                g = groups[gi]
                s = gi % 2
                nfc = g["nfc"]
                wg, wu, wd = WB[s]
                S.dma("pool", wg[:, :, 0:nfc * 128], g["g"].rearrange("(k p) n -> p k n", p=128),
                      writes=[R_WB[s][0]])
                S.dma("pool", wu[:, :, 0:nfc * 128], g["u"].rearrange("(k p) n -> p k n", p=128),
                      writes=[R_WB[s][1]])
                S.dma("pool", wd[:, 0:nfc, :], g["d"].rearrange("(f p) n -> p f n", p=128),
                      writes=[R_WB[s][2]])
                if after_load is not None:
                    after_load()

            def gu(si):
                gi, bi = steps[si]
                g = groups[gi]
                s = gi % 2
                a = si % 2
                wg, wu, wd = WB[s]
                c0, n, tiles = blocks[bi]
                rr = [R_RA[t] for t in tiles]
                for fc in range(g["nfc"]):
                    gb, gr = bank()
                    mmg(gb[:, :n], [(wg[:, kc, fc * 128:(fc + 1) * 128], RA[:, kc, c0:c0 + n]) for kc in range(8)],
                        rr + [R_WB[s][0]], gr)
                    ub, ur = bank()
                    mmg(ub[:, :n], [(wu[:, kc, fc * 128:(fc + 1) * 128], RA[:, kc, c0:c0 + n]) for kc in range(8)],
                        rr + [R_WB[s][1]], ur)
                    sl = (si * 4 + fc) % 2
                    act(SIL[sl][:, :n], gb[:, :n], AF.Silu, gr, [R_SIL[sl]])
                    tt(ACTB[a][:, fc, :n], ub[:, :n], SIL[sl][:, :n], ALU.mult, ur + [R_SIL[sl]], [R_ACTB[a][fc]])

            def down(si):
                gi, bi = steps[si]
                g = groups[gi]
                s = gi % 2
                a = si % 2
                wg, wu, wd = WB[s]
                c0, n, tiles = blocks[bi]
                nfc = g["nfc"]
                for ti, j in enumerate(tiles):
                    yb, yr = bank(pair=True)
                    for half in range(2):
                        mmg(yb[:, half, :], [(ACTB[a][:, fc, ti * 128:(ti + 1) * 128],
                                              wd[:, fc, half * 512:(half + 1) * 512]) for fc in range(nfc)],
                            [R_ACTB[a][fc] for fc in range(nfc)] + [R_WB[s][2]], [yr[half]])
                    yv = yb.rearrange("p b c -> p (b c)")
                    if g["gate"] is None:
                        tt(HT[:, j, :], yv, HT[:, j, :], ALU.add, yr + [R_HT[j]], [R_HT[j]])
                    else:
                        stt(HT[:, j, :], yv, g["gate"](j), HT[:, j, :], ALU.mult, ALU.add,
                            yr + [R_HT[j], R_GT], [R_HT[j]])

            load(0)
            if len(groups) > 1:
                load(1)
            nb = len(blocks)
            for si in range(len(steps)):
                gu(si)
                if si > 0:
                    down(si - 1)
                    gi_prev, bi_prev = steps[si - 1]
                    if bi_prev == nb - 1 and gi_prev + 2 < len(groups):
                        load(gi_prev + 2)
            down(len(steps) - 1)

        R_GT = Res("GT")

        MEMX = arv(PB0 + 10496, 8 * 256).rearrange("p (k c) -> p k c", k=8)
        R_MEMX = [Res("MEMX0"), Res("MEMX1")]
        SQb = [arv(PB0 + 12544 + i * 512, 512) for i in range(2)]
        R_SQb = [Res("SQ%d" % i) for i in range(2)]
        RTb = [arv(PB0 + 13568 + i * 1024, 512, F32) for i in range(2)]
        R_RTb = [Res("RT%d" % i) for i in range(2)]
        hbufs = (SQb, R_SQb, RTb, R_RTb)
        for t in range(2):
            k = ss_state["n"] % 32
            ss_state["n"] += 1
            act(JUNK[:], MEMT[:, t, :], AF.Square, [R_MEM], [R_JUNK, R_SS], accum_out=SS[:, 0, k:k + 1])
            act(SS[:, 1, k:k + 1], SS[:, 0, k:k + 1], AF.Sqrt, [R_SS], [R_SS], scale=1.0 / D, bias=RMS_EPS)
            recip(SS[:, 2, k:k + 1], SS[:, 1, k:k + 1], [R_SS], [R_SS])
            ts(HNB[t][:], MEMT[:, t, :], SS[:, 2, k:k + 1], ALU.mult, [R_MEM, R_SS], [R_HNB[t]])
            pb, pr = bank()
            pv = pb.bitcast(BF16).rearrange("p (k c) -> p k c", k=8)

            def fn(e, pv=pv, t=t):
                ins = None
                for kc in range(8):
                    ins = e.transpose(out=pv[:, kc, :], in_=HNB[t][:, kc * 128:(kc + 1) * 128], identity=IDB[:])
                return ins
            S.op("pe", fn, [R_HNB[t], R_ID], pr)
            g_b = CP[:, C_GM:C_GM + 8].unsqueeze(2).to_broadcast([128, 8, 128])
            tt(MEMX[:, :, t * 128:(t + 1) * 128], pv, g_b, ALU.mult, pr + [R_CP], [R_MEMX[t]])
        for c in range(2):
            kb_, kr_ = bank()
            mmg(kb_[:, :256], [(WKV[:, kc, c * 128:(c + 1) * 128], MEMX[:, kc, :]) for kc in range(8)],
                R_MEMX + [R_WKV], kr_)
            headnorm(kb_[:, :256], kr_, C_MK, MKT[:, c, :], [R_MKV], 256, hbufs)
        for t in range(2):
            vb_, vr_ = bank()
            mmg(vb_[:, :256], [(MEMX[:, kc, t * 128:(t + 1) * 128], WKV[:, kc, 256:512]) for kc in range(8)],
                [R_MEMX[t], R_WKV], vr_)
            copy("act", MV[:, t, :], vb_[:, :256], vr_, [R_MKV])
        dump("mkt", MKT[:].rearrange("p a b -> p (a b)"), [R_MKV])
        dump("mv", MV[:].rearrange("p a b -> p (a b)"), [R_MKV])

        norm_transpose(17, C_GA0, src_ap=XP[0:32, :], src_res=R_XP, npart=32)
        for j in range(NT):
            norm_transpose(j, C_GA0)
        S.barrier()
        prepass(8)
        U = arv(14336, 6 * TC).rearrange("p (c t) -> p c t", c=6)
        R_U = [Res("U%d" % c) for c in range(6)]
        QM0 = arv(27584, 2 * 2176).rearrange("p (c t) -> p c t", c=2)
        R_QM0 = Res("QM0")
        SG = [arv(31936 + i * 1024, 512, F32) for i in range(2)]
        R_SG = [Res("SG%d" % i) for i in range(2)]
        SQb = [arv(33984 + i * 512, 512) for i in range(2)]
        RTb = [arv(35008 + i * 1024, 512, F32) for i in range(2)]
        hbufs = (SQb, R_SQb, RTb, R_RTb)
        TB0 = [(0, 160, [17, 0])] + [(160 + 512 * i, 512, [1 + 4 * i + t for t in range(4)]) for i in range(4)]
        sgi = 0
        for c in range(6):
            for (c0, n, tiles) in TB0:
                rr = [R_RA[t] for t in tiles]
                ab, ar_ = bank()
                mmg(ab[:, :n], [(WIN0[:, kc, c * 128:(c + 1) * 128], RA[:, kc, c0:c0 + n]) for kc in range(8)],
                    rr + [R_WIN0], ar_)
                gb, gr = bank()
                mmg(gb[:, :n], [(WIN0[:, kc, (6 + c) * 128:(7 + c) * 128], RA[:, kc, c0:c0 + n]) for kc in range(8)],
                    rr + [R_WIN0], gr)
                s = sgi % 2
                sgi += 1
                act(SG[s][:, :n], gb[:, :n], AF.Sigmoid, gr + [R_CP], [R_SG[s]], bias=cpc(C_BG + c))
                stt(U[:, c, c0:c0 + n], ab[:, :n], cpc(C_BA + c), SG[s][:, :n], ALU.add, ALU.mult,
                    ar_ + [R_SG[s], R_CP], [R_U[c]])
                if c0 == 0:
                    ts(U[:, c, 0:160], U[:, c, 0:160], cpc(C_FLAG), ALU.mult, [R_U[c], R_CP], [R_U[c]])
        TBC = [(32, 128, [0])] + TB0[1:]
        for c2 in range(2):
            for (c0, n, tiles) in TBC:
                rr = [R_RA[t] for t in tiles]
                qb, qr = bank()
                mmg(qb[:, :n], [(WIN0[:, kc, (12 + c2) * 128:(13 + c2) * 128], RA[:, kc, c0:c0 + n])
                                for kc in range(8)], rr + [R_WIN0], qr)
                headnorm(qb[:, :n], qr, C_MQ0, QM0[:, c2, c0 - 32:c0 - 32 + n], [R_QM0], n, hbufs)
        dump("u", U[:, 0, :], [R_U[0]])
        dump("qm0", QM0[:, 0, :], [R_QM0])
        S.barrier()
        prepass(12)
        DG = [arv(i * 3968, 3968).rearrange("p (k c) -> p k c", k=CONV_W) for i in range(2)]
        R_DG = [Res("DG%d" % i) for i in range(2)]
        for c in range(6):
            s = c % 2
            S.op("dve", lambda e, s=s, c=c: e.tensor_tensor(
                out=DG[s], in0=IDB[:].unsqueeze(1).to_broadcast([128, CONV_W, 128]),
                in1=CP[:, C_DW + c * CONV_W:C_DW + (c + 1) * CONV_W].unsqueeze(2).to_broadcast([128, CONV_W, 128]),
                op=ALU.mult), [R_ID, R_CP], [R_DG[s]])
            for (c0, n, tiles) in TBC:
                cb, cr = bank()
                mmg(cb[:, :n], [(DG[s][:, k, :], U[:, c, c0 - 30 + k:c0 - 30 + k + n]) for k in range(CONV_W)],
                    [R_DG[s], R_U[c]], cr)
                act(RA[:, c, c0:c0 + n], cb[:, :n], AF.Identity, cr + [R_CP], [R_RA[t] for t in tiles],
                    bias=cpc(C_DWB + c))
        dump("conv", RA[:, 0, 32:TC], [R_RA[t] for t in range(NT)])
        SQL = [arv(7936 + i * 512, 512) for i in range(2)]
        R_SQL = [Res("SQL%d" % i) for i in range(2)]
        MSQ = arv(8960, 512, F32)
        VAR = arv(9984, 512, F32)
        R_ST = Res("LNST")
        T1 = [arv(11008 + i * 1024, 512, F32) for i in range(2)]
        R_T1 = [Res("T1%d" % i) for i in range(2)]
        sqi = 0
        for (c0, n, tiles) in TBC:
            rr = [R_RA[t] for t in tiles]
            s1b, s1r = bank()
            mmg(s1b[:, :n], [(ONB[:], RA[:, c, c0:c0 + n]) for c in range(6)], rr + [R_ID], s1r)
            s2b, s2r = bank()
            sqs = []
            for c in range(6):
                s = sqi % 2
                sqi += 1
                act(SQL[s][:, :n], RA[:, c, c0:c0 + n], AF.Square, rr, [R_SQL[s]])
                S.op("pe", lambda e, s2b=s2b, s=s, n=n, c=c: e.matmul(s2b[:, :n], ONB[:], SQL[s][:, :n],
                                                                    start=(c == 0), stop=(c == 5)),
                     [R_SQL[s], R_ID], s2r)
            act(MSQ[:, :n], s1b[:, :n], AF.Square, s1r, [R_ST], scale=1.0 / CONV_CH)
            stt(VAR[:, :n], s2b[:, :n], 1.0 / CONV_CH, MSQ[:, :n], ALU.mult, ALU.subtract, s2r + [R_ST], [R_ST])
            act(VAR[:, :n], VAR[:, :n], AF.Sqrt, [R_ST], [R_ST], bias=LN_EPS)
            recip(VAR[:, :n], VAR[:, :n], [R_ST], [R_ST])
            for c in range(6):
                s = c % 2
                stt(T1[s][:, :n], s1b[:, :n], -1.0 / CONV_CH, RA[:, c, c0:c0 + n], ALU.mult, ALU.add,
                    s1r + rr, [R_T1[s]])
                tt(T1[s][:, :n], T1[s][:, :n], VAR[:, :n], ALU.mult, [R_T1[s], R_ST], [R_T1[s]])
                act(RA[:, c, c0:c0 + n], T1[s][:, :n], AF.Silu, [R_T1[s], R_CP], rr,
                    scale=cpc(C_LNG + c), bias=cpc(C_LNB + c))
        dump("cln", RA[:, 0, 32:TC], [R_RA[t] for t in range(NT)])
        S.barrier()
        PT = [arv(i * 512, 512) for i in range(4)]
        R_PT = [Res("PT%d" % i) for i in range(4)]
        RD = [arv(2048 + i * 1024, 512, F32) for i in range(2)]
        R_RD = [Res("RD%d" % i) for i in range(2)]
        WO0 = arv(14336, 8 * D).rearrange("p (k n) -> p k n", k=8)
        R_WO0 = Res("WO0")
        S.dma("pool", WO0, cv_w_out.rearrange("(k p) n -> p k n", p=128), writes=[R_WO0])
        prepass(8)
        mem_attention(QM0, R_QM0, TBC, 32, (PT, R_PT, RD, R_RD))
        dump("cat0", RA[:, 6, 32:TC], [R_RA[t] for t in range(NT)])
        out_proj(WO0, R_WO0, list(range(NT)))
        dump("h0a", HT[:, 1, :], [R_HT[1]])
        S.barrier()
        WB = []
        R_WB = []
        for s in range(2):
            o = s * 12288
            WB.append((arv(o, 4096).rearrange("p (k n) -> p k n", k=8),
                       arv(o + 4096, 4096).rearrange("p (k n) -> p k n", k=8),
                       arv(o + 8192, 4096).rearrange("p (f n) -> p f n", f=4)))
            R_WB.append([Res("WB%d_%d" % (s, i)) for i in range(3)])
        ACTB = [arv(24576 + a * 2048, 2048).rearrange("p (f n) -> p f n", f=4) for a in range(2)]
        R_ACTB = [[Res("ACTB%d_%d" % (a, f)) for f in range(4)] for a in range(2)]
        SIL = [arv(28672 + i * 512, 512) for i in range(2)]
        R_SIL = [Res("SIL%d" % i) for i in range(2)]
        for j in range(NT):
            norm_transpose(j, C_GF0)
        groups0 = []
        for f0 in range(0, DFF0, 512):
            nf = min(512, DFF0 - f0)
            groups0.append(dict(g=cv_w_gate[:, f0:f0 + nf], u=cv_w_up[:, f0:f0 + nf], d=cv_w_down[f0:f0 + nf, :],
                                nfc=nf // 128, gate=None))
        ffn(groups0, TBC, WB, R_WB, ACTB, R_ACTB, SIL, R_SIL, after_load=lambda: prepass(6))
        dump("h1", HT[:, 1, :], [R_HT[1]])
        S.barrier()

        for j in range(NT):
            norm_transpose(j, C_GA1)
        WIN1 = arv(0, 8 * 1536).rearrange("p (k n) -> p k n", k=8)
        R_WIN1 = Res("WIN1")
        QPAIR = [(0, 3), (1, 4), (2, 5), (6, 9), (7, 10), (8, 11)]
        for i, (ha, hb_) in enumerate(QPAIR):
            for half, hh in enumerate((ha, hb_)):
                S.dma("pool", WIN1[:, :, i * 128 + half * 64:i * 128 + half * 64 + 64],
                      sw_w_in[:, hh * 64:(hh + 1) * 64].rearrange("(k p) n -> p k n", p=128), writes=[R_WIN1])
        S.dma("pool", WIN1[:, :, 768:1536], sw_w_in[:, 768:1536].rearrange("(k p) n -> p k n", p=128),
              writes=[R_WIN1])
        prepass(8)
        QT = arv(12288, 6 * TOK).rearrange("p (a n g q) -> p a n g q", a=2, n=16, g=3)
        R_QT = Res("QT")
        KT = arv(24576, 2 * 2176).rearrange("p (c t) -> p c t", c=2)
        R_KT = Res("KT")
        VT = arv(28928, NT * 256).rearrange("p (j d) -> p j d", j=NT)
        R_VT = Res("VT")
        QM1 = arv(33280, 2 * TOK).rearrange("p (c t) -> p c t", c=2)
        R_QM1 = Res("QM1")
        SQb = [arv(37376 + i * 512, 512) for i in range(2)]
        RTb = [arv(38400 + i * 1024, 512, F32) for i in range(2)]
        hbufs = (SQb, R_SQb, RTb, R_RTb)
        S.barrier()
        TB1 = TB0[1:]
        for i in range(6):
            for bi, (c0, n, tiles) in enumerate(TB1):
                rr = [R_RA[t] for t in tiles]
                qb, qr = bank()
                mmg(qb[:, :n], [(WIN1[:, kc, i * 128:(i + 1) * 128], RA[:, kc, c0:c0 + n]) for kc in range(8)],
                    rr + [R_WIN1], qr)
                headnorm(qb[:, :n], qr, C_QG, QT[:, i // 3, 4 * bi:4 * bi + 4, i % 3, :], [R_QT], n, hbufs,
                         split=128)
        for c2 in range(2):
            for (c0, n, tiles) in TBC:
                rr = [R_RA[t] for t in tiles]
                kb_, kr_ = bank()
                mmg(kb_[:, :n], [(WIN1[:, kc, 768 + c2 * 128:768 + (c2 + 1) * 128], RA[:, kc, c0:c0 + n])
                                 for kc in range(8)], rr + [R_WIN1], kr_)
                headnorm(kb_[:, :n], kr_, C_KG, KT[:, c2, c0 - 32:c0 - 32 + n], [R_KT], n, hbufs)
        for j in range(NT):
            c0, n = tcols(j)
            vb_, vr_ = bank()
            mmg(vb_[:, :256], [(RA[:, kc, c0:c0 + 128], WIN1[:, kc, 1024:1280]) for kc in range(8)],
                [R_RA[j], R_WIN1], vr_)
            copy("act", VT[:, j, :], vb_[:, :256], vr_, [R_VT])
        for c2 in range(2):
            for (c0, n, tiles) in TB1:
                rr = [R_RA[t] for t in tiles]
                qb, qr = bank()
                mmg(qb[:, :n], [(WIN1[:, kc, 1280 + c2 * 128:1280 + (c2 + 1) * 128], RA[:, kc, c0:c0 + n])
                                for kc in range(8)], rr + [R_WIN1], qr)
                headnorm(qb[:, :n], qr, C_MQ1, QM1[:, c2, c0 - 160:c0 - 160 + n], [R_QM1], n, hbufs)
        dump("qt", QT[:, 0, :, 0, :], [R_QT], view=("p (n q) -> p n q", dict(n=16)))
        dump("kt", KT[:, 0, :], [R_KT])
        S.barrier()
        prepass(12)
        ET = [arv(i * 384, 384) for i in range(2)]
        R_ET = [Res("ET%d" % i) for i in range(2)]
        PT1 = [arv(768 + i * 384, 384) for i in range(4)]
        R_PT1 = [Res("PT1%d" % i) for i in range(4)]
        RD1 = [arv(2304 + i * 768, 384, F32) for i in range(2)]
        R_RD1 = [Res("RD1%d" % i) for i in range(2)]
        ei = 0
        pi = 0
        for nblk in range(16):
            j = nblk + 1
            rc0, _ = tcols(j)
            for kvh in range(4):
                rows = slice((kvh % 2) * 64, (kvh % 2) * 64 + 64)
                qc0 = (kvh // 2) * 3
                pts = []
                for kb in range(2):
                    jk = j - 1 + kb
                    sb_, sr_ = bank()
                    mmg(sb_[:, :384], [(KT[rows, kvh // 2, jk * 128:(jk + 1) * 128],
                                        QT[rows, kvh // 2, nblk, :, :].rearrange("p g q -> p (g q)"))],
                        [R_KT, R_QT], sr_)
                    s = ei % 2
                    ei += 1
                    act(ET[s][:], sb_[:, :384], AF.Exp, sr_, [R_ET[s]], scale=0.125)
                    p = pi % 4
                    pi += 1
                    msk = ME0[:, kvh, :] if (nblk == 0 and kb == 0) else ME[:, kb, kvh, :]
                    tt(PT1[p][:], ET[s][:], msk, ALU.mult, [R_ET[s], R_ME], [R_PT1[p]])
                    pts.append((p, jk))
                ob, orr = bank()
                mmg(ob[rows, :384], [(VT[:, jk, kvh * 64:(kvh + 1) * 64], PT1[p][:]) for (p, jk) in pts],
                    [R_VT] + [R_PT1[p] for (p, _) in pts], orr)
                db, dr = bank()
                mmg(db[rows, :384], [(ONB[:, 0:64], PT1[p][:]) for (p, _) in pts] + [(ONB[0:1, 0:64], SRB[0:1, kvh, :])],
                    [R_ID, R_SR] + [R_PT1[p] for (p, _) in pts], dr)
                s2 = (nblk * 4 + kvh) % 2
                recip(RD1[s2][rows, :], db[rows, :384], dr, [R_RD1[s2]])
                tt(RA[rows, qc0:qc0 + 3, rc0:rc0 + 128], ob[rows, :384].rearrange("p (g q) -> p g q", g=3),
                   RD1[s2][rows, :].rearrange("p (g q) -> p g q", g=3), ALU.mult,
                   orr + [R_RD1[s2]], [R_RA[j]])
        dump("swa", RA[:, 0, 160:TC], [R_RA[t] for t in range(1, NT)])
        S.barrier()
        WO1 = arv(14336, 8 * D).rearrange("p (k n) -> p k n", k=8)
        R_WO1 = Res("WO1")
        for i, (ha, hb_) in enumerate(QPAIR):
            S.dma("pool", WO1[0:64, i, :], sw_w_out[ha * 64:(ha + 1) * 64, :], writes=[R_WO1])
            S.dma("pool", WO1[64:128, i, :], sw_w_out[hb_ * 64:(hb_ + 1) * 64, :], writes=[R_WO1])
        S.dma("pool", WO1[:, 6:8, :], sw_w_out[768:1024, :].rearrange("(k p) n -> p k n", p=128), writes=[R_WO1])
        prepass(8)
        PT = [arv(i * 512, 512) for i in range(4)]
        RD = [arv(2048 + i * 1024, 512, F32) for i in range(2)]
        mem_attention(QM1, R_QM1, TB1, 160, (PT, R_PT, RD, R_RD))
        out_proj(WO1, R_WO1, list(range(1, NT)))
        dump("h1a", HT[:, 1, :], [R_HT[1]])
        S.barrier()
        HN32 = [arv(29696 + i * 2048, 1024, F32) for i in range(2)]
        R_HN32 = [Res("HN32%d" % i) for i in range(2)]
        H32T = [arv(33792 + i * 2048, 1024, F32).rearrange("p (k c) -> p k c", k=8) for i in range(2)]
        R_H32T = [Res("H32T%d" % i) for i in range(2)]
        prepass(1000)
        reserved.add(7)
        LG = PS[:, 7, 0:128].rearrange("p (j e) -> p j e", j=16)
        R_LG = banks[7]
        g_b128 = CP[:, C_GF1:C_GF1 + 8].unsqueeze(2).to_broadcast([128, 8, 128])
        rs_col = {}
        for j in range(1, NT):
            k = ss_state["n"] % 32
            ss_state["n"] += 1
            rs_col[j] = k
            s = j % 2
            act(JUNK[:], HT[:, j, :], AF.Square, [R_HT[j]], [R_JUNK, R_SS], accum_out=SS[:, 0, k:k + 1])
            act(SS[:, 1, k:k + 1], SS[:, 0, k:k + 1], AF.Sqrt, [R_SS], [R_SS], scale=1.0 / D, bias=RMS_EPS)
            recip(SS[:, 2, k:k + 1], SS[:, 1, k:k + 1], [R_SS], [R_SS])
            ts(HN32[s], HT[:, j, :], SS[:, 2, k:k + 1], ALU.mult, [R_HT[j], R_SS], [R_HN32[s]])
            pb, pr = bank(pair=True)
            pv = pb.rearrange("p b (k c) -> p (b k) c", k=4)

            def fn(e, pv=pv, s=s):
                ins = None
                for kc in range(8):
                    ins = e.transpose(out=pv[:, kc, :], in_=HN32[s][:, kc * 128:(kc + 1) * 128], identity=IDF[:])
                return ins
            S.op("pe", fn, [R_HN32[s], R_ID], pr)
            tt(H32T[s], pv, g_b128, ALU.mult, pr + [R_CP], [R_H32T[s]])
            mmg(LG[:, j - 1, :], [(H32T[s][:, kc, :], WR[:, kc, :]) for kc in range(8)],
                [R_H32T[s], R_WR], [R_LG])
        ga_state = {"o": 0}

        def ga(n, dt=F32):
            o = ga_state["o"]
            ga_state["o"] += 2 * n if dt != BF16 else n
            if dt == BF16:
                return arv(o, n)
            if dt == F32:
                return arv(o, n, F32)
            return AR[:, o:o + 2 * n].bitcast(dt)

        def v168(ap):
            return ap.rearrange("p (j e) -> p j e", j=16)

        def b168(ap16):
            return ap16.unsqueeze(2).to_broadcast([128, 16, 8])
        R_G = Res("GATE")
        gL, gEQ1, gL2, gEQ2, gSEL, gCUM, gTMP, gSLOT = [ga(128) for _ in range(8)]
        gV1, gV2, gE, gS1, gS2, gET = [ga(16) for _ in range(6)]
        W12 = sb("W12", [128, 32], F32)
        gW1, gW2 = W12[:, 0:16], W12[:, 16:32]
        gNE, gTL, gOE, gOF = [ga(8) for _ in range(4)]
        SELB, CUMB = ga(128, BF16), ga(128, BF16)
        SI = sb("SIT", [128, 32], I32)
        ETI = ga(16, I32)
        LTB = ga(128, BF16)
        R_C2 = Res("C2")
        C2F = ga(296)
        S.dma("sp", C2F, cst2[:, :], writes=[R_C2])
        copy("act", LTB, C2F[:, 0:128], [R_C2], [R_C2])
        RG, WG_ = [R_G], [R_G]
        copy("act", gL, LG.rearrange("p j e -> p (j e)"), [R_LG], WG_)
        S.op("dve", lambda e: e.tensor_reduce(out=gV1, in_=v168(gL), op=ALU.max, axis=AX.X), RG, WG_)
        tt(v168(gEQ1), v168(gL), b168(gV1), ALU.is_equal, RG, WG_)
        stt(gL2, gEQ1, -1e30, gL, ALU.mult, ALU.add, RG, WG_)
        S.op("dve", lambda e: e.tensor_reduce(out=gV2, in_=v168(gL2), op=ALU.max, axis=AX.X), RG, WG_)
        tt(v168(gEQ2), v168(gL2), b168(gV2), ALU.is_equal, RG, WG_)
        tt(gE, gV2, gV1, ALU.subtract, RG, WG_)
        act(gE, gE, AF.Exp, RG, WG_)
        ts(gW1, gE, 1.0, ALU.add, RG, WG_)
        recip(gW1, gW1, RG, WG_)
        tt(gW2, gE, gW1, ALU.mult, RG, WG_)
        tt(gSEL, gEQ1, gEQ2, ALU.add, RG, WG_)
        copy("dve", SELB, gSEL, RG, WG_)
        S.op("dve", lambda e: e.memset(gCUM[:, 0:8], 0.0), [], WG_)
        for j in range(1, 16):
            tt(gCUM[:, j * 8:(j + 1) * 8], gCUM[:, (j - 1) * 8:j * 8], gSEL[:, (j - 1) * 8:j * 8], ALU.add, RG, WG_)
        copy("dve", CUMB, gCUM, RG, WG_)
        rkb, rkr = bank()
        mmg(rkb[:, 0:128], [(LTB, SELB), (ONB[:], CUMB)], RG + [R_C2, R_ID], rkr)
        tob, tor = bank()
        mmg(tob[:, 0:128], [(ONB[:], SELB)], RG + [R_ID], tor)
        S.op("dve", lambda e: e.tensor_reduce(out=gNE, in_=tob[:, 0:128].rearrange("p (j e) -> p e j", j=16),
                                              op=ALU.add, axis=AX.X), tor, WG_)
        ts(gTL, gNE, 0.0, ALU.is_gt, RG, WG_)
        for thr in (512.0, 1024.0, 1536.0):
            stt(gTL, gNE, thr, gTL, ALU.is_gt, ALU.add, RG, WG_)
        copy("dve", gOE[:, 0:1], gTL[:, 0:1], RG, WG_)
        for ex in range(1, NE):
            tt(gOE[:, ex:ex + 1], gOE[:, ex - 1:ex], gTL[:, ex:ex + 1], ALU.add, RG, WG_)
        tt(gOF, gOE, gTL, ALU.subtract, RG, WG_)
        ts(gOF, gOF, 512.0, ALU.mult, RG, WG_)
        tt(v168(gSLOT), v168(rkb[:, 0:128]), gOF.unsqueeze(1).to_broadcast([128, 16, 8]), ALU.add, rkr + RG, WG_)
        tt(gTMP, gEQ1, gSLOT, ALU.mult, RG, WG_)
        S.op("dve", lambda e: e.tensor_reduce(out=gS1, in_=v168(gTMP), op=ALU.add, axis=AX.X), RG, WG_)
        copy("dve", SI[:, 0:16], gS1, RG, WG_)
        tt(gTMP, gEQ2, gSLOT, ALU.mult, RG, WG_)
        S.op("dve", lambda e: e.tensor_reduce(out=gS2, in_=v168(gTMP), op=ALU.add, axis=AX.X), RG, WG_)
        copy("dve", SI[:, 16:32], gS2, RG, WG_)
        tt(v168(gTMP), gOE.unsqueeze(1).to_broadcast([128, 16, 8]), v168(C2F[:, 128:256]), ALU.is_le,
           RG + [R_C2], WG_)
        S.op("dve", lambda e: e.tensor_reduce(out=gET, in_=v168(gTMP), op=ALU.add, axis=AX.X), RG, WG_)
        ts(gET, gET, 7.0, ALU.min, RG, WG_)
        copy("dve", ETI, gET, RG, WG_)
        dump("gw1", gW1, RG)
        dump("gs1", gS1, RG)
        dump("gs2", gS2, RG)
        dump("get", gET, RG)
        reserved.discard(7)
        IDXG = sb("IDXG", [128, 112], I32)
        IDXD = sb("IDXD", [128, 448], I32)
        gIG, gID = ga(112), ga(448)
        stt(gIG.rearrange("p (t g) -> p t g", t=16), gET.unsqueeze(2).to_broadcast([128, 16, 7]), 896.0,
            C2F[:, 256:263].unsqueeze(1).to_broadcast([128, 16, 7]), ALU.mult, ALU.add, RG + [R_C2], WG_)
        copy("dve", IDXG[:], gIG, RG, WG_)
        stt(gID.rearrange("p (t g) -> p t g", t=16), gET.unsqueeze(2).to_broadcast([128, 16, 28]), 3584.0,
            C2F[:, 263:291].unsqueeze(1).to_broadcast([128, 16, 28]), ALU.mult, ALU.add, RG + [R_C2], WG_)
        copy("dve", IDXD[:], gID, RG, WG_)
        for j in range(1, NT):
            s = j % 2
            k = rs_col[j]
            ts(HNB[s][:], HT[:, j, :], SS[:, 2, k:k + 1], ALU.mult, [R_HT[j], R_SS], [R_HNB[s]])
            for kk in range(2):
                col = kk * 16 + j - 1
                S.dma("pool", None, None, reads=[R_HNB[s]] + RG, writes=[R_XS], owner=R_XS,
                      fn=lambda e, s=s, col=col: e.indirect_dma_start(
                          out=XS[:, :], out_offset=bass.IndirectOffsetOnAxis(ap=SI[:, col:col + 1], axis=0),
                          in_=HNB[s][:], in_offset=None))
        NTILE = 16
        WB3 = []
        R_WB3 = []
        for s in range(2):
            o = 8192 + s * 12288
            WB3.append((arv(o, 4096).rearrange("p (k n) -> p k n", k=8),
                        arv(o + 4096, 4096).rearrange("p (k n) -> p k n", k=8),
                        arv(o + 8192, 4096).rearrange("p (f n) -> p f n", f=4)))
            R_WB3.append([Res("WB3%d_%d" % (s, i)) for i in range(3)])
        ACT3 = [arv(32768 + a * 2048, 2048).rearrange("p (f n) -> p f n", f=4) for a in range(2)]
        R_ACT3 = [[Res("ACT3%d_%d" % (a, f)) for f in range(4)] for a in range(2)]
        SIL3 = [arv(36864 + i * 512, 512) for i in range(2)]
        R_SIL3 = [Res("SIL3%d" % i) for i in range(2)]
        YACC = [arv(c * 2048, 1024, F32) for c in range(4)]
        R_YACC = [Res("YACC%d" % c) for c in range(4)]
        RAf = RA[:].rearrange("p k t -> p (k t)")
        XT = [RAf[:, i * 4096:(i + 1) * 4096].rearrange("p (c d) -> p c d", c=4) for i in range(2)]
        R_XT = [Res("XT%d" % i) for i in range(2)]
        XF = [RAf[:, 8192 + i * 4096:8192 + (i + 1) * 4096].rearrange("p (k n) -> p k n", k=8) for i in range(2)]
        R_XF = [Res("XF%d" % i) for i in range(2)]
        R_YS = Res("YS")
        S.barrier()

        def prep_tile(tau):
            s = tau % 2
            S.dma("sp", XT[s], XS[512 * tau:512 * (tau + 1), :].rearrange("(c p) d -> p c d", p=128),
                  reads=[R_XS], writes=[R_XT[s]])
            for c in range(4):
                pb, pr = bank()
                pv = pb.bitcast(BF16).rearrange("p (k c) -> p k c", k=8)

                def fn(e, pv=pv, s=s, c=c):
                    ins = None
                    for kc in range(8):
                        ins = e.transpose(out=pv[:, kc, :], in_=XT[s][:, c, kc * 128:(kc + 1) * 128], identity=IDB[:])
                    return ins
                S.op("pe", fn, [R_XT[s], R_ID], pr)
                tt(XF[s][:, :, c * 128:(c + 1) * 128], pv, g_b128, ALU.mult, pr + [R_CP], [R_XF[s]])

        steps = [(tau, g) for tau in range(NTILE) for g in range(7)]

        wdflat = sw_we_down.rearrange("e r f -> (e r) f")

        def load_w(si):
            tau, g = steps[si]
            s = si % 2
            o = 8192 + s * 12288
            col = tau * 7 + g
            for (dst, src, res_) in ((arv(o, 4096), WSG, R_WB3[s][0]), (arv(o + 4096, 4096), WSU, R_WB3[s][1])):
                S.dma("pool", None, None, reads=[R_WS] + RG, writes=[res_],
                      fn=lambda e, dst=dst, src=src, col=col: e.indirect_dma_start(
                          out=dst, out_offset=None, in_=src[:, :],
                          in_offset=bass.IndirectOffsetOnAxis(ap=IDXG[:, col:col + 1], axis=0)))
            for fc in range(4):
                S.dma("pool", None, None, reads=RG, writes=[R_WB3[s][2]],
                      fn=lambda e, s=s, fc=fc, col=col: e.indirect_dma_start(
                          out=WB3[s][2][:, fc, :], out_offset=None, in_=wdflat[:, :],
                          in_offset=bass.IndirectOffsetOnAxis(ap=IDXD[:, col * 4 + fc:col * 4 + fc + 1], axis=0)))

        def gu3(si):
            tau, g = steps[si]
            s, a, x = si % 2, si % 2, tau % 2
            wg, wu, wd = WB3[s]
            for fc in range(4):
                gb, gr = bank()
                mmg(gb[:, :], [(wg[:, kc, fc * 128:(fc + 1) * 128], XF[x][:, kc, :]) for kc in range(8)],
                    [R_XF[x], R_WB3[s][0]], gr)
                ub, ur = bank()
                mmg(ub[:, :], [(wu[:, kc, fc * 128:(fc + 1) * 128], XF[x][:, kc, :]) for kc in range(8)],
                    [R_XF[x], R_WB3[s][1]], ur)
                sl = fc % 2
                act(SIL3[sl][:], gb[:, :], AF.Silu, gr, [R_SIL3[sl]])
                tt(ACT3[a][:, fc, :], ub[:, :], SIL3[sl][:], ALU.mult, ur + [R_SIL3[sl]], [R_ACT3[a][fc]])

        def down3(si):
            tau, g = steps[si]
            s, a = si % 2, si % 2
            wg, wu, wd = WB3[s]
            for c in range(4):
                yb, yr = bank(pair=True)
                for half in range(2):
                    mmg(yb[:, half, :], [(ACT3[a][:, fc, c * 128:(c + 1) * 128], wd[:, fc, half * 512:(half + 1) * 512])
                                         for fc in range(4)], R_ACT3[a] + [R_WB3[s][2]], [yr[half]])
                yv = yb.rearrange("p b c -> p (b c)")
                if g == 0:
                    copy("act", YACC[c], yv, yr, [R_YACC[c]])
                else:
                    tt(YACC[c], yv, YACC[c], ALU.add, yr + [R_YACC[c]], [R_YACC[c]])
            if g == 6:
                S.dma("sp", YS[512 * tau:512 * (tau + 1), :].rearrange("(c p) d -> p c d", p=128),
                      AR[:, 0:8192].bitcast(F32).rearrange("p (c d) -> p c d", c=4),
                      reads=R_YACC, writes=[R_YS], owner=R_YS)

        prep_tile(0)
        load_w(0)
        load_w(1)
        for si in range(len(steps)):
            tau, g = steps[si]
            gu3(si)
            if si > 0:
                down3(si - 1)
                if si + 1 < len(steps):
                    load_w(si + 1)
            if g == 2 and tau + 1 < NTILE:
                prep_tile(tau + 1)
        down3(len(steps) - 1)
        S.barrier()
        G1 = [arv(8192 + i * 2048, 1024, F32) for i in range(2)]
        G2 = [arv(12288 + i * 2048, 1024, F32) for i in range(2)]
        R_G1 = [Res("G1%d" % i) for i in range(2)]
        R_G2 = [Res("G2%d" % i) for i in range(2)]
        R_OUT = Res("OUT")
        for j in range(1, NT):
            s = j % 2
            for (GB, RGB, base, wv) in ((G1, R_G1, 0, gW1), (G2, R_G2, 16, gW2)):
                col = base + j - 1
                S.dma("pool", None, None, reads=[R_YS] + RG, writes=[RGB[s]],
                      fn=lambda e, GB=GB, s=s, col=col: e.indirect_dma_start(
                          out=GB[s][:], out_offset=None, in_=YS[:, :],
                          in_offset=bass.IndirectOffsetOnAxis(ap=SI[:, col:col + 1], axis=0)))
                stt(HT[:, j, :], GB[s], wv[:, j - 1:j], HT[:, j, :], ALU.mult, ALU.add,
                    [RGB[s], R_HT[j]] + RG, [R_HT[j]])
            S.dma("sp", out[(j - 1) * 128:j * 128, :], HT[:, j, :], reads=[R_HT[j]], owner=R_OUT)
        S.final_wait("sp")

        block = es.enter_context(nc.Block())

        @block.tensor
        def _(e):
            for f in S.q["pe"]:
                f(e)

        @block.scalar
        def _(e):
            for f in S.q["act"]:
                f(e)

        @block.vector
        def _(e):
            for f in S.q["dve"]:
                f(e)

        @block.gpsimd
        def _(e):
            for f in S.q["pool"]:
                f(e)

        @block.sync
        def _(e):
            for f in S.q["sp"]:
                f(e)
    return nc


def _consts():
    ident = np.eye(128, dtype=np.float32)
    bd = np.zeros((128, 128), np.float32)
    bd[:64, :64] = 1.0 / 64
    bd[64:, 64:] = 1.0 / 64
    slopes = np.exp2(-8.0 * (np.arange(12, dtype=np.float32) + 1.0) / 12).astype(np.float32)
    k = np.arange(128)[:, None].astype(np.float32)
    q = np.arange(128)[None, :].astype(np.float32)
    me = np.zeros((128, 2, 4, 3, 128), np.float32)
    for kb in range(2):
        dist = q + 128.0 - k if kb == 0 else q - k
        valid = (dist >= 0) & (dist < 128)
        for kvh in range(4):
            for g in range(3):
                s = slopes[kvh * 3 + g]
                me[:, kb, kvh, g, :] = np.where(valid, np.exp(-s * dist), 0.0)
    c2 = np.zeros((128, 296), np.float32)
    pidx = np.arange(128, dtype=np.float32)[:, None]
    c2[:, 256:263] = np.arange(7, dtype=np.float32)[None, :] * 128 + pidx
    c2[:, 263:291] = (np.arange(7, dtype=np.float32)[:, None] * 512
                      + np.arange(4, dtype=np.float32)[None, :] * 128).reshape(1, 28) + pidx
    c2[:, 0:128] = (np.arange(128)[:, None] < np.arange(128)[None, :]).astype(np.float32)
    c2[:, 128:256] = np.repeat(np.arange(16, dtype=np.float32), 8)[None, :]
    return ident, bd, me.reshape(128, 3072), c2


def _cpack(inp, flag):
    cp = np.zeros((128, NCP), np.float32)

    def pk(v):
        return np.asarray(v, np.float32).reshape(8, 128).T
    cp[:, C_GA0:C_GA0 + 8] = pk(inp["cv_attn_norm_g"][0])
    cp[:, C_GF0:C_GF0 + 8] = pk(inp["cv_ffn_norm_g"][0])
    cp[:, C_GA1:C_GA1 + 8] = pk(inp["sw_attn_norm_g"][0])
    cp[:, C_GF1:C_GF1 + 8] = pk(inp["sw_ffn_norm_g"][0])
    cp[:, C_GM:C_GM + 8] = pk(inp["mem_norm_g"])
    bg = np.asarray(inp["cv_b_glu"][0], np.float32)
    cp[:, C_BA:C_BA + 6] = bg[:768].reshape(6, 128).T
    cp[:, C_BG:C_BG + 6] = bg[768:].reshape(6, 128).T
    dw = np.asarray(inp["cv_dw_w"][0], np.float32)
    cp[:, C_DW:C_DW + 186] = dw.reshape(31, 6, 128).transpose(2, 1, 0).reshape(128, 186)
    cp[:, C_DWB:C_DWB + 6] = np.asarray(inp["cv_dw_b"][0], np.float32).reshape(6, 128).T
    cp[:, C_LNG:C_LNG + 6] = np.asarray(inp["cv_ln_g"][0], np.float32).reshape(6, 128).T
    cp[:, C_LNB:C_LNB + 6] = np.asarray(inp["cv_ln_b"][0], np.float32).reshape(6, 128).T

    def h2(v):
        v = np.asarray(v, np.float32).reshape(64)
        return np.concatenate([v, v])
    cp[:, C_MQ0] = h2(inp["cv_memq_norm_g"][0])
    cp[:, C_MQ1] = h2(inp["sw_memq_norm_g"][0])
    cp[:, C_MK] = h2(inp["mem_k_norm_g"])
    cp[:, C_QG] = h2(inp["sw_q_norm_g"][0])
    cp[:, C_KG] = h2(inp["sw_k_norm_g"][0])
    cp[:, C_FLAG] = flag
    return cp


def make_in_maps(inp):
    f = lambda a: np.ascontiguousarray(np.asarray(a, dtype=np.float32))
    ident, bd, me, c2 = _consts()
    x = f(inp["x"])
    memf = f(inp["mem"])
    sinks = f(inp["sw_sinks"][0])
    sinks_rep = np.ascontiguousarray(np.repeat(sinks, 128)[None, :])
    shared = dict(
        ident=ident, bdm=bd, maskexp=me, cst2=c2, sinks_rep=sinks_rep,
        w_mem_kv=f(inp["w_mem_kv"]), cv_w_in=f(inp["cv_w_in"][0]), cv_w_out=f(inp["cv_w_out"][0]),
        cv_w_gate=f(inp["cv_w_gate"][0]), cv_w_up=f(inp["cv_w_up"][0]), cv_w_down=f(inp["cv_w_down"][0]),
        sw_w_in=f(inp["sw_w_in"][0]), sw_w_out=f(inp["sw_w_out"][0]), sw_router=f(inp["sw_router"][0]),
        sw_we_gate=f(inp["sw_we_gate"][0]), sw_we_up=f(inp["sw_we_up"][0]), sw_we_down=f(inp["sw_we_down"][0]),
    )
    maps = []
    for c in range(NCORES):
        b, qtr = divmod(c, 4)
        xin = np.zeros((TC, D), np.float32)
        t0 = qtr * TOK
        if qtr == 0:
            xin[160:] = x[b, 0:TOK]
        else:
            xin[:] = x[b, t0 - 160:t0 + TOK]
        m = dict(shared)
        m["xin"] = xin
        m["mem"] = memf[b]
        m["cpack"] = _cpack(inp, 0.0 if qtr == 0 else 1.0)
        maps.append(m)
    return maps


def kernel(**inputs):
    nc = build_program()
    maps = make_in_maps(inputs)
    res = run_bass_kernel_spmd(nc, maps, core_ids=list(range(NCORES)))
    outs = [np.asarray(r["out"], np.float32) for r in res.results]
    full = np.concatenate(outs, axis=0).reshape(2, SEQ, D)
    return full
```

```python
import os
from contextlib import ExitStack

import numpy as np
import concourse.bass as bass
import concourse.mybir as mybir
from concourse.bass_utils import run_bass_kernel_spmd

F32 = mybir.dt.float32
BF16 = mybir.dt.bfloat16
I32 = mybir.dt.int32
AF = mybir.ActivationFunctionType
ALU = mybir.AluOpType
AX = mybir.AxisListType

NCORES = 8
D = 1024
SEQ = 8192
TOK = 2048
NT = 17
TC = 2208
HD = 64
DFF0 = 2816
DFFE = 3584
NE = 8
CONV_W = 31
CONV_CH = 768
RMS_EPS = 1e-6
LN_EPS = 1e-5

C_GA0, C_GF0, C_GA1, C_GF1, C_GM = 0, 8, 16, 24, 32
C_BA, C_BG = 40, 46
C_DW = 52
C_DWB, C_LNG, C_LNB = 238, 244, 250
C_MQ0, C_MQ1, C_MK, C_QG, C_KG, C_FLAG = 256, 257, 258, 259, 260, 261
NCP = 264

ENGS = ("pe", "act", "dve", "pool", "sp")
PROFILE_MARKS = bool(os.environ.get("MK_MARKS"))


class Res:
    __slots__ = ("name", "w", "r", "dsem", "dcnt")

    def __init__(self, name):
        self.name = name
        self.w = None
        self.r = []
        self.dsem = None
        self.dcnt = 0


class Sched:
    def __init__(self, esem, dsems):
        self.esem = dict(esem)
        self.free_dsems = list(dsems)
        self.cnt = {e: 0 for e in ENGS}
        self.seen = {e: {} for e in ENGS}
        self.q = {e: [] for e in ENGS}
        self.dlast = {}
        self.nbank = 0

    def _deps(self, eng, reads, writes, skip_key=None):
        deps = {}

        def add(ev, same_ok):
            if ev is None:
                return
            k, v = ev
            if (k == eng or k == skip_key) and not same_ok:
                return
            if deps.get(k, 0) < v:
                deps[k] = v
        for r in reads:
            add(r.w, True)
        for w in writes:
            add(w.w, False)
            for ev in w.r:
                add(ev, False)
        waits = []
        for k, v in deps.items():
            if self.seen[eng].get(k, 0) < v:
                self.seen[eng][k] = v
                waits.append((self.esem[k], v))
        return waits

    def _commit(self, ev, reads, writes):
        for r in reads:
            r.r.append(ev)
        for w in writes:
            w.w = ev
            w.r = []

    def op(self, eng, fn, reads=(), writes=()):
        waits = self._deps(eng, reads, writes)
        self.cnt[eng] += 1
        ev = (eng, self.cnt[eng])
        sem = self.esem[eng]

        def run(e, waits=waits, fn=fn, sem=sem):
            for s, v in waits:
                e.wait_ge(s, v)
            fn(e).then_inc(sem, 1)
        self.q[eng].append(run)
        self._commit(ev, reads, writes)
        return ev

    def dma(self, q, out, in_, reads=(), writes=(), owner=None, fn=None, **kw):
        owner = owner or (writes[0] if writes else reads[0])
        if owner.dsem is None:
            owner.dsem = self.free_dsems.pop()
            self.esem[("d", id(owner))] = owner.dsem
        key = ("d", id(owner))
        waits = self._deps(q, reads, writes, skip_key=key)
        owner.dcnt += 16
        ev = (key, owner.dcnt)
        self.dlast[key] = owner.dcnt
        sem = owner.dsem

        def run(e, waits=waits, sem=sem, out=out, in_=in_, kw=kw, fn=fn):
            for s, v in waits:
                e.wait_ge(s, v)
            if fn is not None:
                fn(e).then_inc(sem, 16)
            else:
                e.dma_start(out=out, in_=in_, **kw).then_inc(sem, 16)
        self.q[q].append(run)
        self._commit(ev, reads, writes)
        return ev

    def raw(self, eng, fn, reads=()):
        waits = self._deps(eng, reads, ())

        def run(e, waits=waits, fn=fn):
            for s, v in waits:
                e.wait_ge(s, v)
            fn(e)
        self.q[eng].append(run)

    def barrier(self):
        evs = [(e, self.cnt[e]) for e in ENGS if self.cnt[e] > 0]
        evs += list(self.dlast.items())
        for eng in ENGS:
            waits = []
            for k, v in evs:
                if k == eng:
                    continue
                if self.seen[eng].get(k, 0) < v:
                    self.seen[eng][k] = v
                    waits.append((self.esem[k], v))
            if waits:
                def run(e, waits=waits):
                    for s, v in waits:
                        e.wait_ge(s, v)
                self.q[eng].append(run)

    def mark(self, nc, name):
        st = self.__dict__.setdefault("_scope", {})

        for eng in ENGS:
            def run(e, eng=eng, name=name):
                if eng in st:
                    nc.leave_named_scope(st[eng][0], st[eng][1], False)
                sid, _ = nc.enter_named_scope(name, False)
                st[eng] = (name, sid)
            self.q[eng].append(run)

    def final_wait(self, eng):
        waits = []
        for k, v in self.dlast.items():
            if self.seen[eng].get(k, 0) < v:
                self.seen[eng][k] = v
                waits.append((self.esem[k], v))

        def run(e, waits=waits):
            for s, v in waits:
                e.wait_ge(s, v)
        self.q[eng].append(run)


def build_program(debug=()):
    nc = bass.Bass("TRN2", target_bir_lowering=False)

    def din(name, shape):
        return nc.dram_tensor(name, list(shape), F32, kind="ExternalInput").ap()

    xin = din("xin", [TC, D])
    mem = din("mem", [256, D])
    cpack = din("cpack", [128, NCP])
    ident = din("ident", [128, 128])
    bdm = din("bdm", [128, 128])
    maskexp = din("maskexp", [128, 3072])
    sinks_rep = din("sinks_rep", [1, 1536])
    w_mem_kv = din("w_mem_kv", [D, 512])
    cv_w_in = din("cv_w_in", [D, 1792])
    cv_w_out = din("cv_w_out", [D, D])
    cv_w_gate = din("cv_w_gate", [D, DFF0])
    cv_w_up = din("cv_w_up", [D, DFF0])
    cv_w_down = din("cv_w_down", [DFF0, D])
    sw_w_in = din("sw_w_in", [D, 1536])
    sw_w_out = din("sw_w_out", [D, D])
    sw_router = din("sw_router", [D, NE])
    sw_we_gate = din("sw_we_gate", [NE, D, DFFE])
    sw_we_up = din("sw_we_up", [NE, D, DFFE])
    sw_we_down = din("sw_we_down", [NE, DFFE, D])
    cst2 = din("cst2", [128, 296])
    WSG = nc.dram_tensor("wsg_scr", [NE * 7 * 128, 4096], BF16, kind="Internal").ap()
    WSU = nc.dram_tensor("wsu_scr", [NE * 7 * 128, 4096], BF16, kind="Internal").ap()
    XS = nc.dram_tensor("xs_scr", [8192, D], BF16, kind="Internal").ap()
    YS = nc.dram_tensor("ys_scr", [8192, D], F32, kind="Internal").ap()
    out = nc.dram_tensor("out", [TOK, D], F32, kind="ExternalOutput").ap()
    dbg_out = {}
    for name, shape in debug:
        dbg_out[name] = nc.dram_tensor("dbg_" + name, list(shape), F32, kind="ExternalOutput").ap()

    es = ExitStack()
    with es:
        def sb(name, shape, dt):
            return es.enter_context(nc.sbuf_tensor(name, list(shape), dt))

        HT = sb("HT", [128, NT, D], F32)
        RA = sb("RA", [128, 8, TC], BF16)
        AR = sb("AR", [128, 40960], BF16)
        CP = sb("CP", [128, NCP], F32)
        IDF = sb("IDF", [128, 128], F32)
        IDB = sb("IDB", [128, 128], BF16)
        BDB = sb("BDB", [128, 128], BF16)
        ONB = sb("ONB", [128, 128], BF16)
        ME = sb("ME", [128, 2, 4, 384], BF16)
        ME0 = sb("ME0", [128, 4, 384], BF16)
        SRB = sb("SRB", [1, 4, 384], BF16)
        MKT = sb("MKT", [128, 2, 256], BF16)
        MV = sb("MV", [128, 2, 256], BF16)
        SS = sb("SS", [128, 3, 32], F32)
        GT = sb("GT", [128, 16, 8], F32)
        WR = sb("WR", [128, 8, NE], F32)
        PS = es.enter_context(nc.psum_tensor("PS", [128, 8, 512], F32))

        esem = {e: es.enter_context(nc.semaphore("sem_" + e)) for e in ENGS}
        dsems = [es.enter_context(nc.semaphore("dsem%d" % i)) for i in range(72)]
        S = Sched(esem, dsems)

        banks = [Res("bank%d" % i) for i in range(8)]
        reserved = set()

        def bank(pair=False):
            while True:
                b = S.nbank % 8
                if pair and (b % 2 == 1 or (b + 1) in reserved):
                    S.nbank += 1
                    continue
                if b in reserved:
                    S.nbank += 1
                    continue
                break
            if pair:
                S.nbank += 2
                return PS[:, b:b + 2, :], [banks[b], banks[b + 1]]
            S.nbank += 1
            return PS[:, b, :], [banks[b]]

        def mmg(out_ap, pairs, reads, writes):
            def fn(e, out_ap=out_ap, pairs=pairs):
                n = len(pairs)
                ins = None
                for i, (l, r) in enumerate(pairs):
                    ins = e.matmul(out_ap, l, r, start=(i == 0), stop=(i == n - 1))
                return ins
            S.op("pe", fn, reads, writes)

        def act(out_ap, in_ap, func, reads, writes, **kw):
            S.op("act", lambda e: e.activation(out=out_ap, in_=in_ap, func=func, **kw), reads, writes)

        def tt(out_ap, in0, in1, op, reads, writes, eng="dve"):
            S.op(eng, lambda e: e.tensor_tensor(out=out_ap, in0=in0, in1=in1, op=op), reads, writes)

        def ts(out_ap, in0, s1, op0, reads, writes, s2=None, op1=None, eng="dve"):
            if op1 is None:
                S.op(eng, lambda e: e.tensor_scalar(out=out_ap, in0=in0, scalar1=s1, scalar2=None, op0=op0),
                     reads, writes)
            else:
                S.op(eng, lambda e: e.tensor_scalar(out=out_ap, in0=in0, scalar1=s1, scalar2=s2, op0=op0, op1=op1),
                     reads, writes)

        def stt(out_ap, in0, scalar, in1, op0, op1, reads, writes):
            S.op("dve", lambda e: e.scalar_tensor_tensor(out=out_ap, in0=in0, scalar=scalar, in1=in1,
                                                         op0=op0, op1=op1), reads, writes)

        def recip(out_ap, in_ap, reads, writes):
            S.op("dve", lambda e: e.reciprocal(out=out_ap, in_=in_ap), reads, writes)

        def copy(eng, out_ap, in_ap, reads, writes):
            if eng == "act":
                act(out_ap, in_ap, AF.Copy, reads, writes)
            else:
                S.op(eng, lambda e: e.tensor_copy(out=out_ap, in_=in_ap), reads, writes)

        def dump(name, ap, reads, view=None):
            if name in dbg_out:
                dst = dbg_out[name]
                if view:
                    dst = dst.rearrange(view[0], **view[1])
                S.dma("pool", dst, ap, reads=reads, writes=(), owner=R_dbg)

        R_dbg = Res("dbg")

        def cpc(c, n=1):
            return CP[:, c:c + n]

        def arv(off, n, dt=BF16):
            if dt == F32:
                return AR[:, off:off + 2 * n].bitcast(F32)
            return AR[:, off:off + n]

        R_HT = [Res("HT%d" % j) for j in range(NT)]
        R_RA = [Res("RA%d" % j) for j in range(NT + 1)]
        R_CP, R_ID, R_ME, R_SR = Res("CP"), Res("ID"), Res("ME"), Res("SR")
        R_MKV = Res("MKV")
        R_SS = Res("SS")

        def tcols(j):
            if j == 17:
                return 0, 32
            return 32 + 128 * j, 128

        S.dma("sp", CP[:], cpack[:, :], writes=[R_CP])
        S.dma("sp", IDF[:], ident[:, :], writes=[R_ID])
        R_BD = Res("BD")
        PB0 = 22016
        BDF = arv(PB0, 128, F32)
        S.dma("sp", BDF, bdm[:, :], writes=[R_BD])
        MEf = ME[:].rearrange("p a b c -> p (a b c)")
        S.dma("pool", MEf[:, 0:1536], maskexp[:, 0:1536], writes=[R_ME])
        S.dma("pool", MEf[:, 1536:3072], maskexp[:, 1536:3072], writes=[R_ME])
        SRF = arv(14336, 1536, F32)[0:1, :]
        S.dma("sp", SRF, sinks_rep[:, :], writes=[R_SR])
        WIN0 = arv(0, 8 * 1792).rearrange("p (k n) -> p k n", k=8)
        R_WIN0 = Res("WIN0")
        S.dma("pool", WIN0, cv_w_in.rearrange("(k p) n -> p k n", p=128), writes=[R_WIN0])
        R_XP = Res("XP")
        R_WS = Res("WS")
        pp_list = [(ws_, w_, ex, g_) for ex in range(NE) for g_ in range(7) for (ws_, w_) in ((WSG, sw_we_gate), (WSU, sw_we_up))]
        pp_state = {"i": 0}

        def prepass(n):
            for _ in range(n):
                if pp_state["i"] >= len(pp_list):
                    return
                ws_, w_, ex, g_ = pp_list[pp_state["i"]]
                pp_state["i"] += 1
                r0 = (ex * 7 + g_) * 128
                S.dma("pool", ws_[r0:r0 + 128, :].rearrange("p (k n) -> p k n", k=8),
                      w_[ex, :, g_ * 512:(g_ + 1) * 512].rearrange("(k p) n -> p k n", p=128),
                      writes=[R_WS], owner=R_WS)
        ZT = sb("ZT", [128, 2048], BF16)
        R_ZT = Res("ZT")
        R_XS = Res("XS")
        S.op("dve", lambda e: e.memset(ZT[:], 0.0), [], [R_ZT])
        XP = arv(PB0 + 256, D, F32)
        S.dma("sp", XP[0:32, :], xin[0:32, :], writes=[R_XP])
        R_MEM = Res("MEM")
        MEMT = arv(PB0 + 2304, 2 * D, F32).rearrange("p (t d) -> p t d", t=2)
        S.dma("sp", MEMT, mem.rearrange("(t p) d -> p t d", p=128), writes=[R_MEM])
        R_WKV = Res("WKV")
        WKV = arv(PB0 + 6400, 8 * 512).rearrange("p (k n) -> p k n", k=8)
        S.dma("pool", WKV, w_mem_kv.rearrange("(k p) n -> p k n", p=128), writes=[R_WKV])
        for j0, j1 in ((0, 2), (2, 5), (5, 9), (9, 13), (13, 17)):
            S.dma("sp", HT[:, j0:j1, :],
                  xin[32 + 128 * j0:32 + 128 * j1, :].rearrange("(j p) d -> p j d", p=128),
                  writes=R_HT[j0:j1])
        R_WR = Res("WR")
        S.dma("sp", WR[:], sw_router.rearrange("(k p) n -> p k n", p=128), writes=[R_WR])
        XSz = XS.rearrange("(a p r) d -> a p (r d)", p=128, r=2)
        for a_ in range(32):
            S.dma("sp", XSz[a_], ZT[:], reads=[R_ZT], writes=[R_XS], owner=R_XS)
        prepass(8)

        copy("act", IDB[:], IDF[:], [R_ID], [R_ID])
        copy("act", BDB[:], BDF, [R_BD], [R_BD])
        S.op("dve", lambda e: e.memset(ONB[:], 1.0), [], [R_ID])
        act(SRB[:].rearrange("p a b -> p (a b)"), SRF, AF.Exp, [R_SR], [R_SR])
        ts(ME0[:], ME[:, 0], cpc(C_FLAG), ALU.mult, [R_ME, R_CP], [R_ME])

        A_TAIL = 37888
        HNB = [arv(A_TAIL + i * 1024, 1024) for i in range(2)]
        R_HNB = [Res("HNB%d" % i) for i in range(2)]
        JUNK = arv(A_TAIL + 2048, 1024)
        R_JUNK = Res("JUNK")
        ss_state = {"n": 0}

        def norm_transpose(j, gcol, src_ap=None, src_res=None, npart=128, fp32_path=None):
            k = ss_state["n"] % 32
            ss_state["n"] += 1
            if src_ap is None:
                src_ap, src_res = HT[:, j, :], R_HT[j]
            c0, n = tcols(j)
            P = slice(0, npart)
            act(JUNK[P, :], src_ap, AF.Square, [src_res], [R_JUNK, R_SS], accum_out=SS[P, 0, k:k + 1])
            act(SS[P, 1, k:k + 1], SS[P, 0, k:k + 1], AF.Sqrt, [R_SS], [R_SS], scale=1.0 / D, bias=RMS_EPS)
            recip(SS[P, 2, k:k + 1], SS[P, 1, k:k + 1], [R_SS], [R_SS])
            if fp32_path is None:
                s = j % 2
                ts(HNB[s][P, :], src_ap, SS[P, 2, k:k + 1], ALU.mult, [src_res, R_SS], [R_HNB[s]])
                pb, pr = bank()
                pv = pb.bitcast(BF16).rearrange("p (k c) -> p k c", k=8)

                def fn(e, pv=pv, s=s, P=P, n=n):
                    ins = None
                    for kc in range(8):
                        ins = e.transpose(out=pv[:, kc, 0:n], in_=HNB[s][P, kc * 128:(kc + 1) * 128],
                                          identity=IDB[P, 0:n])
                    return ins
                S.op("pe", fn, [R_HNB[s], R_ID], pr)
                g_b = CP[:, gcol:gcol + 8].unsqueeze(2).to_broadcast([128, 8, n])
                tt(RA[:, :, c0:c0 + n], pv[:, :, 0:n], g_b, ALU.mult, pr + [R_CP], [R_RA[j]])
            else:
                HN32, R_HN32, H32T, R_H32T = fp32_path
                s = j % 2
                ts(HN32[s], src_ap, SS[:, 2, k:k + 1], ALU.mult, [src_res, R_SS], [R_HN32[s]])
                pb, pr = bank(pair=True)
                pv = pb.rearrange("p b (k c) -> p (b k) c", k=4)

                def fn(e, pv=pv, s=s):
                    ins = None
                    for kc in range(8):
                        ins = e.transpose(out=pv[:, kc, :], in_=HN32[s][:, kc * 128:(kc + 1) * 128],
                                          identity=IDF[:])
                    return ins
                S.op("pe", fn, [R_HN32[s], R_ID], pr)
                g_b = CP[:, gcol:gcol + 8].unsqueeze(2).to_broadcast([128, 8, 128])
                tt(RA[:, :, c0:c0 + n], pv, g_b, ALU.mult, pr + [R_CP], [R_RA[j]])
                tt(H32T[s], pv, g_b, ALU.mult, pr + [R_CP], [R_H32T[s]])

        hn_state = {"n": 0}

        hn_pending = []

        def hn_flush(keep=0):
            while len(hn_pending) > keep:
                hn_pending.pop(0)()

        def headnorm(q_ap, q_res, gcol, out_ap, out_res, n, bufs, split=None):
            SQ, R_SQ, RT, R_RT = bufs
            s = hn_state["n"] % 2
            hn_state["n"] += 1
            act(SQ[s][:, :n], q_ap, AF.Square, q_res, [R_SQ[s]])

            def rest():
                sb_, sr_ = bank()
                mmg(sb_[:, :n], [(BDB[:], SQ[s][:, :n])], [R_SQ[s], R_BD], sr_)
                act(RT[s][:, :n], sb_[:, :n], AF.Sqrt, sr_, [R_RT[s]], bias=RMS_EPS)
                recip(RT[s][:, :n], RT[s][:, :n], [R_RT[s]], [R_RT[s]])
                qv, rv = q_ap, RT[s][:, :n]
                if split:
                    qv = qv.rearrange("p (a b) -> p a b", b=split)
                    rv = rv.rearrange("p (a b) -> p a b", b=split)
                stt(out_ap, qv, cpc(gcol), rv, ALU.mult, ALU.mult, q_res + [R_RT[s], R_CP], out_res)
            hn_flush(keep=0)
            hn_pending.append(rest)

        def mem_attention(QM, R_QM, blocks, qcol_off, bufs):
            PT, R_PT, RD, R_RD = bufs
            npt = len(PT)
            st = {"n": 0, "r": 0}
            its = [(c0, n, tiles, hd) for (c0, n, tiles) in blocks for hd in range(4)]
            pend = {}

            def stage_a(i):
                c0, n, tiles, hd = its[i]
                q0 = c0 - qcol_off
                c2, hb = hd // 2, hd % 2
                rows = slice(hb * 64, hb * 64 + 64)
                pts = []
                for mc in range(2):
                    s = st["n"] % npt
                    st["n"] += 1
                    sb_, sr_ = bank()
                    mmg(sb_[:, :n], [(MKT[rows, c2, mc * 128:(mc + 1) * 128], QM[rows, c2, q0:q0 + n])],
                        [R_MKV, R_QM], sr_)
                    act(PT[s][:, :n], sb_[:, :n], AF.Exp, sr_, [R_PT[s]], scale=0.125)
                    pts.append(s)
                pend[i] = pts

            def stage_b(i):
                c0, n, tiles, hd = its[i]
                c2, hb = hd // 2, hd % 2
                rows = slice(hb * 64, hb * 64 + 64)
                pts = pend.pop(i)
                ob, orr = bank()
                mmg(ob[rows, :n], [(MV[:, mc, hd * 64:(hd + 1) * 64], PT[pts[mc]][:, :n]) for mc in range(2)],
                    [R_MKV] + [R_PT[s] for s in pts], orr)
                db, dr = bank()
                mmg(db[rows, :n], [(ONB[:, 0:64], PT[pts[mc]][:, :n]) for mc in range(2)],
                    [R_ID] + [R_PT[s] for s in pts], dr)
                s2 = st["r"] % 2
                st["r"] += 1
                recip(RD[s2][rows, :n], db[rows, :n], dr, [R_RD[s2]])
                tt(RA[rows, 6 + c2, c0:c0 + n], ob[rows, :n], RD[s2][rows, :n], ALU.mult,
                   orr + [R_RD[s2]], [R_RA[t] for t in tiles])
            stage_a(0)
            for i in range(len(its)):
                if i + 1 < len(its):
                    stage_a(i + 1)
                stage_b(i)

        def out_proj(WO, R_WO, tiles):
            for j in tiles:
                c0, n = tcols(j)
                yb, yr = bank(pair=True)
                for half in range(2):
                    mmg(yb[:, half, :], [(RA[:, kc, c0:c0 + 128], WO[:, kc, half * 512:(half + 1) * 512])
                                         for kc in range(8)], [R_RA[j], R_WO], [yr[half]])
                tt(HT[:, j, :], yb.rearrange("p b c -> p (b c)"), HT[:, j, :], ALU.add, yr + [R_HT[j]], [R_HT[j]])

        def ffn(groups, blocks, WB, R_WB, ACTB, R_ACTB, SIL, R_SIL, after_load=None):
            steps = [(gi, bi) for gi in range(len(groups)) for bi in range(len(blocks))]

            def load(gi):
                g = groups[gi]
                s = gi % 2
                nfc = g["nfc"]
                wg, wu, wd = WB[s]
                S.dma("pool", wg[:, :, 0:nfc * 128], g["g"].rearrange("(k p) n -> p k n", p=128),
                      writes=[R_WB[s][0]])
                S.dma("pool", wu[:, :, 0:nfc * 128], g["u"].rearrange("(k p) n -> p k n", p=128),
                      writes=[R_WB[s][1]])
                S.dma("pool", wd[:, 0:nfc, :], g["d"].rearrange("(f p) n -> p f n", p=128),
                      writes=[R_WB[s][2]])
                if after_load is not None:
                    after_load()

            def gu(si):
                gi, bi = steps[si]
                g = groups[gi]
                s = gi % 2
                a = si % 2
                wg, wu, wd = WB[s]
                c0, n, tiles = blocks[bi]
                rr = [R_RA[t] for t in tiles]
                for fc in range(g["nfc"]):
                    gb, gr = bank()
                    mmg(gb[:, :n], [(wg[:, kc, fc * 128:(fc + 1) * 128], RA[:, kc, c0:c0 + n]) for kc in range(8)],
                        rr + [R_WB[s][0]], gr)
                    ub, ur = bank()
                    mmg(ub[:, :n], [(wu[:, kc, fc * 128:(fc + 1) * 128], RA[:, kc, c0:c0 + n]) for kc in range(8)],
                        rr + [R_WB[s][1]], ur)
                    sl = (si * 4 + fc) % 2
                    act(SIL[sl][:, :n], gb[:, :n], AF.Silu, gr, [R_SIL[sl]])
                    tt(ACTB[a][:, fc, :n], ub[:, :n], SIL[sl][:, :n], ALU.mult, ur + [R_SIL[sl]], [R_ACTB[a][fc]])

            def down(si):
                gi, bi = steps[si]
                g = groups[gi]
                s = gi % 2
                a = si % 2
                wg, wu, wd = WB[s]
                c0, n, tiles = blocks[bi]
                nfc = g["nfc"]
                for ti, j in enumerate(tiles):
                    yb, yr = bank(pair=True)
                    for half in range(2):
                        mmg(yb[:, half, :], [(ACTB[a][:, fc, ti * 128:(ti + 1) * 128],
                                              wd[:, fc, half * 512:(half + 1) * 512]) for fc in range(nfc)],
                            [R_ACTB[a][fc] for fc in range(nfc)] + [R_WB[s][2]], [yr[half]])
                    yv = yb.rearrange("p b c -> p (b c)")
                    if g["gate"] is None:
                        tt(HT[:, j, :], yv, HT[:, j, :], ALU.add, yr + [R_HT[j]], [R_HT[j]])
                    else:
                        stt(HT[:, j, :], yv, g["gate"](j), HT[:, j, :], ALU.mult, ALU.add,
                            yr + [R_HT[j], R_GT], [R_HT[j]])

            load(0)
            if len(groups) > 1:
                load(1)
            nb = len(blocks)
            for si in range(len(steps)):
                gu(si)
                if si > 0:
                    down(si - 1)
                    gi_prev, bi_prev = steps[si - 1]
                    if bi_prev == nb - 1 and gi_prev + 2 < len(groups):
                        load(gi_prev + 2)
            down(len(steps) - 1)

        R_GT = Res("GT")

        if PROFILE_MARKS:
            S.mark(nc, "P_mem")
        MEMX = arv(PB0 + 10496, 8 * 256).rearrange("p (k c) -> p k c", k=8)
        R_MEMX = [Res("MEMX0"), Res("MEMX1")]
        SQb = [arv(PB0 + 12544 + i * 512, 512) for i in range(2)]
        R_SQb = [Res("SQ%d" % i) for i in range(2)]
        RTb = [arv(PB0 + 13568 + i * 1024, 512, F32) for i in range(2)]
        R_RTb = [Res("RT%d" % i) for i in range(2)]
        hbufs = (SQb, R_SQb, RTb, R_RTb)
        for t in range(2):
            k = ss_state["n"] % 32
            ss_state["n"] += 1
            act(JUNK[:], MEMT[:, t, :], AF.Square, [R_MEM], [R_JUNK, R_SS], accum_out=SS[:, 0, k:k + 1])
            act(SS[:, 1, k:k + 1], SS[:, 0, k:k + 1], AF.Sqrt, [R_SS], [R_SS], scale=1.0 / D, bias=RMS_EPS)
            recip(SS[:, 2, k:k + 1], SS[:, 1, k:k + 1], [R_SS], [R_SS])
            ts(HNB[t][:], MEMT[:, t, :], SS[:, 2, k:k + 1], ALU.mult, [R_MEM, R_SS], [R_HNB[t]])
            pb, pr = bank()
            pv = pb.bitcast(BF16).rearrange("p (k c) -> p k c", k=8)

            def fn(e, pv=pv, t=t):
                ins = None
                for kc in range(8):
                    ins = e.transpose(out=pv[:, kc, :], in_=HNB[t][:, kc * 128:(kc + 1) * 128], identity=IDB[:])
                return ins
            S.op("pe", fn, [R_HNB[t], R_ID], pr)
            g_b = CP[:, C_GM:C_GM + 8].unsqueeze(2).to_broadcast([128, 8, 128])
            tt(MEMX[:, :, t * 128:(t + 1) * 128], pv, g_b, ALU.mult, pr + [R_CP], [R_MEMX[t]])
        for c in range(2):
            kb_, kr_ = bank()
            mmg(kb_[:, :256], [(WKV[:, kc, c * 128:(c + 1) * 128], MEMX[:, kc, :]) for kc in range(8)],
                R_MEMX + [R_WKV], kr_)
            headnorm(kb_[:, :256], kr_, C_MK, MKT[:, c, :], [R_MKV], 256, hbufs)
        hn_flush()
        for t in range(2):
            vb_, vr_ = bank()
            mmg(vb_[:, :256], [(MEMX[:, kc, t * 128:(t + 1) * 128], WKV[:, kc, 256:512]) for kc in range(8)],
                [R_MEMX[t], R_WKV], vr_)
            copy("act", MV[:, t, :], vb_[:, :256], vr_, [R_MKV])
        hn_flush()
        dump("mkt", MKT[:].rearrange("p a b -> p (a b)"), [R_MKV])
        dump("mv", MV[:].rearrange("p a b -> p (a b)"), [R_MKV])

        if PROFILE_MARKS:
            S.mark(nc, "L0A_norm")
        norm_transpose(17, C_GA0, src_ap=XP[0:32, :], src_res=R_XP, npart=32)
        for j in range(NT):
            norm_transpose(j, C_GA0)
        hn_flush()
        S.barrier()
        if PROFILE_MARKS:
            S.mark(nc, "L0B_inproj")
        prepass(8)
        U = arv(14336, 6 * TC).rearrange("p (c t) -> p c t", c=6)
        R_U = [Res("U%d" % c) for c in range(6)]
        QM0 = arv(27584, 2 * 2176).rearrange("p (c t) -> p c t", c=2)
        R_QM0 = Res("QM0")
        SG = [arv(31936 + i * 1024, 512, F32) for i in range(2)]
        R_SG = [Res("SG%d" % i) for i in range(2)]
        SQb = [arv(33984 + i * 512, 512) for i in range(2)]
        RTb = [arv(35008 + i * 1024, 512, F32) for i in range(2)]
        hbufs = (SQb, R_SQb, RTb, R_RTb)
        TB0 = [(0, 160, [17, 0])] + [(160 + 512 * i, 512, [1 + 4 * i + t for t in range(4)]) for i in range(4)]
        sgi = 0
        for c in range(6):
            for (c0, n, tiles) in TB0:
                rr = [R_RA[t] for t in tiles]
                ab, ar_ = bank()
                mmg(ab[:, :n], [(WIN0[:, kc, c * 128:(c + 1) * 128], RA[:, kc, c0:c0 + n]) for kc in range(8)],
                    rr + [R_WIN0], ar_)
                gb, gr = bank()
                mmg(gb[:, :n], [(WIN0[:, kc, (6 + c) * 128:(7 + c) * 128], RA[:, kc, c0:c0 + n]) for kc in range(8)],
                    rr + [R_WIN0], gr)
                s = sgi % 2
                sgi += 1
                act(SG[s][:, :n], gb[:, :n], AF.Sigmoid, gr + [R_CP], [R_SG[s]], bias=cpc(C_BG + c))
                stt(U[:, c, c0:c0 + n], ab[:, :n], cpc(C_BA + c), SG[s][:, :n], ALU.add, ALU.mult,
                    ar_ + [R_SG[s], R_CP], [R_U[c]])
                if c0 == 0:
                    ts(U[:, c, 0:160], U[:, c, 0:160], cpc(C_FLAG), ALU.mult, [R_U[c], R_CP], [R_U[c]])
        TBC = [(32, 128, [0])] + TB0[1:]
        for c2 in range(2):
            for (c0, n, tiles) in TBC:
                rr = [R_RA[t] for t in tiles]
                qb, qr = bank()
                mmg(qb[:, :n], [(WIN0[:, kc, (12 + c2) * 128:(13 + c2) * 128], RA[:, kc, c0:c0 + n])
                                for kc in range(8)], rr + [R_WIN0], qr)
                headnorm(qb[:, :n], qr, C_MQ0, QM0[:, c2, c0 - 32:c0 - 32 + n], [R_QM0], n, hbufs)
        hn_flush()
        dump("u", U[:, 0, :], [R_U[0]])
        dump("qm0", QM0[:, 0, :], [R_QM0])
        hn_flush()
        S.barrier()
        if PROFILE_MARKS:
            S.mark(nc, "L0C_conv")
        prepass(12)
        DG = [arv(i * 3968, 3968).rearrange("p (k c) -> p k c", k=CONV_W) for i in range(2)]
        R_DG = [Res("DG%d" % i) for i in range(2)]
        R_C = [[Res("C%d_%d" % (c, j)) for j in range(NT)] for c in range(6)]

        def build_dg(c):
            s = c % 2
            S.op("dve", lambda e, s=s, c=c: e.tensor_tensor(
                out=DG[s], in0=IDB[:].unsqueeze(1).to_broadcast([128, CONV_W, 128]),
                in1=CP[:, C_DW + c * CONV_W:C_DW + (c + 1) * CONV_W].unsqueeze(2).to_broadcast([128, CONV_W, 128]),
                op=ALU.mult), [R_ID, R_CP], [R_DG[s]])

        def conv_block(c, blk):
            s = c % 2
            c0, n, tiles = blk
            cb, cr = bank()
            mmg(cb[:, :n], [(DG[s][:, k, :], U[:, c, c0 - 30 + k:c0 - 30 + k + n]) for k in range(CONV_W)],
                [R_DG[s], R_U[c]], cr)
            act(RA[:, c, c0:c0 + n], cb[:, :n], AF.Identity, cr + [R_CP], [R_C[c][t] for t in tiles],
                bias=cpc(C_DWB + c))
        SQL = [arv(7936 + i * 512, 512) for i in range(2)]
        R_SQL = [Res("SQL%d" % i) for i in range(2)]
        MSQ = arv(8960, 512, F32)
        VAR = arv(9984, 512, F32)
        R_ST = Res("LNST")
        T1 = [arv(11008 + i * 1024, 512, F32) for i in range(2)]
        R_T1 = [Res("T1%d" % i) for i in range(2)]
        ln_st = {"q": 0}

        def ln_block(blk):
            c0, n, tiles = blk
            rall = [R_C[c][t] for c in range(6) for t in tiles]
            s1b, s1r = bank()
            mmg(s1b[:, :n], [(ONB[:], RA[:, c, c0:c0 + n]) for c in range(6)], rall + [R_ID], s1r)
            s2b, s2r = bank()
            for c in range(6):
                s = ln_st["q"] % 2
                ln_st["q"] += 1
                act(SQL[s][:, :n], RA[:, c, c0:c0 + n], AF.Square, [R_C[c][t] for t in tiles], [R_SQL[s]])
                S.op("pe", lambda e, s2b=s2b, s=s, n=n, c=c: e.matmul(s2b[:, :n], ONB[:], SQL[s][:, :n],
                                                                    start=(c == 0), stop=(c == 5)),
                     [R_SQL[s], R_ID], s2r)
            act(MSQ[:, :n], s1b[:, :n], AF.Square, s1r, [R_ST], scale=1.0 / CONV_CH)
            stt(VAR[:, :n], s2b[:, :n], 1.0 / CONV_CH, MSQ[:, :n], ALU.mult, ALU.subtract, s2r + [R_ST], [R_ST])
            act(VAR[:, :n], VAR[:, :n], AF.Sqrt, [R_ST], [R_ST], bias=LN_EPS)
            recip(VAR[:, :n], VAR[:, :n], [R_ST], [R_ST])
            for c in range(6):
                s = c % 2
                rc = [R_C[c][t] for t in tiles]
                stt(T1[s][:, :n], s1b[:, :n], -1.0 / CONV_CH, RA[:, c, c0:c0 + n], ALU.mult, ALU.add,
                    s1r + rc, [R_T1[s]])
                tt(T1[s][:, :n], T1[s][:, :n], VAR[:, :n], ALU.mult, [R_T1[s], R_ST], [R_T1[s]])
                act(RA[:, c, c0:c0 + n], T1[s][:, :n], AF.Silu, [R_T1[s], R_CP], rc,
                    scale=cpc(C_LNG + c), bias=cpc(C_LNB + c))
        for c in range(5):
            build_dg(c)
            for blk in TBC:
                conv_block(c, blk)
        build_dg(5)
        conv_block(5, TBC[0])
        for b in range(len(TBC)):
            if b + 1 < len(TBC):
                conv_block(5, TBC[b + 1])
            ln_block(TBC[b])
        dump("cln", RA[:, 0, 32:TC], [R_C[0][t] for t in range(NT)])
        hn_flush()
        S.barrier()
        if PROFILE_MARKS:
            S.mark(nc, "L0D_memattn")
        PT = [arv(i * 512, 512) for i in range(6)]
        R_PT = [Res("PT%d" % i) for i in range(6)]
        RD = [arv(3072 + i * 1024, 512, F32) for i in range(2)]
        R_RD = [Res("RD%d" % i) for i in range(2)]
        WO0 = arv(14336, 8 * D).rearrange("p (k n) -> p k n", k=8)
        R_WO0 = Res("WO0")
        S.dma("pool", WO0, cv_w_out.rearrange("(k p) n -> p k n", p=128), writes=[R_WO0])
        prepass(8)
        mem_attention(QM0, R_QM0, TBC, 32, (PT, R_PT, RD, R_RD))
        dump("cat0", RA[:, 6, 32:TC], [R_RA[t] for t in range(NT)])
        if PROFILE_MARKS:
            S.mark(nc, "L0E_outproj")
        out_proj(WO0, R_WO0, list(range(NT)))
        dump("h0a", HT[:, 1, :], [R_HT[1]])
        dump("hf0a", HT[:, 1:NT, :].rearrange("p j d -> p (j d)"), R_HT[1:NT])
        hn_flush()
        S.barrier()
        if PROFILE_MARKS:
            S.mark(nc, "L0F_ffn")
        WB = []
        R_WB = []
        for s in range(2):
            o = s * 12288
            WB.append((arv(o, 4096).rearrange("p (k n) -> p k n", k=8),
                       arv(o + 4096, 4096).rearrange("p (k n) -> p k n", k=8),
                       arv(o + 8192, 4096).rearrange("p (f n) -> p f n", f=4)))
            R_WB.append([Res("WB%d_%d" % (s, i)) for i in range(3)])
        ACTB = [arv(24576 + a * 2048, 2048).rearrange("p (f n) -> p f n", f=4) for a in range(2)]
        R_ACTB = [[Res("ACTB%d_%d" % (a, f)) for f in range(4)] for a in range(2)]
        SIL = [arv(28672 + i * 512, 512) for i in range(2)]
        R_SIL = [Res("SIL%d" % i) for i in range(2)]
        for j in range(NT):
            norm_transpose(j, C_GF0)
        groups0 = []
        for f0 in range(0, DFF0, 512):
            nf = min(512, DFF0 - f0)
            groups0.append(dict(g=cv_w_gate[:, f0:f0 + nf], u=cv_w_up[:, f0:f0 + nf], d=cv_w_down[f0:f0 + nf, :],
                                nfc=nf // 128, gate=None))
        ffn(groups0, TBC, WB, R_WB, ACTB, R_ACTB, SIL, R_SIL, after_load=lambda: prepass(6))
        dump("h1", HT[:, 1, :], [R_HT[1]])
        dump("hf1", HT[:, 1:NT, :].rearrange("p j d -> p (j d)"), R_HT[1:NT])
        hn_flush()
        S.barrier()

        if PROFILE_MARKS:
            S.mark(nc, "L1A_norm")
        for j in range(NT):
            norm_transpose(j, C_GA1)
        if PROFILE_MARKS:
            S.mark(nc, "L1B_inproj")
        WIN1 = arv(0, 8 * 1536).rearrange("p (k n) -> p k n", k=8)
        R_WIN1 = Res("WIN1")
        QPAIR = [(0, 3), (1, 4), (2, 5), (6, 9), (7, 10), (8, 11)]
        for i, (ha, hb_) in enumerate(QPAIR):
            for half, hh in enumerate((ha, hb_)):
                S.dma("pool", WIN1[:, :, i * 128 + half * 64:i * 128 + half * 64 + 64],
                      sw_w_in[:, hh * 64:(hh + 1) * 64].rearrange("(k p) n -> p k n", p=128), writes=[R_WIN1])
        S.dma("pool", WIN1[:, :, 768:1536], sw_w_in[:, 768:1536].rearrange("(k p) n -> p k n", p=128),
              writes=[R_WIN1])
        prepass(8)
        QT = arv(12288, 6 * TOK).rearrange("p (a n g q) -> p a n g q", a=2, n=16, g=3)
        R_QT = Res("QT")
        KT = arv(24576, 2 * 2176).rearrange("p (c t) -> p c t", c=2)
        R_KT = Res("KT")
        VT = arv(28928, NT * 256).rearrange("p (j d) -> p j d", j=NT)
        R_VT = Res("VT")
        QM1 = arv(33280, 2 * TOK).rearrange("p (c t) -> p c t", c=2)
        R_QM1 = Res("QM1")
        SQb = [arv(37376 + i * 512, 512) for i in range(2)]
        RTb = [arv(38400 + i * 1024, 512, F32) for i in range(2)]
        hbufs = (SQb, R_SQb, RTb, R_RTb)
        hn_flush()
        S.barrier()
        TB1 = TB0[1:]
        for i in range(6):
            for bi, (c0, n, tiles) in enumerate(TB1):
                rr = [R_RA[t] for t in tiles]
                qb, qr = bank()
                mmg(qb[:, :n], [(WIN1[:, kc, i * 128:(i + 1) * 128], RA[:, kc, c0:c0 + n]) for kc in range(8)],
                    rr + [R_WIN1], qr)
                headnorm(qb[:, :n], qr, C_QG, QT[:, i // 3, 4 * bi:4 * bi + 4, i % 3, :], [R_QT], n, hbufs,
                         split=128)
        for c2 in range(2):
            for (c0, n, tiles) in TBC:
                rr = [R_RA[t] for t in tiles]
                kb_, kr_ = bank()
                mmg(kb_[:, :n], [(WIN1[:, kc, 768 + c2 * 128:768 + (c2 + 1) * 128], RA[:, kc, c0:c0 + n])
                                 for kc in range(8)], rr + [R_WIN1], kr_)
                headnorm(kb_[:, :n], kr_, C_KG, KT[:, c2, c0 - 32:c0 - 32 + n], [R_KT], n, hbufs)
        hn_flush()
        for j in range(NT):
            c0, n = tcols(j)
            vb_, vr_ = bank()
            mmg(vb_[:, :256], [(RA[:, kc, c0:c0 + 128], WIN1[:, kc, 1024:1280]) for kc in range(8)],
                [R_RA[j], R_WIN1], vr_)
            copy("act", VT[:, j, :], vb_[:, :256], vr_, [R_VT])
        for c2 in range(2):
            for (c0, n, tiles) in TB1:
                rr = [R_RA[t] for t in tiles]
                qb, qr = bank()
                mmg(qb[:, :n], [(WIN1[:, kc, 1280 + c2 * 128:1280 + (c2 + 1) * 128], RA[:, kc, c0:c0 + n])
                                for kc in range(8)], rr + [R_WIN1], qr)
                headnorm(qb[:, :n], qr, C_MQ1, QM1[:, c2, c0 - 160:c0 - 160 + n], [R_QM1], n, hbufs)
        hn_flush()
        dump("qt", QT[:, 0, :, 0, :], [R_QT], view=("p (n q) -> p n q", dict(n=16)))
        dump("kt", KT[:, 0, :], [R_KT])
        hn_flush()
        S.barrier()
        if PROFILE_MARKS:
            S.mark(nc, "L1C_swa")
        prepass(12)
        ET = [arv(i * 384, 384) for i in range(4)]
        R_ET = [Res("ET%d" % i) for i in range(4)]
        PT1 = [arv(1536 + i * 384, 384) for i in range(8)]
        R_PT1 = [Res("PT1%d" % i) for i in range(8)]
        RD1 = [arv(4608 + i * 768, 384, F32) for i in range(2)]
        R_RD1 = [Res("RD1%d" % i) for i in range(2)]
        swa_its = [(nblk, kvh) for nblk in range(16) for kvh in range(4)]
        swa_st = {"e": 0, "p": 0}
        swa_pend = {}

        def swa_a(i):
            nblk, kvh = swa_its[i]
            j = nblk + 1
            rows = slice((kvh % 2) * 64, (kvh % 2) * 64 + 64)
            pts = []
            for kb in range(2):
                jk = j - 1 + kb
                sb_, sr_ = bank()
                mmg(sb_[:, :384], [(KT[rows, kvh // 2, jk * 128:(jk + 1) * 128],
                                    QT[rows, kvh // 2, nblk, :, :].rearrange("p g q -> p (g q)"))],
                    [R_KT, R_QT], sr_)
                s = swa_st["e"] % 4
                swa_st["e"] += 1
                act(ET[s][:], sb_[:, :384], AF.Exp, sr_, [R_ET[s]], scale=0.125)
                p = swa_st["p"] % 8
                swa_st["p"] += 1
                msk = ME0[:, kvh, :] if (nblk == 0 and kb == 0) else ME[:, kb, kvh, :]
                tt(PT1[p][:], ET[s][:], msk, ALU.mult, [R_ET[s], R_ME], [R_PT1[p]])
                pts.append((p, jk))
            swa_pend[i] = pts

        def swa_b(i):
            nblk, kvh = swa_its[i]
            j = nblk + 1
            rc0, _ = tcols(j)
            rows = slice((kvh % 2) * 64, (kvh % 2) * 64 + 64)
            qc0 = (kvh // 2) * 3
            pts = swa_pend.pop(i)
            ob, orr = bank()
            mmg(ob[rows, :384], [(VT[:, jk, kvh * 64:(kvh + 1) * 64], PT1[p][:]) for (p, jk) in pts],
                [R_VT] + [R_PT1[p] for (p, _) in pts], orr)
            db, dr = bank()
            mmg(db[rows, :384], [(ONB[:, 0:64], PT1[p][:]) for (p, _) in pts] + [(ONB[0:1, 0:64], SRB[0:1, kvh, :])],
                [R_ID, R_SR] + [R_PT1[p] for (p, _) in pts], dr)
            s2 = i % 2
            recip(RD1[s2][rows, :], db[rows, :384], dr, [R_RD1[s2]])
            tt(RA[rows, qc0:qc0 + 3, rc0:rc0 + 128], ob[rows, :384].rearrange("p (g q) -> p g q", g=3),
               RD1[s2][rows, :].rearrange("p (g q) -> p g q", g=3), ALU.mult,
               orr + [R_RD1[s2]], [R_RA[j]])
        SWA_LOOK = 2
        for i in range(min(SWA_LOOK, len(swa_its))):
            swa_a(i)
        for i in range(len(swa_its)):
            if i + SWA_LOOK < len(swa_its):
                swa_a(i + SWA_LOOK)
            swa_b(i)
        dump("swa", RA[:, 0, 160:TC], [R_RA[t] for t in range(1, NT)])
        hn_flush()
        S.barrier()
        if PROFILE_MARKS:
            S.mark(nc, "L1D_mem_out")
        WO1 = arv(14336, 8 * D).rearrange("p (k n) -> p k n", k=8)
        R_WO1 = Res("WO1")
        for i, (ha, hb_) in enumerate(QPAIR):
            S.dma("pool", WO1[0:64, i, :], sw_w_out[ha * 64:(ha + 1) * 64, :], writes=[R_WO1])
            S.dma("pool", WO1[64:128, i, :], sw_w_out[hb_ * 64:(hb_ + 1) * 64, :], writes=[R_WO1])
        S.dma("pool", WO1[:, 6:8, :], sw_w_out[768:1024, :].rearrange("(k p) n -> p k n", p=128), writes=[R_WO1])
        prepass(8)
        PT = [arv(i * 512, 512) for i in range(6)]
        RD = [arv(3072 + i * 1024, 512, F32) for i in range(2)]
        mem_attention(QM1, R_QM1, TB1, 160, (PT, R_PT, RD, R_RD))
        out_proj(WO1, R_WO1, list(range(1, NT)))
        dump("h1a", HT[:, 1, :], [R_HT[1]])
        dump("hf1a", HT[:, 1:NT, :].rearrange("p j d -> p (j d)"), R_HT[1:NT])
        hn_flush()
        S.barrier()
        if PROFILE_MARKS:
            S.mark(nc, "L1R_router")
        HN32 = [arv(29696 + i * 2048, 1024, F32) for i in range(2)]
        R_HN32 = [Res("HN32%d" % i) for i in range(2)]
        H32T = [arv(33792 + i * 2048, 1024, F32).rearrange("p (k c) -> p k c", k=8) for i in range(2)]
        R_H32T = [Res("H32T%d" % i) for i in range(2)]
        prepass(1000)
        reserved.add(7)
        LG = PS[:, 7, 0:128].rearrange("p (j e) -> p j e", j=16)
        R_LG = banks[7]
        g_b128 = CP[:, C_GF1:C_GF1 + 8].unsqueeze(2).to_broadcast([128, 8, 128])
        rs_col = {}
        for j in range(1, NT):
            k = ss_state["n"] % 32
            ss_state["n"] += 1
            rs_col[j] = k
            s = j % 2
            act(JUNK[:], HT[:, j, :], AF.Square, [R_HT[j]], [R_JUNK, R_SS], accum_out=SS[:, 0, k:k + 1])
            act(SS[:, 1, k:k + 1], SS[:, 0, k:k + 1], AF.Sqrt, [R_SS], [R_SS], scale=1.0 / D, bias=RMS_EPS)
            recip(SS[:, 2, k:k + 1], SS[:, 1, k:k + 1], [R_SS], [R_SS])
            ts(HN32[s], HT[:, j, :], SS[:, 2, k:k + 1], ALU.mult, [R_HT[j], R_SS], [R_HN32[s]])
            pb, pr = bank(pair=True)
            pv = pb.rearrange("p b (k c) -> p (b k) c", k=4)

            def fn(e, pv=pv, s=s):
                ins = None
                for kc in range(8):
                    ins = e.transpose(out=pv[:, kc, :], in_=HN32[s][:, kc * 128:(kc + 1) * 128], identity=IDF[:])
                return ins
            S.op("pe", fn, [R_HN32[s], R_ID], pr)
            tt(H32T[s], pv, g_b128, ALU.mult, pr + [R_CP], [R_H32T[s]])
            mmg(LG[:, j - 1, :], [(H32T[s][:, kc, :], WR[:, kc, :]) for kc in range(8)],
                [R_H32T[s], R_WR], [R_LG])
        ga_state = {"o": 0}

        def ga(n, dt=F32):
            o = ga_state["o"]
            ga_state["o"] += 2 * n if dt != BF16 else n
            if dt == BF16:
                return arv(o, n)
            if dt == F32:
                return arv(o, n, F32)
            return AR[:, o:o + 2 * n].bitcast(dt)

        def v168(ap):
            return ap.rearrange("p (j e) -> p j e", j=16)

        def b168(ap16):
            return ap16.unsqueeze(2).to_broadcast([128, 16, 8])
        R_G = Res("GATE")
        gL, gEQ1, gL2, gEQ2, gSEL, gCUM, gTMP, gSLOT = [ga(128) for _ in range(8)]
        gV1, gV2, gE, gS1, gS2, gET = [ga(16) for _ in range(6)]
        W12 = sb("W12", [128, 32], F32)
        gW1, gW2 = W12[:, 0:16], W12[:, 16:32]
        gNE, gTL, gOE, gOF = [ga(8) for _ in range(4)]
        SELB, CUMB = ga(128, BF16), ga(128, BF16)
        SI = sb("SIT", [128, 32], I32)
        ETI = ga(16, I32)
        LTB = ga(128, BF16)
        R_C2 = Res("C2")
        C2F = ga(296)
        S.dma("sp", C2F, cst2[:, :], writes=[R_C2])
        copy("act", LTB, C2F[:, 0:128], [R_C2], [R_C2])
        RG, WG_ = [R_G], [R_G]
        copy("act", gL, LG.rearrange("p j e -> p (j e)"), [R_LG], WG_)
        S.op("dve", lambda e: e.tensor_reduce(out=gV1, in_=v168(gL), op=ALU.max, axis=AX.X), RG, WG_)
        tt(v168(gEQ1), v168(gL), b168(gV1), ALU.is_equal, RG, WG_)
        stt(gL2, gEQ1, -1e30, gL, ALU.mult, ALU.add, RG, WG_)
        S.op("dve", lambda e: e.tensor_reduce(out=gV2, in_=v168(gL2), op=ALU.max, axis=AX.X), RG, WG_)
        tt(v168(gEQ2), v168(gL2), b168(gV2), ALU.is_equal, RG, WG_)
        tt(gE, gV2, gV1, ALU.subtract, RG, WG_)
        act(gE, gE, AF.Exp, RG, WG_)
        ts(gW1, gE, 1.0, ALU.add, RG, WG_)
        recip(gW1, gW1, RG, WG_)
        tt(gW2, gE, gW1, ALU.mult, RG, WG_)
        tt(gSEL, gEQ1, gEQ2, ALU.add, RG, WG_)
        copy("dve", SELB, gSEL, RG, WG_)
        S.op("dve", lambda e: e.memset(gCUM[:, 0:8], 0.0), [], WG_)
        for j in range(1, 16):
            tt(gCUM[:, j * 8:(j + 1) * 8], gCUM[:, (j - 1) * 8:j * 8], gSEL[:, (j - 1) * 8:j * 8], ALU.add, RG, WG_)
        copy("dve", CUMB, gCUM, RG, WG_)
        rkb, rkr = bank()
        mmg(rkb[:, 0:128], [(LTB, SELB), (ONB[:], CUMB)], RG + [R_C2, R_ID], rkr)
        tob, tor = bank()
        mmg(tob[:, 0:128], [(ONB[:], SELB)], RG + [R_ID], tor)
        S.op("dve", lambda e: e.tensor_reduce(out=gNE, in_=tob[:, 0:128].rearrange("p (j e) -> p e j", j=16),
                                              op=ALU.add, axis=AX.X), tor, WG_)
        ts(gTL, gNE, 0.0, ALU.is_gt, RG, WG_)
        for thr in (512.0, 1024.0, 1536.0):
            stt(gTL, gNE, thr, gTL, ALU.is_gt, ALU.add, RG, WG_)
        copy("dve", gOE[:, 0:1], gTL[:, 0:1], RG, WG_)
        for ex in range(1, NE):
            tt(gOE[:, ex:ex + 1], gOE[:, ex - 1:ex], gTL[:, ex:ex + 1], ALU.add, RG, WG_)
        tt(gOF, gOE, gTL, ALU.subtract, RG, WG_)
        ts(gOF, gOF, 512.0, ALU.mult, RG, WG_)
        tt(v168(gSLOT), v168(rkb[:, 0:128]), gOF.unsqueeze(1).to_broadcast([128, 16, 8]), ALU.add, rkr + RG, WG_)
        tt(gTMP, gEQ1, gSLOT, ALU.mult, RG, WG_)
        S.op("dve", lambda e: e.tensor_reduce(out=gS1, in_=v168(gTMP), op=ALU.add, axis=AX.X), RG, WG_)
        copy("dve", SI[:, 0:16], gS1, RG, WG_)
        tt(gTMP, gEQ2, gSLOT, ALU.mult, RG, WG_)
        S.op("dve", lambda e: e.tensor_reduce(out=gS2, in_=v168(gTMP), op=ALU.add, axis=AX.X), RG, WG_)
        copy("dve", SI[:, 16:32], gS2, RG, WG_)
        tt(v168(gTMP), gOE.unsqueeze(1).to_broadcast([128, 16, 8]), v168(C2F[:, 128:256]), ALU.is_le,
           RG + [R_C2], WG_)
        S.op("dve", lambda e: e.tensor_reduce(out=gET, in_=v168(gTMP), op=ALU.add, axis=AX.X), RG, WG_)
        ts(gET, gET, 7.0, ALU.min, RG, WG_)
        copy("dve", ETI, gET, RG, WG_)
        dump("gw1", gW1, RG)
        dump("gs1", gS1, RG)
        dump("gs2", gS2, RG)
        dump("get", gET, RG)
        reserved.discard(7)
        IDXG = sb("IDXG", [128, 112], I32)
        IDXD = sb("IDXD", [128, 448], I32)
        gIG, gID = ga(112), ga(448)
        stt(gIG.rearrange("p (t g) -> p t g", t=16), gET.unsqueeze(2).to_broadcast([128, 16, 7]), 896.0,
            C2F[:, 256:263].unsqueeze(1).to_broadcast([128, 16, 7]), ALU.mult, ALU.add, RG + [R_C2], WG_)
        copy("dve", IDXG[:], gIG, RG, WG_)
        stt(gID.rearrange("p (t g) -> p t g", t=16), gET.unsqueeze(2).to_broadcast([128, 16, 28]), 3584.0,
            C2F[:, 263:291].unsqueeze(1).to_broadcast([128, 16, 28]), ALU.mult, ALU.add, RG + [R_C2], WG_)
        copy("dve", IDXD[:], gID, RG, WG_)
        if PROFILE_MARKS:
            S.mark(nc, "L1S_scatter")
        for j in range(1, NT):
            s = j % 2
            k = rs_col[j]
            ts(HNB[s][:], HT[:, j, :], SS[:, 2, k:k + 1], ALU.mult, [R_HT[j], R_SS], [R_HNB[s]])
            for kk in range(2):
                col = kk * 16 + j - 1
                S.dma("pool", None, None, reads=[R_HNB[s]] + RG, writes=[R_XS], owner=R_XS,
                      fn=lambda e, s=s, col=col: e.indirect_dma_start(
                          out=XS[:, :], out_offset=bass.IndirectOffsetOnAxis(ap=SI[:, col:col + 1], axis=0),
                          in_=HNB[s][:], in_offset=None))
        if PROFILE_MARKS:
            S.mark(nc, "L1T_tiles")
        NTILE = int(os.environ.get("MK_NTILE", "15"))
        WB3 = []
        R_WB3 = []
        for s in range(2):
            o = 8192 + s * 12288
            WB3.append((arv(o, 4096).rearrange("p (k n) -> p k n", k=8),
                        arv(o + 4096, 4096).rearrange("p (k n) -> p k n", k=8),
                        arv(o + 8192, 4096).rearrange("p (f n) -> p f n", f=4)))
            R_WB3.append([Res("WB3%d_%d" % (s, i)) for i in range(3)])
        ACT3 = [arv(32768 + a * 2048, 2048).rearrange("p (f n) -> p f n", f=4) for a in range(2)]
        R_ACT3 = [[Res("ACT3%d_%d" % (a, f)) for f in range(4)] for a in range(2)]
        SIL3 = [arv(36864 + i * 512, 512) for i in range(2)]
        R_SIL3 = [Res("SIL3%d" % i) for i in range(2)]
        YACC = [arv(c * 2048, 1024, F32) for c in range(4)]
        R_YACC = [Res("YACC%d" % c) for c in range(4)]
        RAf = RA[:].rearrange("p k t -> p (k t)")
        XT = [RAf[:, i * 4096:(i + 1) * 4096].rearrange("p (c d) -> p c d", c=4) for i in range(2)]
        R_XT = [Res("XT%d" % i) for i in range(2)]
        XF = [RAf[:, 8192 + i * 4096:8192 + (i + 1) * 4096].rearrange("p (k n) -> p k n", k=8) for i in range(2)]
        R_XF = [Res("XF%d" % i) for i in range(2)]
        R_YS = Res("YS")
        hn_flush()
        S.barrier()

        def prep_tile(tau):
            s = tau % 2
            S.dma("sp", XT[s], XS[512 * tau:512 * (tau + 1), :].rearrange("(c p) d -> p c d", p=128),
                  reads=[R_XS], writes=[R_XT[s]])
            for c in range(4):
                pb, pr = bank()
                pv = pb.bitcast(BF16).rearrange("p (k c) -> p k c", k=8)

                def fn(e, pv=pv, s=s, c=c):
                    ins = None
                    for kc in range(8):
                        ins = e.transpose(out=pv[:, kc, :], in_=XT[s][:, c, kc * 128:(kc + 1) * 128], identity=IDB[:])
                    return ins
                S.op("pe", fn, [R_XT[s], R_ID], pr)
                tt(XF[s][:, :, c * 128:(c + 1) * 128], pv, g_b128, ALU.mult, pr + [R_CP], [R_XF[s]])

        steps = [(tau, g) for tau in range(NTILE) for g in range(7)]

        wdflat = sw_we_down.rearrange("e r f -> (e r) f")

        def load_w(si):
            tau, g = steps[si]
            s = si % 2
            o = 8192 + s * 12288
            col = tau * 7 + g
            for (dst, src, res_) in ((arv(o, 4096), WSG, R_WB3[s][0]), (arv(o + 4096, 4096), WSU, R_WB3[s][1])):
                S.dma("pool", None, None, reads=[R_WS] + RG, writes=[res_],
                      fn=lambda e, dst=dst, src=src, col=col: e.indirect_dma_start(
                          out=dst, out_offset=None, in_=src[:, :],
                          in_offset=bass.IndirectOffsetOnAxis(ap=IDXG[:, col:col + 1], axis=0)))
            for fc in range(4):
                S.dma("pool", None, None, reads=RG, writes=[R_WB3[s][2]],
                      fn=lambda e, s=s, fc=fc, col=col: e.indirect_dma_start(
                          out=WB3[s][2][:, fc, :], out_offset=None, in_=wdflat[:, :],
                          in_offset=bass.IndirectOffsetOnAxis(ap=IDXD[:, col * 4 + fc:col * 4 + fc + 1], axis=0)))

        def gu3(si):
            tau, g = steps[si]
            s, a, x = si % 2, si % 2, tau % 2
            wg, wu, wd = WB3[s]
            for fc in range(4):
                gb, gr = bank()
                mmg(gb[:, :], [(wg[:, kc, fc * 128:(fc + 1) * 128], XF[x][:, kc, :]) for kc in range(8)],
                    [R_XF[x], R_WB3[s][0]], gr)
                ub, ur = bank()
                mmg(ub[:, :], [(wu[:, kc, fc * 128:(fc + 1) * 128], XF[x][:, kc, :]) for kc in range(8)],
                    [R_XF[x], R_WB3[s][1]], ur)
                sl = fc % 2
                act(SIL3[sl][:], gb[:, :], AF.Silu, gr, [R_SIL3[sl]])
                tt(ACT3[a][:, fc, :], ub[:, :], SIL3[sl][:], ALU.mult, ur + [R_SIL3[sl]], [R_ACT3[a][fc]])

        def down3(si):
            tau, g = steps[si]
            s, a = si % 2, si % 2
            wg, wu, wd = WB3[s]
            for c in range(4):
                yb, yr = bank(pair=True)
                for half in range(2):
                    mmg(yb[:, half, :], [(ACT3[a][:, fc, c * 128:(c + 1) * 128], wd[:, fc, half * 512:(half + 1) * 512])
                                         for fc in range(4)], R_ACT3[a] + [R_WB3[s][2]], [yr[half]])
                yv = yb.rearrange("p b c -> p (b c)")
                if g == 0:
                    copy("act", YACC[c], yv, yr, [R_YACC[c]])
                else:
                    tt(YACC[c], yv, YACC[c], ALU.add, yr + [R_YACC[c]], [R_YACC[c]])
            if g == 6:
                S.dma("sp", YS[512 * tau:512 * (tau + 1), :].rearrange("(c p) d -> p c d", p=128),
                      AR[:, 0:8192].bitcast(F32).rearrange("p (c d) -> p c d", c=4),
                      reads=R_YACC, writes=[R_YS], owner=R_YS)

        prep_tile(0)
        load_w(0)
        load_w(1)
        for si in range(len(steps)):
            tau, g = steps[si]
            gu3(si)
            if si > 0:
                down3(si - 1)
                if si + 1 < len(steps):
                    load_w(si + 1)
            if g == 2 and tau + 1 < NTILE:
                prep_tile(tau + 1)
        down3(len(steps) - 1)
        hn_flush()
        S.barrier()
        if PROFILE_MARKS:
            S.mark(nc, "L1Z_combine")
        G1 = [arv(8192 + i * 2048, 1024, F32) for i in range(2)]
        G2 = [arv(12288 + i * 2048, 1024, F32) for i in range(2)]
        R_G1 = [Res("G1%d" % i) for i in range(2)]
        R_G2 = [Res("G2%d" % i) for i in range(2)]
        R_OUT = Res("OUT")
        for j in range(1, NT):
            s = j % 2
            for (GB, RGB, base, wv) in ((G1, R_G1, 0, gW1), (G2, R_G2, 16, gW2)):
                col = base + j - 1
                S.dma("pool", None, None, reads=[R_YS] + RG, writes=[RGB[s]],
                      fn=lambda e, GB=GB, s=s, col=col: e.indirect_dma_start(
                          out=GB[s][:], out_offset=None, in_=YS[:, :],
                          in_offset=bass.IndirectOffsetOnAxis(ap=SI[:, col:col + 1], axis=0)))
                stt(HT[:, j, :], GB[s], wv[:, j - 1:j], HT[:, j, :], ALU.mult, ALU.add,
                    [RGB[s], R_HT[j]] + RG, [R_HT[j]])
            S.dma("sp", out[(j - 1) * 128:j * 128, :], HT[:, j, :], reads=[R_HT[j]], owner=R_OUT)
        S.final_wait("sp")

        block = es.enter_context(nc.Block())

        @block.tensor
        def _(e):
            for f in S.q["pe"]:
                f(e)

        @block.scalar
        def _(e):
            for f in S.q["act"]:
                f(e)

        @block.vector
        def _(e):
            for f in S.q["dve"]:
                f(e)

        @block.gpsimd
        def _(e):
            for f in S.q["pool"]:
                f(e)

        @block.sync
        def _(e):
            for f in S.q["sp"]:
                f(e)
    return nc


def _consts():
    ident = np.eye(128, dtype=np.float32)
    bd = np.zeros((128, 128), np.float32)
    bd[:64, :64] = 1.0 / 64
    bd[64:, 64:] = 1.0 / 64
    slopes = np.exp2(-8.0 * (np.arange(12, dtype=np.float32) + 1.0) / 12).astype(np.float32)
    k = np.arange(128)[:, None].astype(np.float32)
    q = np.arange(128)[None, :].astype(np.float32)
    me = np.zeros((128, 2, 4, 3, 128), np.float32)
    for kb in range(2):
        dist = q + 128.0 - k if kb == 0 else q - k
        valid = (dist >= 0) & (dist < 128)
        for kvh in range(4):
            for g in range(3):
                s = slopes[kvh * 3 + g]
                me[:, kb, kvh, g, :] = np.where(valid, np.exp(-s * dist), 0.0)
    c2 = np.zeros((128, 296), np.float32)
    pidx = np.arange(128, dtype=np.float32)[:, None]
    c2[:, 256:263] = np.arange(7, dtype=np.float32)[None, :] * 128 + pidx
    c2[:, 263:291] = (np.arange(7, dtype=np.float32)[:, None] * 512
                      + np.arange(4, dtype=np.float32)[None, :] * 128).reshape(1, 28) + pidx
    c2[:, 0:128] = (np.arange(128)[:, None] < np.arange(128)[None, :]).astype(np.float32)
    c2[:, 128:256] = np.repeat(np.arange(16, dtype=np.float32), 8)[None, :]
    return ident, bd, me.reshape(128, 3072), c2


def _cpack(inp, flag):
    cp = np.zeros((128, NCP), np.float32)

    def pk(v):
        return np.asarray(v, np.float32).reshape(8, 128).T
    cp[:, C_GA0:C_GA0 + 8] = pk(inp["cv_attn_norm_g"][0])
    cp[:, C_GF0:C_GF0 + 8] = pk(inp["cv_ffn_norm_g"][0])
    cp[:, C_GA1:C_GA1 + 8] = pk(inp["sw_attn_norm_g"][0])
    cp[:, C_GF1:C_GF1 + 8] = pk(inp["sw_ffn_norm_g"][0])
    cp[:, C_GM:C_GM + 8] = pk(inp["mem_norm_g"])
    bg = np.asarray(inp["cv_b_glu"][0], np.float32)
    cp[:, C_BA:C_BA + 6] = bg[:768].reshape(6, 128).T
    cp[:, C_BG:C_BG + 6] = bg[768:].reshape(6, 128).T
    dw = np.asarray(inp["cv_dw_w"][0], np.float32)
    cp[:, C_DW:C_DW + 186] = dw.reshape(31, 6, 128).transpose(2, 1, 0).reshape(128, 186)
    cp[:, C_DWB:C_DWB + 6] = np.asarray(inp["cv_dw_b"][0], np.float32).reshape(6, 128).T
    cp[:, C_LNG:C_LNG + 6] = np.asarray(inp["cv_ln_g"][0], np.float32).reshape(6, 128).T
    cp[:, C_LNB:C_LNB + 6] = np.asarray(inp["cv_ln_b"][0], np.float32).reshape(6, 128).T

    def h2(v):
        v = np.asarray(v, np.float32).reshape(64)
        return np.concatenate([v, v])
    cp[:, C_MQ0] = h2(inp["cv_memq_norm_g"][0])
    cp[:, C_MQ1] = h2(inp["sw_memq_norm_g"][0])
    cp[:, C_MK] = h2(inp["mem_k_norm_g"])
    cp[:, C_QG] = h2(inp["sw_q_norm_g"][0])
    cp[:, C_KG] = h2(inp["sw_k_norm_g"][0])
    cp[:, C_FLAG] = flag
    return cp


def make_in_maps(inp):
    f = lambda a: np.ascontiguousarray(np.asarray(a, dtype=np.float32))
    ident, bd, me, c2 = _consts()
    x = f(inp["x"])
    memf = f(inp["mem"])
    sinks = f(inp["sw_sinks"][0])
    sinks_rep = np.ascontiguousarray(np.repeat(sinks, 128)[None, :])
    shared = dict(
        ident=ident, bdm=bd, maskexp=me, cst2=c2, sinks_rep=sinks_rep,
        w_mem_kv=f(inp["w_mem_kv"]), cv_w_in=f(inp["cv_w_in"][0]), cv_w_out=f(inp["cv_w_out"][0]),
        cv_w_gate=f(inp["cv_w_gate"][0]), cv_w_up=f(inp["cv_w_up"][0]), cv_w_down=f(inp["cv_w_down"][0]),
        sw_w_in=f(inp["sw_w_in"][0]), sw_w_out=f(inp["sw_w_out"][0]), sw_router=f(inp["sw_router"][0]),
        sw_we_gate=f(inp["sw_we_gate"][0]), sw_we_up=f(inp["sw_we_up"][0]), sw_we_down=f(inp["sw_we_down"][0]),
    )
    maps = []
    for c in range(NCORES):
        b, qtr = divmod(c, 4)
        xin = np.zeros((TC, D), np.float32)
        t0 = qtr * TOK
        if qtr == 0:
            xin[160:] = x[b, 0:TOK]
        else:
            xin[:] = x[b, t0 - 160:t0 + TOK]
        m = dict(shared)
        m["xin"] = xin
        m["mem"] = memf[b]
        m["cpack"] = _cpack(inp, 0.0 if qtr == 0 else 1.0)
        maps.append(m)
    return maps


def kernel(**inputs):
    nc = build_program()
    maps = make_in_maps(inputs)
    res = run_bass_kernel_spmd(nc, maps, core_ids=list(range(NCORES)))
    outs = [np.asarray(r["out"], np.float32) for r in res.results]
    full = np.concatenate(outs, axis=0).reshape(2, SEQ, D)
    return full
```

```python
import os
from contextlib import ExitStack

import numpy as np
import concourse.bass as bass
import concourse.mybir as mybir
from concourse.bass_utils import run_bass_kernel_spmd

F32 = mybir.dt.float32
BF16 = mybir.dt.bfloat16
I32 = mybir.dt.int32
AF = mybir.ActivationFunctionType
ALU = mybir.AluOpType
AX = mybir.AxisListType

NCORES = 8
D = 1024
SEQ = 8192
TOK = 2048
NT = 17
TC = 2208
HD = 64
DFF0 = 2816
DFFE = 3584
NE = 8
CONV_W = 31
CONV_CH = 768
RMS_EPS = 1e-6
LN_EPS = 1e-5

C_GA0, C_GF0, C_GA1, C_GF1, C_GM = 0, 8, 16, 24, 32
C_BA, C_BG = 40, 46
C_DW = 52
C_DWB, C_LNG, C_LNB = 238, 244, 250
C_MQ0, C_MQ1, C_MK, C_QG, C_KG, C_FLAG = 256, 257, 258, 259, 260, 261
NCP = 264

ENGS = ("pe", "act", "dve", "pool", "sp")
PROFILE_MARKS = bool(os.environ.get("MK_MARKS"))


class Res:
    __slots__ = ("name", "w", "r", "dsem", "dcnt")

    def __init__(self, name):
        self.name = name
        self.w = None
        self.r = []
        self.dsem = None
        self.dcnt = 0


class Sched:
    def __init__(self, esem, dsems):
        self.esem = dict(esem)
        self.free_dsems = list(dsems)
        self.cnt = {e: 0 for e in ENGS}
        self.seen = {e: {} for e in ENGS}
        self.q = {e: [] for e in ENGS}
        self.dlast = {}
        self.nbank = 0
        self._reg = None

    def _deps(self, eng, reads, writes, skip_key=None):
        deps = {}

        def add(ev, same_ok):
            if ev is None:
                return
            k, v = ev
            if (k == eng or k == skip_key) and not same_ok:
                return
            if deps.get(k, 0) < v:
                deps[k] = v
        for r in reads:
            add(r.w, True)
        for w in writes:
            add(w.w, False)
            for ev in w.r:
                add(ev, False)
        waits = []
        for k, v in deps.items():
            if self.seen[eng].get(k, 0) < v:
                self.seen[eng][k] = v
                waits.append((self.esem[k], v))
        return waits

    def _commit(self, ev, reads, writes):
        for r in reads:
            r.r.append(ev)
        for w in writes:
            w.w = ev
            w.r = []

    def op(self, eng, fn, reads=(), writes=()):
        waits = self._deps(eng, reads, writes)
        self.cnt[eng] += 1
        ev = (eng, self.cnt[eng])
        sem = self.esem[eng]

        def run(e, waits=waits, fn=fn, sem=sem):
            for s, v in waits:
                e.wait_ge(s, v)
            fn(e).then_inc(sem, 1)
        self.q[eng].append(run)
        self._commit(ev, reads, writes)
        return ev

    def dma(self, q, out, in_, reads=(), writes=(), owner=None, fn=None, **kw):
        owner = owner or (writes[0] if writes else reads[0])
        if owner.dsem is None:
            owner.dsem = self.free_dsems.pop()
            self.esem[("d", id(owner))] = owner.dsem
        key = ("d", id(owner))
        waits = self._deps(q, reads, writes, skip_key=key)
        owner.dcnt += 16
        ev = (key, owner.dcnt)
        self.dlast[key] = owner.dcnt
        sem = owner.dsem
        if self._reg is not None:
            self._reg["dma"][q][key] = self._reg["dma"][q].get(key, 0) + 16

        def run(e, waits=waits, sem=sem, out=out, in_=in_, kw=kw, fn=fn):
            for s, v in waits:
                e.wait_ge(s, v)
            if fn is not None:
                fn(e).then_inc(sem, 16)
            else:
                e.dma_start(out=out, in_=in_, **kw).then_inc(sem, 16)
        self.q[q].append(run)
        self._commit(ev, reads, writes)
        return ev

    def raw(self, eng, fn, reads=()):
        waits = self._deps(eng, reads, ())

        def run(e, waits=waits, fn=fn):
            for s, v in waits:
                e.wait_ge(s, v)
            fn(e)
        self.q[eng].append(run)

    def barrier(self):
        evs = [(e, self.cnt[e]) for e in ENGS if self.cnt[e] > 0]
        evs += list(self.dlast.items())
        for eng in ENGS:
            waits = []
            for k, v in evs:
                if k == eng:
                    continue
                if self.seen[eng].get(k, 0) < v:
                    self.seen[eng][k] = v
                    waits.append((self.esem[k], v))
            if waits:
                def run(e, waits=waits):
                    for s, v in waits:
                        e.wait_ge(s, v)
                self.q[eng].append(run)

    def region_begin(self, tag):
        self._reg = dict(tag=tag, cnt0=dict(self.cnt), dma={e: {} for e in ENGS},
                         seen0={e: dict(v) for e, v in self.seen.items()})
        for eng in ENGS:
            self.q[eng].append(("begin", tag))

    def region_end(self):
        r = self._reg
        for eng in ENGS:
            self.q[eng].append(("end", dict(own=self.cnt[eng] - r["cnt0"][eng], dma=dict(r["dma"][eng]))))
        self.seen = r["seen0"]
        self._reg = None

    def replay(self, eng, e, regs):
        items = self.q[eng]
        i = 0
        while i < len(items):
            it = items[i]
            if isinstance(it, tuple) and it[0] == "begin":
                j = i + 1
                body = []
                while not (isinstance(items[j], tuple) and items[j][0] == "end"):
                    body.append(items[j])
                    j += 1
                comp = items[j][1]
                rt, rk = regs[eng]
                e.reg_mov(rk, it[1])
                with e.If_lt(rk, rt):
                    for f in body:
                        f(e)
                with e.Else():
                    n = comp["own"]
                    while n > 0:
                        e.sem_inc(self.esem[eng], min(n, 8))
                        n -= min(n, 8)
                    for key, v in comp["dma"].items():
                        for _ in range(v // 16):
                            e.sem_inc(self.esem[key], 16)
                i = j + 1
            else:
                it(e)
                i += 1

    def mark(self, nc, name):
        st = self.__dict__.setdefault("_scope", {})

        for eng in ENGS:
            def run(e, eng=eng, name=name):
                if eng in st:
                    nc.leave_named_scope(st[eng][0], st[eng][1], False)
                sid, _ = nc.enter_named_scope(name, False)
                st[eng] = (name, sid)
            self.q[eng].append(run)

    def final_wait(self, eng):
        waits = []
        for k, v in self.dlast.items():
            if self.seen[eng].get(k, 0) < v:
                self.seen[eng][k] = v
                waits.append((self.esem[k], v))

        def run(e, waits=waits):
            for s, v in waits:
                e.wait_ge(s, v)
        self.q[eng].append(run)


def build_program(debug=()):
    nc = bass.Bass("TRN2", target_bir_lowering=False)

    def din(name, shape):
        return nc.dram_tensor(name, list(shape), F32, kind="ExternalInput").ap()

    xin = din("xin", [TC, D])
    mem = din("mem", [256, D])
    cpack = din("cpack", [128, NCP])
    ident = din("ident", [128, 128])
    bdm = din("bdm", [128, 128])
    maskexp = din("maskexp", [128, 3072])
    sinks_rep = din("sinks_rep", [1, 1536])
    w_mem_kv = din("w_mem_kv", [D, 512])
    cv_w_in = din("cv_w_in", [D, 1792])
    cv_w_out = din("cv_w_out", [D, D])
    cv_w_gate = din("cv_w_gate", [D, DFF0])
    cv_w_up = din("cv_w_up", [D, DFF0])
    cv_w_down = din("cv_w_down", [DFF0, D])
    sw_w_in = din("sw_w_in", [D, 1536])
    sw_w_out = din("sw_w_out", [D, D])
    sw_router = din("sw_router", [D, NE])
    sw_we_gate = din("sw_we_gate", [NE, D, DFFE])
    sw_we_up = din("sw_we_up", [NE, D, DFFE])
    sw_we_down = din("sw_we_down", [NE, DFFE, D])
    cst2 = din("cst2", [128, 296])
    WSG = nc.dram_tensor("wsg_scr", [NE * 7 * 128, 4096], BF16, kind="Internal").ap()
    WSU = nc.dram_tensor("wsu_scr", [NE * 7 * 128, 4096], BF16, kind="Internal").ap()
    XS = nc.dram_tensor("xs_scr", [8192, D], BF16, kind="Internal").ap()
    YS = nc.dram_tensor("ys_scr", [8192, D], F32, kind="Internal").ap()
    out = nc.dram_tensor("out", [TOK, D], F32, kind="ExternalOutput").ap()
    dbg_out = {}
    for name, shape in debug:
        dbg_out[name] = nc.dram_tensor("dbg_" + name, list(shape), F32, kind="ExternalOutput").ap()

    es = ExitStack()
    with es:
        def sb(name, shape, dt):
            return es.enter_context(nc.sbuf_tensor(name, list(shape), dt))

        HT = sb("HT", [128, NT, D], F32)
        RA = sb("RA", [128, 8, TC], BF16)
        AR = sb("AR", [128, 40960], BF16)
        CP = sb("CP", [128, NCP], F32)
        IDF = sb("IDF", [128, 128], F32)
        IDB = sb("IDB", [128, 128], BF16)
        BDB = sb("BDB", [128, 128], BF16)
        ONB = sb("ONB", [128, 128], BF16)
        ME = sb("ME", [128, 2, 4, 384], BF16)
        ME0 = sb("ME0", [128, 4, 384], BF16)
        SRB = sb("SRB", [1, 4, 384], BF16)
        MKT = sb("MKT", [128, 2, 256], BF16)
        MV = sb("MV", [128, 2, 256], BF16)
        SS = sb("SS", [128, 3, 32], F32)
        GT = sb("GT", [128, 16, 8], F32)
        WR = sb("WR", [128, 8, NE], F32)
        PS = es.enter_context(nc.psum_tensor("PS", [128, 8, 512], F32))

        esem = {e: es.enter_context(nc.semaphore("sem_" + e)) for e in ENGS}
        dsems = [es.enter_context(nc.semaphore("dsem%d" % i)) for i in range(72)]
        S = Sched(esem, dsems)

        banks = [Res("bank%d" % i) for i in range(8)]
        reserved = set()

        def bank(pair=False):
            while True:
                b = S.nbank % 8
                if pair and (b % 2 == 1 or (b + 1) in reserved):
                    S.nbank += 1
                    continue
                if b in reserved:
                    S.nbank += 1
                    continue
                break
            if pair:
                S.nbank += 2
                return PS[:, b:b + 2, :], [banks[b], banks[b + 1]]
            S.nbank += 1
            return PS[:, b, :], [banks[b]]

        def mmg(out_ap, pairs, reads, writes):
            def fn(e, out_ap=out_ap, pairs=pairs):
                n = len(pairs)
                ins = None
                for i, (l, r) in enumerate(pairs):
                    ins = e.matmul(out_ap, l, r, start=(i == 0), stop=(i == n - 1))
                return ins
            S.op("pe", fn, reads, writes)

        def act(out_ap, in_ap, func, reads, writes, **kw):
            S.op("act", lambda e: e.activation(out=out_ap, in_=in_ap, func=func, **kw), reads, writes)

        def tt(out_ap, in0, in1, op, reads, writes, eng="dve"):
            S.op(eng, lambda e: e.tensor_tensor(out=out_ap, in0=in0, in1=in1, op=op), reads, writes)

        def ts(out_ap, in0, s1, op0, reads, writes, s2=None, op1=None, eng="dve"):
            if op1 is None:
                S.op(eng, lambda e: e.tensor_scalar(out=out_ap, in0=in0, scalar1=s1, scalar2=None, op0=op0),
                     reads, writes)
            else:
                S.op(eng, lambda e: e.tensor_scalar(out=out_ap, in0=in0, scalar1=s1, scalar2=s2, op0=op0, op1=op1),
                     reads, writes)

        def stt(out_ap, in0, scalar, in1, op0, op1, reads, writes):
            S.op("dve", lambda e: e.scalar_tensor_tensor(out=out_ap, in0=in0, scalar=scalar, in1=in1,
                                                         op0=op0, op1=op1), reads, writes)

        def recip(out_ap, in_ap, reads, writes):
            S.op("dve", lambda e: e.reciprocal(out=out_ap, in_=in_ap), reads, writes)

        def copy(eng, out_ap, in_ap, reads, writes):
            if eng == "act":
                act(out_ap, in_ap, AF.Copy, reads, writes)
            else:
                S.op(eng, lambda e: e.tensor_copy(out=out_ap, in_=in_ap), reads, writes)

        def dump(name, ap, reads, view=None):
            if name in dbg_out:
                dst = dbg_out[name]
                if view:
                    dst = dst.rearrange(view[0], **view[1])
                S.dma("pool", dst, ap, reads=reads, writes=(), owner=R_dbg)

        R_dbg = Res("dbg")

        def cpc(c, n=1):
            return CP[:, c:c + n]

        def arv(off, n, dt=BF16):
            if dt == F32:
                return AR[:, off:off + 2 * n].bitcast(F32)
            return AR[:, off:off + n]

        R_HT = [Res("HT%d" % j) for j in range(NT)]
        R_RA = [Res("RA%d" % j) for j in range(NT + 1)]
        R_CP, R_ID, R_ME, R_SR = Res("CP"), Res("ID"), Res("ME"), Res("SR")
        R_MKV = Res("MKV")
        R_SS = Res("SS")

        def tcols(j):
            if j == 17:
                return 0, 32
            return 32 + 128 * j, 128

        S.dma("sp", CP[:], cpack[:, :], writes=[R_CP])
        S.dma("sp", IDF[:], ident[:, :], writes=[R_ID])
        R_BD = Res("BD")
        PB0 = 22016
        BDF = arv(PB0, 128, F32)
        S.dma("sp", BDF, bdm[:, :], writes=[R_BD])
        MEf = ME[:].rearrange("p a b c -> p (a b c)")
        S.dma("pool", MEf[:, 0:1536], maskexp[:, 0:1536], writes=[R_ME])
        S.dma("pool", MEf[:, 1536:3072], maskexp[:, 1536:3072], writes=[R_ME])
        SRF = arv(14336, 1536, F32)[0:1, :]
        S.dma("sp", SRF, sinks_rep[:, :], writes=[R_SR])
        WIN0 = arv(0, 8 * 1792).rearrange("p (k n) -> p k n", k=8)
        R_WIN0 = Res("WIN0")
        S.dma("pool", WIN0, cv_w_in.rearrange("(k p) n -> p k n", p=128), writes=[R_WIN0])
        R_XP = Res("XP")
        R_WS = Res("WS")
        pp_list = [(ws_, w_, ex, g_) for ex in range(NE) for g_ in range(7) for (ws_, w_) in ((WSG, sw_we_gate), (WSU, sw_we_up))]
        pp_state = {"i": 0}

        def prepass(n):
            for _ in range(n):
                if pp_state["i"] >= len(pp_list):
                    return
                ws_, w_, ex, g_ = pp_list[pp_state["i"]]
                pp_state["i"] += 1
                r0 = (ex * 7 + g_) * 128
                S.dma("pool", ws_[r0:r0 + 128, :].rearrange("p (k n) -> p k n", k=8),
                      w_[ex, :, g_ * 512:(g_ + 1) * 512].rearrange("(k p) n -> p k n", p=128),
                      writes=[R_WS], owner=R_WS)
        ZT = sb("ZT", [128, 2048], BF16)
        R_ZT = Res("ZT")
        R_XS = Res("XS")
        S.op("dve", lambda e: e.memset(ZT[:], 0.0), [], [R_ZT])
        XP = arv(PB0 + 256, D, F32)
        S.dma("sp", XP[0:32, :], xin[0:32, :], writes=[R_XP])
        R_MEM = Res("MEM")
        MEMT = arv(PB0 + 2304, 2 * D, F32).rearrange("p (t d) -> p t d", t=2)
        S.dma("sp", MEMT, mem.rearrange("(t p) d -> p t d", p=128), writes=[R_MEM])
        R_WKV = Res("WKV")
        WKV = arv(PB0 + 6400, 8 * 512).rearrange("p (k n) -> p k n", k=8)
        S.dma("pool", WKV, w_mem_kv.rearrange("(k p) n -> p k n", p=128), writes=[R_WKV])
        for j0, j1 in ((0, 2), (2, 5), (5, 9), (9, 13), (13, 17)):
            S.dma("sp", HT[:, j0:j1, :],
                  xin[32 + 128 * j0:32 + 128 * j1, :].rearrange("(j p) d -> p j d", p=128),
                  writes=R_HT[j0:j1])
        R_WR = Res("WR")
        S.dma("sp", WR[:], sw_router.rearrange("(k p) n -> p k n", p=128), writes=[R_WR])
        XSz = XS.rearrange("(a p r) d -> a p (r d)", p=128, r=2)
        for a_ in range(32):
            S.dma("sp", XSz[a_], ZT[:], reads=[R_ZT], writes=[R_XS], owner=R_XS)
        prepass(8)

        copy("act", IDB[:], IDF[:], [R_ID], [R_ID])
        copy("act", BDB[:], BDF, [R_BD], [R_BD])
        S.op("dve", lambda e: e.memset(ONB[:], 1.0), [], [R_ID])
        act(SRB[:].rearrange("p a b -> p (a b)"), SRF, AF.Exp, [R_SR], [R_SR])
        ts(ME0[:], ME[:, 0], cpc(C_FLAG), ALU.mult, [R_ME, R_CP], [R_ME])

        A_TAIL = 37888
        HNB = [arv(A_TAIL + i * 1024, 1024) for i in range(2)]
        R_HNB = [Res("HNB%d" % i) for i in range(2)]
        JUNK = arv(A_TAIL + 2048, 1024)
        R_JUNK = Res("JUNK")
        ss_state = {"n": 0}

        def norm_transpose(j, gcol, src_ap=None, src_res=None, npart=128, fp32_path=None):
            k = ss_state["n"] % 32
            ss_state["n"] += 1
            if src_ap is None:
                src_ap, src_res = HT[:, j, :], R_HT[j]
            c0, n = tcols(j)
            P = slice(0, npart)
            act(JUNK[P, :], src_ap, AF.Square, [src_res], [R_JUNK, R_SS], accum_out=SS[P, 0, k:k + 1])
            act(SS[P, 1, k:k + 1], SS[P, 0, k:k + 1], AF.Sqrt, [R_SS], [R_SS], scale=1.0 / D, bias=RMS_EPS)
            recip(SS[P, 2, k:k + 1], SS[P, 1, k:k + 1], [R_SS], [R_SS])
            if fp32_path is None:
                s = j % 2
                ts(HNB[s][P, :], src_ap, SS[P, 2, k:k + 1], ALU.mult, [src_res, R_SS], [R_HNB[s]])
                pb, pr = bank()
                pv = pb.bitcast(BF16).rearrange("p (k c) -> p k c", k=8)

                def fn(e, pv=pv, s=s, P=P, n=n):
                    ins = None
                    for kc in range(8):
                        ins = e.transpose(out=pv[:, kc, 0:n], in_=HNB[s][P, kc * 128:(kc + 1) * 128],
                                          identity=IDB[P, 0:n])
                    return ins
                S.op("pe", fn, [R_HNB[s], R_ID], pr)
                g_b = CP[:, gcol:gcol + 8].unsqueeze(2).to_broadcast([128, 8, n])
                tt(RA[:, :, c0:c0 + n], pv[:, :, 0:n], g_b, ALU.mult, pr + [R_CP], [R_RA[j]])
            else:
                HN32, R_HN32, H32T, R_H32T = fp32_path
                s = j % 2
                ts(HN32[s], src_ap, SS[:, 2, k:k + 1], ALU.mult, [src_res, R_SS], [R_HN32[s]])
                pb, pr = bank(pair=True)
                pv = pb.rearrange("p b (k c) -> p (b k) c", k=4)

                def fn(e, pv=pv, s=s):
                    ins = None
                    for kc in range(8):
                        ins = e.transpose(out=pv[:, kc, :], in_=HN32[s][:, kc * 128:(kc + 1) * 128],
                                          identity=IDF[:])
                    return ins
                S.op("pe", fn, [R_HN32[s], R_ID], pr)
                g_b = CP[:, gcol:gcol + 8].unsqueeze(2).to_broadcast([128, 8, 128])
                tt(RA[:, :, c0:c0 + n], pv, g_b, ALU.mult, pr + [R_CP], [R_RA[j]])
                tt(H32T[s], pv, g_b, ALU.mult, pr + [R_CP], [R_H32T[s]])

        hn_state = {"n": 0}

        hn_pending = []

        def hn_flush(keep=0):
            while len(hn_pending) > keep:
                hn_pending.pop(0)()

        def headnorm(q_ap, q_res, gcol, out_ap, out_res, n, bufs, split=None):
            SQ, R_SQ, RT, R_RT = bufs
            s = hn_state["n"] % 2
            hn_state["n"] += 1
            act(SQ[s][:, :n], q_ap, AF.Square, q_res, [R_SQ[s]])

            def rest():
                sb_, sr_ = bank()
                mmg(sb_[:, :n], [(BDB[:], SQ[s][:, :n])], [R_SQ[s], R_BD], sr_)
                act(RT[s][:, :n], sb_[:, :n], AF.Sqrt, sr_, [R_RT[s]], bias=RMS_EPS)
                recip(RT[s][:, :n], RT[s][:, :n], [R_RT[s]], [R_RT[s]])
                qv, rv = q_ap, RT[s][:, :n]
                if split:
                    qv = qv.rearrange("p (a b) -> p a b", b=split)
                    rv = rv.rearrange("p (a b) -> p a b", b=split)
                stt(out_ap, qv, cpc(gcol), rv, ALU.mult, ALU.mult, q_res + [R_RT[s], R_CP], out_res)
            hn_flush(keep=0)
            hn_pending.append(rest)

        def mem_attention(QM, R_QM, blocks, qcol_off, bufs):
            PT, R_PT, RD, R_RD = bufs
            npt = len(PT)
            st = {"n": 0, "r": 0}
            its = [(c0, n, tiles, hd) for (c0, n, tiles) in blocks for hd in range(4)]
            pend = {}

            def stage_a(i):
                c0, n, tiles, hd = its[i]
                q0 = c0 - qcol_off
                c2, hb = hd // 2, hd % 2
                rows = slice(hb * 64, hb * 64 + 64)
                pts = []
                for mc in range(2):
                    s = st["n"] % npt
                    st["n"] += 1
                    sb_, sr_ = bank()
                    mmg(sb_[:, :n], [(MKT[rows, c2, mc * 128:(mc + 1) * 128], QM[rows, c2, q0:q0 + n])],
                        [R_MKV, R_QM], sr_)
                    act(PT[s][:, :n], sb_[:, :n], AF.Exp, sr_, [R_PT[s]], scale=0.125)
                    pts.append(s)
                pend[i] = pts

            def stage_b(i):
                c0, n, tiles, hd = its[i]
                c2, hb = hd // 2, hd % 2
                rows = slice(hb * 64, hb * 64 + 64)
                pts = pend.pop(i)
                ob, orr = bank()
                mmg(ob[rows, :n], [(MV[:, mc, hd * 64:(hd + 1) * 64], PT[pts[mc]][:, :n]) for mc in range(2)],
                    [R_MKV] + [R_PT[s] for s in pts], orr)
                db, dr = bank()
                mmg(db[rows, :n], [(ONB[:, 0:64], PT[pts[mc]][:, :n]) for mc in range(2)],
                    [R_ID] + [R_PT[s] for s in pts], dr)
                s2 = st["r"] % 2
                st["r"] += 1
                recip(RD[s2][rows, :n], db[rows, :n], dr, [R_RD[s2]])
                tt(RA[rows, 6 + c2, c0:c0 + n], ob[rows, :n], RD[s2][rows, :n], ALU.mult,
                   orr + [R_RD[s2]], [R_RA[t] for t in tiles])
            stage_a(0)
            for i in range(len(its)):
                if i + 1 < len(its):
                    stage_a(i + 1)
                stage_b(i)

        def out_proj(WO, R_WO, tiles):
            for j in tiles:
                c0, n = tcols(j)
                yb, yr = bank(pair=True)
                for half in range(2):
                    mmg(yb[:, half, :], [(RA[:, kc, c0:c0 + 128], WO[:, kc, half * 512:(half + 1) * 512])
                                         for kc in range(8)], [R_RA[j], R_WO], [yr[half]])
                tt(HT[:, j, :], yb.rearrange("p b c -> p (b c)"), HT[:, j, :], ALU.add, yr + [R_HT[j]], [R_HT[j]])

        def ffn(groups, blocks, WB, R_WB, ACTB, R_ACTB, SIL, R_SIL, after_load=None):
            steps = [(gi, bi) for gi in range(len(groups)) for bi in range(len(blocks))]

            def load(gi):
                g = groups[gi]
                s = gi % 2
                nfc = g["nfc"]
                wg, wu, wd = WB[s]
                S.dma("pool", wg[:, :, 0:nfc * 128], g["g"].rearrange("(k p) n -> p k n", p=128),
                      writes=[R_WB[s][0]])
                S.dma("pool", wu[:, :, 0:nfc * 128], g["u"].rearrange("(k p) n -> p k n", p=128),
                      writes=[R_WB[s][1]])
                S.dma("pool", wd[:, 0:nfc, :], g["d"].rearrange("(f p) n -> p f n", p=128),
                      writes=[R_WB[s][2]])
                if after_load is not None:
                    after_load()

            def gu(si):
                gi, bi = steps[si]
                g = groups[gi]
                s = gi % 2
                a = si % 2
                wg, wu, wd = WB[s]
                c0, n, tiles = blocks[bi]
                rr = [R_RA[t] for t in tiles]
                for fc in range(g["nfc"]):
                    gb, gr = bank()
                    mmg(gb[:, :n], [(wg[:, kc, fc * 128:(fc + 1) * 128], RA[:, kc, c0:c0 + n]) for kc in range(8)],
                        rr + [R_WB[s][0]], gr)
                    ub, ur = bank()
                    mmg(ub[:, :n], [(wu[:, kc, fc * 128:(fc + 1) * 128], RA[:, kc, c0:c0 + n]) for kc in range(8)],
                        rr + [R_WB[s][1]], ur)
                    sl = (si * 4 + fc) % 2
                    act(SIL[sl][:, :n], gb[:, :n], AF.Silu, gr, [R_SIL[sl]])
                    tt(ACTB[a][:, fc, :n], ub[:, :n], SIL[sl][:, :n], ALU.mult, ur + [R_SIL[sl]], [R_ACTB[a][fc]])

            def down(si):
                gi, bi = steps[si]
                g = groups[gi]
                s = gi % 2
                a = si % 2
                wg, wu, wd = WB[s]
                c0, n, tiles = blocks[bi]
                nfc = g["nfc"]
                for ti, j in enumerate(tiles):
                    yb, yr = bank(pair=True)
                    for half in range(2):
                        mmg(yb[:, half, :], [(ACTB[a][:, fc, ti * 128:(ti + 1) * 128],
                                              wd[:, fc, half * 512:(half + 1) * 512]) for fc in range(nfc)],
                            [R_ACTB[a][fc] for fc in range(nfc)] + [R_WB[s][2]], [yr[half]])
                    yv = yb.rearrange("p b c -> p (b c)")
                    if g["gate"] is None:
                        tt(HT[:, j, :], yv, HT[:, j, :], ALU.add, yr + [R_HT[j]], [R_HT[j]])
                    else:
                        stt(HT[:, j, :], yv, g["gate"](j), HT[:, j, :], ALU.mult, ALU.add,
                            yr + [R_HT[j], R_GT], [R_HT[j]])

            load(0)
            if len(groups) > 1:
                load(1)
            nb = len(blocks)
            for si in range(len(steps)):
                gu(si)
                if si > 0:
                    down(si - 1)
                    gi_prev, bi_prev = steps[si - 1]
                    if bi_prev == nb - 1 and gi_prev + 2 < len(groups):
                        load(gi_prev + 2)
            down(len(steps) - 1)

        R_GT = Res("GT")

        if PROFILE_MARKS:
            S.mark(nc, "P_mem")
        MEMX = arv(PB0 + 10496, 8 * 256).rearrange("p (k c) -> p k c", k=8)
        R_MEMX = [Res("MEMX0"), Res("MEMX1")]
        SQb = [arv(PB0 + 12544 + i * 512, 512) for i in range(2)]
        R_SQb = [Res("SQ%d" % i) for i in range(2)]
        RTb = [arv(PB0 + 13568 + i * 1024, 512, F32) for i in range(2)]
        R_RTb = [Res("RT%d" % i) for i in range(2)]
        hbufs = (SQb, R_SQb, RTb, R_RTb)
        for t in range(2):
            k = ss_state["n"] % 32
            ss_state["n"] += 1
            act(JUNK[:], MEMT[:, t, :], AF.Square, [R_MEM], [R_JUNK, R_SS], accum_out=SS[:, 0, k:k + 1])
            act(SS[:, 1, k:k + 1], SS[:, 0, k:k + 1], AF.Sqrt, [R_SS], [R_SS], scale=1.0 / D, bias=RMS_EPS)
            recip(SS[:, 2, k:k + 1], SS[:, 1, k:k + 1], [R_SS], [R_SS])
            ts(HNB[t][:], MEMT[:, t, :], SS[:, 2, k:k + 1], ALU.mult, [R_MEM, R_SS], [R_HNB[t]])
            pb, pr = bank()
            pv = pb.bitcast(BF16).rearrange("p (k c) -> p k c", k=8)

            def fn(e, pv=pv, t=t):
                ins = None
                for kc in range(8):
                    ins = e.transpose(out=pv[:, kc, :], in_=HNB[t][:, kc * 128:(kc + 1) * 128], identity=IDB[:])
                return ins
            S.op("pe", fn, [R_HNB[t], R_ID], pr)
            g_b = CP[:, C_GM:C_GM + 8].unsqueeze(2).to_broadcast([128, 8, 128])
            tt(MEMX[:, :, t * 128:(t + 1) * 128], pv, g_b, ALU.mult, pr + [R_CP], [R_MEMX[t]])
        for c in range(2):
            kb_, kr_ = bank()
            mmg(kb_[:, :256], [(WKV[:, kc, c * 128:(c + 1) * 128], MEMX[:, kc, :]) for kc in range(8)],
                R_MEMX + [R_WKV], kr_)
            headnorm(kb_[:, :256], kr_, C_MK, MKT[:, c, :], [R_MKV], 256, hbufs)
        hn_flush()
        for t in range(2):
            vb_, vr_ = bank()
            mmg(vb_[:, :256], [(MEMX[:, kc, t * 128:(t + 1) * 128], WKV[:, kc, 256:512]) for kc in range(8)],
                [R_MEMX[t], R_WKV], vr_)
            copy("act", MV[:, t, :], vb_[:, :256], vr_, [R_MKV])
        hn_flush()
        dump("mkt", MKT[:].rearrange("p a b -> p (a b)"), [R_MKV])
        dump("mv", MV[:].rearrange("p a b -> p (a b)"), [R_MKV])

        if PROFILE_MARKS:
            S.mark(nc, "L0A_norm")
        norm_transpose(17, C_GA0, src_ap=XP[0:32, :], src_res=R_XP, npart=32)
        for j in range(NT):
            norm_transpose(j, C_GA0)
        hn_flush()
        S.barrier()
        if PROFILE_MARKS:
            S.mark(nc, "L0B_inproj")
        prepass(8)
        U = arv(14336, 6 * TC).rearrange("p (c t) -> p c t", c=6)
        R_U = [Res("U%d" % c) for c in range(6)]
        QM0 = arv(27584, 2 * 2176).rearrange("p (c t) -> p c t", c=2)
        R_QM0 = Res("QM0")
        SG = [arv(31936 + i * 1024, 512, F32) for i in range(2)]
        R_SG = [Res("SG%d" % i) for i in range(2)]
        SQb = [arv(33984 + i * 512, 512) for i in range(2)]
        RTb = [arv(35008 + i * 1024, 512, F32) for i in range(2)]
        hbufs = (SQb, R_SQb, RTb, R_RTb)
        TB0 = [(0, 160, [17, 0])] + [(160 + 512 * i, 512, [1 + 4 * i + t for t in range(4)]) for i in range(4)]
        sgi = 0
        for c in range(6):
            for (c0, n, tiles) in TB0:
                rr = [R_RA[t] for t in tiles]
                ab, ar_ = bank()
                mmg(ab[:, :n], [(WIN0[:, kc, c * 128:(c + 1) * 128], RA[:, kc, c0:c0 + n]) for kc in range(8)],
                    rr + [R_WIN0], ar_)
                gb, gr = bank()
                mmg(gb[:, :n], [(WIN0[:, kc, (6 + c) * 128:(7 + c) * 128], RA[:, kc, c0:c0 + n]) for kc in range(8)],
                    rr + [R_WIN0], gr)
                s = sgi % 2
                sgi += 1
                act(SG[s][:, :n], gb[:, :n], AF.Sigmoid, gr + [R_CP], [R_SG[s]], bias=cpc(C_BG + c))
                stt(U[:, c, c0:c0 + n], ab[:, :n], cpc(C_BA + c), SG[s][:, :n], ALU.add, ALU.mult,
                    ar_ + [R_SG[s], R_CP], [R_U[c]])
                if c0 == 0:
                    ts(U[:, c, 0:160], U[:, c, 0:160], cpc(C_FLAG), ALU.mult, [R_U[c], R_CP], [R_U[c]])
        TBC = [(32, 128, [0])] + TB0[1:]
        for c2 in range(2):
            for (c0, n, tiles) in TBC:
                rr = [R_RA[t] for t in tiles]
                qb, qr = bank()
                mmg(qb[:, :n], [(WIN0[:, kc, (12 + c2) * 128:(13 + c2) * 128], RA[:, kc, c0:c0 + n])
                                for kc in range(8)], rr + [R_WIN0], qr)
                headnorm(qb[:, :n], qr, C_MQ0, QM0[:, c2, c0 - 32:c0 - 32 + n], [R_QM0], n, hbufs)
        hn_flush()
        dump("u", U[:, 0, :], [R_U[0]])
        dump("qm0", QM0[:, 0, :], [R_QM0])
        hn_flush()
        S.barrier()
        if PROFILE_MARKS:
            S.mark(nc, "L0C_conv")
        prepass(12)
        DG = [arv(i * 3968, 3968).rearrange("p (k c) -> p k c", k=CONV_W) for i in range(2)]
        R_DG = [Res("DG%d" % i) for i in range(2)]
        R_C = [[Res("C%d_%d" % (c, j)) for j in range(NT)] for c in range(6)]

        def build_dg(c):
            s = c % 2
            S.op("dve", lambda e, s=s, c=c: e.tensor_tensor(
                out=DG[s], in0=IDB[:].unsqueeze(1).to_broadcast([128, CONV_W, 128]),
                in1=CP[:, C_DW + c * CONV_W:C_DW + (c + 1) * CONV_W].unsqueeze(2).to_broadcast([128, CONV_W, 128]),
                op=ALU.mult), [R_ID, R_CP], [R_DG[s]])

        def conv_block(c, blk):
            s = c % 2
            c0, n, tiles = blk
            cb, cr = bank()
            mmg(cb[:, :n], [(DG[s][:, k, :], U[:, c, c0 - 30 + k:c0 - 30 + k + n]) for k in range(CONV_W)],
                [R_DG[s], R_U[c]], cr)
            act(RA[:, c, c0:c0 + n], cb[:, :n], AF.Identity, cr + [R_CP], [R_C[c][t] for t in tiles],
                bias=cpc(C_DWB + c))
        SQL = [arv(7936 + i * 512, 512) for i in range(2)]
        R_SQL = [Res("SQL%d" % i) for i in range(2)]
        MSQ = arv(8960, 512, F32)
        VAR = arv(9984, 512, F32)
        R_ST = Res("LNST")
        T1 = [arv(11008 + i * 1024, 512, F32) for i in range(2)]
        R_T1 = [Res("T1%d" % i) for i in range(2)]
        ln_st = {"q": 0}

        def ln_block(blk):
            c0, n, tiles = blk
            rall = [R_C[c][t] for c in range(6) for t in tiles]
            s1b, s1r = bank()
            mmg(s1b[:, :n], [(ONB[:], RA[:, c, c0:c0 + n]) for c in range(6)], rall + [R_ID], s1r)
            s2b, s2r = bank()
            for c in range(6):
                s = ln_st["q"] % 2
                ln_st["q"] += 1
                act(SQL[s][:, :n], RA[:, c, c0:c0 + n], AF.Square, [R_C[c][t] for t in tiles], [R_SQL[s]])
                S.op("pe", lambda e, s2b=s2b, s=s, n=n, c=c: e.matmul(s2b[:, :n], ONB[:], SQL[s][:, :n],
                                                                    start=(c == 0), stop=(c == 5)),
                     [R_SQL[s], R_ID], s2r)
            act(MSQ[:, :n], s1b[:, :n], AF.Square, s1r, [R_ST], scale=1.0 / CONV_CH)
            stt(VAR[:, :n], s2b[:, :n], 1.0 / CONV_CH, MSQ[:, :n], ALU.mult, ALU.subtract, s2r + [R_ST], [R_ST])
            act(VAR[:, :n], VAR[:, :n], AF.Sqrt, [R_ST], [R_ST], bias=LN_EPS)
            recip(VAR[:, :n], VAR[:, :n], [R_ST], [R_ST])
            for c in range(6):
                s = c % 2
                rc = [R_C[c][t] for t in tiles]
                stt(T1[s][:, :n], s1b[:, :n], -1.0 / CONV_CH, RA[:, c, c0:c0 + n], ALU.mult, ALU.add,
                    s1r + rc, [R_T1[s]])
                tt(T1[s][:, :n], T1[s][:, :n], VAR[:, :n], ALU.mult, [R_T1[s], R_ST], [R_T1[s]])
                act(RA[:, c, c0:c0 + n], T1[s][:, :n], AF.Silu, [R_T1[s], R_CP], rc,
                    scale=cpc(C_LNG + c), bias=cpc(C_LNB + c))
        for c in range(5):
            build_dg(c)
            for blk in TBC:
                conv_block(c, blk)
        build_dg(5)
        conv_block(5, TBC[0])
        for b in range(len(TBC)):
            if b + 1 < len(TBC):
                conv_block(5, TBC[b + 1])
            ln_block(TBC[b])
        dump("cln", RA[:, 0, 32:TC], [R_C[0][t] for t in range(NT)])
        hn_flush()
        S.barrier()
        if PROFILE_MARKS:
            S.mark(nc, "L0D_memattn")
        PT = [arv(i * 512, 512) for i in range(6)]
        R_PT = [Res("PT%d" % i) for i in range(6)]
        RD = [arv(3072 + i * 1024, 512, F32) for i in range(2)]
        R_RD = [Res("RD%d" % i) for i in range(2)]
        WO0 = arv(14336, 8 * D).rearrange("p (k n) -> p k n", k=8)
        R_WO0 = Res("WO0")
        S.dma("pool", WO0, cv_w_out.rearrange("(k p) n -> p k n", p=128), writes=[R_WO0])
        prepass(8)
        mem_attention(QM0, R_QM0, TBC, 32, (PT, R_PT, RD, R_RD))
        dump("cat0", RA[:, 6, 32:TC], [R_RA[t] for t in range(NT)])
        if PROFILE_MARKS:
            S.mark(nc, "L0E_outproj")
        out_proj(WO0, R_WO0, list(range(NT)))
        dump("h0a", HT[:, 1, :], [R_HT[1]])
        dump("hf0a", HT[:, 1:NT, :].rearrange("p j d -> p (j d)"), R_HT[1:NT])
        hn_flush()
        S.barrier()
        if PROFILE_MARKS:
            S.mark(nc, "L0F_ffn")
        WB = []
        R_WB = []
        for s in range(2):
            o = s * 12288
            WB.append((arv(o, 4096).rearrange("p (k n) -> p k n", k=8),
                       arv(o + 4096, 4096).rearrange("p (k n) -> p k n", k=8),
                       arv(o + 8192, 4096).rearrange("p (f n) -> p f n", f=4)))
            R_WB.append([Res("WB%d_%d" % (s, i)) for i in range(3)])
        ACTB = [arv(24576 + a * 2048, 2048).rearrange("p (f n) -> p f n", f=4) for a in range(2)]
        R_ACTB = [[Res("ACTB%d_%d" % (a, f)) for f in range(4)] for a in range(2)]
        SIL = [arv(28672 + i * 512, 512) for i in range(2)]
        R_SIL = [Res("SIL%d" % i) for i in range(2)]
        for j in range(NT):
            norm_transpose(j, C_GF0)
        groups0 = []
        for f0 in range(0, DFF0, 512):
            nf = min(512, DFF0 - f0)
            groups0.append(dict(g=cv_w_gate[:, f0:f0 + nf], u=cv_w_up[:, f0:f0 + nf], d=cv_w_down[f0:f0 + nf, :],
                                nfc=nf // 128, gate=None))
        ffn(groups0, TBC, WB, R_WB, ACTB, R_ACTB, SIL, R_SIL, after_load=lambda: prepass(6))
        dump("h1", HT[:, 1, :], [R_HT[1]])
        dump("hf1", HT[:, 1:NT, :].rearrange("p j d -> p (j d)"), R_HT[1:NT])
        hn_flush()
        S.barrier()

        if PROFILE_MARKS:
            S.mark(nc, "L1A_norm")
        for j in range(NT):
            norm_transpose(j, C_GA1)
        if PROFILE_MARKS:
            S.mark(nc, "L1B_inproj")
        WIN1 = arv(0, 8 * 1536).rearrange("p (k n) -> p k n", k=8)
        R_WIN1 = Res("WIN1")
        QPAIR = [(0, 3), (1, 4), (2, 5), (6, 9), (7, 10), (8, 11)]
        for i, (ha, hb_) in enumerate(QPAIR):
            for half, hh in enumerate((ha, hb_)):
                S.dma("pool", WIN1[:, :, i * 128 + half * 64:i * 128 + half * 64 + 64],
                      sw_w_in[:, hh * 64:(hh + 1) * 64].rearrange("(k p) n -> p k n", p=128), writes=[R_WIN1])
        S.dma("pool", WIN1[:, :, 768:1536], sw_w_in[:, 768:1536].rearrange("(k p) n -> p k n", p=128),
              writes=[R_WIN1])
        prepass(8)
        QT = arv(12288, 6 * TOK).rearrange("p (a n g q) -> p a n g q", a=2, n=16, g=3)
        R_QT = Res("QT")
        KT = arv(24576, 2 * 2176).rearrange("p (c t) -> p c t", c=2)
        R_KT = Res("KT")
        VT = arv(28928, NT * 256).rearrange("p (j d) -> p j d", j=NT)
        R_VT = Res("VT")
        QM1 = arv(33280, 2 * TOK).rearrange("p (c t) -> p c t", c=2)
        R_QM1 = Res("QM1")
        SQb = [arv(37376 + i * 512, 512) for i in range(2)]
        RTb = [arv(38400 + i * 1024, 512, F32) for i in range(2)]
        hbufs = (SQb, R_SQb, RTb, R_RTb)
        hn_flush()
        S.barrier()
        TB1 = TB0[1:]
        for i in range(6):
            for bi, (c0, n, tiles) in enumerate(TB1):
                rr = [R_RA[t] for t in tiles]
                qb, qr = bank()
                mmg(qb[:, :n], [(WIN1[:, kc, i * 128:(i + 1) * 128], RA[:, kc, c0:c0 + n]) for kc in range(8)],
                    rr + [R_WIN1], qr)
                headnorm(qb[:, :n], qr, C_QG, QT[:, i // 3, 4 * bi:4 * bi + 4, i % 3, :], [R_QT], n, hbufs,
                         split=128)
        for c2 in range(2):
            for (c0, n, tiles) in TBC:
                rr = [R_RA[t] for t in tiles]
                kb_, kr_ = bank()
                mmg(kb_[:, :n], [(WIN1[:, kc, 768 + c2 * 128:768 + (c2 + 1) * 128], RA[:, kc, c0:c0 + n])
                                 for kc in range(8)], rr + [R_WIN1], kr_)
                headnorm(kb_[:, :n], kr_, C_KG, KT[:, c2, c0 - 32:c0 - 32 + n], [R_KT], n, hbufs)
        hn_flush()
        for j in range(NT):
            c0, n = tcols(j)
            vb_, vr_ = bank()
            mmg(vb_[:, :256], [(RA[:, kc, c0:c0 + 128], WIN1[:, kc, 1024:1280]) for kc in range(8)],
                [R_RA[j], R_WIN1], vr_)
            copy("act", VT[:, j, :], vb_[:, :256], vr_, [R_VT])
        for c2 in range(2):
            for (c0, n, tiles) in TB1:
                rr = [R_RA[t] for t in tiles]
                qb, qr = bank()
                mmg(qb[:, :n], [(WIN1[:, kc, 1280 + c2 * 128:1280 + (c2 + 1) * 128], RA[:, kc, c0:c0 + n])
                                for kc in range(8)], rr + [R_WIN1], qr)
                headnorm(qb[:, :n], qr, C_MQ1, QM1[:, c2, c0 - 160:c0 - 160 + n], [R_QM1], n, hbufs)
        hn_flush()
        dump("qt", QT[:, 0, :, 0, :], [R_QT], view=("p (n q) -> p n q", dict(n=16)))
        dump("kt", KT[:, 0, :], [R_KT])
        hn_flush()
        S.barrier()
        if PROFILE_MARKS:
            S.mark(nc, "L1C_swa")
        prepass(12)
        ET = [arv(i * 384, 384) for i in range(4)]
        R_ET = [Res("ET%d" % i) for i in range(4)]
        PT1 = [arv(1536 + i * 384, 384) for i in range(8)]
        R_PT1 = [Res("PT1%d" % i) for i in range(8)]
        RD1 = [arv(4608 + i * 768, 384, F32) for i in range(2)]
        R_RD1 = [Res("RD1%d" % i) for i in range(2)]
        swa_its = [(nblk, kvh) for nblk in range(16) for kvh in range(4)]
        swa_st = {"e": 0, "p": 0}
        swa_pend = {}

        def swa_a(i):
            nblk, kvh = swa_its[i]
            j = nblk + 1
            rows = slice((kvh % 2) * 64, (kvh % 2) * 64 + 64)
            pts = []
            for kb in range(2):
                jk = j - 1 + kb
                sb_, sr_ = bank()
                mmg(sb_[:, :384], [(KT[rows, kvh // 2, jk * 128:(jk + 1) * 128],
                                    QT[rows, kvh // 2, nblk, :, :].rearrange("p g q -> p (g q)"))],
                    [R_KT, R_QT], sr_)
                s = swa_st["e"] % 4
                swa_st["e"] += 1
                act(ET[s][:], sb_[:, :384], AF.Exp, sr_, [R_ET[s]], scale=0.125)
                p = swa_st["p"] % 8
                swa_st["p"] += 1
                msk = ME0[:, kvh, :] if (nblk == 0 and kb == 0) else ME[:, kb, kvh, :]
                tt(PT1[p][:], ET[s][:], msk, ALU.mult, [R_ET[s], R_ME], [R_PT1[p]])
                pts.append((p, jk))
            swa_pend[i] = pts

        def swa_b(i):
            nblk, kvh = swa_its[i]
            j = nblk + 1
            rc0, _ = tcols(j)
            rows = slice((kvh % 2) * 64, (kvh % 2) * 64 + 64)
            qc0 = (kvh // 2) * 3
            pts = swa_pend.pop(i)
            ob, orr = bank()
            mmg(ob[rows, :384], [(VT[:, jk, kvh * 64:(kvh + 1) * 64], PT1[p][:]) for (p, jk) in pts],
                [R_VT] + [R_PT1[p] for (p, _) in pts], orr)
            db, dr = bank()
            mmg(db[rows, :384], [(ONB[:, 0:64], PT1[p][:]) for (p, _) in pts] + [(ONB[0:1, 0:64], SRB[0:1, kvh, :])],
                [R_ID, R_SR] + [R_PT1[p] for (p, _) in pts], dr)
            s2 = i % 2
            recip(RD1[s2][rows, :], db[rows, :384], dr, [R_RD1[s2]])
            tt(RA[rows, qc0:qc0 + 3, rc0:rc0 + 128], ob[rows, :384].rearrange("p (g q) -> p g q", g=3),
               RD1[s2][rows, :].rearrange("p (g q) -> p g q", g=3), ALU.mult,
               orr + [R_RD1[s2]], [R_RA[j]])
        SWA_LOOK = 2
        for i in range(min(SWA_LOOK, len(swa_its))):
            swa_a(i)
        for i in range(len(swa_its)):
            if i + SWA_LOOK < len(swa_its):
                swa_a(i + SWA_LOOK)
            swa_b(i)
        dump("swa", RA[:, 0, 160:TC], [R_RA[t] for t in range(1, NT)])
        hn_flush()
        S.barrier()
        if PROFILE_MARKS:
            S.mark(nc, "L1D_mem_out")
        WO1 = arv(14336, 8 * D).rearrange("p (k n) -> p k n", k=8)
        R_WO1 = Res("WO1")
        for i, (ha, hb_) in enumerate(QPAIR):
            S.dma("pool", WO1[0:64, i, :], sw_w_out[ha * 64:(ha + 1) * 64, :], writes=[R_WO1])
            S.dma("pool", WO1[64:128, i, :], sw_w_out[hb_ * 64:(hb_ + 1) * 64, :], writes=[R_WO1])
        S.dma("pool", WO1[:, 6:8, :], sw_w_out[768:1024, :].rearrange("(k p) n -> p k n", p=128), writes=[R_WO1])
        prepass(8)
        PT = [arv(i * 512, 512) for i in range(6)]
        RD = [arv(3072 + i * 1024, 512, F32) for i in range(2)]
        mem_attention(QM1, R_QM1, TB1, 160, (PT, R_PT, RD, R_RD))
        out_proj(WO1, R_WO1, list(range(1, NT)))
        dump("h1a", HT[:, 1, :], [R_HT[1]])
        dump("hf1a", HT[:, 1:NT, :].rearrange("p j d -> p (j d)"), R_HT[1:NT])
        hn_flush()
        S.barrier()
        if PROFILE_MARKS:
            S.mark(nc, "L1R_router")
        HN32 = [arv(29696 + i * 2048, 1024, F32) for i in range(2)]
        R_HN32 = [Res("HN32%d" % i) for i in range(2)]
        H32T = [arv(33792 + i * 2048, 1024, F32).rearrange("p (k c) -> p k c", k=8) for i in range(2)]
        R_H32T = [Res("H32T%d" % i) for i in range(2)]
        prepass(1000)
        reserved.add(7)
        LG = PS[:, 7, 0:128].rearrange("p (j e) -> p j e", j=16)
        R_LG = banks[7]
        g_b128 = CP[:, C_GF1:C_GF1 + 8].unsqueeze(2).to_broadcast([128, 8, 128])
        rs_col = {}
        for j in range(1, NT):
            k = ss_state["n"] % 32
            ss_state["n"] += 1
            rs_col[j] = k
            s = j % 2
            act(JUNK[:], HT[:, j, :], AF.Square, [R_HT[j]], [R_JUNK, R_SS], accum_out=SS[:, 0, k:k + 1])
            act(SS[:, 1, k:k + 1], SS[:, 0, k:k + 1], AF.Sqrt, [R_SS], [R_SS], scale=1.0 / D, bias=RMS_EPS)
            recip(SS[:, 2, k:k + 1], SS[:, 1, k:k + 1], [R_SS], [R_SS])
            ts(HN32[s], HT[:, j, :], SS[:, 2, k:k + 1], ALU.mult, [R_HT[j], R_SS], [R_HN32[s]])
            pb, pr = bank(pair=True)
            pv = pb.rearrange("p b (k c) -> p (b k) c", k=4)

            def fn(e, pv=pv, s=s):
                ins = None
                for kc in range(8):
                    ins = e.transpose(out=pv[:, kc, :], in_=HN32[s][:, kc * 128:(kc + 1) * 128], identity=IDF[:])
                return ins
            S.op("pe", fn, [R_HN32[s], R_ID], pr)
            tt(H32T[s], pv, g_b128, ALU.mult, pr + [R_CP], [R_H32T[s]])
            mmg(LG[:, j - 1, :], [(H32T[s][:, kc, :], WR[:, kc, :]) for kc in range(8)],
                [R_H32T[s], R_WR], [R_LG])
        ga_state = {"o": 0}

        def ga(n, dt=F32):
            o = ga_state["o"]
            ga_state["o"] += 2 * n if dt != BF16 else n
            if dt == BF16:
                return arv(o, n)
            if dt == F32:
                return arv(o, n, F32)
            return AR[:, o:o + 2 * n].bitcast(dt)

        def v168(ap):
            return ap.rearrange("p (j e) -> p j e", j=16)

        def b168(ap16):
            return ap16.unsqueeze(2).to_broadcast([128, 16, 8])
        R_G = Res("GATE")
        gL, gEQ1, gL2, gEQ2, gSEL, gCUM, gTMP, gSLOT = [ga(128) for _ in range(8)]
        gV1, gV2, gE, gS1, gS2, gET = [ga(16) for _ in range(6)]
        W12 = sb("W12", [128, 32], F32)
        gW1, gW2 = W12[:, 0:16], W12[:, 16:32]
        gNE, gTL, gOE, gOF = [ga(8) for _ in range(4)]
        SELB, CUMB = ga(128, BF16), ga(128, BF16)
        SI = sb("SIT", [128, 32], I32)
        ETI = ga(16, I32)
        LTB = ga(128, BF16)
        R_C2 = Res("C2")
        C2F = ga(296)
        S.dma("sp", C2F, cst2[:, :], writes=[R_C2])
        copy("act", LTB, C2F[:, 0:128], [R_C2], [R_C2])
        RG, WG_ = [R_G], [R_G]
        copy("act", gL, LG.rearrange("p j e -> p (j e)"), [R_LG], WG_)
        S.op("dve", lambda e: e.tensor_reduce(out=gV1, in_=v168(gL), op=ALU.max, axis=AX.X), RG, WG_)
        tt(v168(gEQ1), v168(gL), b168(gV1), ALU.is_equal, RG, WG_)
        stt(gL2, gEQ1, -1e30, gL, ALU.mult, ALU.add, RG, WG_)
        S.op("dve", lambda e: e.tensor_reduce(out=gV2, in_=v168(gL2), op=ALU.max, axis=AX.X), RG, WG_)
        tt(v168(gEQ2), v168(gL2), b168(gV2), ALU.is_equal, RG, WG_)
        tt(gE, gV2, gV1, ALU.subtract, RG, WG_)
        act(gE, gE, AF.Exp, RG, WG_)
        ts(gW1, gE, 1.0, ALU.add, RG, WG_)
        recip(gW1, gW1, RG, WG_)
        tt(gW2, gE, gW1, ALU.mult, RG, WG_)
        tt(gSEL, gEQ1, gEQ2, ALU.add, RG, WG_)
        copy("dve", SELB, gSEL, RG, WG_)
        S.op("dve", lambda e: e.memset(gCUM[:, 0:8], 0.0), [], WG_)
        for j in range(1, 16):
            tt(gCUM[:, j * 8:(j + 1) * 8], gCUM[:, (j - 1) * 8:j * 8], gSEL[:, (j - 1) * 8:j * 8], ALU.add, RG, WG_)
        copy("dve", CUMB, gCUM, RG, WG_)
        rkb, rkr = bank()
        mmg(rkb[:, 0:128], [(LTB, SELB), (ONB[:], CUMB)], RG + [R_C2, R_ID], rkr)
        tob, tor = bank()
        mmg(tob[:, 0:128], [(ONB[:], SELB)], RG + [R_ID], tor)
        S.op("dve", lambda e: e.tensor_reduce(out=gNE, in_=tob[:, 0:128].rearrange("p (j e) -> p e j", j=16),
                                              op=ALU.add, axis=AX.X), tor, WG_)
        ts(gTL, gNE, 0.0, ALU.is_gt, RG, WG_)
        for thr in (512.0, 1024.0, 1536.0):
            stt(gTL, gNE, thr, gTL, ALU.is_gt, ALU.add, RG, WG_)
        copy("dve", gOE[:, 0:1], gTL[:, 0:1], RG, WG_)
        for ex in range(1, NE):
            tt(gOE[:, ex:ex + 1], gOE[:, ex - 1:ex], gTL[:, ex:ex + 1], ALU.add, RG, WG_)
        tt(gOF, gOE, gTL, ALU.subtract, RG, WG_)
        ts(gOF, gOF, 512.0, ALU.mult, RG, WG_)
        tt(v168(gSLOT), v168(rkb[:, 0:128]), gOF.unsqueeze(1).to_broadcast([128, 16, 8]), ALU.add, rkr + RG, WG_)
        tt(gTMP, gEQ1, gSLOT, ALU.mult, RG, WG_)
        S.op("dve", lambda e: e.tensor_reduce(out=gS1, in_=v168(gTMP), op=ALU.add, axis=AX.X), RG, WG_)
        copy("dve", SI[:, 0:16], gS1, RG, WG_)
        tt(gTMP, gEQ2, gSLOT, ALU.mult, RG, WG_)
        S.op("dve", lambda e: e.tensor_reduce(out=gS2, in_=v168(gTMP), op=ALU.add, axis=AX.X), RG, WG_)
        copy("dve", SI[:, 16:32], gS2, RG, WG_)
        tt(v168(gTMP), gOE.unsqueeze(1).to_broadcast([128, 16, 8]), v168(C2F[:, 128:256]), ALU.is_le,
           RG + [R_C2], WG_)
        S.op("dve", lambda e: e.tensor_reduce(out=gET, in_=v168(gTMP), op=ALU.add, axis=AX.X), RG, WG_)
        ts(gET, gET, 7.0, ALU.min, RG, WG_)
        copy("dve", ETI, gET, RG, WG_)
        dump("gw1", gW1, RG)
        dump("gs1", gS1, RG)
        dump("gs2", gS2, RG)
        dump("get", gET, RG)
        reserved.discard(7)
        TTI = ga(1, I32)
        copy("dve", TTI, gOE[:, 7:8], RG, WG_)
        eng_regs = {}

        def mk_setup(eng):
            def fn(e, eng=eng):
                rt = e.alloc_register()
                rk = e.alloc_register()
                e.reg_load(rt, TTI[0:1, 0:1])
                eng_regs[eng] = (rt, rk)
            return fn
        for eng in ENGS:
            S.raw(eng, mk_setup(eng), RG)
        IDXG = sb("IDXG", [128, 112], I32)
        IDXD = sb("IDXD", [128, 448], I32)
        gIG, gID = ga(112), ga(448)
        stt(gIG.rearrange("p (t g) -> p t g", t=16), gET.unsqueeze(2).to_broadcast([128, 16, 7]), 896.0,
            C2F[:, 256:263].unsqueeze(1).to_broadcast([128, 16, 7]), ALU.mult, ALU.add, RG + [R_C2], WG_)
        copy("dve", IDXG[:], gIG, RG, WG_)
        stt(gID.rearrange("p (t g) -> p t g", t=16), gET.unsqueeze(2).to_broadcast([128, 16, 28]), 3584.0,
            C2F[:, 263:291].unsqueeze(1).to_broadcast([128, 16, 28]), ALU.mult, ALU.add, RG + [R_C2], WG_)
        copy("dve", IDXD[:], gID, RG, WG_)
        if PROFILE_MARKS:
            S.mark(nc, "L1S_scatter")
        for j in range(1, NT):
            s = j % 2
            k = rs_col[j]
            ts(HNB[s][:], HT[:, j, :], SS[:, 2, k:k + 1], ALU.mult, [R_HT[j], R_SS], [R_HNB[s]])
            for kk in range(2):
                col = kk * 16 + j - 1
                S.dma("pool", None, None, reads=[R_HNB[s]] + RG, writes=[R_XS], owner=R_XS,
                      fn=lambda e, s=s, col=col: e.indirect_dma_start(
                          out=XS[:, :], out_offset=bass.IndirectOffsetOnAxis(ap=SI[:, col:col + 1], axis=0),
                          in_=HNB[s][:], in_offset=None))
        if PROFILE_MARKS:
            S.mark(nc, "L1T_tiles")
        NTILE = int(os.environ.get("MK_NTILE", "15"))
        WB3 = []
        R_WB3 = []
        for s in range(2):
            o = 8192 + s * 12288
            WB3.append((arv(o, 4096).rearrange("p (k n) -> p k n", k=8),
                        arv(o + 4096, 4096).rearrange("p (k n) -> p k n", k=8),
                        arv(o + 8192, 4096).rearrange("p (f n) -> p f n", f=4)))
            R_WB3.append([Res("WB3%d_%d" % (s, i)) for i in range(3)])
        ACT3 = [arv(32768 + a * 2048, 2048).rearrange("p (f n) -> p f n", f=4) for a in range(2)]
        R_ACT3 = [[Res("ACT3%d_%d" % (a, f)) for f in range(4)] for a in range(2)]
        SIL3 = [arv(36864 + i * 512, 512) for i in range(2)]
        R_SIL3 = [Res("SIL3%d" % i) for i in range(2)]
        YACC = [arv(c * 2048, 1024, F32) for c in range(4)]
        R_YACC = [Res("YACC%d" % c) for c in range(4)]
        RAf = RA[:].rearrange("p k t -> p (k t)")
        XT = [RAf[:, i * 4096:(i + 1) * 4096].rearrange("p (c d) -> p c d", c=4) for i in range(2)]
        R_XT = [Res("XT%d" % i) for i in range(2)]
        XF = [RAf[:, 8192 + i * 4096:8192 + (i + 1) * 4096].rearrange("p (k n) -> p k n", k=8) for i in range(2)]
        R_XF = [Res("XF%d" % i) for i in range(2)]
        R_YS = Res("YS")
        hn_flush()
        S.barrier()

        def prep_tile(tau):
            s = tau % 2
            S.dma("sp", XT[s], XS[512 * tau:512 * (tau + 1), :].rearrange("(c p) d -> p c d", p=128),
                  reads=[R_XS], writes=[R_XT[s]])
            for c in range(4):
                pb, pr = bank()
                pv = pb.bitcast(BF16).rearrange("p (k c) -> p k c", k=8)

                def fn(e, pv=pv, s=s, c=c):
                    ins = None
                    for kc in range(8):
                        ins = e.transpose(out=pv[:, kc, :], in_=XT[s][:, c, kc * 128:(kc + 1) * 128], identity=IDB[:])
                    return ins
                S.op("pe", fn, [R_XT[s], R_ID], pr)
                tt(XF[s][:, :, c * 128:(c + 1) * 128], pv, g_b128, ALU.mult, pr + [R_CP], [R_XF[s]])

        steps = [(tau, g) for tau in range(NTILE) for g in range(7)]

        wdflat = sw_we_down.rearrange("e r f -> (e r) f")

        def load_w(si):
            tau, g = steps[si]
            s = si % 2
            o = 8192 + s * 12288
            col = tau * 7 + g
            for (dst, src, res_) in ((arv(o, 4096), WSG, R_WB3[s][0]), (arv(o + 4096, 4096), WSU, R_WB3[s][1])):
                S.dma("pool", None, None, reads=[R_WS] + RG, writes=[res_],
                      fn=lambda e, dst=dst, src=src, col=col: e.indirect_dma_start(
                          out=dst, out_offset=None, in_=src[:, :],
                          in_offset=bass.IndirectOffsetOnAxis(ap=IDXG[:, col:col + 1], axis=0)))
            for fc in range(4):
                S.dma("pool", None, None, reads=RG, writes=[R_WB3[s][2]],
                      fn=lambda e, s=s, fc=fc, col=col: e.indirect_dma_start(
                          out=WB3[s][2][:, fc, :], out_offset=None, in_=wdflat[:, :],
                          in_offset=bass.IndirectOffsetOnAxis(ap=IDXD[:, col * 4 + fc:col * 4 + fc + 1], axis=0)))

        def gu3(si):
            tau, g = steps[si]
            s, a, x = si % 2, si % 2, tau % 2
            wg, wu, wd = WB3[s]
            for fc in range(4):
                gb, gr = bank()
                mmg(gb[:, :], [(wg[:, kc, fc * 128:(fc + 1) * 128], XF[x][:, kc, :]) for kc in range(8)],
                    [R_XF[x], R_WB3[s][0]], gr)
                ub, ur = bank()
                mmg(ub[:, :], [(wu[:, kc, fc * 128:(fc + 1) * 128], XF[x][:, kc, :]) for kc in range(8)],
                    [R_XF[x], R_WB3[s][1]], ur)
                sl = fc % 2
                act(SIL3[sl][:], gb[:, :], AF.Silu, gr, [R_SIL3[sl]])
                tt(ACT3[a][:, fc, :], ub[:, :], SIL3[sl][:], ALU.mult, ur + [R_SIL3[sl]], [R_ACT3[a][fc]])

        def down3(si):
            tau, g = steps[si]
            s, a = si % 2, si % 2
            wg, wu, wd = WB3[s]
            for c in range(4):
                yb, yr = bank(pair=True)
                for half in range(2):
                    mmg(yb[:, half, :], [(ACT3[a][:, fc, c * 128:(c + 1) * 128], wd[:, fc, half * 512:(half + 1) * 512])
                                         for fc in range(4)], R_ACT3[a] + [R_WB3[s][2]], [yr[half]])
                yv = yb.rearrange("p b c -> p (b c)")
                if g == 0:
                    copy("act", YACC[c], yv, yr, [R_YACC[c]])
                else:
                    tt(YACC[c], yv, YACC[c], ALU.add, yr + [R_YACC[c]], [R_YACC[c]])
            if g == 6:
                S.dma("sp", YS[512 * tau:512 * (tau + 1), :].rearrange("(c p) d -> p c d", p=128),
                      AR[:, 0:8192].bitcast(F32).rearrange("p (c d) -> p c d", c=4),
                      reads=R_YACC, writes=[R_YS], owner=R_YS)

        CT0 = int(os.environ.get("MK_CT0", "8"))

        def run_steps(si0, si1):
            prep_tile(steps[si0][0])
            load_w(si0)
            if si0 + 1 < si1:
                load_w(si0 + 1)
            for si in range(si0, si1):
                tau, g = steps[si]
                gu3(si)
                if si > si0:
                    down3(si - 1)
                    if si + 1 < si1:
                        load_w(si + 1)
                if g == 2 and (tau + 1) * 7 < si1:
                    prep_tile(tau + 1)
            down3(si1 - 1)
        nuncond = min(CT0, NTILE)
        run_steps(0, nuncond * 7)
        for tau in range(nuncond, NTILE):
            S.region_begin(tau)
            run_steps(tau * 7, (tau + 1) * 7)
            S.region_end()
        hn_flush()
        S.barrier()
        if PROFILE_MARKS:
            S.mark(nc, "L1Z_combine")
        G1 = [arv(8192 + i * 2048, 1024, F32) for i in range(2)]
        G2 = [arv(12288 + i * 2048, 1024, F32) for i in range(2)]
        R_G1 = [Res("G1%d" % i) for i in range(2)]
        R_G2 = [Res("G2%d" % i) for i in range(2)]
        R_OUT = Res("OUT")
        for j in range(1, NT):
            s = j % 2
            for (GB, RGB, base, wv) in ((G1, R_G1, 0, gW1), (G2, R_G2, 16, gW2)):
                col = base + j - 1
                S.dma("pool", None, None, reads=[R_YS] + RG, writes=[RGB[s]],
                      fn=lambda e, GB=GB, s=s, col=col: e.indirect_dma_start(
                          out=GB[s][:], out_offset=None, in_=YS[:, :],
                          in_offset=bass.IndirectOffsetOnAxis(ap=SI[:, col:col + 1], axis=0)))
                stt(HT[:, j, :], GB[s], wv[:, j - 1:j], HT[:, j, :], ALU.mult, ALU.add,
                    [RGB[s], R_HT[j]] + RG, [R_HT[j]])
            S.dma("sp", out[(j - 1) * 128:j * 128, :], HT[:, j, :], reads=[R_HT[j]], owner=R_OUT)
        S.final_wait("sp")

        block = es.enter_context(nc.Block())

        @block.tensor
        def _(e):
            S.replay("pe", e, eng_regs)

        @block.scalar
        def _(e):
            S.replay("act", e, eng_regs)

        @block.vector
        def _(e):
            S.replay("dve", e, eng_regs)

        @block.gpsimd
        def _(e):
            S.replay("pool", e, eng_regs)

        @block.sync
        def _(e):
            S.replay("sp", e, eng_regs)
    return nc


def _consts():
    ident = np.eye(128, dtype=np.float32)
    bd = np.zeros((128, 128), np.float32)
    bd[:64, :64] = 1.0 / 64
    bd[64:, 64:] = 1.0 / 64
    slopes = np.exp2(-8.0 * (np.arange(12, dtype=np.float32) + 1.0) / 12).astype(np.float32)
    k = np.arange(128)[:, None].astype(np.float32)
    q = np.arange(128)[None, :].astype(np.float32)
    me = np.zeros((128, 2, 4, 3, 128), np.float32)
    for kb in range(2):
        dist = q + 128.0 - k if kb == 0 else q - k
        valid = (dist >= 0) & (dist < 128)
        for kvh in range(4):
            for g in range(3):
                s = slopes[kvh * 3 + g]
                me[:, kb, kvh, g, :] = np.where(valid, np.exp(-s * dist), 0.0)
    c2 = np.zeros((128, 296), np.float32)
    pidx = np.arange(128, dtype=np.float32)[:, None]
    c2[:, 256:263] = np.arange(7, dtype=np.float32)[None, :] * 128 + pidx
    c2[:, 263:291] = (np.arange(7, dtype=np.float32)[:, None] * 512
                      + np.arange(4, dtype=np.float32)[None, :] * 128).reshape(1, 28) + pidx
    c2[:, 0:128] = (np.arange(128)[:, None] < np.arange(128)[None, :]).astype(np.float32)
    c2[:, 128:256] = np.repeat(np.arange(16, dtype=np.float32), 8)[None, :]
    return ident, bd, me.reshape(128, 3072), c2


def _cpack(inp, flag):
    cp = np.zeros((128, NCP), np.float32)

    def pk(v):
        return np.asarray(v, np.float32).reshape(8, 128).T
    cp[:, C_GA0:C_GA0 + 8] = pk(inp["cv_attn_norm_g"][0])
    cp[:, C_GF0:C_GF0 + 8] = pk(inp["cv_ffn_norm_g"][0])
    cp[:, C_GA1:C_GA1 + 8] = pk(inp["sw_attn_norm_g"][0])
    cp[:, C_GF1:C_GF1 + 8] = pk(inp["sw_ffn_norm_g"][0])
    cp[:, C_GM:C_GM + 8] = pk(inp["mem_norm_g"])
    bg = np.asarray(inp["cv_b_glu"][0], np.float32)
    cp[:, C_BA:C_BA + 6] = bg[:768].reshape(6, 128).T
    cp[:, C_BG:C_BG + 6] = bg[768:].reshape(6, 128).T
    dw = np.asarray(inp["cv_dw_w"][0], np.float32)
    cp[:, C_DW:C_DW + 186] = dw.reshape(31, 6, 128).transpose(2, 1, 0).reshape(128, 186)
    cp[:, C_DWB:C_DWB + 6] = np.asarray(inp["cv_dw_b"][0], np.float32).reshape(6, 128).T
    cp[:, C_LNG:C_LNG + 6] = np.asarray(inp["cv_ln_g"][0], np.float32).reshape(6, 128).T
    cp[:, C_LNB:C_LNB + 6] = np.asarray(inp["cv_ln_b"][0], np.float32).reshape(6, 128).T

    def h2(v):
        v = np.asarray(v, np.float32).reshape(64)
        return np.concatenate([v, v])
    cp[:, C_MQ0] = h2(inp["cv_memq_norm_g"][0])
    cp[:, C_MQ1] = h2(inp["sw_memq_norm_g"][0])
    cp[:, C_MK] = h2(inp["mem_k_norm_g"])
    cp[:, C_QG] = h2(inp["sw_q_norm_g"][0])
    cp[:, C_KG] = h2(inp["sw_k_norm_g"][0])
    cp[:, C_FLAG] = flag
    return cp


def make_in_maps(inp):
    f = lambda a: np.ascontiguousarray(np.asarray(a, dtype=np.float32))
    ident, bd, me, c2 = _consts()
    x = f(inp["x"])
    memf = f(inp["mem"])
    sinks = f(inp["sw_sinks"][0])
    sinks_rep = np.ascontiguousarray(np.repeat(sinks, 128)[None, :])
    shared = dict(
        ident=ident, bdm=bd, maskexp=me, cst2=c2, sinks_rep=sinks_rep,
        w_mem_kv=f(inp["w_mem_kv"]), cv_w_in=f(inp["cv_w_in"][0]), cv_w_out=f(inp["cv_w_out"][0]),
        cv_w_gate=f(inp["cv_w_gate"][0]), cv_w_up=f(inp["cv_w_up"][0]), cv_w_down=f(inp["cv_w_down"][0]),
        sw_w_in=f(inp["sw_w_in"][0]), sw_w_out=f(inp["sw_w_out"][0]), sw_router=f(inp["sw_router"][0]),
        sw_we_gate=f(inp["sw_we_gate"][0]), sw_we_up=f(inp["sw_we_up"][0]), sw_we_down=f(inp["sw_we_down"][0]),
    )
    maps = []
    for c in range(NCORES):
        b, qtr = divmod(c, 4)
        xin = np.zeros((TC, D), np.float32)
        t0 = qtr * TOK
        if qtr == 0:
            xin[160:] = x[b, 0:TOK]
        else:
            xin[:] = x[b, t0 - 160:t0 + TOK]
        m = dict(shared)
        m["xin"] = xin
        m["mem"] = memf[b]
        m["cpack"] = _cpack(inp, 0.0 if qtr == 0 else 1.0)
        maps.append(m)
    return maps


def kernel(**inputs):
    nc = build_program()
    maps = make_in_maps(inputs)
    res = run_bass_kernel_spmd(nc, maps, core_ids=list(range(NCORES)))
    outs = [np.asarray(r["out"], np.float32) for r in res.results]
    full = np.concatenate(outs, axis=0).reshape(2, SEQ, D)
    return full
```

```python
import os
from contextlib import ExitStack

import numpy as np
import concourse.bass as bass
import concourse.mybir as mybir
from concourse.bass_utils import run_bass_kernel_spmd

F32 = mybir.dt.float32
BF16 = mybir.dt.bfloat16
I32 = mybir.dt.int32
AF = mybir.ActivationFunctionType
ALU = mybir.AluOpType
AX = mybir.AxisListType

NCORES = 8
D = 1024
SEQ = 8192
TOK = 2048
NT = 17
TC = 2208
HD = 64
DFF0 = 2816
DFFE = 3584
NE = 8
CONV_W = 31
CONV_CH = 768
RMS_EPS = 1e-6
LN_EPS = 1e-5

C_GA0, C_GF0, C_GA1, C_GF1, C_GM = 0, 8, 16, 24, 32
C_BA, C_BG = 40, 46
C_DW = 52
C_DWB, C_LNG, C_LNB = 238, 244, 250
C_MQ0, C_MQ1, C_MK, C_QG, C_KG, C_FLAG = 256, 257, 258, 259, 260, 261
NCP = 264

ENGS = ("pe", "act", "dve", "pool", "sp")
PROFILE_MARKS = bool(os.environ.get("MK_MARKS"))


class Res:
    __slots__ = ("name", "w", "r", "dsem", "dcnt")

    def __init__(self, name):
        self.name = name
        self.w = None
        self.r = []
        self.dsem = None
        self.dcnt = 0


class Sched:
    def __init__(self, esem, dsems):
        self.esem = dict(esem)
        self.free_dsems = list(dsems)
        self.cnt = {e: 0 for e in ENGS}
        self.seen = {e: {} for e in ENGS}
        self.q = {e: [] for e in ENGS}
        self.dlast = {}
        self.nbank = 0
        self._reg = None

    def _deps(self, eng, reads, writes, skip_key=None):
        deps = {}

        def add(ev, same_ok):
            if ev is None:
                return
            k, v = ev
            if (k == eng or k == skip_key) and not same_ok:
                return
            if deps.get(k, 0) < v:
                deps[k] = v
        for r in reads:
            add(r.w, True)
        for w in writes:
            add(w.w, False)
            for ev in w.r:
                add(ev, False)
        waits = []
        for k, v in deps.items():
            if self.seen[eng].get(k, 0) < v:
                self.seen[eng][k] = v
                waits.append((self.esem[k], v))
        return waits

    def _commit(self, ev, reads, writes):
        for r in reads:
            r.r.append(ev)
        for w in writes:
            w.w = ev
            w.r = []

    def op(self, eng, fn, reads=(), writes=()):
        waits = self._deps(eng, reads, writes)
        self.cnt[eng] += 1
        ev = (eng, self.cnt[eng])
        sem = self.esem[eng]

        def run(e, waits=waits, fn=fn, sem=sem):
            for s, v in waits:
                e.wait_ge(s, v)
            fn(e).then_inc(sem, 1)
        self.q[eng].append(run)
        self._commit(ev, reads, writes)
        return ev

    def dma(self, q, out, in_, reads=(), writes=(), owner=None, fn=None, **kw):
        owner = owner or (writes[0] if writes else reads[0])
        if owner.dsem is None:
            owner.dsem = self.free_dsems.pop()
            self.esem[("d", id(owner))] = owner.dsem
        key = ("d", id(owner))
        waits = self._deps(q, reads, writes, skip_key=key)
        owner.dcnt += 16
        ev = (key, owner.dcnt)
        self.dlast[key] = owner.dcnt
        sem = owner.dsem
        if self._reg is not None:
            self._reg["dma"][q][key] = self._reg["dma"][q].get(key, 0) + 16

        def run(e, waits=waits, sem=sem, out=out, in_=in_, kw=kw, fn=fn):
            for s, v in waits:
                e.wait_ge(s, v)
            if fn is not None:
                fn(e).then_inc(sem, 16)
            else:
                e.dma_start(out=out, in_=in_, **kw).then_inc(sem, 16)
        self.q[q].append(run)
        self._commit(ev, reads, writes)
        return ev

    def raw(self, eng, fn, reads=()):
        waits = self._deps(eng, reads, ())

        def run(e, waits=waits, fn=fn):
            for s, v in waits:
                e.wait_ge(s, v)
            fn(e)
        self.q[eng].append(run)

    def barrier(self, dma=True):
        evs = [(e, self.cnt[e]) for e in ENGS if self.cnt[e] > 0]
        if dma:
            evs += list(self.dlast.items())
        for eng in ENGS:
            waits = []
            for k, v in evs:
                if k == eng:
                    continue
                if self.seen[eng].get(k, 0) < v:
                    self.seen[eng][k] = v
                    waits.append((self.esem[k], v))
            if waits:
                def run(e, waits=waits):
                    for s, v in waits:
                        e.wait_ge(s, v)
                self.q[eng].append(run)

    def region_begin(self, tag):
        self._reg = dict(tag=tag, cnt0=dict(self.cnt), dma={e: {} for e in ENGS},
                         seen0={e: dict(v) for e, v in self.seen.items()})
        for eng in ENGS:
            self.q[eng].append(("begin", tag))

    def region_end(self):
        r = self._reg
        for eng in ENGS:
            self.q[eng].append(("end", dict(own=self.cnt[eng] - r["cnt0"][eng], dma=dict(r["dma"][eng]))))
        self.seen = r["seen0"]
        self._reg = None

    def replay(self, eng, e, regs):
        items = self.q[eng]
        i = 0
        while i < len(items):
            it = items[i]
            if isinstance(it, tuple) and it[0] == "begin":
                j = i + 1
                body = []
                while not (isinstance(items[j], tuple) and items[j][0] == "end"):
                    body.append(items[j])
                    j += 1
                comp = items[j][1]
                rt, rk = regs[eng]
                e.reg_mov(rk, it[1])
                with e.If_lt(rk, rt):
                    for f in body:
                        f(e)
                with e.Else():
                    n = comp["own"]
                    while n > 0:
                        e.sem_inc(self.esem[eng], min(n, 8))
                        n -= min(n, 8)
                    for key, v in comp["dma"].items():
                        for _ in range(v // 16):
                            e.sem_inc(self.esem[key], 16)
                i = j + 1
            else:
                it(e)
                i += 1

    def mark(self, nc, name):
        st = self.__dict__.setdefault("_scope", {})

        for eng in ENGS:
            def run(e, eng=eng, name=name):
                if eng in st:
                    nc.leave_named_scope(st[eng][0], st[eng][1], False)
                sid, _ = nc.enter_named_scope(name, False)
                st[eng] = (name, sid)
            self.q[eng].append(run)

    def final_wait(self, eng):
        waits = []
        for k, v in self.dlast.items():
            if self.seen[eng].get(k, 0) < v:
                self.seen[eng][k] = v
                waits.append((self.esem[k], v))

        def run(e, waits=waits):
            for s, v in waits:
                e.wait_ge(s, v)
        self.q[eng].append(run)


def build_program(debug=()):
    nc = bass.Bass("TRN2", target_bir_lowering=False)

    def din(name, shape):
        return nc.dram_tensor(name, list(shape), F32, kind="ExternalInput").ap()

    xin = din("xin", [TC, D])
    mem = din("mem", [256, D])
    cpack = din("cpack", [128, NCP])
    ident = din("ident", [128, 128])
    bdm = din("bdm", [128, 128])
    maskexp = din("maskexp", [128, 3072])
    sinks_rep = din("sinks_rep", [1, 1536])
    w_mem_kv = din("w_mem_kv", [D, 512])
    cv_w_in = din("cv_w_in", [D, 1792])
    cv_w_out = din("cv_w_out", [D, D])
    cv_w_gate = din("cv_w_gate", [D, DFF0])
    cv_w_up = din("cv_w_up", [D, DFF0])
    cv_w_down = din("cv_w_down", [DFF0, D])
    sw_w_in = din("sw_w_in", [D, 1536])
    sw_w_out = din("sw_w_out", [D, D])
    sw_router = din("sw_router", [D, NE])
    sw_we_gate = din("sw_we_gate", [NE, D, DFFE])
    sw_we_up = din("sw_we_up", [NE, D, DFFE])
    sw_we_down = din("sw_we_down", [NE, DFFE, D])
    cst2 = din("cst2", [128, 296])
    WSG = nc.dram_tensor("wsg_scr", [NE * 7 * 128, 4096], BF16, kind="Internal").ap()
    WSU = nc.dram_tensor("wsu_scr", [NE * 7 * 128, 4096], BF16, kind="Internal").ap()
    XS = nc.dram_tensor("xs_scr", [8192, D], BF16, kind="Internal").ap()
    YS = nc.dram_tensor("ys_scr", [8192, D], F32, kind="Internal").ap()
    out = nc.dram_tensor("out", [TOK, D], F32, kind="ExternalOutput").ap()
    dbg_out = {}
    for name, shape in debug:
        dbg_out[name] = nc.dram_tensor("dbg_" + name, list(shape), F32, kind="ExternalOutput").ap()

    es = ExitStack()
    with es:
        def sb(name, shape, dt):
            return es.enter_context(nc.sbuf_tensor(name, list(shape), dt))

        HT = sb("HT", [128, NT, D], F32)
        RA = sb("RA", [128, 8, TC], BF16)
        AR = sb("AR", [128, 40960], BF16)
        CP = sb("CP", [128, NCP], F32)
        IDF = sb("IDF", [128, 128], F32)
        IDB = sb("IDB", [128, 128], BF16)
        BDB = sb("BDB", [128, 128], BF16)
        ONB = sb("ONB", [128, 128], BF16)
        ME = sb("ME", [128, 2, 4, 384], BF16)
        ME0 = sb("ME0", [128, 4, 384], BF16)
        SRB = sb("SRB", [128, 4, 384], BF16)
        MKT = sb("MKT", [128, 2, 256], BF16)
        MV = sb("MV", [128, 2, 256], BF16)
        SS = sb("SS", [128, 3, 32], F32)
        GT = sb("GT", [128, 16, 8], F32)
        WR = sb("WR", [128, 8, NE], F32)
        PS = es.enter_context(nc.psum_tensor("PS", [128, 8, 512], F32))

        esem = {e: es.enter_context(nc.semaphore("sem_" + e)) for e in ENGS}
        dsems = [es.enter_context(nc.semaphore("dsem%d" % i)) for i in range(72)]
        S = Sched(esem, dsems)

        banks = [Res("bank%d" % i) for i in range(8)]
        reserved = set()

        def bank(pair=False):
            while True:
                b = S.nbank % 8
                if pair and (b % 2 == 1 or (b + 1) in reserved):
                    S.nbank += 1
                    continue
                if b in reserved:
                    S.nbank += 1
                    continue
                break
            if pair:
                S.nbank += 2
                return PS[:, b:b + 2, :], [banks[b], banks[b + 1]]
            S.nbank += 1
            return PS[:, b, :], [banks[b]]

        def mmg(out_ap, pairs, reads, writes):
            def fn(e, out_ap=out_ap, pairs=pairs):
                n = len(pairs)
                ins = None
                for i, (l, r) in enumerate(pairs):
                    ins = e.matmul(out_ap, l, r, start=(i == 0), stop=(i == n - 1))
                return ins
            S.op("pe", fn, reads, writes)

        def act(out_ap, in_ap, func, reads, writes, **kw):
            S.op("act", lambda e: e.activation(out=out_ap, in_=in_ap, func=func, **kw), reads, writes)

        def tt(out_ap, in0, in1, op, reads, writes, eng="dve"):
            S.op(eng, lambda e: e.tensor_tensor(out=out_ap, in0=in0, in1=in1, op=op), reads, writes)

        def ts(out_ap, in0, s1, op0, reads, writes, s2=None, op1=None, eng="dve"):
            if op1 is None:
                S.op(eng, lambda e: e.tensor_scalar(out=out_ap, in0=in0, scalar1=s1, scalar2=None, op0=op0),
                     reads, writes)
            else:
                S.op(eng, lambda e: e.tensor_scalar(out=out_ap, in0=in0, scalar1=s1, scalar2=s2, op0=op0, op1=op1),
                     reads, writes)

        def stt(out_ap, in0, scalar, in1, op0, op1, reads, writes):
            S.op("dve", lambda e: e.scalar_tensor_tensor(out=out_ap, in0=in0, scalar=scalar, in1=in1,
                                                         op0=op0, op1=op1), reads, writes)

        def recip(out_ap, in_ap, reads, writes):
            S.op("dve", lambda e: e.reciprocal(out=out_ap, in_=in_ap), reads, writes)

        def copy(eng, out_ap, in_ap, reads, writes):
            if eng == "act":
                act(out_ap, in_ap, AF.Copy, reads, writes)
            else:
                S.op(eng, lambda e: e.tensor_copy(out=out_ap, in_=in_ap), reads, writes)

        def dump(name, ap, reads, view=None):
            if name in dbg_out:
                dst = dbg_out[name]
                if view:
                    dst = dst.rearrange(view[0], **view[1])
                S.dma("pool", dst, ap, reads=reads, writes=(), owner=R_dbg)

        R_dbg = Res("dbg")

        def cpc(c, n=1):
            return CP[:, c:c + n]

        def arv(off, n, dt=BF16):
            if dt == F32:
                return AR[:, off:off + 2 * n].bitcast(F32)
            return AR[:, off:off + n]

        R_HT = [Res("HT%d" % j) for j in range(NT)]
        R_RA = [Res("RA%d" % j) for j in range(NT + 1)]
        R_CP, R_ID, R_ME, R_SR = Res("CP"), Res("ID"), Res("ME"), Res("SR")
        R_MKV = Res("MKV")
        R_SS = Res("SS")

        def tcols(j):
            if j == 17:
                return 0, 32
            return 32 + 128 * j, 128

        S.dma("sp", CP[:], cpack[:, :], writes=[R_CP])
        S.dma("sp", IDF[:], ident[:, :], writes=[R_ID])
        R_BD = Res("BD")
        PB0 = 22016
        BDF = arv(PB0, 128, F32)
        S.dma("sp", BDF, bdm[:, :], writes=[R_BD])
        MEf = ME[:].rearrange("p a b c -> p (a b c)")
        S.dma("pool", MEf[:, 0:1536], maskexp[:, 0:1536], writes=[R_ME])
        S.dma("pool", MEf[:, 1536:3072], maskexp[:, 1536:3072], writes=[R_ME])
        SRF = arv(14336, 1536, F32)[0:1, :]
        S.dma("sp", SRF, sinks_rep[:, :], writes=[R_SR])
        WIN0 = arv(0, 8 * 1792).rearrange("p (k n) -> p k n", k=8)
        R_WIN0 = Res("WIN0")
        S.dma("pool", WIN0, cv_w_in.rearrange("(k p) n -> p k n", p=128), writes=[R_WIN0])
        R_XP = Res("XP")
        R_WS = Res("WS")
        pp_list = [(ws_, w_, ex, g_) for ex in range(NE) for g_ in range(7) for (ws_, w_) in ((WSG, sw_we_gate), (WSU, sw_we_up))]
        pp_state = {"i": 0}

        def prepass(n):
            for _ in range(n):
                if pp_state["i"] >= len(pp_list):
                    return
                ws_, w_, ex, g_ = pp_list[pp_state["i"]]
                pp_state["i"] += 1
                r0 = (ex * 7 + g_) * 128
                S.dma("pool", ws_[r0:r0 + 128, :].rearrange("p (k n) -> p k n", k=8),
                      w_[ex, :, g_ * 512:(g_ + 1) * 512].rearrange("(k p) n -> p k n", p=128),
                      writes=[R_WS], owner=R_WS)
        ZT = sb("ZT", [128, 2048], BF16)
        R_ZT = Res("ZT")
        R_XS = Res("XS")
        S.op("dve", lambda e: e.memset(ZT[:], 0.0), [], [R_ZT])
        XP = arv(PB0 + 256, D, F32)
        S.dma("sp", XP[0:32, :], xin[0:32, :], writes=[R_XP])
        R_MEM = Res("MEM")
        MEMT = arv(PB0 + 2304, 2 * D, F32).rearrange("p (t d) -> p t d", t=2)
        S.dma("sp", MEMT, mem.rearrange("(t p) d -> p t d", p=128), writes=[R_MEM])
        R_WKV = Res("WKV")
        WKV = arv(PB0 + 6400, 8 * 512).rearrange("p (k n) -> p k n", k=8)
        S.dma("pool", WKV, w_mem_kv.rearrange("(k p) n -> p k n", p=128), writes=[R_WKV])
        for j0, j1 in ((0, 2), (2, 5), (5, 9), (9, 13), (13, 17)):
            S.dma("sp", HT[:, j0:j1, :],
                  xin[32 + 128 * j0:32 + 128 * j1, :].rearrange("(j p) d -> p j d", p=128),
                  writes=R_HT[j0:j1])
        R_WR = Res("WR")
        S.dma("sp", WR[:], sw_router.rearrange("(k p) n -> p k n", p=128), writes=[R_WR])
        XSz = XS.rearrange("(a p r) d -> a p (r d)", p=128, r=2)
        for a_ in range(32):
            S.dma("sp", XSz[a_], ZT[:], reads=[R_ZT], writes=[R_XS], owner=R_XS)
        prepass(8)

        copy("act", IDB[:], IDF[:], [R_ID], [R_ID])
        copy("act", BDB[:], BDF, [R_BD], [R_BD])
        S.op("dve", lambda e: e.memset(ONB[:], 1.0), [], [R_ID])
        S.op("dve", lambda e: e.memset(SRB[:], 0.0), [], [R_SR])
        act(SRB[0:1].rearrange("p a b -> p (a b)"), SRF, AF.Exp, [R_SR], [R_SR])
        ts(ME0[:], ME[:, 0], cpc(C_FLAG), ALU.mult, [R_ME, R_CP], [R_ME])

        A_TAIL = 37888
        HNB = [arv(A_TAIL + i * 1024, 1024) for i in range(2)]
        R_HNB = [Res("HNB%d" % i) for i in range(2)]
        JUNK = arv(A_TAIL + 2048, 1024)
        R_JUNK = Res("JUNK")
        ss_state = {"n": 0}

        def norm_transpose(j, gcol, src_ap=None, src_res=None, npart=128, fp32_path=None):
            k = ss_state["n"] % 32
            ss_state["n"] += 1
            if src_ap is None:
                src_ap, src_res = HT[:, j, :], R_HT[j]
            c0, n = tcols(j)
            P = slice(0, npart)
            act(JUNK[P, :], src_ap, AF.Square, [src_res], [R_JUNK, R_SS], accum_out=SS[P, 0, k:k + 1])
            act(SS[P, 1, k:k + 1], SS[P, 0, k:k + 1], AF.Sqrt, [R_SS], [R_SS], scale=1.0 / D, bias=RMS_EPS)
            recip(SS[P, 2, k:k + 1], SS[P, 1, k:k + 1], [R_SS], [R_SS])
            if fp32_path is None:
                s = j % 2
                ts(HNB[s][P, :], src_ap, SS[P, 2, k:k + 1], ALU.mult, [src_res, R_SS], [R_HNB[s]])
                pb, pr = bank()
                pv = pb.bitcast(BF16).rearrange("p (k c) -> p k c", k=8)

                def fn(e, pv=pv, s=s, P=P, n=n):
                    ins = None
                    for kc in range(8):
                        ins = e.transpose(out=pv[:, kc, 0:n], in_=HNB[s][P, kc * 128:(kc + 1) * 128],
                                          identity=IDB[P, 0:n])
                    return ins
                S.op("pe", fn, [R_HNB[s], R_ID], pr)
                g_b = CP[:, gcol:gcol + 8].unsqueeze(2).to_broadcast([128, 8, n])
                tt(RA[:, :, c0:c0 + n], pv[:, :, 0:n], g_b, ALU.mult, pr + [R_CP], [R_RA[j]])
            else:
                HN32, R_HN32, H32T, R_H32T = fp32_path
                s = j % 2
                ts(HN32[s], src_ap, SS[:, 2, k:k + 1], ALU.mult, [src_res, R_SS], [R_HN32[s]])
                pb, pr = bank(pair=True)
                pv = pb.rearrange("p b (k c) -> p (b k) c", k=4)

                def fn(e, pv=pv, s=s):
                    ins = None
                    for kc in range(8):
                        ins = e.transpose(out=pv[:, kc, :], in_=HN32[s][:, kc * 128:(kc + 1) * 128],
                                          identity=IDF[:])
                    return ins
                S.op("pe", fn, [R_HN32[s], R_ID], pr)
                g_b = CP[:, gcol:gcol + 8].unsqueeze(2).to_broadcast([128, 8, 128])
                tt(RA[:, :, c0:c0 + n], pv, g_b, ALU.mult, pr + [R_CP], [R_RA[j]])
                tt(H32T[s], pv, g_b, ALU.mult, pr + [R_CP], [R_H32T[s]])

        hn_state = {"n": 0}

        hn_pending = []

        def hn_flush(keep=0):
            while len(hn_pending) > keep:
                hn_pending.pop(0)()

        def headnorm(q_ap, q_res, gcol, out_ap, out_res, n, bufs, split=None):
            SQ, R_SQ, RT, R_RT = bufs
            s = hn_state["n"] % 2
            hn_state["n"] += 1
            act(SQ[s][:, :n], q_ap, AF.Square, q_res, [R_SQ[s]])

            def rest():
                sb_, sr_ = bank()
                mmg(sb_[:, :n], [(BDB[:], SQ[s][:, :n])], [R_SQ[s], R_BD], sr_)
                act(RT[s][:, :n], sb_[:, :n], AF.Sqrt, sr_, [R_RT[s]], bias=RMS_EPS)
                recip(RT[s][:, :n], RT[s][:, :n], [R_RT[s]], [R_RT[s]])
                qv, rv = q_ap, RT[s][:, :n]
                if split:
                    qv = qv.rearrange("p (a b) -> p a b", b=split)
                    rv = rv.rearrange("p (a b) -> p a b", b=split)
                stt(out_ap, qv, cpc(gcol), rv, ALU.mult, ALU.mult, q_res + [R_RT[s], R_CP], out_res)
            hn_flush(keep=0)
            hn_pending.append(rest)

        def mem_attention(QM, R_QM, blocks, qcol_off, bufs):
            PT, R_PT, RD, R_RD = bufs
            npt = len(PT)
            st = {"n": 0, "r": 0}
            its = [(c0, n, tiles, hd) for (c0, n, tiles) in blocks for hd in range(4)]
            pend = {}

            def stage_a(i):
                c0, n, tiles, hd = its[i]
                q0 = c0 - qcol_off
                c2, hb = hd // 2, hd % 2
                rows = slice(hb * 64, hb * 64 + 64)
                pts = []
                for mc in range(2):
                    s = st["n"] % npt
                    st["n"] += 1
                    sb_, sr_ = bank()
                    mmg(sb_[:, :n], [(MKT[rows, c2, mc * 128:(mc + 1) * 128], QM[rows, c2, q0:q0 + n])],
                        [R_MKV, R_QM], sr_)
                    act(PT[s][:, :n], sb_[:, :n], AF.Exp, sr_, [R_PT[s]], scale=0.125)
                    pts.append(s)
                pend[i] = pts

            def stage_b(i):
                c0, n, tiles, hd = its[i]
                c2, hb = hd // 2, hd % 2
                rows = slice(hb * 64, hb * 64 + 64)
                pts = pend.pop(i)
                ob, orr = bank()
                mmg(ob[:, :n], [(MV[:, mc, c2 * 128:(c2 + 1) * 128], PT[pts[mc]][:, :n]) for mc in range(2)],
                    [R_MKV] + [R_PT[s] for s in pts], orr)
                db, dr = bank()
                mmg(db[:, :n], [(ONB[:], PT[pts[mc]][:, :n]) for mc in range(2)],
                    [R_ID] + [R_PT[s] for s in pts], dr)
                s2 = st["r"] % 2
                st["r"] += 1
                recip(RD[s2][rows, :n], db[rows, :n], dr, [R_RD[s2]])
                tt(RA[rows, 6 + c2, c0:c0 + n], ob[rows, :n], RD[s2][rows, :n], ALU.mult,
                   orr + [R_RD[s2]], [R_RA[t] for t in tiles])
            nun = len(its) // 2
            stage_a(0)
            stage_a(1)
            for u in range(nun):
                if u + 1 < nun:
                    stage_a(2 * u + 2)
                    stage_a(2 * u + 3)
                stage_b(2 * u)
                stage_b(2 * u + 1)

        def out_proj(WO, R_WO, tiles):
            for j in tiles:
                c0, n = tcols(j)
                yb, yr = bank(pair=True)
                for half in range(2):
                    mmg(yb[:, half, :], [(RA[:, kc, c0:c0 + 128], WO[:, kc, half * 512:(half + 1) * 512])
                                         for kc in range(8)], [R_RA[j], R_WO], [yr[half]])
                tt(HT[:, j, :], yb.rearrange("p b c -> p (b c)"), HT[:, j, :], ALU.add, yr + [R_HT[j]], [R_HT[j]])

        def ffn(groups, blocks, WB, R_WB, ACTB, R_ACTB, SIL, R_SIL, after_load=None):
            steps = [(gi, bi) for gi in range(len(groups)) for bi in range(len(blocks))]

            def load(gi):
                g = groups[gi]
                s = gi % 2
                nfc = g["nfc"]
                wg, wu, wd = WB[s]
                S.dma("pool", wg[:, :, 0:nfc * 128], g["g"].rearrange("(k p) n -> p k n", p=128),
                      writes=[R_WB[s][0]])
                S.dma("pool", wu[:, :, 0:nfc * 128], g["u"].rearrange("(k p) n -> p k n", p=128),
                      writes=[R_WB[s][1]])
                S.dma("pool", wd[:, 0:nfc, :], g["d"].rearrange("(f p) n -> p f n", p=128),
                      writes=[R_WB[s][2]])
                if after_load is not None:
                    after_load()

            def gu(si):
                gi, bi = steps[si]
                g = groups[gi]
                s = gi % 2
                a = si % 2
                wg, wu, wd = WB[s]
                c0, n, tiles = blocks[bi]
                rr = [R_RA[t] for t in tiles]
                for fc in range(g["nfc"]):
                    gb, gr = bank()
                    mmg(gb[:, :n], [(wg[:, kc, fc * 128:(fc + 1) * 128], RA[:, kc, c0:c0 + n]) for kc in range(8)],
                        rr + [R_WB[s][0]], gr)
                    ub, ur = bank()
                    mmg(ub[:, :n], [(wu[:, kc, fc * 128:(fc + 1) * 128], RA[:, kc, c0:c0 + n]) for kc in range(8)],
                        rr + [R_WB[s][1]], ur)
                    sl = (si * 4 + fc) % 2
                    act(SIL[sl][:, :n], gb[:, :n], AF.Silu, gr, [R_SIL[sl]])
                    tt(ACTB[a][:, fc, :n], ub[:, :n], SIL[sl][:, :n], ALU.mult, ur + [R_SIL[sl]], [R_ACTB[a][fc]])

            def down(si):
                gi, bi = steps[si]
                g = groups[gi]
                s = gi % 2
                a = si % 2
                wg, wu, wd = WB[s]
                c0, n, tiles = blocks[bi]
                nfc = g["nfc"]
                for ti, j in enumerate(tiles):
                    yb, yr = bank(pair=True)
                    for half in range(2):
                        mmg(yb[:, half, :], [(ACTB[a][:, fc, ti * 128:(ti + 1) * 128],
                                              wd[:, fc, half * 512:(half + 1) * 512]) for fc in range(nfc)],
                            [R_ACTB[a][fc] for fc in range(nfc)] + [R_WB[s][2]], [yr[half]])
                    yv = yb.rearrange("p b c -> p (b c)")
                    if g["gate"] is None:
                        tt(HT[:, j, :], yv, HT[:, j, :], ALU.add, yr + [R_HT[j]], [R_HT[j]])
                    else:
                        stt(HT[:, j, :], yv, g["gate"](j), HT[:, j, :], ALU.mult, ALU.add,
                            yr + [R_HT[j], R_GT], [R_HT[j]])

            load(0)
            if len(groups) > 1:
                load(1)
            nb = len(blocks)
            for si in range(len(steps)):
                gu(si)
                if si > 0:
                    down(si - 1)
                    gi_prev, bi_prev = steps[si - 1]
                    if bi_prev == nb - 1 and gi_prev + 2 < len(groups):
                        load(gi_prev + 2)
            down(len(steps) - 1)

        R_GT = Res("GT")

        if PROFILE_MARKS:
            S.mark(nc, "P_mem")
        MEMX = arv(PB0 + 10496, 8 * 256).rearrange("p (k c) -> p k c", k=8)
        R_MEMX = [Res("MEMX0"), Res("MEMX1")]
        SQb = [arv(PB0 + 12544 + i * 512, 512) for i in range(2)]
        R_SQb = [Res("SQ%d" % i) for i in range(2)]
        RTb = [arv(PB0 + 13568 + i * 1024, 512, F32) for i in range(2)]
        R_RTb = [Res("RT%d" % i) for i in range(2)]
        hbufs = (SQb, R_SQb, RTb, R_RTb)
        for t in range(2):
            k = ss_state["n"] % 32
            ss_state["n"] += 1
            act(JUNK[:], MEMT[:, t, :], AF.Square, [R_MEM], [R_JUNK, R_SS], accum_out=SS[:, 0, k:k + 1])
            act(SS[:, 1, k:k + 1], SS[:, 0, k:k + 1], AF.Sqrt, [R_SS], [R_SS], scale=1.0 / D, bias=RMS_EPS)
            recip(SS[:, 2, k:k + 1], SS[:, 1, k:k + 1], [R_SS], [R_SS])
            ts(HNB[t][:], MEMT[:, t, :], SS[:, 2, k:k + 1], ALU.mult, [R_MEM, R_SS], [R_HNB[t]])
            pb, pr = bank()
            pv = pb.bitcast(BF16).rearrange("p (k c) -> p k c", k=8)

            def fn(e, pv=pv, t=t):
                ins = None
                for kc in range(8):
                    ins = e.transpose(out=pv[:, kc, :], in_=HNB[t][:, kc * 128:(kc + 1) * 128], identity=IDB[:])
                return ins
            S.op("pe", fn, [R_HNB[t], R_ID], pr)
            g_b = CP[:, C_GM:C_GM + 8].unsqueeze(2).to_broadcast([128, 8, 128])
            tt(MEMX[:, :, t * 128:(t + 1) * 128], pv, g_b, ALU.mult, pr + [R_CP], [R_MEMX[t]])
        for c in range(2):
            kb_, kr_ = bank()
            mmg(kb_[:, :256], [(WKV[:, kc, c * 128:(c + 1) * 128], MEMX[:, kc, :]) for kc in range(8)],
                R_MEMX + [R_WKV], kr_)
            headnorm(kb_[:, :256], kr_, C_MK, MKT[:, c, :], [R_MKV], 256, hbufs)
        hn_flush()
        for t in range(2):
            vb_, vr_ = bank()
            mmg(vb_[:, :256], [(MEMX[:, kc, t * 128:(t + 1) * 128], WKV[:, kc, 256:512]) for kc in range(8)],
                [R_MEMX[t], R_WKV], vr_)
            copy("act", MV[:, t, :], vb_[:, :256], vr_, [R_MKV])
        hn_flush()
        dump("mkt", MKT[:].rearrange("p a b -> p (a b)"), [R_MKV])
        dump("mv", MV[:].rearrange("p a b -> p (a b)"), [R_MKV])

        if PROFILE_MARKS:
            S.mark(nc, "L0A_norm")
        hn_flush()
        S.barrier(dma=False)
        norm_transpose(17, C_GA0, src_ap=XP[0:32, :], src_res=R_XP, npart=32)
        for j in range(NT):
            norm_transpose(j, C_GA0)
        if PROFILE_MARKS:
            S.mark(nc, "L0B_inproj")
        prepass(8)
        U = arv(14336, 6 * TC).rearrange("p (c t) -> p c t", c=6)
        R_U = [Res("U%d" % c) for c in range(6)]
        QM0 = arv(27584, 2 * 2176).rearrange("p (c t) -> p c t", c=2)
        R_QM0 = Res("QM0")
        SG = [arv(31936 + i * 1024, 512, F32) for i in range(2)]
        R_SG = [Res("SG%d" % i) for i in range(2)]
        SQb = [arv(33984 + i * 512, 512) for i in range(2)]
        RTb = [arv(35008 + i * 1024, 512, F32) for i in range(2)]
        hbufs = (SQb, R_SQb, RTb, R_RTb)
        TB0 = [(0, 160, [17, 0])] + [(160 + 512 * i, 512, [1 + 4 * i + t for t in range(4)]) for i in range(4)]
        sgi = 0
        for c in range(6):
            for (c0, n, tiles) in TB0:
                rr = [R_RA[t] for t in tiles]
                ab, ar_ = bank()
                mmg(ab[:, :n], [(WIN0[:, kc, c * 128:(c + 1) * 128], RA[:, kc, c0:c0 + n]) for kc in range(8)],
                    rr + [R_WIN0], ar_)
                gb, gr = bank()
                mmg(gb[:, :n], [(WIN0[:, kc, (6 + c) * 128:(7 + c) * 128], RA[:, kc, c0:c0 + n]) for kc in range(8)],
                    rr + [R_WIN0], gr)
                s = sgi % 2
                sgi += 1
                act(SG[s][:, :n], gb[:, :n], AF.Sigmoid, gr + [R_CP], [R_SG[s]], bias=cpc(C_BG + c))
                stt(U[:, c, c0:c0 + n], ab[:, :n], cpc(C_BA + c), SG[s][:, :n], ALU.add, ALU.mult,
                    ar_ + [R_SG[s], R_CP], [R_U[c]])
                if c0 == 0:
                    ts(U[:, c, 0:160], U[:, c, 0:160], cpc(C_FLAG), ALU.mult, [R_U[c], R_CP], [R_U[c]])
        TBC = [(32, 128, [0])] + TB0[1:]
        for c2 in range(2):
            for (c0, n, tiles) in TBC:
                rr = [R_RA[t] for t in tiles]
                qb, qr = bank()
                mmg(qb[:, :n], [(WIN0[:, kc, (12 + c2) * 128:(13 + c2) * 128], RA[:, kc, c0:c0 + n])
                                for kc in range(8)], rr + [R_WIN0], qr)
                headnorm(qb[:, :n], qr, C_MQ0, QM0[:, c2, c0 - 32:c0 - 32 + n], [R_QM0], n, hbufs)
        hn_flush()
        dump("u", U[:, 0, :], [R_U[0]])
        dump("qm0", QM0[:, 0, :], [R_QM0])
        hn_flush()
        S.barrier()
        if PROFILE_MARKS:
            S.mark(nc, "L0C_conv")
        prepass(12)
        DG = [arv(i * 3968, 3968).rearrange("p (k c) -> p k c", k=CONV_W) for i in range(2)]
        R_DG = [Res("DG%d" % i) for i in range(2)]
        R_C = [[Res("C%d_%d" % (c, j)) for j in range(NT)] for c in range(6)]

        def build_dg(c):
            s = c % 2
            S.op("dve", lambda e, s=s, c=c: e.tensor_tensor(
                out=DG[s], in0=IDB[:].unsqueeze(1).to_broadcast([128, CONV_W, 128]),
                in1=CP[:, C_DW + c * CONV_W:C_DW + (c + 1) * CONV_W].unsqueeze(2).to_broadcast([128, CONV_W, 128]),
                op=ALU.mult), [R_ID, R_CP], [R_DG[s]])

        def conv_block(c, blk):
            s = c % 2
            c0, n, tiles = blk
            cb, cr = bank()
            mmg(cb[:, :n], [(DG[s][:, k, :], U[:, c, c0 - 30 + k:c0 - 30 + k + n]) for k in range(CONV_W)],
                [R_DG[s], R_U[c]], cr)
            act(RA[:, c, c0:c0 + n], cb[:, :n], AF.Identity, cr + [R_CP], [R_C[c][t] for t in tiles],
                bias=cpc(C_DWB + c))
        SQL = [arv(7936 + i * 512, 512) for i in range(2)]
        R_SQL = [Res("SQL%d" % i) for i in range(2)]
        MSQ = arv(8960, 512, F32)
        VAR = arv(9984, 512, F32)
        R_ST = Res("LNST")
        T1 = [arv(11008 + i * 1024, 512, F32) for i in range(2)]
        R_T1 = [Res("T1%d" % i) for i in range(2)]
        ln_st = {"q": 0}

        def ln_block(blk):
            c0, n, tiles = blk
            rall = [R_C[c][t] for c in range(6) for t in tiles]
            s1b, s1r = bank()
            mmg(s1b[:, :n], [(ONB[:], RA[:, c, c0:c0 + n]) for c in range(6)], rall + [R_ID], s1r)
            s2b, s2r = bank()
            for c in range(6):
                s = ln_st["q"] % 2
                ln_st["q"] += 1
                act(SQL[s][:, :n], RA[:, c, c0:c0 + n], AF.Square, [R_C[c][t] for t in tiles], [R_SQL[s]])
                S.op("pe", lambda e, s2b=s2b, s=s, n=n, c=c: e.matmul(s2b[:, :n], ONB[:], SQL[s][:, :n],
                                                                    start=(c == 0), stop=(c == 5)),
                     [R_SQL[s], R_ID], s2r)
            act(MSQ[:, :n], s1b[:, :n], AF.Square, s1r, [R_ST], scale=1.0 / CONV_CH)
            stt(VAR[:, :n], s2b[:, :n], 1.0 / CONV_CH, MSQ[:, :n], ALU.mult, ALU.subtract, s2r + [R_ST], [R_ST])
            act(VAR[:, :n], VAR[:, :n], AF.Sqrt, [R_ST], [R_ST], bias=LN_EPS)
            recip(VAR[:, :n], VAR[:, :n], [R_ST], [R_ST])
            for c in range(6):
                s = c % 2
                rc = [R_C[c][t] for t in tiles]
                stt(T1[s][:, :n], s1b[:, :n], -1.0 / CONV_CH, RA[:, c, c0:c0 + n], ALU.mult, ALU.add,
                    s1r + rc, [R_T1[s]])
                tt(T1[s][:, :n], T1[s][:, :n], VAR[:, :n], ALU.mult, [R_T1[s], R_ST], [R_T1[s]])
                act(RA[:, c, c0:c0 + n], T1[s][:, :n], AF.Silu, [R_T1[s], R_CP], rc,
                    scale=cpc(C_LNG + c), bias=cpc(C_LNB + c))
        for c in range(5):
            build_dg(c)
            for blk in TBC:
                conv_block(c, blk)
        build_dg(5)
        conv_block(5, TBC[0])
        for b in range(len(TBC)):
            if b + 1 < len(TBC):
                conv_block(5, TBC[b + 1])
            ln_block(TBC[b])
        dump("cln", RA[:, 0, 32:TC], [R_C[0][t] for t in range(NT)])
        hn_flush()
        S.barrier()
        if PROFILE_MARKS:
            S.mark(nc, "L0D_memattn")
        PT = [arv(i * 512, 512) for i in range(8)]
        R_PT = [Res("PT%d" % i) for i in range(8)]
        RD = [arv(4096 + i * 1024, 512, F32) for i in range(2)]
        R_RD = [Res("RD%d" % i) for i in range(2)]
        WO0 = arv(14336, 8 * D).rearrange("p (k n) -> p k n", k=8)
        R_WO0 = Res("WO0")
        S.dma("pool", WO0, cv_w_out.rearrange("(k p) n -> p k n", p=128), writes=[R_WO0])
        prepass(8)
        mem_attention(QM0, R_QM0, TBC, 32, (PT, R_PT, RD, R_RD))
        dump("cat0", RA[:, 6, 32:TC], [R_RA[t] for t in range(NT)])
        if PROFILE_MARKS:
            S.mark(nc, "L0E_outproj")
        out_proj(WO0, R_WO0, list(range(NT)))
        dump("h0a", HT[:, 1, :], [R_HT[1]])
        dump("hf0a", HT[:, 1:NT, :].rearrange("p j d -> p (j d)"), R_HT[1:NT])
        hn_flush()
        S.barrier()
        if PROFILE_MARKS:
            S.mark(nc, "L0F_ffn")
        WB = []
        R_WB = []
        for s in range(2):
            o = s * 12288
            WB.append((arv(o, 4096).rearrange("p (k n) -> p k n", k=8),
                       arv(o + 4096, 4096).rearrange("p (k n) -> p k n", k=8),
                       arv(o + 8192, 4096).rearrange("p (f n) -> p f n", f=4)))
            R_WB.append([Res("WB%d_%d" % (s, i)) for i in range(3)])
        ACTB = [arv(24576 + a * 2048, 2048).rearrange("p (f n) -> p f n", f=4) for a in range(2)]
        R_ACTB = [[Res("ACTB%d_%d" % (a, f)) for f in range(4)] for a in range(2)]
        SIL = [arv(28672 + i * 512, 512) for i in range(2)]
        R_SIL = [Res("SIL%d" % i) for i in range(2)]
        for j in range(NT):
            norm_transpose(j, C_GF0)
        groups0 = []
        for f0 in range(0, DFF0, 512):
            nf = min(512, DFF0 - f0)
            groups0.append(dict(g=cv_w_gate[:, f0:f0 + nf], u=cv_w_up[:, f0:f0 + nf], d=cv_w_down[f0:f0 + nf, :],
                                nfc=nf // 128, gate=None))
        ffn(groups0, TBC, WB, R_WB, ACTB, R_ACTB, SIL, R_SIL, after_load=lambda: prepass(6))
        dump("h1", HT[:, 1, :], [R_HT[1]])
        dump("hf1", HT[:, 1:NT, :].rearrange("p j d -> p (j d)"), R_HT[1:NT])
        hn_flush()
        S.barrier()

        if PROFILE_MARKS:
            S.mark(nc, "L1A_norm")
        for j in range(NT):
            norm_transpose(j, C_GA1)
        if PROFILE_MARKS:
            S.mark(nc, "L1B_inproj")
        WIN1 = arv(0, 8 * 1536).rearrange("p (k n) -> p k n", k=8)
        R_WIN1 = Res("WIN1")
        QPAIR = [(0, 3), (1, 4), (2, 5), (6, 9), (7, 10), (8, 11)]
        for i, (ha, hb_) in enumerate(QPAIR):
            for half, hh in enumerate((ha, hb_)):
                S.dma("pool", WIN1[:, :, i * 128 + half * 64:i * 128 + half * 64 + 64],
                      sw_w_in[:, hh * 64:(hh + 1) * 64].rearrange("(k p) n -> p k n", p=128), writes=[R_WIN1])
        S.dma("pool", WIN1[:, :, 768:1536], sw_w_in[:, 768:1536].rearrange("(k p) n -> p k n", p=128),
              writes=[R_WIN1])
        prepass(8)
        QT = arv(12288, 6 * TOK).rearrange("p (a n g q) -> p a n g q", a=2, n=16, g=3)
        R_QT = Res("QT")
        KT = arv(24576, 2 * 2176).rearrange("p (c t) -> p c t", c=2)
        R_KT = Res("KT")
        VT = arv(28928, NT * 256).rearrange("p (j d) -> p j d", j=NT)
        R_VT = Res("VT")
        QM1 = arv(33280, 2 * TOK).rearrange("p (c t) -> p c t", c=2)
        R_QM1 = Res("QM1")
        SQb = [arv(37376 + i * 512, 512) for i in range(2)]
        RTb = [arv(38400 + i * 1024, 512, F32) for i in range(2)]
        hbufs = (SQb, R_SQb, RTb, R_RTb)
        hn_flush()
        S.barrier()
        TB1 = TB0[1:]
        for i in range(6):
            for bi, (c0, n, tiles) in enumerate(TB1):
                rr = [R_RA[t] for t in tiles]
                qb, qr = bank()
                mmg(qb[:, :n], [(WIN1[:, kc, i * 128:(i + 1) * 128], RA[:, kc, c0:c0 + n]) for kc in range(8)],
                    rr + [R_WIN1], qr)
                headnorm(qb[:, :n], qr, C_QG, QT[:, i // 3, 4 * bi:4 * bi + 4, i % 3, :], [R_QT], n, hbufs,
                         split=128)
        for c2 in range(2):
            for (c0, n, tiles) in TBC:
                rr = [R_RA[t] for t in tiles]
                kb_, kr_ = bank()
                mmg(kb_[:, :n], [(WIN1[:, kc, 768 + c2 * 128:768 + (c2 + 1) * 128], RA[:, kc, c0:c0 + n])
                                 for kc in range(8)], rr + [R_WIN1], kr_)
                headnorm(kb_[:, :n], kr_, C_KG, KT[:, c2, c0 - 32:c0 - 32 + n], [R_KT], n, hbufs)
        hn_flush()
        for j in range(NT):
            c0, n = tcols(j)
            vb_, vr_ = bank()
            mmg(vb_[:, :256], [(RA[:, kc, c0:c0 + 128], WIN1[:, kc, 1024:1280]) for kc in range(8)],
                [R_RA[j], R_WIN1], vr_)
            copy("act", VT[:, j, :], vb_[:, :256], vr_, [R_VT])
        for c2 in range(2):
            for (c0, n, tiles) in TB1:
                rr = [R_RA[t] for t in tiles]
                qb, qr = bank()
                mmg(qb[:, :n], [(WIN1[:, kc, 1280 + c2 * 128:1280 + (c2 + 1) * 128], RA[:, kc, c0:c0 + n])
                                for kc in range(8)], rr + [R_WIN1], qr)
                headnorm(qb[:, :n], qr, C_MQ1, QM1[:, c2, c0 - 160:c0 - 160 + n], [R_QM1], n, hbufs)
        hn_flush()
        dump("qt", QT[:, 0, :, 0, :], [R_QT], view=("p (n q) -> p n q", dict(n=16)))
        dump("kt", KT[:, 0, :], [R_KT])
        hn_flush()
        S.barrier()
        if PROFILE_MARKS:
            S.mark(nc, "L1C_swa")
        prepass(12)
        ET = [arv(i * 384, 384) for i in range(8)]
        R_ET = [Res("ET%d" % i) for i in range(8)]
        PT1 = [arv(3072 + i * 384, 384) for i in range(8)]
        R_PT1 = [Res("PT1%d" % i) for i in range(8)]
        RD1 = [arv(6144 + i * 768, 384, F32) for i in range(2)]
        R_RD1 = [Res("RD1%d" % i) for i in range(2)]
        swa_its = [(nblk, kvh) for nblk in range(16) for kvh in range(4)]
        swa_st = {"e": 0, "p": 0}
        swa_pend = {}

        def swa_a(i):
            nblk, kvh = swa_its[i]
            j = nblk + 1
            rows = slice((kvh % 2) * 64, (kvh % 2) * 64 + 64)
            pts = []
            for kb in range(2):
                jk = j - 1 + kb
                sb_, sr_ = bank()
                mmg(sb_[:, :384], [(KT[rows, kvh // 2, jk * 128:(jk + 1) * 128],
                                    QT[rows, kvh // 2, nblk, :, :].rearrange("p g q -> p (g q)"))],
                    [R_KT, R_QT], sr_)
                s = swa_st["e"] % 8
                swa_st["e"] += 1
                act(ET[s][:], sb_[:, :384], AF.Exp, sr_, [R_ET[s]], scale=0.125)
                p = swa_st["p"] % 8
                swa_st["p"] += 1
                msk = ME0[:, kvh, :] if (nblk == 0 and kb == 0) else ME[:, kb, kvh, :]
                tt(PT1[p][:], ET[s][:], msk, ALU.mult, [R_ET[s], R_ME], [R_PT1[p]],
                   eng=os.environ.get("MK_SWA_MASK_ENG", "pool"))
                pts.append((p, jk))
            swa_pend[i] = pts

        def swa_b(i):
            nblk, kvh = swa_its[i]
            j = nblk + 1
            rc0, _ = tcols(j)
            rows = slice((kvh % 2) * 64, (kvh % 2) * 64 + 64)
            qc0 = (kvh // 2) * 3
            pts = swa_pend.pop(i)
            ob, orr = bank()
            vc0 = (kvh // 2) * 128
            mmg(ob[:, :384], [(VT[:, jk, vc0:vc0 + 128], PT1[p][:]) for (p, jk) in pts],
                [R_VT] + [R_PT1[p] for (p, _) in pts], orr)
            db, dr = bank()
            mmg(db[:, :384], [(ONB[:], PT1[p][:]) for (p, _) in pts] + [(ONB[:], SRB[:, kvh, :])],
                [R_ID, R_SR] + [R_PT1[p] for (p, _) in pts], dr)
            s2 = i % 2
            recip(RD1[s2][rows, :], db[rows, :384], dr, [R_RD1[s2]])
            tt(RA[rows, qc0:qc0 + 3, rc0:rc0 + 128], ob[rows, :384].rearrange("p (g q) -> p g q", g=3),
               RD1[s2][rows, :].rearrange("p (g q) -> p g q", g=3), ALU.mult,
               orr + [R_RD1[s2]], [R_RA[j]])
        nun = len(swa_its) // 2
        swa_a(0)
        swa_a(1)
        for u in range(nun):
            if u + 1 < nun:
                swa_a(2 * u + 2)
                swa_a(2 * u + 3)
            swa_b(2 * u)
            swa_b(2 * u + 1)
        dump("swa", RA[:, 0, 160:TC], [R_RA[t] for t in range(1, NT)])
        hn_flush()
        S.barrier()
        if PROFILE_MARKS:
            S.mark(nc, "L1D_mem_out")
        WO1 = arv(14336, 8 * D).rearrange("p (k n) -> p k n", k=8)
        R_WO1 = Res("WO1")
        for i, (ha, hb_) in enumerate(QPAIR):
            S.dma("pool", WO1[0:64, i, :], sw_w_out[ha * 64:(ha + 1) * 64, :], writes=[R_WO1])
            S.dma("pool", WO1[64:128, i, :], sw_w_out[hb_ * 64:(hb_ + 1) * 64, :], writes=[R_WO1])
        S.dma("pool", WO1[:, 6:8, :], sw_w_out[768:1024, :].rearrange("(k p) n -> p k n", p=128), writes=[R_WO1])
        prepass(8)
        PT = [arv(i * 512, 512) for i in range(8)]
        RD = [arv(4096 + i * 1024, 512, F32) for i in range(2)]
        mem_attention(QM1, R_QM1, TB1, 160, (PT, R_PT, RD, R_RD))
        out_proj(WO1, R_WO1, list(range(1, NT)))
        dump("h1a", HT[:, 1, :], [R_HT[1]])
        dump("hf1a", HT[:, 1:NT, :].rearrange("p j d -> p (j d)"), R_HT[1:NT])
        hn_flush()
        S.barrier()
        if PROFILE_MARKS:
            S.mark(nc, "L1R_router")
        HN32 = [arv(29696 + i * 2048, 1024, F32) for i in range(2)]
        R_HN32 = [Res("HN32%d" % i) for i in range(2)]
        H32T = [arv(33792 + i * 2048, 1024, F32).rearrange("p (k c) -> p k c", k=8) for i in range(2)]
        R_H32T = [Res("H32T%d" % i) for i in range(2)]
        prepass(1000)
        reserved.add(7)
        LG = PS[:, 7, 0:128].rearrange("p (j e) -> p j e", j=16)
        R_LG = banks[7]
        g_b128 = CP[:, C_GF1:C_GF1 + 8].unsqueeze(2).to_broadcast([128, 8, 128])
        rs_col = {}
        for j in range(1, NT):
            k = ss_state["n"] % 32
            ss_state["n"] += 1
            rs_col[j] = k
            s = j % 2
            act(JUNK[:], HT[:, j, :], AF.Square, [R_HT[j]], [R_JUNK, R_SS], accum_out=SS[:, 0, k:k + 1])
            act(SS[:, 1, k:k + 1], SS[:, 0, k:k + 1], AF.Sqrt, [R_SS], [R_SS], scale=1.0 / D, bias=RMS_EPS)
            recip(SS[:, 2, k:k + 1], SS[:, 1, k:k + 1], [R_SS], [R_SS])
            ts(HN32[s], HT[:, j, :], SS[:, 2, k:k + 1], ALU.mult, [R_HT[j], R_SS], [R_HN32[s]])
            pb, pr = bank(pair=True)
            pv = pb.rearrange("p b (k c) -> p (b k) c", k=4)

            def fn(e, pv=pv, s=s):
                ins = None
                for kc in range(8):
                    ins = e.transpose(out=pv[:, kc, :], in_=HN32[s][:, kc * 128:(kc + 1) * 128], identity=IDF[:])
                return ins
            S.op("pe", fn, [R_HN32[s], R_ID], pr)
            tt(H32T[s], pv, g_b128, ALU.mult, pr + [R_CP], [R_H32T[s]])
            mmg(LG[:, j - 1, :], [(H32T[s][:, kc, :], WR[:, kc, :]) for kc in range(8)],
                [R_H32T[s], R_WR], [R_LG])
        ga_state = {"o": 0}

        def ga(n, dt=F32):
            o = ga_state["o"]
            ga_state["o"] += 2 * n if dt != BF16 else n
            if dt == BF16:
                return arv(o, n)
            if dt == F32:
                return arv(o, n, F32)
            return AR[:, o:o + 2 * n].bitcast(dt)

        def v168(ap):
            return ap.rearrange("p (j e) -> p j e", j=16)

        def b168(ap16):
            return ap16.unsqueeze(2).to_broadcast([128, 16, 8])
        R_G = Res("GATE")
        gL, gEQ1, gL2, gEQ2, gSEL, gCUM, gTMP, gSLOT = [ga(128) for _ in range(8)]
        gV1, gV2, gE, gS1, gS2, gET = [ga(16) for _ in range(6)]
        W12 = sb("W12", [128, 32], F32)
        gW1, gW2 = W12[:, 0:16], W12[:, 16:32]
        gNE, gTL, gOE, gOF = [ga(8) for _ in range(4)]
        SELB, CUMB = ga(128, BF16), ga(128, BF16)
        SI = sb("SIT", [128, 32], I32)
        ETI = ga(16, I32)
        LTB = ga(128, BF16)
        R_C2 = Res("C2")
        C2F = ga(296)
        S.dma("sp", C2F, cst2[:, :], writes=[R_C2])
        copy("act", LTB, C2F[:, 0:128], [R_C2], [R_C2])
        RG, WG_ = [R_G], [R_G]
        copy("act", gL, LG.rearrange("p j e -> p (j e)"), [R_LG], WG_)
        S.op("dve", lambda e: e.tensor_reduce(out=gV1, in_=v168(gL), op=ALU.max, axis=AX.X), RG, WG_)
        tt(v168(gEQ1), v168(gL), b168(gV1), ALU.is_equal, RG, WG_)
        stt(gL2, gEQ1, -1e30, gL, ALU.mult, ALU.add, RG, WG_)
        S.op("dve", lambda e: e.tensor_reduce(out=gV2, in_=v168(gL2), op=ALU.max, axis=AX.X), RG, WG_)
        tt(v168(gEQ2), v168(gL2), b168(gV2), ALU.is_equal, RG, WG_)
        tt(gE, gV2, gV1, ALU.subtract, RG, WG_)
        act(gE, gE, AF.Exp, RG, WG_)
        ts(gW1, gE, 1.0, ALU.add, RG, WG_)
        recip(gW1, gW1, RG, WG_)
        tt(gW2, gE, gW1, ALU.mult, RG, WG_)
        tt(gSEL, gEQ1, gEQ2, ALU.add, RG, WG_)
        copy("dve", SELB, gSEL, RG, WG_)
        S.op("dve", lambda e: e.memset(gCUM[:, 0:8], 0.0), [], WG_)
        for j in range(1, 16):
            tt(gCUM[:, j * 8:(j + 1) * 8], gCUM[:, (j - 1) * 8:j * 8], gSEL[:, (j - 1) * 8:j * 8], ALU.add, RG, WG_)
        copy("dve", CUMB, gCUM, RG, WG_)
        rkb, rkr = bank()
        mmg(rkb[:, 0:128], [(LTB, SELB), (ONB[:], CUMB)], RG + [R_C2, R_ID], rkr)
        tob, tor = bank()
        mmg(tob[:, 0:128], [(ONB[:], SELB)], RG + [R_ID], tor)
        S.op("dve", lambda e: e.tensor_reduce(out=gNE, in_=tob[:, 0:128].rearrange("p (j e) -> p e j", j=16),
                                              op=ALU.add, axis=AX.X), tor, WG_)
        ts(gTL, gNE, 0.0, ALU.is_gt, RG, WG_)
        for thr in (512.0, 1024.0, 1536.0):
            stt(gTL, gNE, thr, gTL, ALU.is_gt, ALU.add, RG, WG_)
        copy("dve", gOE[:, 0:1], gTL[:, 0:1], RG, WG_)
        for ex in range(1, NE):
            tt(gOE[:, ex:ex + 1], gOE[:, ex - 1:ex], gTL[:, ex:ex + 1], ALU.add, RG, WG_)
        tt(gOF, gOE, gTL, ALU.subtract, RG, WG_)
        ts(gOF, gOF, 512.0, ALU.mult, RG, WG_)
        tt(v168(gSLOT), v168(rkb[:, 0:128]), gOF.unsqueeze(1).to_broadcast([128, 16, 8]), ALU.add, rkr + RG, WG_)
        tt(gTMP, gEQ1, gSLOT, ALU.mult, RG, WG_)
        S.op("dve", lambda e: e.tensor_reduce(out=gS1, in_=v168(gTMP), op=ALU.add, axis=AX.X), RG, WG_)
        copy("dve", SI[:, 0:16], gS1, RG, WG_)
        tt(gTMP, gEQ2, gSLOT, ALU.mult, RG, WG_)
        S.op("dve", lambda e: e.tensor_reduce(out=gS2, in_=v168(gTMP), op=ALU.add, axis=AX.X), RG, WG_)
        copy("dve", SI[:, 16:32], gS2, RG, WG_)
        tt(v168(gTMP), gOE.unsqueeze(1).to_broadcast([128, 16, 8]), v168(C2F[:, 128:256]), ALU.is_le,
           RG + [R_C2], WG_)
        S.op("dve", lambda e: e.tensor_reduce(out=gET, in_=v168(gTMP), op=ALU.add, axis=AX.X), RG, WG_)
        ts(gET, gET, 7.0, ALU.min, RG, WG_)
        copy("dve", ETI, gET, RG, WG_)
        dump("gw1", gW1, RG)
        dump("gs1", gS1, RG)
        dump("gs2", gS2, RG)
        dump("get", gET, RG)
        reserved.discard(7)
        TTI = ga(1, I32)
        copy("dve", TTI, gOE[:, 7:8], RG, WG_)
        eng_regs = {}

        def mk_setup(eng):
            def fn(e, eng=eng):
                rt = e.alloc_register()
                rk = e.alloc_register()
                e.reg_load(rt, TTI[0:1, 0:1])
                eng_regs[eng] = (rt, rk)
            return fn
        for eng in ENGS:
            S.raw(eng, mk_setup(eng), RG)
        IDXG = sb("IDXG", [128, 112], I32)
        IDXD = sb("IDXD", [128, 448], I32)
        gIG, gID = ga(112), ga(448)
        stt(gIG.rearrange("p (t g) -> p t g", t=16), gET.unsqueeze(2).to_broadcast([128, 16, 7]), 896.0,
            C2F[:, 256:263].unsqueeze(1).to_broadcast([128, 16, 7]), ALU.mult, ALU.add, RG + [R_C2], WG_)
        copy("dve", IDXG[:], gIG, RG, WG_)
        stt(gID.rearrange("p (t g) -> p t g", t=16), gET.unsqueeze(2).to_broadcast([128, 16, 28]), 3584.0,
            C2F[:, 263:291].unsqueeze(1).to_broadcast([128, 16, 28]), ALU.mult, ALU.add, RG + [R_C2], WG_)
        copy("dve", IDXD[:], gID, RG, WG_)
        if PROFILE_MARKS:
            S.mark(nc, "L1S_scatter")
        for j in range(1, NT):
            s = j % 2
            k = rs_col[j]
            ts(HNB[s][:], HT[:, j, :], SS[:, 2, k:k + 1], ALU.mult, [R_HT[j], R_SS], [R_HNB[s]])
            for kk in range(2):
                col = kk * 16 + j - 1
                S.dma("pool", None, None, reads=[R_HNB[s]] + RG, writes=[R_XS], owner=R_XS,
                      fn=lambda e, s=s, col=col: e.indirect_dma_start(
                          out=XS[:, :], out_offset=bass.IndirectOffsetOnAxis(ap=SI[:, col:col + 1], axis=0),
                          in_=HNB[s][:], in_offset=None))
        if PROFILE_MARKS:
            S.mark(nc, "L1T_tiles")
        NTILE = int(os.environ.get("MK_NTILE", "15"))
        WB3 = []
        R_WB3 = []
        for s in range(2):
            o = 8192 + s * 12288
            WB3.append((arv(o, 4096).rearrange("p (k n) -> p k n", k=8),
                        arv(o + 4096, 4096).rearrange("p (k n) -> p k n", k=8),
                        arv(o + 8192, 4096).rearrange("p (f n) -> p f n", f=4)))
            R_WB3.append([Res("WB3%d_%d" % (s, i)) for i in range(3)])
        ACT3 = [arv(32768 + a * 2048, 2048).rearrange("p (f n) -> p f n", f=4) for a in range(2)]
        R_ACT3 = [[Res("ACT3%d_%d" % (a, f)) for f in range(4)] for a in range(2)]
        SIL3 = [arv(36864 + i * 512, 512) for i in range(2)]
        R_SIL3 = [Res("SIL3%d" % i) for i in range(2)]
        YACC = [arv(c * 2048, 1024, F32) for c in range(4)]
        R_YACC = [Res("YACC%d" % c) for c in range(4)]
        RAf = RA[:].rearrange("p k t -> p (k t)")
        XT = [RAf[:, i * 4096:(i + 1) * 4096].rearrange("p (c d) -> p c d", c=4) for i in range(2)]
        R_XT = [Res("XT%d" % i) for i in range(2)]
        XF = [RAf[:, 8192 + i * 4096:8192 + (i + 1) * 4096].rearrange("p (k n) -> p k n", k=8) for i in range(2)]
        R_XF = [Res("XF%d" % i) for i in range(2)]
        R_YS = Res("YS")
        hn_flush()
        S.barrier()

        def prep_tile(tau):
            s = tau % 2
            S.dma("sp", XT[s], XS[512 * tau:512 * (tau + 1), :].rearrange("(c p) d -> p c d", p=128),
                  reads=[R_XS], writes=[R_XT[s]])
            for c in range(4):
                pb, pr = bank()
                pv = pb.bitcast(BF16).rearrange("p (k c) -> p k c", k=8)

                def fn(e, pv=pv, s=s, c=c):
                    ins = None
                    for kc in range(8):
                        ins = e.transpose(out=pv[:, kc, :], in_=XT[s][:, c, kc * 128:(kc + 1) * 128], identity=IDB[:])
                    return ins
                S.op("pe", fn, [R_XT[s], R_ID], pr)
                tt(XF[s][:, :, c * 128:(c + 1) * 128], pv, g_b128, ALU.mult, pr + [R_CP], [R_XF[s]])

        steps = [(tau, g) for tau in range(NTILE) for g in range(7)]

        wdflat = sw_we_down.rearrange("e r f -> (e r) f")

        def load_w(si):
            tau, g = steps[si]
            s = si % 2
            o = 8192 + s * 12288
            col = tau * 7 + g
            for (dst, src, res_) in ((arv(o, 4096), WSG, R_WB3[s][0]), (arv(o + 4096, 4096), WSU, R_WB3[s][1])):
                S.dma("pool", None, None, reads=[R_WS] + RG, writes=[res_],
                      fn=lambda e, dst=dst, src=src, col=col: e.indirect_dma_start(
                          out=dst, out_offset=None, in_=src[:, :],
                          in_offset=bass.IndirectOffsetOnAxis(ap=IDXG[:, col:col + 1], axis=0)))
            for fc in range(4):
                S.dma("pool", None, None, reads=RG, writes=[R_WB3[s][2]],
                      fn=lambda e, s=s, fc=fc, col=col: e.indirect_dma_start(
                          out=WB3[s][2][:, fc, :], out_offset=None, in_=wdflat[:, :],
                          in_offset=bass.IndirectOffsetOnAxis(ap=IDXD[:, col * 4 + fc:col * 4 + fc + 1], axis=0)))

        def gu3(si):
            tau, g = steps[si]
            s, a, x = si % 2, si % 2, tau % 2
            wg, wu, wd = WB3[s]
            for fc in range(4):
                gb, gr = bank()
                mmg(gb[:, :], [(wg[:, kc, fc * 128:(fc + 1) * 128], XF[x][:, kc, :]) for kc in range(8)],
                    [R_XF[x], R_WB3[s][0]], gr)
                ub, ur = bank()
                mmg(ub[:, :], [(wu[:, kc, fc * 128:(fc + 1) * 128], XF[x][:, kc, :]) for kc in range(8)],
                    [R_XF[x], R_WB3[s][1]], ur)
                sl = fc % 2
                act(SIL3[sl][:], gb[:, :], AF.Silu, gr, [R_SIL3[sl]])
                tt(ACT3[a][:, fc, :], ub[:, :], SIL3[sl][:], ALU.mult, ur + [R_SIL3[sl]], [R_ACT3[a][fc]])

        def down3(si):
            tau, g = steps[si]
            s, a = si % 2, si % 2
            wg, wu, wd = WB3[s]
            for c in range(4):
                yb, yr = bank(pair=True)
                for half in range(2):
                    mmg(yb[:, half, :], [(ACT3[a][:, fc, c * 128:(c + 1) * 128], wd[:, fc, half * 512:(half + 1) * 512])
                                         for fc in range(4)], R_ACT3[a] + [R_WB3[s][2]], [yr[half]])
                yv = yb.rearrange("p b c -> p (b c)")
                if g == 0:
                    copy("act", YACC[c], yv, yr, [R_YACC[c]])
                else:
                    tt(YACC[c], yv, YACC[c], ALU.add, yr + [R_YACC[c]], [R_YACC[c]])
            if g == 6:
                S.dma("sp", YS[512 * tau:512 * (tau + 1), :].rearrange("(c p) d -> p c d", p=128),
                      AR[:, 0:8192].bitcast(F32).rearrange("p (c d) -> p c d", c=4),
                      reads=R_YACC, writes=[R_YS], owner=R_YS)

        CT0 = int(os.environ.get("MK_CT0", "8"))

        loaded, prepped = set(), set()

        def ld(si):
            if si < len(steps) and si not in loaded:
                loaded.add(si)
                load_w(si)

        def pt(tau):
            if tau < NTILE and tau not in prepped:
                prepped.add(tau)
                prep_tile(tau)

        def run_steps(si0, si1):
            pt(steps[si0][0])
            ld(si0)
            ld(si0 + 1)
            for si in range(si0, si1):
                tau, g = steps[si]
                gu3(si)
                if si > si0:
                    down3(si - 1)
                    ld(si + 1)
                if g == 2:
                    pt(tau + 1)
            down3(si1 - 1)
            ld(si1)
            ld(si1 + 1)
        nuncond = min(CT0, NTILE)
        run_steps(0, nuncond * 7)
        for tau in range(nuncond, NTILE):
            S.region_begin(tau)
            run_steps(tau * 7, (tau + 1) * 7)
            S.region_end()
        hn_flush()
        S.barrier()
        if PROFILE_MARKS:
            S.mark(nc, "L1Z_combine")
        G1 = [arv(8192 + i * 2048, 1024, F32) for i in range(2)]
        G2 = [arv(12288 + i * 2048, 1024, F32) for i in range(2)]
        R_G1 = [Res("G1%d" % i) for i in range(2)]
        R_G2 = [Res("G2%d" % i) for i in range(2)]
        R_OUT = Res("OUT")
        for j in range(1, NT):
            s = j % 2
            for (GB, RGB, base, wv) in ((G1, R_G1, 0, gW1), (G2, R_G2, 16, gW2)):
                col = base + j - 1
                S.dma("pool", None, None, reads=[R_YS] + RG, writes=[RGB[s]],
                      fn=lambda e, GB=GB, s=s, col=col: e.indirect_dma_start(
                          out=GB[s][:], out_offset=None, in_=YS[:, :],
                          in_offset=bass.IndirectOffsetOnAxis(ap=SI[:, col:col + 1], axis=0)))
                stt(HT[:, j, :], GB[s], wv[:, j - 1:j], HT[:, j, :], ALU.mult, ALU.add,
                    [RGB[s], R_HT[j]] + RG, [R_HT[j]])
            S.dma("sp", out[(j - 1) * 128:j * 128, :], HT[:, j, :], reads=[R_HT[j]], owner=R_OUT)
        S.final_wait("sp")

        block = es.enter_context(nc.Block())

        @block.tensor
        def _(e):
            S.replay("pe", e, eng_regs)

        @block.scalar
        def _(e):
            S.replay("act", e, eng_regs)

        @block.vector
        def _(e):
            S.replay("dve", e, eng_regs)

        @block.gpsimd
        def _(e):
            S.replay("pool", e, eng_regs)

        @block.sync
        def _(e):
            S.replay("sp", e, eng_regs)
    return nc


def _consts():
    ident = np.eye(128, dtype=np.float32)
    bd = np.zeros((128, 128), np.float32)
    bd[:64, :64] = 1.0 / 64
    bd[64:, 64:] = 1.0 / 64
    slopes = np.exp2(-8.0 * (np.arange(12, dtype=np.float32) + 1.0) / 12).astype(np.float32)
    k = np.arange(128)[:, None].astype(np.float32)
    q = np.arange(128)[None, :].astype(np.float32)
    me = np.zeros((128, 2, 4, 3, 128), np.float32)
    for kb in range(2):
        dist = q + 128.0 - k if kb == 0 else q - k
        valid = (dist >= 0) & (dist < 128)
        for kvh in range(4):
            for g in range(3):
                s = slopes[kvh * 3 + g]
                me[:, kb, kvh, g, :] = np.where(valid, np.exp(-s * dist), 0.0)
    c2 = np.zeros((128, 296), np.float32)
    pidx = np.arange(128, dtype=np.float32)[:, None]
    c2[:, 256:263] = np.arange(7, dtype=np.float32)[None, :] * 128 + pidx
    c2[:, 263:291] = (np.arange(7, dtype=np.float32)[:, None] * 512
                      + np.arange(4, dtype=np.float32)[None, :] * 128).reshape(1, 28) + pidx
    c2[:, 0:128] = (np.arange(128)[:, None] < np.arange(128)[None, :]).astype(np.float32)
    c2[:, 128:256] = np.repeat(np.arange(16, dtype=np.float32), 8)[None, :]
    return ident, bd, me.reshape(128, 3072), c2


def _cpack(inp, flag):
    cp = np.zeros((128, NCP), np.float32)

    def pk(v):
        return np.asarray(v, np.float32).reshape(8, 128).T
    cp[:, C_GA0:C_GA0 + 8] = pk(inp["cv_attn_norm_g"][0])
    cp[:, C_GF0:C_GF0 + 8] = pk(inp["cv_ffn_norm_g"][0])
    cp[:, C_GA1:C_GA1 + 8] = pk(inp["sw_attn_norm_g"][0])
    cp[:, C_GF1:C_GF1 + 8] = pk(inp["sw_ffn_norm_g"][0])
    cp[:, C_GM:C_GM + 8] = pk(inp["mem_norm_g"])
    bg = np.asarray(inp["cv_b_glu"][0], np.float32)
    cp[:, C_BA:C_BA + 6] = bg[:768].reshape(6, 128).T
    cp[:, C_BG:C_BG + 6] = bg[768:].reshape(6, 128).T
    dw = np.asarray(inp["cv_dw_w"][0], np.float32)
    cp[:, C_DW:C_DW + 186] = dw.reshape(31, 6, 128).transpose(2, 1, 0).reshape(128, 186)
    cp[:, C_DWB:C_DWB + 6] = np.asarray(inp["cv_dw_b"][0], np.float32).reshape(6, 128).T
    cp[:, C_LNG:C_LNG + 6] = np.asarray(inp["cv_ln_g"][0], np.float32).reshape(6, 128).T
    cp[:, C_LNB:C_LNB + 6] = np.asarray(inp["cv_ln_b"][0], np.float32).reshape(6, 128).T

    def h2(v):
        v = np.asarray(v, np.float32).reshape(64)
        return np.concatenate([v, v])
    cp[:, C_MQ0] = h2(inp["cv_memq_norm_g"][0])
    cp[:, C_MQ1] = h2(inp["sw_memq_norm_g"][0])
    cp[:, C_MK] = h2(inp["mem_k_norm_g"])
    cp[:, C_QG] = h2(inp["sw_q_norm_g"][0])
    cp[:, C_KG] = h2(inp["sw_k_norm_g"][0])
    cp[:, C_FLAG] = flag
    return cp


def make_in_maps(inp):
    f = lambda a: np.ascontiguousarray(np.asarray(a, dtype=np.float32))
    ident, bd, me, c2 = _consts()
    x = f(inp["x"])
    memf = f(inp["mem"])
    sinks = f(inp["sw_sinks"][0])
    sinks_rep = np.ascontiguousarray(np.repeat(sinks, 128)[None, :])
    shared = dict(
        ident=ident, bdm=bd, maskexp=me, cst2=c2, sinks_rep=sinks_rep,
        w_mem_kv=f(inp["w_mem_kv"]), cv_w_in=f(inp["cv_w_in"][0]), cv_w_out=f(inp["cv_w_out"][0]),
        cv_w_gate=f(inp["cv_w_gate"][0]), cv_w_up=f(inp["cv_w_up"][0]), cv_w_down=f(inp["cv_w_down"][0]),
        sw_w_in=f(inp["sw_w_in"][0]), sw_w_out=f(inp["sw_w_out"][0]), sw_router=f(inp["sw_router"][0]),
        sw_we_gate=f(inp["sw_we_gate"][0]), sw_we_up=f(inp["sw_we_up"][0]), sw_we_down=f(inp["sw_we_down"][0]),
    )
    maps = []
    for c in range(NCORES):
        b, qtr = divmod(c, 4)
        xin = np.zeros((TC, D), np.float32)
        t0 = qtr * TOK
        if qtr == 0:
            xin[160:] = x[b, 0:TOK]
        else:
            xin[:] = x[b, t0 - 160:t0 + TOK]
        m = dict(shared)
        m["xin"] = xin
        m["mem"] = memf[b]
        m["cpack"] = _cpack(inp, 0.0 if qtr == 0 else 1.0)
        maps.append(m)
    return maps


def kernel(**inputs):
    nc = build_program()
    maps = make_in_maps(inputs)
    res = run_bass_kernel_spmd(nc, maps, core_ids=list(range(NCORES)))
    outs = [np.asarray(r["out"], np.float32) for r in res.results]
    full = np.concatenate(outs, axis=0).reshape(2, SEQ, D)
    return full
```

```python
import os
from contextlib import ExitStack

import numpy as np
import concourse.bass as bass
import concourse.mybir as mybir
from concourse.bass_utils import run_bass_kernel_spmd

F32 = mybir.dt.float32
BF16 = mybir.dt.bfloat16
I32 = mybir.dt.int32
AF = mybir.ActivationFunctionType
ALU = mybir.AluOpType
AX = mybir.AxisListType

NCORES = 8
D = 1024
SEQ = 8192
TOK = 2048
NT = 17
TC = 2208
HD = 64
DFF0 = 2816
DFFE = 3584
NE = 8
CONV_W = 31
CONV_CH = 768
RMS_EPS = 1e-6
LN_EPS = 1e-5

C_GA0, C_GF0, C_GA1, C_GF1, C_GM = 0, 8, 16, 24, 32
C_BA, C_BG = 40, 46
C_DW = 52
C_DWB, C_LNG, C_LNB = 238, 244, 250
C_MQ0, C_MQ1, C_MK, C_QG, C_KG, C_FLAG = 256, 257, 258, 259, 260, 261
NCP = 264

ENGS = ("pe", "act", "dve", "pool", "sp")
PROFILE_MARKS = bool(os.environ.get("MK_MARKS"))


class Res:
    __slots__ = ("name", "w", "r", "dsem", "dcnt")

    def __init__(self, name):
        self.name = name
        self.w = None
        self.r = []
        self.dsem = None
        self.dcnt = 0


class Sched:
    def __init__(self, esem, dsems):
        self.esem = dict(esem)
        self.free_dsems = list(dsems)
        self.cnt = {e: 0 for e in ENGS}
        self.seen = {e: {} for e in ENGS}
        self.q = {e: [] for e in ENGS}
        self.dlast = {}
        self.nbank = 0
        self._reg = None

    def _deps(self, eng, reads, writes, skip_key=None):
        deps = {}

        def add(ev, same_ok):
            if ev is None:
                return
            k, v = ev
            if (k == eng or k == skip_key) and not same_ok:
                return
            if deps.get(k, 0) < v:
                deps[k] = v
        for r in reads:
            add(r.w, True)
        for w in writes:
            add(w.w, False)
            for ev in w.r:
                add(ev, False)
        waits = []
        for k, v in deps.items():
            if self.seen[eng].get(k, 0) < v:
                self.seen[eng][k] = v
                waits.append((self.esem[k], v))
        return waits

    def _commit(self, ev, reads, writes):
        for r in reads:
            r.r.append(ev)
        for w in writes:
            w.w = ev
            w.r = []

    def op(self, eng, fn, reads=(), writes=()):
        waits = self._deps(eng, reads, writes)
        self.cnt[eng] += 1
        ev = (eng, self.cnt[eng])
        sem = self.esem[eng]

        def run(e, waits=waits, fn=fn, sem=sem):
            for s, v in waits:
                e.wait_ge(s, v)
            fn(e).then_inc(sem, 1)
        self.q[eng].append(run)
        self._commit(ev, reads, writes)
        return ev

    def dma(self, q, out, in_, reads=(), writes=(), owner=None, fn=None, **kw):
        owner = owner or (writes[0] if writes else reads[0])
        if owner.dsem is None:
            owner.dsem = self.free_dsems.pop()
            self.esem[("d", id(owner))] = owner.dsem
        key = ("d", id(owner))
        waits = self._deps(q, reads, writes, skip_key=key)
        owner.dcnt += 16
        ev = (key, owner.dcnt)
        self.dlast[key] = owner.dcnt
        sem = owner.dsem
        if self._reg is not None:
            self._reg["dma"][q][key] = self._reg["dma"][q].get(key, 0) + 16

        def run(e, waits=waits, sem=sem, out=out, in_=in_, kw=kw, fn=fn):
            for s, v in waits:
                e.wait_ge(s, v)
            if fn is not None:
                fn(e).then_inc(sem, 16)
            else:
                e.dma_start(out=out, in_=in_, **kw).then_inc(sem, 16)
        self.q[q].append(run)
        self._commit(ev, reads, writes)
        return ev

    def raw(self, eng, fn, reads=()):
        waits = self._deps(eng, reads, ())

        def run(e, waits=waits, fn=fn):
            for s, v in waits:
                e.wait_ge(s, v)
            fn(e)
        self.q[eng].append(run)

    def barrier(self, dma=True):
        evs = [(e, self.cnt[e]) for e in ENGS if self.cnt[e] > 0]
        if dma:
            evs += list(self.dlast.items())
        for eng in ENGS:
            waits = []
            for k, v in evs:
                if k == eng:
                    continue
                if self.seen[eng].get(k, 0) < v:
                    self.seen[eng][k] = v
                    waits.append((self.esem[k], v))
            if waits:
                def run(e, waits=waits):
                    for s, v in waits:
                        e.wait_ge(s, v)
                self.q[eng].append(run)

    def region_begin(self, tag):
        self._reg = dict(tag=tag, cnt0=dict(self.cnt), dma={e: {} for e in ENGS},
                         seen0={e: dict(v) for e, v in self.seen.items()})
        for eng in ENGS:
            self.q[eng].append(("begin", tag))

    def region_end(self):
        r = self._reg
        for eng in ENGS:
            self.q[eng].append(("end", dict(own=self.cnt[eng] - r["cnt0"][eng], dma=dict(r["dma"][eng]))))
        self.seen = r["seen0"]
        self._reg = None

    def replay(self, eng, e, regs):
        items = self.q[eng]
        i = 0
        while i < len(items):
            it = items[i]
            if isinstance(it, tuple) and it[0] == "begin":
                j = i + 1
                body = []
                while not (isinstance(items[j], tuple) and items[j][0] == "end"):
                    body.append(items[j])
                    j += 1
                comp = items[j][1]
                with e.If_eq(regs[eng], it[1]):
                    for f in body:
                        f(e)
                with e.Else():
                    n = comp["own"]
                    while n > 0:
                        e.sem_inc(self.esem[eng], min(n, 8))
                        n -= min(n, 8)
                    for key, v in comp["dma"].items():
                        for _ in range(v // 16):
                            e.sem_inc(self.esem[key], 16)
                i = j + 1
            else:
                it(e)
                i += 1

    def mark(self, nc, name):
        st = self.__dict__.setdefault("_scope", {})

        for eng in ENGS:
            def run(e, eng=eng, name=name):
                if eng in st:
                    nc.leave_named_scope(st[eng][0], st[eng][1], False)
                sid, _ = nc.enter_named_scope(name, False)
                st[eng] = (name, sid)
            self.q[eng].append(run)

    def final_wait(self, eng):
        waits = []
        for k, v in self.dlast.items():
            if self.seen[eng].get(k, 0) < v:
                self.seen[eng][k] = v
                waits.append((self.esem[k], v))

        def run(e, waits=waits):
            for s, v in waits:
                e.wait_ge(s, v)
        self.q[eng].append(run)


def build_program(debug=()):
    nc = bass.Bass("TRN2", target_bir_lowering=False)

    def din(name, shape):
        return nc.dram_tensor(name, list(shape), F32, kind="ExternalInput").ap()

    xin = din("xin", [TC, D])
    mem = din("mem", [256, D])
    cpack = din("cpack", [128, NCP])
    ident = din("ident", [128, 128])
    bdm = din("bdm", [128, 128])
    maskexp = din("maskexp", [128, 3072])
    sinks_rep = din("sinks_rep", [1, 1536])
    w_mem_kv = din("w_mem_kv", [D, 512])
    cv_w_in = din("cv_w_in", [D, 1792])
    cv_w_out = din("cv_w_out", [D, D])
    cv_w_gate = din("cv_w_gate", [D, DFF0])
    cv_w_up = din("cv_w_up", [D, DFF0])
    cv_w_down = din("cv_w_down", [DFF0, D])
    sw_w_in = din("sw_w_in", [D, 1536])
    sw_w_out = din("sw_w_out", [D, D])
    sw_router = din("sw_router", [D, NE])
    sw_we_gate = din("sw_we_gate", [NE, D, DFFE])
    sw_we_up = din("sw_we_up", [NE, D, DFFE])
    sw_we_down = din("sw_we_down", [NE, DFFE, D])
    cst2 = din("cst2", [128, 296])
    WSG = nc.dram_tensor("wsg_scr", [NE * 7 * 128, 4096], BF16, kind="Internal").ap()
    WSU = nc.dram_tensor("wsu_scr", [NE * 7 * 128, 4096], BF16, kind="Internal").ap()
    XS = nc.dram_tensor("xs_scr", [8192, D], BF16, kind="Internal").ap()
    YS = nc.dram_tensor("ys_scr", [8192, D], F32, kind="Internal").ap()
    out = nc.dram_tensor("out", [TOK, D], F32, kind="ExternalOutput").ap()
    dbg_out = {}
    for name, shape in debug:
        dbg_out[name] = nc.dram_tensor("dbg_" + name, list(shape), F32, kind="ExternalOutput").ap()

    es = ExitStack()
    with es:
        def sb(name, shape, dt):
            return es.enter_context(nc.sbuf_tensor(name, list(shape), dt))

        HT = sb("HT", [128, NT, D], F32)
        RA = sb("RA", [128, 8, TC], BF16)
        AR = sb("AR", [128, 40960], BF16)
        CP = sb("CP", [128, NCP], F32)
        IDF = sb("IDF", [128, 128], F32)
        IDB = sb("IDB", [128, 128], BF16)
        BDB = sb("BDB", [128, 128], BF16)
        ONB = sb("ONB", [128, 128], BF16)
        ME = sb("ME", [128, 2, 4, 384], BF16)
        ME0 = sb("ME0", [128, 4, 384], BF16)
        SRB = sb("SRB", [128, 4, 384], BF16)
        MKT = sb("MKT", [128, 2, 256], BF16)
        MV = sb("MV", [128, 2, 256], BF16)
        SS = sb("SS", [128, 3, 32], F32)
        GT = sb("GT", [128, 16, 8], F32)
        WR = sb("WR", [128, 8, NE], F32)
        PS = es.enter_context(nc.psum_tensor("PS", [128, 8, 512], F32))

        esem = {e: es.enter_context(nc.semaphore("sem_" + e)) for e in ENGS}
        dsems = [es.enter_context(nc.semaphore("dsem%d" % i)) for i in range(72)]
        S = Sched(esem, dsems)

        banks = [Res("bank%d" % i) for i in range(8)]
        reserved = set()

        def bank(pair=False):
            while True:
                b = S.nbank % 8
                if pair and (b % 2 == 1 or (b + 1) in reserved):
                    S.nbank += 1
                    continue
                if b in reserved:
                    S.nbank += 1
                    continue
                break
            if pair:
                S.nbank += 2
                return PS[:, b:b + 2, :], [banks[b], banks[b + 1]]
            S.nbank += 1
            return PS[:, b, :], [banks[b]]

        def mmg(out_ap, pairs, reads, writes):
            def fn(e, out_ap=out_ap, pairs=pairs):
                n = len(pairs)
                ins = None
                for i, (l, r) in enumerate(pairs):
                    ins = e.matmul(out_ap, l, r, start=(i == 0), stop=(i == n - 1))
                return ins
            S.op("pe", fn, reads, writes)

        def act(out_ap, in_ap, func, reads, writes, **kw):
            S.op("act", lambda e: e.activation(out=out_ap, in_=in_ap, func=func, **kw), reads, writes)

        def tt(out_ap, in0, in1, op, reads, writes, eng="dve"):
            S.op(eng, lambda e: e.tensor_tensor(out=out_ap, in0=in0, in1=in1, op=op), reads, writes)

        def ts(out_ap, in0, s1, op0, reads, writes, s2=None, op1=None, eng="dve"):
            if op1 is None:
                S.op(eng, lambda e: e.tensor_scalar(out=out_ap, in0=in0, scalar1=s1, scalar2=None, op0=op0),
                     reads, writes)
            else:
                S.op(eng, lambda e: e.tensor_scalar(out=out_ap, in0=in0, scalar1=s1, scalar2=s2, op0=op0, op1=op1),
                     reads, writes)

        def stt(out_ap, in0, scalar, in1, op0, op1, reads, writes):
            S.op("dve", lambda e: e.scalar_tensor_tensor(out=out_ap, in0=in0, scalar=scalar, in1=in1,
                                                         op0=op0, op1=op1), reads, writes)

        def recip(out_ap, in_ap, reads, writes):
            S.op("dve", lambda e: e.reciprocal(out=out_ap, in_=in_ap), reads, writes)

        def copy(eng, out_ap, in_ap, reads, writes):
            if eng == "act":
                act(out_ap, in_ap, AF.Copy, reads, writes)
            else:
                S.op(eng, lambda e: e.tensor_copy(out=out_ap, in_=in_ap), reads, writes)

        def dump(name, ap, reads, view=None):
            if name in dbg_out:
                dst = dbg_out[name]
                if view:
                    dst = dst.rearrange(view[0], **view[1])
                S.dma("pool", dst, ap, reads=reads, writes=(), owner=R_dbg)

        R_dbg = Res("dbg")

        def cpc(c, n=1):
            return CP[:, c:c + n]

        def arv(off, n, dt=BF16):
            if dt == F32:
                return AR[:, off:off + 2 * n].bitcast(F32)
            return AR[:, off:off + n]

        R_HT = [Res("HT%d" % j) for j in range(NT)]
        R_RA = [Res("RA%d" % j) for j in range(NT + 1)]
        R_CP, R_ID, R_ME, R_SR = Res("CP"), Res("ID"), Res("ME"), Res("SR")
        R_MKV = Res("MKV")
        R_SS = Res("SS")

        def tcols(j):
            if j == 17:
                return 0, 32
            return 32 + 128 * j, 128

        S.dma("sp", CP[:], cpack[:, :], writes=[R_CP])
        S.dma("sp", IDF[:], ident[:, :], writes=[R_ID])
        R_BD = Res("BD")
        PB0 = 22016
        BDF = arv(PB0, 128, F32)
        S.dma("sp", BDF, bdm[:, :], writes=[R_BD])
        MEf = ME[:].rearrange("p a b c -> p (a b c)")
        S.dma("pool", MEf[:, 0:1536], maskexp[:, 0:1536], writes=[R_ME])
        S.dma("pool", MEf[:, 1536:3072], maskexp[:, 1536:3072], writes=[R_ME])
        SRF = arv(14336, 1536, F32)[0:1, :]
        S.dma("sp", SRF, sinks_rep[:, :], writes=[R_SR])
        WIN0 = arv(0, 8 * 1792).rearrange("p (k n) -> p k n", k=8)
        R_WIN0 = Res("WIN0")
        S.dma("pool", WIN0, cv_w_in.rearrange("(k p) n -> p k n", p=128), writes=[R_WIN0])
        R_XP = Res("XP")
        R_WS = Res("WS")
        pp_list = [(ws_, w_, ex, g_) for ex in range(NE) for g_ in range(7) for (ws_, w_) in ((WSG, sw_we_gate), (WSU, sw_we_up))]
        pp_state = {"i": 0}

        def prepass(n):
            for _ in range(n):
                if pp_state["i"] >= len(pp_list):
                    return
                ws_, w_, ex, g_ = pp_list[pp_state["i"]]
                pp_state["i"] += 1
                r0 = (ex * 7 + g_) * 128
                S.dma("pool", ws_[r0:r0 + 128, :].rearrange("p (k n) -> p k n", k=8),
                      w_[ex, :, g_ * 512:(g_ + 1) * 512].rearrange("(k p) n -> p k n", p=128),
                      writes=[R_WS], owner=R_WS)
        ZT = sb("ZT", [128, 2048], BF16)
        R_ZT = Res("ZT")
        R_XS = Res("XS")
        S.op("dve", lambda e: e.memset(ZT[:], 0.0), [], [R_ZT])
        XP = arv(PB0 + 256, D, F32)
        S.dma("sp", XP[0:32, :], xin[0:32, :], writes=[R_XP])
        R_MEM = Res("MEM")
        MEMT = arv(PB0 + 2304, 2 * D, F32).rearrange("p (t d) -> p t d", t=2)
        S.dma("sp", MEMT, mem.rearrange("(t p) d -> p t d", p=128), writes=[R_MEM])
        R_WKV = Res("WKV")
        WKV = arv(PB0 + 6400, 8 * 512).rearrange("p (k n) -> p k n", k=8)
        S.dma("pool", WKV, w_mem_kv.rearrange("(k p) n -> p k n", p=128), writes=[R_WKV])
        for j0, j1 in ((0, 2), (2, 5), (5, 9), (9, 13), (13, 17)):
            S.dma("sp", HT[:, j0:j1, :],
                  xin[32 + 128 * j0:32 + 128 * j1, :].rearrange("(j p) d -> p j d", p=128),
                  writes=R_HT[j0:j1])
        R_WR = Res("WR")
        S.dma("sp", WR[:], sw_router.rearrange("(k p) n -> p k n", p=128), writes=[R_WR])
        XSz = XS.rearrange("(a p r) d -> a p (r d)", p=128, r=2)
        for a_ in range(32):
            S.dma("sp", XSz[a_], ZT[:], reads=[R_ZT], writes=[R_XS], owner=R_XS)
        prepass(8)

        copy("act", IDB[:], IDF[:], [R_ID], [R_ID])
        copy("act", BDB[:], BDF, [R_BD], [R_BD])
        S.op("dve", lambda e: e.memset(ONB[:], 1.0), [], [R_ID])
        S.op("dve", lambda e: e.memset(SRB[:], 0.0), [], [R_SR])
        act(SRB[0:1].rearrange("p a b -> p (a b)"), SRF, AF.Exp, [R_SR], [R_SR])
        ts(ME0[:], ME[:, 0], cpc(C_FLAG), ALU.mult, [R_ME, R_CP], [R_ME])

        A_TAIL = 37888
        HNB = [arv(A_TAIL + i * 1024, 1024) for i in range(2)]
        R_HNB = [Res("HNB%d" % i) for i in range(2)]
        JUNK = arv(A_TAIL + 2048, 1024)
        R_JUNK = Res("JUNK")
        ss_state = {"n": 0}

        def norm_transpose(j, gcol, src_ap=None, src_res=None, npart=128, fp32_path=None):
            k = ss_state["n"] % 32
            ss_state["n"] += 1
            if src_ap is None:
                src_ap, src_res = HT[:, j, :], R_HT[j]
            c0, n = tcols(j)
            P = slice(0, npart)
            act(JUNK[P, :], src_ap, AF.Square, [src_res], [R_JUNK, R_SS], accum_out=SS[P, 0, k:k + 1])
            act(SS[P, 1, k:k + 1], SS[P, 0, k:k + 1], AF.Sqrt, [R_SS], [R_SS], scale=1.0 / D, bias=RMS_EPS)
            recip(SS[P, 2, k:k + 1], SS[P, 1, k:k + 1], [R_SS], [R_SS])
            if fp32_path is None:
                s = j % 2
                ts(HNB[s][P, :], src_ap, SS[P, 2, k:k + 1], ALU.mult, [src_res, R_SS], [R_HNB[s]])
                pb, pr = bank()
                pv = pb.bitcast(BF16).rearrange("p (k c) -> p k c", k=8)

                def fn(e, pv=pv, s=s, P=P, n=n):
                    ins = None
                    for kc in range(8):
                        ins = e.transpose(out=pv[:, kc, 0:n], in_=HNB[s][P, kc * 128:(kc + 1) * 128],
                                          identity=IDB[P, 0:n])
                    return ins
                S.op("pe", fn, [R_HNB[s], R_ID], pr)
                g_b = CP[:, gcol:gcol + 8].unsqueeze(2).to_broadcast([128, 8, n])
                tt(RA[:, :, c0:c0 + n], pv[:, :, 0:n], g_b, ALU.mult, pr + [R_CP], [R_RA[j]])
            else:
                HN32, R_HN32, H32T, R_H32T = fp32_path
                s = j % 2
                ts(HN32[s], src_ap, SS[:, 2, k:k + 1], ALU.mult, [src_res, R_SS], [R_HN32[s]])
                pb, pr = bank(pair=True)
                pv = pb.rearrange("p b (k c) -> p (b k) c", k=4)

                def fn(e, pv=pv, s=s):
                    ins = None
                    for kc in range(8):
                        ins = e.transpose(out=pv[:, kc, :], in_=HN32[s][:, kc * 128:(kc + 1) * 128],
                                          identity=IDF[:])
                    return ins
                S.op("pe", fn, [R_HN32[s], R_ID], pr)
                g_b = CP[:, gcol:gcol + 8].unsqueeze(2).to_broadcast([128, 8, 128])
                tt(RA[:, :, c0:c0 + n], pv, g_b, ALU.mult, pr + [R_CP], [R_RA[j]])
                tt(H32T[s], pv, g_b, ALU.mult, pr + [R_CP], [R_H32T[s]])

        hn_state = {"n": 0}

        hn_pending = []

        def hn_flush(keep=0):
            while len(hn_pending) > keep:
                hn_pending.pop(0)()

        def headnorm(q_ap, q_res, gcol, out_ap, out_res, n, bufs, split=None):
            SQ, R_SQ, RT, R_RT = bufs
            s = hn_state["n"] % 2
            hn_state["n"] += 1
            act(SQ[s][:, :n], q_ap, AF.Square, q_res, [R_SQ[s]])

            def rest():
                sb_, sr_ = bank()
                mmg(sb_[:, :n], [(BDB[:], SQ[s][:, :n])], [R_SQ[s], R_BD], sr_)
                act(RT[s][:, :n], sb_[:, :n], AF.Sqrt, sr_, [R_RT[s]], bias=RMS_EPS)
                recip(RT[s][:, :n], RT[s][:, :n], [R_RT[s]], [R_RT[s]])
                qv, rv = q_ap, RT[s][:, :n]
                if split:
                    qv = qv.rearrange("p (a b) -> p a b", b=split)
                    rv = rv.rearrange("p (a b) -> p a b", b=split)
                stt(out_ap, qv, cpc(gcol), rv, ALU.mult, ALU.mult, q_res + [R_RT[s], R_CP], out_res)
            hn_flush(keep=0)
            hn_pending.append(rest)

        def mem_attention(QM, R_QM, blocks, qcol_off, bufs):
            PT, R_PT, RD, R_RD = bufs
            npt = len(PT)
            st = {"n": 0, "r": 0}
            its = [(c0, n, tiles, hd) for (c0, n, tiles) in blocks for hd in range(4)]
            pend = {}

            def stage_a(i):
                c0, n, tiles, hd = its[i]
                q0 = c0 - qcol_off
                c2, hb = hd // 2, hd % 2
                rows = slice(hb * 64, hb * 64 + 64)
                pts = []
                for mc in range(2):
                    s = st["n"] % npt
                    st["n"] += 1
                    sb_, sr_ = bank()
                    mmg(sb_[:, :n], [(MKT[rows, c2, mc * 128:(mc + 1) * 128], QM[rows, c2, q0:q0 + n])],
                        [R_MKV, R_QM], sr_)
                    act(PT[s][:, :n], sb_[:, :n], AF.Exp, sr_, [R_PT[s]], scale=0.125)
                    pts.append(s)
                pend[i] = pts

            def stage_b(i):
                c0, n, tiles, hd = its[i]
                c2, hb = hd // 2, hd % 2
                rows = slice(hb * 64, hb * 64 + 64)
                pts = pend.pop(i)
                ob, orr = bank()
                mmg(ob[:, :n], [(MV[:, mc, c2 * 128:(c2 + 1) * 128], PT[pts[mc]][:, :n]) for mc in range(2)],
                    [R_MKV] + [R_PT[s] for s in pts], orr)
                db, dr = bank()
                mmg(db[:, :n], [(ONB[:], PT[pts[mc]][:, :n]) for mc in range(2)],
                    [R_ID] + [R_PT[s] for s in pts], dr)
                s2 = st["r"] % 2
                st["r"] += 1
                recip(RD[s2][rows, :n], db[rows, :n], dr, [R_RD[s2]])
                tt(RA[rows, 6 + c2, c0:c0 + n], ob[rows, :n], RD[s2][rows, :n], ALU.mult,
                   orr + [R_RD[s2]], [R_RA[t] for t in tiles])
            nun = len(its) // 2
            stage_a(0)
            stage_a(1)
            for u in range(nun):
                if u + 1 < nun:
                    stage_a(2 * u + 2)
                    stage_a(2 * u + 3)
                stage_b(2 * u)
                stage_b(2 * u + 1)

        def out_proj(WO, R_WO, tiles):
            for j in tiles:
                c0, n = tcols(j)
                yb, yr = bank(pair=True)
                for half in range(2):
                    mmg(yb[:, half, :], [(RA[:, kc, c0:c0 + 128], WO[:, kc, half * 512:(half + 1) * 512])
                                         for kc in range(8)], [R_RA[j], R_WO], [yr[half]])
                tt(HT[:, j, :], yb.rearrange("p b c -> p (b c)"), HT[:, j, :], ALU.add, yr + [R_HT[j]], [R_HT[j]])

        def ffn(groups, blocks, WB, R_WB, ACTB, R_ACTB, SIL, R_SIL, after_load=None):
            steps = [(gi, bi) for gi in range(len(groups)) for bi in range(len(blocks))]

            def load(gi):
                g = groups[gi]
                s = gi % 2
                nfc = g["nfc"]
                wg, wu, wd = WB[s]
                S.dma("pool", wg[:, :, 0:nfc * 128], g["g"].rearrange("(k p) n -> p k n", p=128),
                      writes=[R_WB[s][0]])
                S.dma("pool", wu[:, :, 0:nfc * 128], g["u"].rearrange("(k p) n -> p k n", p=128),
                      writes=[R_WB[s][1]])
                S.dma("pool", wd[:, 0:nfc, :], g["d"].rearrange("(f p) n -> p f n", p=128),
                      writes=[R_WB[s][2]])
                if after_load is not None:
                    after_load()

            def gu(si):
                gi, bi = steps[si]
                g = groups[gi]
                s = gi % 2
                a = si % 2
                wg, wu, wd = WB[s]
                c0, n, tiles = blocks[bi]
                rr = [R_RA[t] for t in tiles]
                for fc in range(g["nfc"]):
                    gb, gr = bank()
                    mmg(gb[:, :n], [(wg[:, kc, fc * 128:(fc + 1) * 128], RA[:, kc, c0:c0 + n]) for kc in range(8)],
                        rr + [R_WB[s][0]], gr)
                    ub, ur = bank()
                    mmg(ub[:, :n], [(wu[:, kc, fc * 128:(fc + 1) * 128], RA[:, kc, c0:c0 + n]) for kc in range(8)],
                        rr + [R_WB[s][1]], ur)
                    sl = (si * 4 + fc) % 2
                    act(SIL[sl][:, :n], gb[:, :n], AF.Silu, gr, [R_SIL[sl]])
                    tt(ACTB[a][:, fc, :n], ub[:, :n], SIL[sl][:, :n], ALU.mult, ur + [R_SIL[sl]], [R_ACTB[a][fc]])

            def down(si):
                gi, bi = steps[si]
                g = groups[gi]
                s = gi % 2
                a = si % 2
                wg, wu, wd = WB[s]
                c0, n, tiles = blocks[bi]
                nfc = g["nfc"]
                for ti, j in enumerate(tiles):
                    yb, yr = bank(pair=True)
                    for half in range(2):
                        mmg(yb[:, half, :], [(ACTB[a][:, fc, ti * 128:(ti + 1) * 128],
                                              wd[:, fc, half * 512:(half + 1) * 512]) for fc in range(nfc)],
                            [R_ACTB[a][fc] for fc in range(nfc)] + [R_WB[s][2]], [yr[half]])
                    yv = yb.rearrange("p b c -> p (b c)")
                    if g["gate"] is None:
                        tt(HT[:, j, :], yv, HT[:, j, :], ALU.add, yr + [R_HT[j]], [R_HT[j]])
                    else:
                        stt(HT[:, j, :], yv, g["gate"](j), HT[:, j, :], ALU.mult, ALU.add,
                            yr + [R_HT[j], R_GT], [R_HT[j]])

            load(0)
            if len(groups) > 1:
                load(1)
            nb = len(blocks)
            for si in range(len(steps)):
                gu(si)
                if si > 0:
                    down(si - 1)
                    gi_prev, bi_prev = steps[si - 1]
                    if bi_prev == nb - 1 and gi_prev + 2 < len(groups):
                        load(gi_prev + 2)
            down(len(steps) - 1)

        R_GT = Res("GT")

        if PROFILE_MARKS:
            S.mark(nc, "P_mem")
        MEMX = arv(PB0 + 10496, 8 * 256).rearrange("p (k c) -> p k c", k=8)
        R_MEMX = [Res("MEMX0"), Res("MEMX1")]
        SQb = [arv(PB0 + 12544 + i * 512, 512) for i in range(2)]
        R_SQb = [Res("SQ%d" % i) for i in range(2)]
        RTb = [arv(PB0 + 13568 + i * 1024, 512, F32) for i in range(2)]
        R_RTb = [Res("RT%d" % i) for i in range(2)]
        hbufs = (SQb, R_SQb, RTb, R_RTb)
        for t in range(2):
            k = ss_state["n"] % 32
            ss_state["n"] += 1
            act(JUNK[:], MEMT[:, t, :], AF.Square, [R_MEM], [R_JUNK, R_SS], accum_out=SS[:, 0, k:k + 1])
            act(SS[:, 1, k:k + 1], SS[:, 0, k:k + 1], AF.Sqrt, [R_SS], [R_SS], scale=1.0 / D, bias=RMS_EPS)
            recip(SS[:, 2, k:k + 1], SS[:, 1, k:k + 1], [R_SS], [R_SS])
            ts(HNB[t][:], MEMT[:, t, :], SS[:, 2, k:k + 1], ALU.mult, [R_MEM, R_SS], [R_HNB[t]])
            pb, pr = bank()
            pv = pb.bitcast(BF16).rearrange("p (k c) -> p k c", k=8)

            def fn(e, pv=pv, t=t):
                ins = None
                for kc in range(8):
                    ins = e.transpose(out=pv[:, kc, :], in_=HNB[t][:, kc * 128:(kc + 1) * 128], identity=IDB[:])
                return ins
            S.op("pe", fn, [R_HNB[t], R_ID], pr)
            g_b = CP[:, C_GM:C_GM + 8].unsqueeze(2).to_broadcast([128, 8, 128])
            tt(MEMX[:, :, t * 128:(t + 1) * 128], pv, g_b, ALU.mult, pr + [R_CP], [R_MEMX[t]])
        for c in range(2):
            kb_, kr_ = bank()
            mmg(kb_[:, :256], [(WKV[:, kc, c * 128:(c + 1) * 128], MEMX[:, kc, :]) for kc in range(8)],
                R_MEMX + [R_WKV], kr_)
            headnorm(kb_[:, :256], kr_, C_MK, MKT[:, c, :], [R_MKV], 256, hbufs)
        hn_flush()
        for t in range(2):
            vb_, vr_ = bank()
            mmg(vb_[:, :256], [(MEMX[:, kc, t * 128:(t + 1) * 128], WKV[:, kc, 256:512]) for kc in range(8)],
                [R_MEMX[t], R_WKV], vr_)
            copy("act", MV[:, t, :], vb_[:, :256], vr_, [R_MKV])
        hn_flush()
        dump("mkt", MKT[:].rearrange("p a b -> p (a b)"), [R_MKV])
        dump("mv", MV[:].rearrange("p a b -> p (a b)"), [R_MKV])

        if PROFILE_MARKS:
            S.mark(nc, "L0A_norm")
        hn_flush()
        S.barrier(dma=False)
        norm_transpose(17, C_GA0, src_ap=XP[0:32, :], src_res=R_XP, npart=32)
        for j in range(NT):
            norm_transpose(j, C_GA0)
        if PROFILE_MARKS:
            S.mark(nc, "L0B_inproj")
        prepass(8)
        U = arv(14336, 6 * TC).rearrange("p (c t) -> p c t", c=6)
        R_U = [Res("U%d" % c) for c in range(6)]
        QM0 = arv(27584, 2 * 2176).rearrange("p (c t) -> p c t", c=2)
        R_QM0 = Res("QM0")
        SG = [arv(31936 + i * 1024, 512, F32) for i in range(2)]
        R_SG = [Res("SG%d" % i) for i in range(2)]
        SQb = [arv(33984 + i * 512, 512) for i in range(2)]
        RTb = [arv(35008 + i * 1024, 512, F32) for i in range(2)]
        hbufs = (SQb, R_SQb, RTb, R_RTb)
        TB0 = [(0, 160, [17, 0])] + [(160 + 512 * i, 512, [1 + 4 * i + t for t in range(4)]) for i in range(4)]
        sgi = 0
        for c in range(6):
            for (c0, n, tiles) in TB0:
                rr = [R_RA[t] for t in tiles]
                ab, ar_ = bank()
                mmg(ab[:, :n], [(WIN0[:, kc, c * 128:(c + 1) * 128], RA[:, kc, c0:c0 + n]) for kc in range(8)],
                    rr + [R_WIN0], ar_)
                gb, gr = bank()
                mmg(gb[:, :n], [(WIN0[:, kc, (6 + c) * 128:(7 + c) * 128], RA[:, kc, c0:c0 + n]) for kc in range(8)],
                    rr + [R_WIN0], gr)
                s = sgi % 2
                sgi += 1
                act(SG[s][:, :n], gb[:, :n], AF.Sigmoid, gr + [R_CP], [R_SG[s]], bias=cpc(C_BG + c))
                stt(U[:, c, c0:c0 + n], ab[:, :n], cpc(C_BA + c), SG[s][:, :n], ALU.add, ALU.mult,
                    ar_ + [R_SG[s], R_CP], [R_U[c]])
                if c0 == 0:
                    ts(U[:, c, 0:160], U[:, c, 0:160], cpc(C_FLAG), ALU.mult, [R_U[c], R_CP], [R_U[c]])
        TBC = [(32, 128, [0])] + TB0[1:]
        for c2 in range(2):
            for (c0, n, tiles) in TBC:
                rr = [R_RA[t] for t in tiles]
                qb, qr = bank()
                mmg(qb[:, :n], [(WIN0[:, kc, (12 + c2) * 128:(13 + c2) * 128], RA[:, kc, c0:c0 + n])
                                for kc in range(8)], rr + [R_WIN0], qr)
                headnorm(qb[:, :n], qr, C_MQ0, QM0[:, c2, c0 - 32:c0 - 32 + n], [R_QM0], n, hbufs)
        hn_flush()
        dump("u", U[:, 0, :], [R_U[0]])
        dump("qm0", QM0[:, 0, :], [R_QM0])
        hn_flush()
        S.barrier()
        if PROFILE_MARKS:
            S.mark(nc, "L0C_conv")
        prepass(12)
        DG = [arv(i * 3968, 3968).rearrange("p (k c) -> p k c", k=CONV_W) for i in range(2)]
        R_DG = [Res("DG%d" % i) for i in range(2)]
        R_C = [[Res("C%d_%d" % (c, j)) for j in range(NT)] for c in range(6)]

        def build_dg(c):
            s = c % 2
            S.op("dve", lambda e, s=s, c=c: e.tensor_tensor(
                out=DG[s], in0=IDB[:].unsqueeze(1).to_broadcast([128, CONV_W, 128]),
                in1=CP[:, C_DW + c * CONV_W:C_DW + (c + 1) * CONV_W].unsqueeze(2).to_broadcast([128, CONV_W, 128]),
                op=ALU.mult), [R_ID, R_CP], [R_DG[s]])

        def conv_block(c, blk):
            s = c % 2
            c0, n, tiles = blk
            cb, cr = bank()
            mmg(cb[:, :n], [(DG[s][:, k, :], U[:, c, c0 - 30 + k:c0 - 30 + k + n]) for k in range(CONV_W)],
                [R_DG[s], R_U[c]], cr)
            act(RA[:, c, c0:c0 + n], cb[:, :n], AF.Identity, cr + [R_CP], [R_C[c][t] for t in tiles],
                bias=cpc(C_DWB + c))
        SQL = [arv(7936 + i * 512, 512) for i in range(2)]
        R_SQL = [Res("SQL%d" % i) for i in range(2)]
        MSQ = arv(8960, 512, F32)
        VAR = arv(9984, 512, F32)
        R_ST = Res("LNST")
        T1 = [arv(11008 + i * 1024, 512, F32) for i in range(2)]
        R_T1 = [Res("T1%d" % i) for i in range(2)]
        ln_st = {"q": 0}

        def ln_block(blk):
            c0, n, tiles = blk
            rall = [R_C[c][t] for c in range(6) for t in tiles]
            s1b, s1r = bank()
            mmg(s1b[:, :n], [(ONB[:], RA[:, c, c0:c0 + n]) for c in range(6)], rall + [R_ID], s1r)
            s2b, s2r = bank()
            for c in range(6):
                s = ln_st["q"] % 2
                ln_st["q"] += 1
                act(SQL[s][:, :n], RA[:, c, c0:c0 + n], AF.Square, [R_C[c][t] for t in tiles], [R_SQL[s]])
                S.op("pe", lambda e, s2b=s2b, s=s, n=n, c=c: e.matmul(s2b[:, :n], ONB[:], SQL[s][:, :n],
                                                                    start=(c == 0), stop=(c == 5)),
                     [R_SQL[s], R_ID], s2r)
            act(MSQ[:, :n], s1b[:, :n], AF.Square, s1r, [R_ST], scale=1.0 / CONV_CH)
            stt(VAR[:, :n], s2b[:, :n], 1.0 / CONV_CH, MSQ[:, :n], ALU.mult, ALU.subtract, s2r + [R_ST], [R_ST])
            act(VAR[:, :n], VAR[:, :n], AF.Sqrt, [R_ST], [R_ST], bias=LN_EPS)
            recip(VAR[:, :n], VAR[:, :n], [R_ST], [R_ST])
            for c in range(6):
                s = c % 2
                rc = [R_C[c][t] for t in tiles]
                stt(T1[s][:, :n], s1b[:, :n], -1.0 / CONV_CH, RA[:, c, c0:c0 + n], ALU.mult, ALU.add,
                    s1r + rc, [R_T1[s]])
                tt(T1[s][:, :n], T1[s][:, :n], VAR[:, :n], ALU.mult, [R_T1[s], R_ST], [R_T1[s]])
                act(RA[:, c, c0:c0 + n], T1[s][:, :n], AF.Silu, [R_T1[s], R_CP], rc,
                    scale=cpc(C_LNG + c), bias=cpc(C_LNB + c))
        for c in range(5):
            build_dg(c)
            for blk in TBC:
                conv_block(c, blk)
        build_dg(5)
        conv_block(5, TBC[0])
        for b in range(len(TBC)):
            if b + 1 < len(TBC):
                conv_block(5, TBC[b + 1])
            ln_block(TBC[b])
        dump("cln", RA[:, 0, 32:TC], [R_C[0][t] for t in range(NT)])
        hn_flush()
        S.barrier()
        if PROFILE_MARKS:
            S.mark(nc, "L0D_memattn")
        PT = [arv(i * 512, 512) for i in range(8)]
        R_PT = [Res("PT%d" % i) for i in range(8)]
        RD = [arv(4096 + i * 1024, 512, F32) for i in range(2)]
        R_RD = [Res("RD%d" % i) for i in range(2)]
        WO0 = arv(14336, 8 * D).rearrange("p (k n) -> p k n", k=8)
        R_WO0 = Res("WO0")
        S.dma("pool", WO0, cv_w_out.rearrange("(k p) n -> p k n", p=128), writes=[R_WO0])
        prepass(8)
        mem_attention(QM0, R_QM0, TBC, 32, (PT, R_PT, RD, R_RD))
        dump("cat0", RA[:, 6, 32:TC], [R_RA[t] for t in range(NT)])
        if PROFILE_MARKS:
            S.mark(nc, "L0E_outproj")
        out_proj(WO0, R_WO0, list(range(NT)))
        dump("h0a", HT[:, 1, :], [R_HT[1]])
        dump("hf0a", HT[:, 1:NT, :].rearrange("p j d -> p (j d)"), R_HT[1:NT])
        hn_flush()
        S.barrier()
        if PROFILE_MARKS:
            S.mark(nc, "L0F_ffn")
        WB = []
        R_WB = []
        for s in range(2):
            o = s * 12288
            WB.append((arv(o, 4096).rearrange("p (k n) -> p k n", k=8),
                       arv(o + 4096, 4096).rearrange("p (k n) -> p k n", k=8),
                       arv(o + 8192, 4096).rearrange("p (f n) -> p f n", f=4)))
            R_WB.append([Res("WB%d_%d" % (s, i)) for i in range(3)])
        ACTB = [arv(24576 + a * 2048, 2048).rearrange("p (f n) -> p f n", f=4) for a in range(2)]
        R_ACTB = [[Res("ACTB%d_%d" % (a, f)) for f in range(4)] for a in range(2)]
        SIL = [arv(28672 + i * 512, 512) for i in range(2)]
        R_SIL = [Res("SIL%d" % i) for i in range(2)]
        for j in range(NT):
            norm_transpose(j, C_GF0)
        groups0 = []
        for f0 in range(0, DFF0, 512):
            nf = min(512, DFF0 - f0)
            groups0.append(dict(g=cv_w_gate[:, f0:f0 + nf], u=cv_w_up[:, f0:f0 + nf], d=cv_w_down[f0:f0 + nf, :],
                                nfc=nf // 128, gate=None))
        ffn(groups0, TBC, WB, R_WB, ACTB, R_ACTB, SIL, R_SIL, after_load=lambda: prepass(6))
        dump("h1", HT[:, 1, :], [R_HT[1]])
        dump("hf1", HT[:, 1:NT, :].rearrange("p j d -> p (j d)"), R_HT[1:NT])
        hn_flush()
        S.barrier()

        if PROFILE_MARKS:
            S.mark(nc, "L1A_norm")
        for j in range(NT):
            norm_transpose(j, C_GA1)
        if PROFILE_MARKS:
            S.mark(nc, "L1B_inproj")
        WIN1 = arv(0, 8 * 1536).rearrange("p (k n) -> p k n", k=8)
        R_WIN1 = Res("WIN1")
        QPAIR = [(0, 3), (1, 4), (2, 5), (6, 9), (7, 10), (8, 11)]
        for i, (ha, hb_) in enumerate(QPAIR):
            for half, hh in enumerate((ha, hb_)):
                S.dma("pool", WIN1[:, :, i * 128 + half * 64:i * 128 + half * 64 + 64],
                      sw_w_in[:, hh * 64:(hh + 1) * 64].rearrange("(k p) n -> p k n", p=128), writes=[R_WIN1])
        S.dma("pool", WIN1[:, :, 768:1536], sw_w_in[:, 768:1536].rearrange("(k p) n -> p k n", p=128),
              writes=[R_WIN1])
        prepass(8)
        QT = arv(12288, 6 * TOK).rearrange("p (a n g q) -> p a n g q", a=2, n=16, g=3)
        R_QT = Res("QT")
        KT = arv(24576, 2 * 2176).rearrange("p (c t) -> p c t", c=2)
        R_KT = Res("KT")
        VT = arv(28928, NT * 256).rearrange("p (j d) -> p j d", j=NT)
        R_VT = Res("VT")
        QM1 = arv(33280, 2 * TOK).rearrange("p (c t) -> p c t", c=2)
        R_QM1 = Res("QM1")
        SQb = [arv(37376 + i * 512, 512) for i in range(2)]
        RTb = [arv(38400 + i * 1024, 512, F32) for i in range(2)]
        hbufs = (SQb, R_SQb, RTb, R_RTb)
        hn_flush()
        S.barrier()
        TB1 = TB0[1:]
        for i in range(6):
            for bi, (c0, n, tiles) in enumerate(TB1):
                rr = [R_RA[t] for t in tiles]
                qb, qr = bank()
                mmg(qb[:, :n], [(WIN1[:, kc, i * 128:(i + 1) * 128], RA[:, kc, c0:c0 + n]) for kc in range(8)],
                    rr + [R_WIN1], qr)
                headnorm(qb[:, :n], qr, C_QG, QT[:, i // 3, 4 * bi:4 * bi + 4, i % 3, :], [R_QT], n, hbufs,
                         split=128)
        for c2 in range(2):
            for (c0, n, tiles) in TBC:
                rr = [R_RA[t] for t in tiles]
                kb_, kr_ = bank()
                mmg(kb_[:, :n], [(WIN1[:, kc, 768 + c2 * 128:768 + (c2 + 1) * 128], RA[:, kc, c0:c0 + n])
                                 for kc in range(8)], rr + [R_WIN1], kr_)
                headnorm(kb_[:, :n], kr_, C_KG, KT[:, c2, c0 - 32:c0 - 32 + n], [R_KT], n, hbufs)
        hn_flush()
        for j in range(NT):
            c0, n = tcols(j)
            vb_, vr_ = bank()
            mmg(vb_[:, :256], [(RA[:, kc, c0:c0 + 128], WIN1[:, kc, 1024:1280]) for kc in range(8)],
                [R_RA[j], R_WIN1], vr_)
            copy("act", VT[:, j, :], vb_[:, :256], vr_, [R_VT])
        for c2 in range(2):
            for (c0, n, tiles) in TB1:
                rr = [R_RA[t] for t in tiles]
                qb, qr = bank()
                mmg(qb[:, :n], [(WIN1[:, kc, 1280 + c2 * 128:1280 + (c2 + 1) * 128], RA[:, kc, c0:c0 + n])
                                for kc in range(8)], rr + [R_WIN1], qr)
                headnorm(qb[:, :n], qr, C_MQ1, QM1[:, c2, c0 - 160:c0 - 160 + n], [R_QM1], n, hbufs)
        hn_flush()
        dump("qt", QT[:, 0, :, 0, :], [R_QT], view=("p (n q) -> p n q", dict(n=16)))
        dump("kt", KT[:, 0, :], [R_KT])
        hn_flush()
        S.barrier()
        if PROFILE_MARKS:
            S.mark(nc, "L1C_swa")
        prepass(12)
        ET = [arv(i * 384, 384) for i in range(8)]
        R_ET = [Res("ET%d" % i) for i in range(8)]
        PT1 = [arv(3072 + i * 384, 384) for i in range(8)]
        R_PT1 = [Res("PT1%d" % i) for i in range(8)]
        RD1 = [arv(6144 + i * 768, 384, F32) for i in range(2)]
        R_RD1 = [Res("RD1%d" % i) for i in range(2)]
        swa_its = [(nblk, kvh) for nblk in range(16) for kvh in range(4)]
        swa_st = {"e": 0, "p": 0}
        swa_pend = {}

        def swa_a(i):
            nblk, kvh = swa_its[i]
            j = nblk + 1
            rows = slice((kvh % 2) * 64, (kvh % 2) * 64 + 64)
            pts = []
            for kb in range(2):
                jk = j - 1 + kb
                sb_, sr_ = bank()
                mmg(sb_[:, :384], [(KT[rows, kvh // 2, jk * 128:(jk + 1) * 128],
                                    QT[rows, kvh // 2, nblk, :, :].rearrange("p g q -> p (g q)"))],
                    [R_KT, R_QT], sr_)
                s = swa_st["e"] % 8
                swa_st["e"] += 1
                act(ET[s][:], sb_[:, :384], AF.Exp, sr_, [R_ET[s]], scale=0.125)
                p = swa_st["p"] % 8
                swa_st["p"] += 1
                msk = ME0[:, kvh, :] if (nblk == 0 and kb == 0) else ME[:, kb, kvh, :]
                tt(PT1[p][:], ET[s][:], msk, ALU.mult, [R_ET[s], R_ME], [R_PT1[p]],
                   eng=os.environ.get("MK_SWA_MASK_ENG", "pool"))
                pts.append((p, jk))
            swa_pend[i] = pts

        def swa_b(i):
            nblk, kvh = swa_its[i]
            j = nblk + 1
            rc0, _ = tcols(j)
            rows = slice((kvh % 2) * 64, (kvh % 2) * 64 + 64)
            qc0 = (kvh // 2) * 3
            pts = swa_pend.pop(i)
            ob, orr = bank()
            vc0 = (kvh // 2) * 128
            mmg(ob[:, :384], [(VT[:, jk, vc0:vc0 + 128], PT1[p][:]) for (p, jk) in pts],
                [R_VT] + [R_PT1[p] for (p, _) in pts], orr)
            db, dr = bank()
            mmg(db[:, :384], [(ONB[:], PT1[p][:]) for (p, _) in pts] + [(ONB[:], SRB[:, kvh, :])],
                [R_ID, R_SR] + [R_PT1[p] for (p, _) in pts], dr)
            s2 = i % 2
            recip(RD1[s2][rows, :], db[rows, :384], dr, [R_RD1[s2]])
            tt(RA[rows, qc0:qc0 + 3, rc0:rc0 + 128], ob[rows, :384].rearrange("p (g q) -> p g q", g=3),
               RD1[s2][rows, :].rearrange("p (g q) -> p g q", g=3), ALU.mult,
               orr + [R_RD1[s2]], [R_RA[j]])
        nun = len(swa_its) // 2
        swa_a(0)
        swa_a(1)
        for u in range(nun):
            if u + 1 < nun:
                swa_a(2 * u + 2)
                swa_a(2 * u + 3)
            swa_b(2 * u)
            swa_b(2 * u + 1)
        dump("swa", RA[:, 0, 160:TC], [R_RA[t] for t in range(1, NT)])
        hn_flush()
        S.barrier()
        if PROFILE_MARKS:
            S.mark(nc, "L1D_mem_out")
        WO1 = arv(14336, 8 * D).rearrange("p (k n) -> p k n", k=8)
        R_WO1 = Res("WO1")
        for i, (ha, hb_) in enumerate(QPAIR):
            S.dma("pool", WO1[0:64, i, :], sw_w_out[ha * 64:(ha + 1) * 64, :], writes=[R_WO1])
            S.dma("pool", WO1[64:128, i, :], sw_w_out[hb_ * 64:(hb_ + 1) * 64, :], writes=[R_WO1])
        S.dma("pool", WO1[:, 6:8, :], sw_w_out[768:1024, :].rearrange("(k p) n -> p k n", p=128), writes=[R_WO1])
        prepass(8)
        PT = [arv(i * 512, 512) for i in range(8)]
        RD = [arv(4096 + i * 1024, 512, F32) for i in range(2)]
        mem_attention(QM1, R_QM1, TB1, 160, (PT, R_PT, RD, R_RD))
        out_proj(WO1, R_WO1, list(range(1, NT)))
        dump("h1a", HT[:, 1, :], [R_HT[1]])
        dump("hf1a", HT[:, 1:NT, :].rearrange("p j d -> p (j d)"), R_HT[1:NT])
        hn_flush()
        S.barrier()
        if PROFILE_MARKS:
            S.mark(nc, "L1R_router")
        HN32 = [arv(29696 + i * 2048, 1024, F32) for i in range(2)]
        R_HN32 = [Res("HN32%d" % i) for i in range(2)]
        H32T = [arv(33792 + i * 2048, 1024, F32).rearrange("p (k c) -> p k c", k=8) for i in range(2)]
        R_H32T = [Res("H32T%d" % i) for i in range(2)]
        prepass(1000)
        reserved.add(7)
        LG = PS[:, 7, 0:128].rearrange("p (j e) -> p j e", j=16)
        R_LG = banks[7]
        g_b128 = CP[:, C_GF1:C_GF1 + 8].unsqueeze(2).to_broadcast([128, 8, 128])
        rs_col = {}
        for j in range(1, NT):
            k = ss_state["n"] % 32
            ss_state["n"] += 1
            rs_col[j] = k
            s = j % 2
            act(JUNK[:], HT[:, j, :], AF.Square, [R_HT[j]], [R_JUNK, R_SS], accum_out=SS[:, 0, k:k + 1])
            act(SS[:, 1, k:k + 1], SS[:, 0, k:k + 1], AF.Sqrt, [R_SS], [R_SS], scale=1.0 / D, bias=RMS_EPS)
            recip(SS[:, 2, k:k + 1], SS[:, 1, k:k + 1], [R_SS], [R_SS])
            ts(HN32[s], HT[:, j, :], SS[:, 2, k:k + 1], ALU.mult, [R_HT[j], R_SS], [R_HN32[s]])
            pb, pr = bank(pair=True)
            pv = pb.rearrange("p b (k c) -> p (b k) c", k=4)

            def fn(e, pv=pv, s=s):
                ins = None
                for kc in range(8):
                    ins = e.transpose(out=pv[:, kc, :], in_=HN32[s][:, kc * 128:(kc + 1) * 128], identity=IDF[:])
                return ins
            S.op("pe", fn, [R_HN32[s], R_ID], pr)
            tt(H32T[s], pv, g_b128, ALU.mult, pr + [R_CP], [R_H32T[s]])
            mmg(LG[:, j - 1, :], [(H32T[s][:, kc, :], WR[:, kc, :]) for kc in range(8)],
                [R_H32T[s], R_WR], [R_LG])
        ga_state = {"o": 0}

        def ga(n, dt=F32):
            o = ga_state["o"]
            ga_state["o"] += 2 * n if dt != BF16 else n
            if dt == BF16:
                return arv(o, n)
            if dt == F32:
                return arv(o, n, F32)
            return AR[:, o:o + 2 * n].bitcast(dt)

        def v168(ap):
            return ap.rearrange("p (j e) -> p j e", j=16)

        def b168(ap16):
            return ap16.unsqueeze(2).to_broadcast([128, 16, 8])
        R_G = Res("GATE")
        gL, gEQ1, gL2, gEQ2, gSEL, gCUM, gTMP, gSLOT = [ga(128) for _ in range(8)]
        gV1, gV2, gE, gS1, gS2, gET = [ga(16) for _ in range(6)]
        W12 = sb("W12", [128, 32], F32)
        gW1, gW2 = W12[:, 0:16], W12[:, 16:32]
        gNE, gTL, gOE, gOF = [ga(8) for _ in range(4)]
        SELB, CUMB = ga(128, BF16), ga(128, BF16)
        SI = sb("SIT", [128, 32], I32)
        ETI = ga(16, I32)
        LTB = ga(128, BF16)
        R_C2 = Res("C2")
        C2F = ga(296)
        S.dma("sp", C2F, cst2[:, :], writes=[R_C2])
        copy("act", LTB, C2F[:, 0:128], [R_C2], [R_C2])
        RG, WG_ = [R_G], [R_G]
        copy("act", gL, LG.rearrange("p j e -> p (j e)"), [R_LG], WG_)
        S.op("dve", lambda e: e.tensor_reduce(out=gV1, in_=v168(gL), op=ALU.max, axis=AX.X), RG, WG_)
        tt(v168(gEQ1), v168(gL), b168(gV1), ALU.is_equal, RG, WG_)
        stt(gL2, gEQ1, -1e30, gL, ALU.mult, ALU.add, RG, WG_)
        S.op("dve", lambda e: e.tensor_reduce(out=gV2, in_=v168(gL2), op=ALU.max, axis=AX.X), RG, WG_)
        tt(v168(gEQ2), v168(gL2), b168(gV2), ALU.is_equal, RG, WG_)
        tt(gE, gV2, gV1, ALU.subtract, RG, WG_)
        act(gE, gE, AF.Exp, RG, WG_)
        ts(gW1, gE, 1.0, ALU.add, RG, WG_)
        recip(gW1, gW1, RG, WG_)
        tt(gW2, gE, gW1, ALU.mult, RG, WG_)
        tt(gSEL, gEQ1, gEQ2, ALU.add, RG, WG_)
        copy("dve", SELB, gSEL, RG, WG_)
        S.op("dve", lambda e: e.memset(gCUM[:, 0:8], 0.0), [], WG_)
        for j in range(1, 16):
            tt(gCUM[:, j * 8:(j + 1) * 8], gCUM[:, (j - 1) * 8:j * 8], gSEL[:, (j - 1) * 8:j * 8], ALU.add, RG, WG_)
        copy("dve", CUMB, gCUM, RG, WG_)
        rkb, rkr = bank()
        mmg(rkb[:, 0:128], [(LTB, SELB), (ONB[:], CUMB)], RG + [R_C2, R_ID], rkr)
        tob, tor = bank()
        mmg(tob[:, 0:128], [(ONB[:], SELB)], RG + [R_ID], tor)
        S.op("dve", lambda e: e.tensor_reduce(out=gNE, in_=tob[:, 0:128].rearrange("p (j e) -> p e j", j=16),
                                              op=ALU.add, axis=AX.X), tor, WG_)
        ts(gTL, gNE, 0.0, ALU.is_gt, RG, WG_)
        for thr in (512.0, 1024.0, 1536.0):
            stt(gTL, gNE, thr, gTL, ALU.is_gt, ALU.add, RG, WG_)
        copy("dve", gOE[:, 0:1], gTL[:, 0:1], RG, WG_)
        for ex in range(1, NE):
            tt(gOE[:, ex:ex + 1], gOE[:, ex - 1:ex], gTL[:, ex:ex + 1], ALU.add, RG, WG_)
        tt(gOF, gOE, gTL, ALU.subtract, RG, WG_)
        ts(gOF, gOF, 512.0, ALU.mult, RG, WG_)
        tt(v168(gSLOT), v168(rkb[:, 0:128]), gOF.unsqueeze(1).to_broadcast([128, 16, 8]), ALU.add, rkr + RG, WG_)
        tt(gTMP, gEQ1, gSLOT, ALU.mult, RG, WG_)
        S.op("dve", lambda e: e.tensor_reduce(out=gS1, in_=v168(gTMP), op=ALU.add, axis=AX.X), RG, WG_)
        copy("dve", SI[:, 0:16], gS1, RG, WG_)
        tt(gTMP, gEQ2, gSLOT, ALU.mult, RG, WG_)
        S.op("dve", lambda e: e.tensor_reduce(out=gS2, in_=v168(gTMP), op=ALU.add, axis=AX.X), RG, WG_)
        copy("dve", SI[:, 16:32], gS2, RG, WG_)
        tt(v168(gTMP), gOE.unsqueeze(1).to_broadcast([128, 16, 8]), v168(C2F[:, 128:256]), ALU.is_le,
           RG + [R_C2], WG_)
        S.op("dve", lambda e: e.tensor_reduce(out=gET, in_=v168(gTMP), op=ALU.add, axis=AX.X), RG, WG_)
        ts(gET, gET, 7.0, ALU.min, RG, WG_)
        copy("dve", ETI, gET, RG, WG_)
        dump("gw1", gW1, RG)
        dump("gs1", gS1, RG)
        dump("gs2", gS2, RG)
        dump("get", gET, RG)
        reserved.discard(7)
        MODEI = sb("MODEI", [128, 16], I32)
        gMA, gMB, gREM = ga(128), ga(128), ga(128)
        gC, gFILL, gMODE = ga(8), ga(16), ga(16)
        TAUv = v168(C2F[:, 128:256])
        tt(v168(gMA), gOF.unsqueeze(1).to_broadcast([128, 16, 8]), TAUv, ALU.is_le, RG + [R_C2], WG_)
        tt(gC, gOE, gTL, ALU.subtract, RG, WG_)
        tt(v168(gMA), gC.unsqueeze(1).to_broadcast([128, 16, 8]), TAUv, ALU.is_le, RG + [R_C2], WG_)
        tt(v168(gMB), TAUv, gOE.unsqueeze(1).to_broadcast([128, 16, 8]), ALU.is_lt, RG + [R_C2], WG_)
        tt(gMA, gMA, gMB, ALU.mult, RG, WG_)
        tt(gC, gNE, gOF, ALU.add, RG, WG_)
        stt(v168(gREM), TAUv, -512.0, gC.unsqueeze(1).to_broadcast([128, 16, 8]), ALU.mult, ALU.add,
            RG + [R_C2], WG_)
        tt(gREM, gREM, gMA, ALU.mult, RG, WG_)
        S.op("dve", lambda e: e.tensor_reduce(out=gFILL, in_=v168(gREM), op=ALU.add, axis=AX.X), RG, WG_)
        ts(gMODE, gFILL, 0.0, ALU.is_gt, RG, WG_)
        stt(gMODE, gFILL, 256.0, gMODE, ALU.is_gt, ALU.add, RG, WG_)
        copy("dve", MODEI[:], gMODE, RG, WG_)
        dump("gmode", gMODE, RG)
        eng_regs = {}

        def mk_setup(eng):
            def fn(e, eng=eng):
                eng_regs[eng] = e.alloc_register(name="rmode_" + eng)
            return fn
        for eng in ENGS:
            S.raw(eng, mk_setup(eng), [])
        IDXG = sb("IDXG", [128, 112], I32)
        IDXD = sb("IDXD", [128, 448], I32)
        gIG, gID = ga(112), ga(448)
        stt(gIG.rearrange("p (t g) -> p t g", t=16), gET.unsqueeze(2).to_broadcast([128, 16, 7]), 896.0,
            C2F[:, 256:263].unsqueeze(1).to_broadcast([128, 16, 7]), ALU.mult, ALU.add, RG + [R_C2], WG_)
        copy("dve", IDXG[:], gIG, RG, WG_)
        stt(gID.rearrange("p (t g) -> p t g", t=16), gET.unsqueeze(2).to_broadcast([128, 16, 28]), 3584.0,
            C2F[:, 263:291].unsqueeze(1).to_broadcast([128, 16, 28]), ALU.mult, ALU.add, RG + [R_C2], WG_)
        copy("dve", IDXD[:], gID, RG, WG_)
        if PROFILE_MARKS:
            S.mark(nc, "L1S_scatter")
        for j in range(1, NT):
            s = j % 2
            k = rs_col[j]
            ts(HNB[s][:], HT[:, j, :], SS[:, 2, k:k + 1], ALU.mult, [R_HT[j], R_SS], [R_HNB[s]])
            for kk in range(2):
                col = kk * 16 + j - 1
                S.dma("pool", None, None, reads=[R_HNB[s]] + RG, writes=[R_XS], owner=R_XS,
                      fn=lambda e, s=s, col=col: e.indirect_dma_start(
                          out=XS[:, :], out_offset=bass.IndirectOffsetOnAxis(ap=SI[:, col:col + 1], axis=0),
                          in_=HNB[s][:], in_offset=None))
        if PROFILE_MARKS:
            S.mark(nc, "L1T_tiles")
        NTILE = int(os.environ.get("MK_NTILE", "15"))
        WB3 = []
        R_WB3 = []
        for s in range(2):
            o = 8192 + s * 12288
            WB3.append((arv(o, 4096).rearrange("p (k n) -> p k n", k=8),
                        arv(o + 4096, 4096).rearrange("p (k n) -> p k n", k=8),
                        arv(o + 8192, 4096).rearrange("p (f n) -> p f n", f=4)))
            R_WB3.append([Res("WB3%d_%d" % (s, i)) for i in range(3)])
        ACT3 = [arv(32768 + a * 2048, 2048).rearrange("p (f n) -> p f n", f=4) for a in range(2)]
        R_ACT3 = [[Res("ACT3%d_%d" % (a, f)) for f in range(4)] for a in range(2)]
        SIL3 = [arv(36864 + i * 512, 512) for i in range(2)]
        R_SIL3 = [Res("SIL3%d" % i) for i in range(2)]
        YACC = [arv(c * 2048, 1024, F32) for c in range(4)]
        R_YACC = [Res("YACC%d" % c) for c in range(4)]
        RAf = RA[:].rearrange("p k t -> p (k t)")
        XT = [RAf[:, i * 4096:(i + 1) * 4096].rearrange("p (c d) -> p c d", c=4) for i in range(2)]
        R_XT = [Res("XT%d" % i) for i in range(2)]
        XF = [RAf[:, 8192 + i * 4096:8192 + (i + 1) * 4096].rearrange("p (k n) -> p k n", k=8) for i in range(2)]
        R_XF = [Res("XF%d" % i) for i in range(2)]
        R_YS = Res("YS")
        hn_flush()
        S.barrier()

        def xs_load(tau):
            if tau >= NTILE:
                return
            s = tau % 2
            S.dma("sp", XT[s], XS[512 * tau:512 * (tau + 1), :].rearrange("(c p) d -> p c d", p=128),
                  reads=[R_XS], writes=[R_XT[s]])

        def prep_tile(tau, nch):
            s = tau % 2
            for c in range(nch):
                pb, pr = bank()
                pv = pb.bitcast(BF16).rearrange("p (k c) -> p k c", k=8)

                def fn(e, pv=pv, s=s, c=c):
                    ins = None
                    for kc in range(8):
                        ins = e.transpose(out=pv[:, kc, :], in_=XT[s][:, c, kc * 128:(kc + 1) * 128], identity=IDB[:])
                    return ins
                S.op("pe", fn, [R_XT[s], R_ID], pr)
                tt(XF[s][:, :, c * 128:(c + 1) * 128], pv, g_b128, ALU.mult, pr + [R_CP], [R_XF[s]])

        steps = [(tau, g) for tau in range(NTILE) for g in range(7)]

        wdflat = sw_we_down.rearrange("e r f -> (e r) f")

        def load_w(si):
            tau, g = steps[si]
            s = si % 2
            o = 8192 + s * 12288
            col = tau * 7 + g
            for (dst, src, res_) in ((arv(o, 4096), WSG, R_WB3[s][0]), (arv(o + 4096, 4096), WSU, R_WB3[s][1])):
                S.dma("pool", None, None, reads=[R_WS] + RG, writes=[res_],
                      fn=lambda e, dst=dst, src=src, col=col: e.indirect_dma_start(
                          out=dst, out_offset=None, in_=src[:, :],
                          in_offset=bass.IndirectOffsetOnAxis(ap=IDXG[:, col:col + 1], axis=0)))
            for fc in range(4):
                S.dma("pool", None, None, reads=RG, writes=[R_WB3[s][2]],
                      fn=lambda e, s=s, fc=fc, col=col: e.indirect_dma_start(
                          out=WB3[s][2][:, fc, :], out_offset=None, in_=wdflat[:, :],
                          in_offset=bass.IndirectOffsetOnAxis(ap=IDXD[:, col * 4 + fc:col * 4 + fc + 1], axis=0)))

        def gu3(si, nch):
            tau, g = steps[si]
            s, a, x = si % 2, si % 2, tau % 2
            wg, wu, wd = WB3[s]
            n = nch * 128
            for fc in range(4):
                gb, gr = bank()
                mmg(gb[:, :n], [(wg[:, kc, fc * 128:(fc + 1) * 128], XF[x][:, kc, :n]) for kc in range(8)],
                    [R_XF[x], R_WB3[s][0]], gr)
                ub, ur = bank()
                mmg(ub[:, :n], [(wu[:, kc, fc * 128:(fc + 1) * 128], XF[x][:, kc, :n]) for kc in range(8)],
                    [R_XF[x], R_WB3[s][1]], ur)
                sl = fc % 2
                act(SIL3[sl][:, :n], gb[:, :n], AF.Silu, gr, [R_SIL3[sl]])
                tt(ACT3[a][:, fc, :n], ub[:, :n], SIL3[sl][:, :n], ALU.mult, ur + [R_SIL3[sl]], [R_ACT3[a][fc]])

        def down3(si, nch):
            tau, g = steps[si]
            s, a = si % 2, si % 2
            wg, wu, wd = WB3[s]
            for c in range(nch):
                yb, yr = bank(pair=True)
                for half in range(2):
                    mmg(yb[:, half, :], [(ACT3[a][:, fc, c * 128:(c + 1) * 128], wd[:, fc, half * 512:(half + 1) * 512])
                                         for fc in range(4)], R_ACT3[a] + [R_WB3[s][2]], [yr[half]])
                yv = yb.rearrange("p b c -> p (b c)")
                if g == 0:
                    copy("act", YACC[c], yv, yr, [R_YACC[c]])
                else:
                    tt(YACC[c], yv, YACC[c], ALU.add, yr + [R_YACC[c]], [R_YACC[c]])
            if g == 6:
                S.dma("sp", YS[512 * tau:512 * tau + 128 * nch, :].rearrange("(c p) d -> p c d", p=128),
                      AR[:, 0:2048 * nch].bitcast(F32).rearrange("p (c d) -> p c d", c=nch),
                      reads=R_YACC[0:nch], writes=[R_YS], owner=R_YS)

        def ld(si):
            if si < len(steps):
                load_w(si)

        def run_tile(tau, nch):
            si0, si1 = tau * 7, (tau + 1) * 7
            for si in range(si0, si1):
                g = steps[si][1]
                gu3(si, nch)
                if si > si0:
                    down3(si - 1, nch)
                    ld(si + 1)
                if g == 1:
                    xs_load(tau + 1)
                if g == 4 and tau + 1 < NTILE:
                    prep_tile(tau + 1, 4)
            down3(si1 - 1, nch)
            ld(si1 + 1)

        def mk_regload(tau):
            def fn(e, tau=tau):
                for k_, v_ in eng_regs.items():
                    pass
                e.reg_load(eng_regs[fn.eng], MODEI[0:1, tau:tau + 1])
            return fn
        xs_load(0)
        ld(0)
        ld(1)
        prep_tile(0, 4)
        for tau in range(NTILE):
            for eng in ENGS:
                f_ = mk_regload(tau)
                f_.eng = eng
                S.raw(eng, f_, RG)
            S.region_begin(2)
            run_tile(tau, 4)
            S.region_end()
            S.region_begin(1)
            run_tile(tau, 2)
            S.region_end()
        hn_flush()
        S.barrier()
        if PROFILE_MARKS:
            S.mark(nc, "L1Z_combine")
        G1 = [arv(8192 + i * 2048, 1024, F32) for i in range(2)]
        G2 = [arv(12288 + i * 2048, 1024, F32) for i in range(2)]
        R_G1 = [Res("G1%d" % i) for i in range(2)]
        R_G2 = [Res("G2%d" % i) for i in range(2)]
        R_OUT = Res("OUT")
        for j in range(1, NT):
            s = j % 2
            for (GB, RGB, base, wv) in ((G1, R_G1, 0, gW1), (G2, R_G2, 16, gW2)):
                col = base + j - 1
                S.dma("pool", None, None, reads=[R_YS] + RG, writes=[RGB[s]],
                      fn=lambda e, GB=GB, s=s, col=col: e.indirect_dma_start(
                          out=GB[s][:], out_offset=None, in_=YS[:, :],
                          in_offset=bass.IndirectOffsetOnAxis(ap=SI[:, col:col + 1], axis=0)))
                stt(HT[:, j, :], GB[s], wv[:, j - 1:j], HT[:, j, :], ALU.mult, ALU.add,
                    [RGB[s], R_HT[j]] + RG, [R_HT[j]])
            S.dma("sp", out[(j - 1) * 128:j * 128, :], HT[:, j, :], reads=[R_HT[j]], owner=R_OUT)
        S.final_wait("sp")

        block = es.enter_context(nc.Block())

        @block.tensor
        def _(e):
            S.replay("pe", e, eng_regs)

        @block.scalar
        def _(e):
            S.replay("act", e, eng_regs)

        @block.vector
        def _(e):
            S.replay("dve", e, eng_regs)

        @block.gpsimd
        def _(e):
            S.replay("pool", e, eng_regs)

        @block.sync
        def _(e):
            S.replay("sp", e, eng_regs)
    return nc


def _consts():
    ident = np.eye(128, dtype=np.float32)
    bd = np.zeros((128, 128), np.float32)
    bd[:64, :64] = 1.0 / 64
    bd[64:, 64:] = 1.0 / 64
    slopes = np.exp2(-8.0 * (np.arange(12, dtype=np.float32) + 1.0) / 12).astype(np.float32)
    k = np.arange(128)[:, None].astype(np.float32)
    q = np.arange(128)[None, :].astype(np.float32)
    me = np.zeros((128, 2, 4, 3, 128), np.float32)
    for kb in range(2):
        dist = q + 128.0 - k if kb == 0 else q - k
        valid = (dist >= 0) & (dist < 128)
        for kvh in range(4):
            for g in range(3):
                s = slopes[kvh * 3 + g]
                me[:, kb, kvh, g, :] = np.where(valid, np.exp(-s * dist), 0.0)
    c2 = np.zeros((128, 296), np.float32)
    pidx = np.arange(128, dtype=np.float32)[:, None]
    c2[:, 256:263] = np.arange(7, dtype=np.float32)[None, :] * 128 + pidx
    c2[:, 263:291] = (np.arange(7, dtype=np.float32)[:, None] * 512
                      + np.arange(4, dtype=np.float32)[None, :] * 128).reshape(1, 28) + pidx
    c2[:, 0:128] = (np.arange(128)[:, None] < np.arange(128)[None, :]).astype(np.float32)
    c2[:, 128:256] = np.repeat(np.arange(16, dtype=np.float32), 8)[None, :]
    return ident, bd, me.reshape(128, 3072), c2


def _cpack(inp, flag):
    cp = np.zeros((128, NCP), np.float32)

    def pk(v):
        return np.asarray(v, np.float32).reshape(8, 128).T
    cp[:, C_GA0:C_GA0 + 8] = pk(inp["cv_attn_norm_g"][0])
    cp[:, C_GF0:C_GF0 + 8] = pk(inp["cv_ffn_norm_g"][0])
    cp[:, C_GA1:C_GA1 + 8] = pk(inp["sw_attn_norm_g"][0])
    cp[:, C_GF1:C_GF1 + 8] = pk(inp["sw_ffn_norm_g"][0])
    cp[:, C_GM:C_GM + 8] = pk(inp["mem_norm_g"])
    bg = np.asarray(inp["cv_b_glu"][0], np.float32)
    cp[:, C_BA:C_BA + 6] = bg[:768].reshape(6, 128).T
    cp[:, C_BG:C_BG + 6] = bg[768:].reshape(6, 128).T
    dw = np.asarray(inp["cv_dw_w"][0], np.float32)
    cp[:, C_DW:C_DW + 186] = dw.reshape(31, 6, 128).transpose(2, 1, 0).reshape(128, 186)
    cp[:, C_DWB:C_DWB + 6] = np.asarray(inp["cv_dw_b"][0], np.float32).reshape(6, 128).T
    cp[:, C_LNG:C_LNG + 6] = np.asarray(inp["cv_ln_g"][0], np.float32).reshape(6, 128).T
    cp[:, C_LNB:C_LNB + 6] = np.asarray(inp["cv_ln_b"][0], np.float32).reshape(6, 128).T

    def h2(v):
        v = np.asarray(v, np.float32).reshape(64)
        return np.concatenate([v, v])
    cp[:, C_MQ0] = h2(inp["cv_memq_norm_g"][0])
    cp[:, C_MQ1] = h2(inp["sw_memq_norm_g"][0])
    cp[:, C_MK] = h2(inp["mem_k_norm_g"])
    cp[:, C_QG] = h2(inp["sw_q_norm_g"][0])
    cp[:, C_KG] = h2(inp["sw_k_norm_g"][0])
    cp[:, C_FLAG] = flag
    return cp


def make_in_maps(inp):
    f = lambda a: np.ascontiguousarray(np.asarray(a, dtype=np.float32))
    ident, bd, me, c2 = _consts()
    x = f(inp["x"])
    memf = f(inp["mem"])
    sinks = f(inp["sw_sinks"][0])
    sinks_rep = np.ascontiguousarray(np.repeat(sinks, 128)[None, :])
    shared = dict(
        ident=ident, bdm=bd, maskexp=me, cst2=c2, sinks_rep=sinks_rep,
        w_mem_kv=f(inp["w_mem_kv"]), cv_w_in=f(inp["cv_w_in"][0]), cv_w_out=f(inp["cv_w_out"][0]),
        cv_w_gate=f(inp["cv_w_gate"][0]), cv_w_up=f(inp["cv_w_up"][0]), cv_w_down=f(inp["cv_w_down"][0]),
        sw_w_in=f(inp["sw_w_in"][0]), sw_w_out=f(inp["sw_w_out"][0]), sw_router=f(inp["sw_router"][0]),
        sw_we_gate=f(inp["sw_we_gate"][0]), sw_we_up=f(inp["sw_we_up"][0]), sw_we_down=f(inp["sw_we_down"][0]),
    )
    maps = []
    for c in range(NCORES):
        b, qtr = divmod(c, 4)
        xin = np.zeros((TC, D), np.float32)
        t0 = qtr * TOK
        if qtr == 0:
            xin[160:] = x[b, 0:TOK]
        else:
            xin[:] = x[b, t0 - 160:t0 + TOK]
        m = dict(shared)
        m["xin"] = xin
        m["mem"] = memf[b]
        m["cpack"] = _cpack(inp, 0.0 if qtr == 0 else 1.0)
        maps.append(m)
    return maps


def kernel(**inputs):
    nc = build_program()
    maps = make_in_maps(inputs)
    res = run_bass_kernel_spmd(nc, maps, core_ids=list(range(NCORES)))
    outs = [np.asarray(r["out"], np.float32) for r in res.results]
    full = np.concatenate(outs, axis=0).reshape(2, SEQ, D)
    return full
```
